# Optimizing a Trainium2 kernel written in Bass

```python
import math
import jax
import jax.numpy as jnp
from jax import lax
import numpy as np

D_MODEL = 1024
BATCH = 8
SEQ = 4096
DEPTH = 4

GRID_W = 64
CTX_LEN = 256

HY_W = 256
N_HEADS = 8
N_KV_HEADS = 2
N_GROUPS = N_HEADS // N_KV_HEADS
HEAD_DIM = 64
ATT_W = N_HEADS * HEAD_DIM
KV_W = N_KV_HEADS * HEAD_DIM
RG_W = 256
RG_BLOCKS = 4
RG_BW = RG_W // RG_BLOCKS
MIX_W = HY_W + ATT_W + RG_W

HY_END = 3 * HY_W
Q_END = HY_END + ATT_W
K_END = Q_END + KV_W
V_END = K_END + KV_W
RGX_END = V_END + RG_W
PROJ_W = RGX_END + RG_W

HY_ORDER = 2
HY_SHORT = 3
HY_BANDS = 8
HY_EMB = 1 + 2 * HY_BANDS
HY_FILTER_W = 64
HY_DECAY_TARGET = 1e-2
HY_SHORT_DECAY_PCT = 0.3
HY_LONG_DECAY_PCT = 1.5
HY_MAX_DECAY = math.log(HY_DECAY_TARGET) / HY_SHORT_DECAY_PCT
HY_MIN_DECAY = math.log(HY_DECAY_TARGET) / HY_LONG_DECAY_PCT

ROPE_THETA = 10000.0
Q_BLOCK = 128
QK_EPS = 1e-6

RG_CONV = 4
RG_C = 8.0

N_EXPERTS = 64
TOP_K = 8
EXPERT_FF = 256
SHARED_FF = 256
ROUTED_SCALE = 2.5
MOE_CHUNK = 1024

LN_EPS = 1e-6
DN_ALPHA = (2 * DEPTH) ** 0.25
DN_BETA = (8 * DEPTH) ** -0.25

kernel_name = 'hybrid_dit_hyena_gqa_rglru_moe'


def layer_norm(u, g, b):
    u32 = u.astype(jnp.float32)
    mu = jnp.mean(u32, axis=-1, keepdims=True)
    var = jnp.mean(jnp.square(u32 - mu), axis=-1, keepdims=True)
    return ((u32 - mu) * lax.rsqrt(var + LN_EPS) * g + b).astype(u.dtype)


def rms_norm(u, g):
    u32 = u.astype(jnp.float32)
    return (u32 * lax.rsqrt(jnp.mean(jnp.square(u32), axis=-1, keepdims=True) + QK_EPS) * g).astype(u.dtype)


def depthwise_conv(u, w, b):
    width = w.shape[0]
    L = u.shape[1]
    left = (width - 1) // 2
    up = jnp.pad(u, ((0, 0), (left, width - 1 - left), (0, 0)))
    return sum(up[:, j:j + L] * w[j] for j in range(width)) + b


def split_proj(p):
    return (p[..., :HY_END], p[..., HY_END:Q_END], p[..., Q_END:K_END],
            p[..., K_END:V_END], p[..., V_END:RGX_END], p[..., RGX_END:])


def hyena_filter_spectra(L, w1, b1, freq, w2, b2, w3):
    t = jnp.linspace(0.0, 1.0, L, dtype=jnp.float32)[:, None]
    w = 2.0 * math.pi * jnp.arange(L, dtype=jnp.float32)[:, None] / L
    bands = jnp.linspace(1e-4, HY_BANDS - 1, HY_BANDS, dtype=jnp.float32)
    z = jnp.concatenate([t, jnp.cos(bands * w), -jnp.sin(bands * w)], axis=-1)
    f = jnp.sin(freq * (z @ w1 + b1))
    f = jnp.sin(freq * (f @ w2 + b2))
    f = (f @ w3).astype(jnp.float32).reshape(L, 2, HY_ORDER, HY_W)
    deltas = jnp.abs(jnp.linspace(HY_MIN_DECAY, HY_MAX_DECAY, HY_W, dtype=jnp.float32))
    f = f * jnp.exp(-t * deltas)[:, None, None, :]
    fwd, bwd = f[:, 0], f[:, 1]
    k = jnp.concatenate([fwd, jnp.zeros_like(fwd[:1]), bwd[:0:-1]], axis=0)
    return jnp.fft.rfft(k, axis=0)


def fft_long_conv(u, k_f, skip):
    L = u.shape[1]
    u32 = u.astype(jnp.float32)
    y = jnp.fft.irfft(jnp.fft.rfft(u32, n=2 * L, axis=1) * k_f, n=2 * L, axis=1)[:, :L]
    return (y + u32 * skip.astype(jnp.float32)).astype(u.dtype)


def hyena_mixer(u, short_w, short_b, w1, b1, freq, w2, b2, w3, skip):
    L = u.shape[1]
    x1, x2, z = jnp.split(depthwise_conv(u, short_w, short_b), 3, axis=-1)
    k_f = hyena_filter_spectra(L, w1, b1, freq, w2, b2, w3)
    for n, gate in enumerate((x1, x2)):
        z = gate * fft_long_conv(z, k_f[:, n], skip[n])
    return z


def axial_rope(n_tokens):
    n_rows = n_tokens // GRID_W
    row = jnp.repeat(jnp.arange(n_rows, dtype=jnp.float32), GRID_W)
    col = jnp.tile(jnp.arange(GRID_W, dtype=jnp.float32), n_rows)
    axis_dim = HEAD_DIM // 2
    inv_freq = ROPE_THETA ** (-jnp.arange(0, axis_dim, 2, dtype=jnp.float32) / axis_dim)
    ang = jnp.concatenate([row[:, None] * inv_freq, col[:, None] * inv_freq], axis=-1)
    return jnp.cos(ang), jnp.sin(ang)


def apply_rope(u, cos, sin):
    u32 = u.astype(jnp.float32).reshape(*u.shape[:-1], HEAD_DIM // 2, 2)
    u1, u2 = u32[..., 0], u32[..., 1]
    out = jnp.stack([u1 * cos - u2 * sin, u1 * sin + u2 * cos], axis=-1)
    return out.reshape(u.shape).astype(u.dtype)


def gqa_q(q, gain):
    B, L, _ = q.shape
    q = rms_norm(q.reshape(B, L, N_KV_HEADS, N_GROUPS, HEAD_DIM), gain)
    return q.transpose(0, 2, 3, 1, 4)


def gqa_kv(k, gain=None):
    B, L, _ = k.shape
    k = k.reshape(B, L, N_KV_HEADS, HEAD_DIM)
    if gain is not None:
        k = rms_norm(k, gain)
    return k.transpose(0, 2, 1, 3)


def sdpa(q, k, v):
    s = jnp.einsum('bkgqd,bksd->bkgqs', q, k, preferred_element_type=jnp.float32) * HEAD_DIM ** -0.5
    p = jax.nn.softmax(s, axis=-1).astype(v.dtype)
    return jnp.einsum('bkgqs,bksd->bkgqd', p, v)


def attention_mixer(q_l, k_l, v_l, q_c, k_c, v_c, q_gain, k_gain, cos, sin):
    B, T, _ = q_l.shape
    kc = gqa_kv(k_c, k_gain)
    vc = gqa_kv(v_c)
    ql = apply_rope(gqa_q(q_l, q_gain), cos, sin)
    k_all = jnp.concatenate([apply_rope(gqa_kv(k_l, k_gain), cos, sin), kc], axis=2)
    v_all = jnp.concatenate([gqa_kv(v_l), vc], axis=2)
    n_blk = T // Q_BLOCK
    q_blocks = ql.reshape(B, N_KV_HEADS, N_GROUPS, n_blk, Q_BLOCK, HEAD_DIM).transpose(3, 0, 1, 2, 4, 5)
    o = lax.map(lambda qb: sdpa(qb, k_all, v_all), q_blocks)
    y_l = o.transpose(1, 0, 4, 2, 3, 5).reshape(B, T, ATT_W)
    if q_c is None:
        return y_l, None
    o_c = sdpa(gqa_q(q_c, q_gain), kc, vc)
    y_c = o_c.transpose(0, 3, 1, 2, 4).reshape(B, q_c.shape[1], ATT_W)
    return y_l, y_c


def _linear_combine(e1, e2):
    a1, b1 = e1
    a2, b2 = e2
    return a1 * a2, a2 * b1 + b2


def rg_lru_scan(u, lam, w_a, b_a, w_x, b_x, h0, reverse):
    B, L, _ = u.shape
    ub = u.reshape(B, L, RG_BLOCKS, RG_BW)
    r = jax.nn.sigmoid(jnp.einsum('blhi,hij->blhj', ub, w_a).reshape(B, L, RG_W) + b_a).astype(jnp.float32)
    i = jax.nn.sigmoid(jnp.einsum('blhi,hij->blhj', ub, w_x).reshape(B, L, RG_W) + b_x)
    log_a = -RG_C * r * jax.nn.softplus(-lam.astype(jnp.float32))
    a = jnp.exp(log_a)
    b = jnp.sqrt(-jnp.expm1(2.0 * log_a)) * (i * u).astype(jnp.float32)
    first = L - 1 if reverse else 0
    b = b.at[:, first].add(a[:, first] * h0)
    _, h = lax.associative_scan(_linear_combine, (a, b), axis=1, reverse=reverse)
    return h


def rglru_mixer(x_l, g_l, x_c, g_c, conv_w, conv_b, lam, w_a, b_a, w_x, b_x):
    u_l = depthwise_conv(x_l, conv_w, conv_b)
    u_c = depthwise_conv(x_c, conv_w, conv_b)
    zeros = jnp.zeros((x_c.shape[0], RG_W), jnp.float32)
    h_l_sum = 0.0
    h_c_sum = 0.0
    for d, rev in enumerate((False, True)):
        params = (lam[d], w_a[d], b_a[d], w_x[d], b_x[d])
        h_c = rg_lru_scan(u_c, *params, zeros, rev)
        h_end = h_c[:, 0] if rev else h_c[:, -1]
        h_l_sum = h_l_sum + rg_lru_scan(u_l, *params, h_end, rev)
        if g_c is not None:
            h_c_sum = h_c_sum + h_c
    y_l = h_l_sum.astype(x_l.dtype) * jax.nn.gelu(g_l)
    y_c = None if g_c is None else h_c_sum.astype(x_c.dtype) * jax.nn.gelu(g_c)
    return y_l, y_c


def moe_ffn(u, w_router, b_router, w_gate, w_up, w_down, ws_gate, ws_up, ws_down):
    n, d = u.shape
    scores = jax.nn.sigmoid(jnp.einsum('nd,de->ne', u, w_router, preferred_element_type=jnp.float32))
    _, idx = lax.top_k(scores + b_router.astype(jnp.float32), TOP_K)
    sel = jnp.take_along_axis(scores, idx, axis=-1)
    wts = sel / jnp.sum(sel, axis=-1, keepdims=True) * ROUTED_SCALE
    gates = jnp.zeros((n, N_EXPERTS), jnp.float32).at[jnp.arange(n)[:, None], idx].add(wts).astype(u.dtype)
    pad = (-n) % MOE_CHUNK
    u_blk = jnp.pad(u, ((0, pad), (0, 0))).reshape(-1, MOE_CHUNK, d)
    g_blk = jnp.pad(gates, ((0, pad), (0, 0))).reshape(-1, MOE_CHUNK, N_EXPERTS)

    def routed(args):
        ub, gb = args
        hid = jax.nn.silu(jnp.einsum('nd,edf->nef', ub, w_gate)) * jnp.einsum('nd,edf->nef', ub, w_up)
        return jnp.einsum('nef,efd->nd', hid * gb[..., None], w_down)

    y = lax.map(routed, (u_blk, g_blk)).reshape(-1, d)[:n]
    shared = (jax.nn.silu(u @ ws_gate) * (u @ ws_up)) @ ws_down
    return y + shared


def setup_inputs(seed: int = 0) -> dict:
    key = jax.random.key(seed)
    ks = iter(jax.random.split(key, 48))

    def nrm(shape, scale):
        return jax.random.normal(next(ks), shape, jnp.float32) * scale

    D = D_MODEL
    u = jax.random.uniform(next(ks), (DEPTH, 2, RG_W), jnp.float32, 0.9, 0.999)
    s = u ** (1.0 / RG_C)
    rg_lambda = jnp.log(s) - jnp.log1p(-s)
    return {
        'x': nrm((BATCH, SEQ, D), 1.0),
        'c': nrm((BATCH, D), 1.0),
        'ctx': nrm((BATCH, CTX_LEN, D), 1.0),
        'c_ctx': nrm((D,), 1.0),
        'w_mod': nrm((DEPTH, D, 6 * D), 0.5 * D ** -0.5),
        'b_mod': nrm((DEPTH, 6 * D), 0.02),
        'ln1_g': 1.0 + nrm((DEPTH, D), 0.02),
        'ln1_b': nrm((DEPTH, D), 0.02),
        'ln2_g': 1.0 + nrm((DEPTH, D), 0.02),
        'ln2_b': nrm((DEPTH, D), 0.02),
        'w_in': nrm((DEPTH, D, PROJ_W), D ** -0.5),
        'w_out': nrm((DEPTH, MIX_W, D), DN_BETA * MIX_W ** -0.5),
        'hy_short_w': nrm((DEPTH, HY_SHORT, 3 * HY_W), HY_SHORT ** -0.5),
        'hy_short_b': nrm((DEPTH, 3 * HY_W), 0.02),
        'hy_f_w1': nrm((DEPTH, HY_EMB, HY_FILTER_W), HY_EMB ** -0.5),
        'hy_f_b1': nrm((DEPTH, HY_FILTER_W), 0.02),
        'hy_f_freq': 1.0 + nrm((DEPTH, HY_FILTER_W), 0.1),
        'hy_f_w2': nrm((DEPTH, HY_FILTER_W, HY_FILTER_W), HY_FILTER_W ** -0.5),
        'hy_f_b2': nrm((DEPTH, HY_FILTER_W), 0.02),
        'hy_f_w3': nrm((DEPTH, HY_FILTER_W, 2 * HY_ORDER * HY_W), 0.04 * HY_FILTER_W ** -0.5),
        'hy_skip': nrm((DEPTH, HY_ORDER, HY_W), 1.0),
        'q_norm': 1.0 + nrm((DEPTH, HEAD_DIM), 0.02),
        'k_norm': 1.0 + nrm((DEPTH, HEAD_DIM), 0.02),
        'rg_conv_w': nrm((DEPTH, RG_CONV, RG_W), RG_CONV ** -0.5),
        'rg_conv_b': nrm((DEPTH, RG_W), 0.02),
        'rg_lambda': rg_lambda,
        'rg_w_a': nrm((DEPTH, 2, RG_BLOCKS, RG_BW, RG_BW), RG_BW ** -0.5),
        'rg_b_a': nrm((DEPTH, 2, RG_W), 0.02),
        'rg_w_x': nrm((DEPTH, 2, RG_BLOCKS, RG_BW, RG_BW), RG_BW ** -0.5),
        'rg_b_x': nrm((DEPTH, 2, RG_W), 0.02),
        'w_router': nrm((DEPTH, D, N_EXPERTS), D ** -0.5),
        'b_router': nrm((DEPTH, N_EXPERTS), 0.01),
        'w_gate': nrm((DEPTH, N_EXPERTS, D, EXPERT_FF), D ** -0.5),
        'w_up': nrm((DEPTH, N_EXPERTS, D, EXPERT_FF), D ** -0.5),
        'w_down': nrm((DEPTH, N_EXPERTS, EXPERT_FF, D), DN_BETA * EXPERT_FF ** -0.5),
        'ws_gate': nrm((DEPTH, D, SHARED_FF), D ** -0.5),
        'ws_up': nrm((DEPTH, D, SHARED_FF), D ** -0.5),
        'ws_down': nrm((DEPTH, SHARED_FF, D), DN_BETA * SHARED_FF ** -0.5),
    }


def reference(x, c, ctx, c_ctx, w_mod, b_mod, ln1_g, ln1_b, ln2_g, ln2_b, w_in, w_out,
              hy_short_w, hy_short_b, hy_f_w1, hy_f_b1, hy_f_freq, hy_f_w2, hy_f_b2, hy_f_w3, hy_skip,
              q_norm, k_norm, rg_conv_w, rg_conv_b, rg_lambda, rg_w_a, rg_b_a, rg_w_x, rg_b_x,
              w_router, b_router, w_gate, w_up, w_down, ws_gate, ws_up, ws_down):
    B, T, D = x.shape
    Lc = ctx.shape[1]
    cos, sin = axial_rope(T)
    s_c = jax.nn.silu(c)
    s_cc = jax.nn.silu(c_ctx)
    h, hc = x, ctx
    for l in range(DEPTH):
        last = l == DEPTH - 1
        sh1, sc1, g1, sh2, sc2, g2 = jnp.split((s_c @ w_mod[l] + b_mod[l])[:, None, :], 6, axis=-1)
        n_mod_c = 2 if last else 6
        mod_c = jnp.split(s_cc @ w_mod[l, :, :n_mod_c * D] + b_mod[l, :n_mod_c * D], n_mod_c)

        a_l = h * (1.0 + sc1) + sh1
        a_c = hc * (1.0 + mod_c[1]) + mod_c[0]
        hy_l, q_l, k_l, v_l, rgx_l, rgg_l = split_proj(a_l @ w_in[l])
        if last:
            k_c, v_c, rgx_c = jnp.split(a_c @ w_in[l, :, Q_END:RGX_END], [KV_W, 2 * KV_W], axis=-1)
            q_c = None
            rgg_c = None
        else:
            hy_c, q_c, k_c, v_c, rgx_c, rgg_c = split_proj(a_c @ w_in[l])
        hy_p = (hy_short_w[l], hy_short_b[l], hy_f_w1[l], hy_f_b1[l], hy_f_freq[l],
                hy_f_w2[l], hy_f_b2[l], hy_f_w3[l], hy_skip[l])
        att_l, att_c = attention_mixer(q_l, k_l, v_l, q_c, k_c, v_c, q_norm[l], k_norm[l], cos, sin)
        rg_l, rg_c = rglru_mixer(rgx_l, rgg_l, rgx_c, rgg_c, rg_conv_w[l], rg_conv_b[l],
                                 rg_lambda[l], rg_w_a[l], rg_b_a[l], rg_w_x[l], rg_b_x[l])
        y_l = jnp.concatenate([hyena_mixer(hy_l, *hy_p), att_l, rg_l], axis=-1) @ w_out[l]
        h = layer_norm(DN_ALPHA * h + g1 * y_l, ln1_g[l], ln1_b[l])

        moe_p = (w_router[l], b_router[l], w_gate[l], w_up[l], w_down[l], ws_gate[l], ws_up[l], ws_down[l])
        m_l = (h * (1.0 + sc2) + sh2).reshape(B * T, D)
        if last:
            f_l = moe_ffn(m_l, *moe_p)
        else:
            y_c = jnp.concatenate([hyena_mixer(hy_c, *hy_p), att_c, rg_c], axis=-1) @ w_out[l]
            hc = layer_norm(DN_ALPHA * hc + mod_c[2] * y_c, ln1_g[l], ln1_b[l])
            m_c = (hc * (1.0 + mod_c[4]) + mod_c[3]).reshape(B * Lc, D)
            f = moe_ffn(jnp.concatenate([m_l, m_c], axis=0), *moe_p)
            f_l = f[:B * T]
            hc = layer_norm(DN_ALPHA * hc + mod_c[5] * f[B * T:].reshape(B, Lc, D), ln2_g[l], ln2_b[l])
        h = layer_norm(DN_ALPHA * h + g2 * f_l.reshape(B, T, D), ln2_g[l], ln2_b[l])
    return h
```

```python
from contextlib import ExitStack
import math
import numpy as np
import ml_dtypes
import concourse.bass as bass
import concourse.mybir as mybir
from concourse.bass_utils import run_bass_kernel_spmd

F32 = mybir.dt.float32
BF16 = mybir.dt.bfloat16
AF = mybir.ActivationFunctionType
ALU = mybir.AluOpType
AX = mybir.AxisListType

D = 1024
T = 4096
LC = 256
NTOK = T + LC
NTILE = NTOK // 128
PROJ_W = 2048
N_EXP = 64
DN_ALPHA = (2 * 4) ** 0.25
LN_EPS = 1e-6
QK_EPS = 1e-6
PT_ROWS = 2 + T + 2 + LC + 2
PT_W = 1536


def pt_row(tok):
    return 2 + tok if tok < T else 4 + tok


class Buf:
    __slots__ = ("t", "w", "r")

    def __init__(self, t):
        self.t = t
        self.w = None
        self.r = {}

    def __getitem__(self, idx):
        return self.t[idx]


class Res:
    __slots__ = ("w", "r")

    def __init__(self):
        self.w = None
        self.r = {}


class KB:
    NSLOT = 28

    def __init__(self, nc):
        self.nc = nc
        self.eng = {"pe": nc.tensor, "act": nc.scalar, "dve": nc.vector, "pool": nc.gpsimd, "sp": nc.sync}
        self.sem = {q: nc.alloc_semaphore("sem_" + q) for q in ("pe", "act", "dve", "pool")}
        self.cnt = {q: 0 for q in self.sem}
        self.slots = [nc.alloc_semaphore("dsl%d" % i) for i in range(self.NSLOT)]
        self.slotcnt = [0] * self.NSLOT
        self.nextslot = 0
        self.seen = {q: {} for q in self.eng}

    def _semof(self, key):
        return self.sem[key] if isinstance(key, str) else self.slots[key]

    def _wait(self, q, key, val):
        if q == "pe" and key == "pe":
            return
        if self.seen[q].get(key, 0) >= val:
            return
        self.eng[q].wait_ge(self._semof(key), val)
        self.seen[q][key] = val

    def _deps(self, q, R, W):
        for r in R:
            if r.w is not None:
                self._wait(q, *r.w)
        for w in W:
            if w.w is not None:
                self._wait(q, *w.w)
            for kk, v in w.r.items():
                self._wait(q, kk, v)

    @staticmethod
    def _mark(tok, R, W):
        kk, v = tok
        for r in R:
            if r.r.get(kk, 0) < v:
                r.r[kk] = v
        for w in W:
            w.w = tok
            w.r = {}

    def op(self, q, fn, R=(), W=()):
        self._deps(q, R, W)
        inst = fn()
        self.cnt[q] += 1
        inst.then_inc(self.sem[q], 1)
        self._mark((q, self.cnt[q]), R, W)

    def dma(self, out, in_, R=(), W=(), q="sp", **kw):
        sl = self.nextslot
        self.nextslot = (sl + 1) % self.NSLOT
        if self.slotcnt[sl]:
            self._wait(q, sl, self.slotcnt[sl])
        self._deps(q, R, W)
        self.eng[q].dma_start(out=out, in_=in_, **kw).then_inc(self.slots[sl], 16)
        self.slotcnt[sl] += 16
        self._mark((sl, self.slotcnt[sl]), R, W)

    def barrier(self):
        for q in self.eng:
            for key in self.sem:
                if self.cnt[key]:
                    self._wait(q, key, self.cnt[key])
            for sl in range(self.NSLOT):
                if self.slotcnt[sl]:
                    self._wait(q, sl, self.slotcnt[sl])


def build(L=4, dbg=False, stop_after=None):
    nc = bass.Bass("TRN2", target_bir_lowering=False)
    k = KB(nc)

    class LT:
        def __init__(self, aps):
            self.aps = aps

        def __getitem__(self, idx):
            if isinstance(idx, tuple):
                l_, rest = idx[0], idx[1:]
                return self.aps[l_][rest] if rest else self.aps[l_]
            return self.aps[idx]

    def din(name, shape, dt=F32):
        if name in WEIGHT_NAMES:
            return LT([nc.dram_tensor("%s_%d" % (name, l_), list(shape[1:]), dt, kind="ExternalInput").ap() for l_ in range(shape[0])])
        return nc.dram_tensor(name, list(shape), dt, kind="ExternalInput").ap()

    def dscr(name, shape, dt=F32):
        return nc.dram_tensor(name, list(shape), dt, kind="ExternalOutput" if dbg else "Internal").ap()

    x = din("x", [T, D])
    ctx = din("ctx", [LC, D])
    cc = din("cc", [128, 8, 2])
    w_mod = din("w_mod", [L, D, 6 * D])
    b_mod = din("b_mod", [L, 6 * D])
    bmT = din("bmT", [128, L, 48])
    ln1_g = din("ln1_g", [L, D]); ln1_b = din("ln1_b", [L, D])
    ln2_g = din("ln2_g", [L, D]); ln2_b = din("ln2_b", [L, D])
    w_in = din("w_in", [L, D, PROJ_W])
    w_out = din("w_out", [L, D, D])
    hy_short_w = din("hy_short_w", [L, 3, 768]); hy_short_b = din("hy_short_b", [L, 768])
    hy_f_w1 = din("hy_f_w1", [L, 17, 64]); hy_f_b1 = din("hy_f_b1", [L, 64])
    hy_f_freq = din("hy_f_freq", [L, 64])
    hy_f_w2 = din("hy_f_w2", [L, 64, 64]); hy_f_b2 = din("hy_f_b2", [L, 64])
    hy_f_w3 = din("hy_f_w3", [L, 64, 1024]); hy_skip = din("hy_skip", [L, 2, 256])
    q_norm = din("q_norm", [L, 64]); k_norm = din("k_norm", [L, 64])
    rg_conv_w = din("rg_conv_w", [L, 4, 256]); rg_conv_b = din("rg_conv_b", [L, 256])
    rg_lambda = din("rg_lambda", [L, 2, 256])
    rg_w_a = din("rg_w_a", [L, 2, 4, 64, 64]); rg_b_a = din("rg_b_a", [L, 2, 256])
    rg_w_x = din("rg_w_x", [L, 2, 4, 64, 64]); rg_b_x = din("rg_b_x", [L, 2, 256])
    w_router = din("w_router", [L, D, N_EXP]); b_router = din("b_router", [L, N_EXP])
    w_gate = din("w_gate", [L, N_EXP, D, 256]); w_up = din("w_up", [L, N_EXP, D, 256])
    w_down = din("w_down", [L, N_EXP, 256, D])
    ws_gate = din("ws_gate", [L, D, 256]); ws_up = din("ws_up", [L, D, 256]); ws_down = din("ws_down", [L, 256, D])
    identb_d = din("identb", [128, 128], BF16)
    identf_d = din("identf", [128, 128])
    WT_d = din("WT", [32, 128, 32, 2, 128], BF16)
    WTc_d = din("WTc", [2, 128, 2, 2, 128], BF16)
    SC_d = din("SC", [128, 32]); SCc_d = din("SCc", [128, 2])
    ZT_d = din("ZT", [17, T]); ZTc_d = din("ZTc", [17, LC])
    WIN_d = din("WIN", [T, 256]); WIN2_d = din("WIN2", [T, 256])
    WINc_d = din("WINc", [LC, 256]); WIN2c_d = din("WIN2c", [LC, 256])
    ROPE_d = din("ROPE", [T, 2, 256])
    SEL_d = din("SEL", [65, 65 * 128], BF16)
    ALT_d = din("ALT", [1, 128], BF16)
    W0I_d = din("W0I", [128, 32, 2, 128], BF16)
    W0Ic_d = din("W0Ic", [128, 2, 2, 128], BF16)

    y_out = nc.dram_tensor("y", [T, D], F32, kind="ExternalOutput").ap()

    HD = dscr("HD", [NTOK, D])
    H1D = dscr("H1D", [NTOK, D])
    PT = dscr("PT", [PT_ROWS, PT_W])
    PR = dscr("PR", [512, NTOK])
    RGT = dscr("RGT", [256, NTOK], BF16)
    MIXTOK = dscr("MIXTOK", [NTOK, 768], BF16)
    HX = dscr("HX", [NTOK, 768])
    V1D = dscr("V1D", [NTOK, 256])
    KFD = dscr("KFD", [32, 128, 2, 512])
    KFDc = dscr("KFDc", [2, 128, 2, 512])
    GMOD = dscr("GMOD", [L, 2, 2, D])

    gs = ExitStack()
    _uid = [0]

    def un(name):
        _uid[0] += 1
        return "%s_%d" % (name, _uid[0])

    def gsb(name, shape, dt=F32):
        return Buf(gs.enter_context(nc.sbuf_tensor(un(name), list(shape), dt)))

    MOD = gsb("MOD", [128, L, 48, 2])
    identb = gsb("identb_s", [128, 128], BF16)
    identf = gsb("identf_s", [128, 128])
    k.dma(identb[:], identb_d[:, :], W=[identb])
    k.dma(identf[:], identf_d[:, :], W=[identf])

    def hsrc(l, i):
        if l == 0:
            return x[i * 128:(i + 1) * 128, :] if i < 32 else ctx[(i - 32) * 128:(i - 31) * 128, :]
        return HD[i * 128:(i + 1) * 128, :]

    def colvec(v1d, n0, n):
        return v1d[n0:n0 + n].rearrange("(p o) -> p o", o=1)

    def phase_mod():
        with ExitStack() as es:
            def sb(name, shape, dt=F32):
                return Buf(es.enter_context(nc.sbuf_tensor(un(name), list(shape), dt)))

            def ps(name, shape, dt=F32):
                return Buf(es.enter_context(nc.psum_tensor(un(name), list(shape), dt)))
            ccs = sb("ccs", [128, 8, 2]); sT = sb("sT", [128, 8, 2]); sg = sb("sg", [128, 8, 2])
            bmTs = sb("bmTs", [128, L, 48])
            wbuf = [sb("wm%d" % i, [128, 8, 1024]) for i in range(2)]
            brow = sb("brow", [2, 1024]); rows = sb("rows", [2, 1024])
            psf = [ps("psf%d" % i, [128, 8, 2]) for i in range(2)]
            psr = [ps("psr%d" % i, [2, 512]) for i in range(2)]
            k.dma(ccs[:], cc[:, :, :], W=[ccs])
            k.dma(bmTs[:], bmT[:, :, :], W=[bmTs])
            k.op("act", lambda: nc.scalar.activation(out=sg[:], in_=ccs[:], func=AF.Sigmoid), R=[ccs], W=[sg])
            k.op("dve", lambda: nc.vector.tensor_tensor(out=sT[:], in0=ccs[:], in1=sg[:], op=ALU.mult), R=[ccs, sg], W=[sT])
            it = 0
            for l in range(L):
                for blk in range(6):
                    wb = wbuf[it % 2]; pf = psf[it % 2]; it += 1
                    c0 = blk * 1024
                    k.dma(wb[:], w_mod[l, :, c0:c0 + 1024].rearrange("(k p) n -> p k n", p=128), W=[wb])
                    for ch in range(8):
                        for kk in range(8):
                            k.op("pe", lambda: nc.tensor.matmul(pf[:, ch, :], wb[:, kk, ch * 128:(ch + 1) * 128], sT[:, kk, :],
                                                                start=(kk == 0), stop=(kk == 7)), R=[wb, sT], W=[pf])
                    for j in range(2):
                        k.op("dve", lambda: nc.vector.tensor_tensor(out=MOD[:, l, blk * 8:(blk + 1) * 8, j], in0=pf[:, :, j],
                                                                    in1=bmTs[:, l, blk * 8:(blk + 1) * 8], op=ALU.add),
                             R=[pf, bmTs], W=[MOD])
                    if blk in (1, 4):
                        k.op("dve", lambda: nc.vector.tensor_scalar(out=MOD[:, l, blk * 8:(blk + 1) * 8, :], in0=MOD[:, l, blk * 8:(blk + 1) * 8, :],
                                                                    scalar1=1.0, scalar2=None, op0=ALU.add), R=[MOD], W=[MOD])
                    if blk in (2, 5):
                        k.dma(brow[:], b_mod[l, c0:c0 + 1024].partition_broadcast(2), W=[brow])
                        for nb in range(2):
                            for kk in range(8):
                                k.op("pe", lambda: nc.tensor.matmul(psr[nb][:, :], sT[:, kk, :], wb[:, kk, nb * 512:(nb + 1) * 512],
                                                                    start=(kk == 0), stop=(kk == 7)), R=[wb, sT], W=[psr[nb]])
                            k.op("dve", lambda: nc.vector.tensor_tensor(out=rows[:, nb * 512:(nb + 1) * 512], in0=psr[nb][:, :],
                                                                        in1=brow[:, nb * 512:(nb + 1) * 512], op=ALU.add),
                                 R=[psr[nb], brow], W=[rows])
                        k.dma(GMOD[l, 0 if blk == 2 else 1, :, :], rows[:], R=[rows])
            k.barrier()

    def phase_inproj(l):
        with ExitStack() as es:
            def sb(name, shape, dt=F32):
                return Buf(es.enter_context(nc.sbuf_tensor(un(name), list(shape), dt)))

            def ps(name, shape, dt=F32):
                return Buf(es.enter_context(nc.psum_tensor(un(name), list(shape), dt)))
            Wst = [sb("wst%d" % i, [128, 8, 512]) for i in range(2)]
            Wb = sb("Wb", [128, 8, PROJ_W], BF16)
            zt = sb("zt", [2, PT_W])
            hf = [sb("hf%d" % i, [128, D]) for i in range(3)]
            hb = [sb("hb%d" % i, [128, D], BF16) for i in range(2)]
            aT = [sb("aT%d" % i, [128, 8, 512], BF16) for i in range(2)]
            ot = [sb("ot%d" % i, [128, PT_W]) for i in range(2)]
            of = [sb("of%d" % i, [128, 512]) for i in range(2)]
            tp = [ps("tp%d" % i, [128, 8, 128], BF16) for i in range(2)]
            po = [ps("po%d" % i, [128, 512]) for i in range(3)]
            pf = [ps("pf%d" % i, [128, 512]) for i in range(2)]
            for cb in range(4):
                st = Wst[cb % 2]
                k.dma(st[:], w_in[l, :, cb * 512:(cb + 1) * 512].rearrange("(k p) n -> p k n", p=128), W=[st])
                k.op("pool", lambda: nc.gpsimd.tensor_copy(out=Wb[:, :, cb * 512:(cb + 1) * 512], in_=st[:]), R=[st], W=[Wb])
            k.op("dve", lambda: nc.vector.memset(zt[:], 0.0), W=[zt])
            for r0 in (0, 2 + T, PT_ROWS - 2):
                k.dma(PT[r0:r0 + 2, :], zt[:], R=[zt])
            ti = 0
            for g in range(9):
                tiles = list(range(4 * g, min(4 * g + 4, NTILE)))
                nt = len(tiles)
                j = 0 if g < 8 else 1
                A = aT[g % 2]
                for s, i in enumerate(tiles):
                    h_f = hf[ti % 3]; h_b = hb[ti % 2]; tpp = tp[ti % 2]; ti += 1
                    k.dma(h_f[:], hsrc(l, i), W=[h_f])
                    k.op("dve", lambda: nc.vector.tensor_copy(out=h_b[:], in_=h_f[:]), R=[h_f], W=[h_b])
                    for kk in range(8):
                        k.op("pe", lambda: nc.tensor.transpose(tpp[:, kk, :], h_b[:, kk * 128:(kk + 1) * 128], identb[:]),
                             R=[h_b, identb], W=[tpp])
                    for kk in range(8):
                        sc = MOD[:, l, 8 + kk, j:j + 1]; sh = MOD[:, l, kk, j:j + 1]
                        if kk % 2 == 0:
                            k.op("act", lambda: nc.scalar.activation(out=A[:, kk, s * 128:(s + 1) * 128], in_=tpp[:, kk, :],
                                                                     func=AF.Identity, bias=sh, scale=sc), R=[tpp, MOD], W=[A])
                        else:
                            k.op("dve", lambda: nc.vector.tensor_scalar(out=A[:, kk, s * 128:(s + 1) * 128], in0=tpp[:, kk, :],
                                                                        scalar1=sc, scalar2=sh, op0=ALU.mult, op1=ALU.add),
                                 R=[tpp, MOD], W=[A])
                for s, i in enumerate(tiles):
                    o_t = ot[i % 2]
                    for nb in range(3):
                        p = po[nb]
                        for kk in range(8):
                            k.op("pe", lambda: nc.tensor.matmul(p[:, :], A[:, kk, s * 128:(s + 1) * 128], Wb[:, kk, nb * 512:(nb + 1) * 512],
                                                                start=(kk == 0), stop=(kk == 7)), R=[A, Wb], W=[p])
                        if nb == 1:
                            k.op("dve", lambda: nc.vector.tensor_copy(out=o_t[:, nb * 512:(nb + 1) * 512], in_=p[:, :]), R=[p], W=[o_t])
                        else:
                            k.op("act", lambda: nc.scalar.copy(out=o_t[:, nb * 512:(nb + 1) * 512], in_=p[:, :]), R=[p], W=[o_t])
                    r0 = pt_row(i * 128)
                    k.dma(PT[r0:r0 + 128, :], o_t[:], R=[o_t])
                n = nt * 128
                for c4 in range(4):
                    p = pf[c4 % 2]; o_f = of[c4 % 2]
                    for kk in range(8):
                        k.op("pe", lambda: nc.tensor.matmul(p[:, 0:n], Wb[:, kk, 1536 + c4 * 128:1536 + (c4 + 1) * 128], A[:, kk, 0:n],
                                                            start=(kk == 0), stop=(kk == 7)), R=[A, Wb], W=[p])
                    k.op("act" if c4 % 2 else "dve",
                         (lambda: nc.scalar.copy(out=o_f[:, 0:n], in_=p[:, 0:n])) if c4 % 2 else
                         (lambda: nc.vector.tensor_copy(out=o_f[:, 0:n], in_=p[:, 0:n])), R=[p], W=[o_f])
                    k.dma(PR[c4 * 128:(c4 + 1) * 128, g * 512:g * 512 + n], o_f[:, 0:n], R=[o_f])
            k.barrier()

    def phase_rg(l):
        NB = PT_ROWS
        LAT0, CTX0 = 2, 4 + T
        with ExitStack() as es:
            def sb(name, shape, dt=F32):
                return Buf(es.enter_context(nc.sbuf_tensor(un(name), list(shape), dt)))

            def ps(name, shape, dt=F32):
                return Buf(es.enter_context(nc.psum_tensor(un(name), list(shape), dt)))
            xp = sb("xp", [128, NB]); gp = sb("gp", [128, NTOK]); u = sb("u", [128, NB])
            Aa = sb("Aa", [128, NB]); Bv = sb("Bv", [128, NB]); HS = sb("HS", [128, NTOK]); tmp = sb("tmpS", [128, NTOK])
            ob = sb("ob", [128, NTOK], BF16)
            cw = sb("cw", [128, 4]); cbv = sb("cbv", [128, 1])
            WA = sb("WA", [128, 128]); WX = sb("WX", [128, 128])
            ba = sb("ba", [128, 1]); bx = sb("bx", [128, 1]); lam = sb("lam", [128, 1]); m8 = sb("m8", [128, 1])
            rT = [sb("rT%d" % i, [128, 512]) for i in range(2)]
            iT = [sb("iT%d" % i, [128, 512]) for i in range(2)]
            t1 = [sb("t1%d" % i, [128, 512]) for i in range(2)]
            pr = [ps("pr%d" % i, [128, 512]) for i in range(2)]
            pi = [ps("pi%d" % i, [128, 512]) for i in range(2)]
            segs = [(LAT0 + b * 512, 512) for b in range(8)] + [(CTX0, 256)]
            for c in range(2):
                ch0 = c * 128
                k.op("dve", lambda: nc.vector.memset(xp[:], 0.0), W=[xp])
                k.dma(xp[:, LAT0:LAT0 + T], PR[ch0:ch0 + 128, 0:T], W=[xp])
                k.dma(xp[:, CTX0:CTX0 + LC], PR[ch0:ch0 + 128, T:NTOK], W=[xp])
                k.dma(gp[:], PR[256 + ch0:256 + ch0 + 128, :], W=[gp])
                k.dma(cw[:], rg_conv_w[l, :, ch0:ch0 + 128].rearrange("j p -> p j"), W=[cw], allow_slow_non_contiguous=True)
                k.dma(cbv[:], colvec(rg_conv_b[l], ch0, 128), W=[cbv], allow_slow_non_contiguous=True)
                n = NB - 4
                k.op("dve", lambda: nc.vector.tensor_scalar(out=u[:, 2:2 + n], in0=xp[:, 1:1 + n], scalar1=cw[:, 0:1], scalar2=cbv[:, 0:1],
                                                            op0=ALU.mult, op1=ALU.add), R=[xp, cw, cbv], W=[u])
                for jj in range(1, 4):
                    k.op("dve", lambda: nc.vector.scalar_tensor_tensor(out=u[:, 2:2 + n], in0=xp[:, 1 + jj:1 + jj + n], scalar=cw[:, jj:jj + 1],
                                                                       in1=u[:, 2:2 + n], op0=ALU.mult, op1=ALU.add), R=[xp, cw, u], W=[u])
                for d in range(2):
                    k.op("dve", lambda: nc.vector.memset(WA[:], 0.0), W=[WA])
                    k.op("dve", lambda: nc.vector.memset(WX[:], 0.0), W=[WX])
                    for hh in range(2):
                        k.dma(WA[hh * 64:(hh + 1) * 64, hh * 64:(hh + 1) * 64], rg_w_a[l, d, 2 * c + hh, :, :], W=[WA])
                        k.dma(WX[hh * 64:(hh + 1) * 64, hh * 64:(hh + 1) * 64], rg_w_x[l, d, 2 * c + hh, :, :], W=[WX])
                    k.dma(ba[:], colvec(rg_b_a[l, d], ch0, 128), W=[ba], allow_slow_non_contiguous=True)
                    k.dma(bx[:], colvec(rg_b_x[l, d], ch0, 128), W=[bx], allow_slow_non_contiguous=True)
                    k.dma(lam[:], colvec(rg_lambda[l, d], ch0, 128), W=[lam], allow_slow_non_contiguous=True)
                    k.op("act", lambda: nc.scalar.activation(out=m8[:], in_=lam[:], func=AF.Exp, scale=-1.0), R=[lam], W=[m8])
                    k.op("act", lambda: nc.scalar.activation(out=m8[:], in_=m8[:], func=AF.Ln, bias=1.0), R=[m8], W=[m8])
                    k.op("dve", lambda: nc.vector.tensor_scalar(out=m8[:], in0=m8[:], scalar1=-8.0, scalar2=None, op0=ALU.mult), R=[m8], W=[m8])
                    for bi, (p0, nn) in enumerate(segs):
                        p_r = pr[bi % 2]; p_i = pi[bi % 2]; r_ = rT[bi % 2]; i_ = iT[bi % 2]; t_ = t1[bi % 2]
                        k.op("pe", lambda: nc.tensor.matmul(p_r[:, 0:nn], WA[:], u[:, p0:p0 + nn], start=True, stop=True), R=[WA, u], W=[p_r])
                        k.op("pe", lambda: nc.tensor.matmul(p_i[:, 0:nn], WX[:], u[:, p0:p0 + nn], start=True, stop=True), R=[WX, u], W=[p_i])
                        k.op("act", lambda: nc.scalar.activation(out=r_[:, 0:nn], in_=p_r[:, 0:nn], func=AF.Sigmoid, bias=ba[:, 0:1]), R=[p_r, ba], W=[r_])
                        k.op("act", lambda: nc.scalar.activation(out=Aa[:, p0:p0 + nn], in_=r_[:, 0:nn], func=AF.Exp, scale=m8[:, 0:1]), R=[r_, m8], W=[Aa])
                        k.op("act", lambda: nc.scalar.activation(out=i_[:, 0:nn], in_=p_i[:, 0:nn], func=AF.Sigmoid, bias=bx[:, 0:1]), R=[p_i, bx], W=[i_])
                        k.op("dve", lambda: nc.vector.tensor_tensor(out=t_[:, 0:nn], in0=Aa[:, p0:p0 + nn], in1=Aa[:, p0:p0 + nn], op=ALU.mult), R=[Aa], W=[t_])
                        k.op("dve", lambda: nc.vector.tensor_scalar(out=t_[:, 0:nn], in0=t_[:, 0:nn], scalar1=-1.0, scalar2=1.0, op0=ALU.mult, op1=ALU.add), R=[t_], W=[t_])
                        k.op("act", lambda: nc.scalar.activation(out=t_[:, 0:nn], in_=t_[:, 0:nn], func=AF.Sqrt), R=[t_], W=[t_])
                        k.op("dve", lambda: nc.vector.tensor_tensor(out=i_[:, 0:nn], in0=i_[:, 0:nn], in1=u[:, p0:p0 + nn], op=ALU.mult), R=[i_, u], W=[i_])
                        k.op("dve", lambda: nc.vector.tensor_tensor(out=Bv[:, p0:p0 + nn], in0=t_[:, 0:nn], in1=i_[:, 0:nn], op=ALU.mult), R=[t_, i_], W=[Bv])
                    dst = HS if d == 0 else tmp
                    if d == 0:
                        k.op("dve", lambda: nc.vector.tensor_tensor_scan(out=dst[:, T:NTOK], data0=Aa[:, CTX0:CTX0 + LC], data1=Bv[:, CTX0:CTX0 + LC],
                                                                         initial=0.0, op0=ALU.mult, op1=ALU.add), R=[Aa, Bv], W=[dst])
                        k.op("dve", lambda: nc.vector.tensor_tensor_scan(out=dst[:, 0:T], data0=Aa[:, LAT0:LAT0 + T], data1=Bv[:, LAT0:LAT0 + T],
                                                                         initial=dst[:, NTOK - 1:NTOK], op0=ALU.mult, op1=ALU.add), R=[Aa, Bv, dst], W=[dst])
                    else:
                        k.op("dve", lambda: nc.vector.tensor_tensor_scan(out=dst[:, T:NTOK][:, ::-1], data0=Aa[:, CTX0:CTX0 + LC][:, ::-1],
                                                                         data1=Bv[:, CTX0:CTX0 + LC][:, ::-1],
                                                                         initial=0.0, op0=ALU.mult, op1=ALU.add), R=[Aa, Bv], W=[dst])
                        k.op("dve", lambda: nc.vector.tensor_tensor_scan(out=dst[:, 0:T][:, ::-1], data0=Aa[:, LAT0:LAT0 + T][:, ::-1],
                                                                         data1=Bv[:, LAT0:LAT0 + T][:, ::-1],
                                                                         initial=dst[:, T:T + 1], op0=ALU.mult, op1=ALU.add), R=[Aa, Bv, dst], W=[dst])
                        k.op("dve", lambda: nc.vector.tensor_tensor(out=HS[:], in0=HS[:], in1=tmp[:], op=ALU.add), R=[HS, tmp], W=[HS])
                k.op("dve", lambda: nc.vector.tensor_tensor(out=tmp[:], in0=gp[:], in1=gp[:], op=ALU.mult), R=[gp], W=[tmp])
                k.op("dve", lambda: nc.vector.tensor_scalar(out=tmp[:], in0=tmp[:], scalar1=0.044715, scalar2=1.0, op0=ALU.mult, op1=ALU.add), R=[tmp], W=[tmp])
                k.op("dve", lambda: nc.vector.tensor_tensor(out=tmp[:], in0=tmp[:], in1=gp[:], op=ALU.mult), R=[tmp, gp], W=[tmp])
                k.op("act", lambda: nc.scalar.activation(out=tmp[:], in_=tmp[:], func=AF.Sigmoid, scale=2.0 * math.sqrt(2.0 / math.pi)), R=[tmp], W=[tmp])
                k.op("dve", lambda: nc.vector.tensor_tensor(out=tmp[:], in0=tmp[:], in1=gp[:], op=ALU.mult), R=[tmp, gp], W=[tmp])
                k.op("dve", lambda: nc.vector.tensor_tensor(out=ob[:], in0=tmp[:], in1=HS[:], op=ALU.mult), R=[tmp, HS], W=[ob])
                k.dma(RGT[ch0:ch0 + 128, :], ob[:], R=[ob])
            k.barrier()

    def phase_attn(l):
        with ExitStack() as es:
            def sb(name, shape, dt=F32):
                return Buf(es.enter_context(nc.sbuf_tensor(un(name), list(shape), dt)))

            def ps(name, shape, dt=F32):
                return Buf(es.enter_context(nc.psum_tensor(un(name), list(shape), dt)))
            QT = sb("QT", [64, 8, NTOK], BF16)
            KT = sb("KT", [64, 2, NTOK], BF16)
            VA = sb("VA", [128, NTILE, 2, 65], BF16)
            qg = sb("qg", [128, 64]); kg = sb("kg", [128, 64])
            qkv = [sb("qkv%d" % i, [128, 768]) for i in range(2)]
            rope = [sb("rope%d" % i, [128, 2, 256]) for i in range(2)]
            sq = sb("sq", [128, 640]); ss = sb("ss", [128, 10]); qn = sb("qn", [128, 640])
            ra = sb("ra", [128, 320]); rb = sb("rb", [128, 320])
            qr = [sb("qr%d" % i, [128, 640], BF16) for i in range(2)]
            esp = ExitStack()
            ptq = [Buf(esp.enter_context(nc.psum_tensor(un("ptq%d" % i), [64, 8, 128], BF16))) for i in range(2)]
            ptk = Buf(esp.enter_context(nc.psum_tensor(un("ptk"), [64, 2, 128], BF16)))
            k.dma(qg[:], q_norm[l].partition_broadcast(128), W=[qg])
            k.dma(kg[:], k_norm[l].partition_broadcast(128), W=[kg])
            k.op("dve", lambda: nc.vector.memset(VA[:], 1.0), W=[VA])
            for i in range(NTILE):
                t_ = qkv[i % 2]; rp = rope[i % 2]; q_r = qr[i % 2]; pq = ptq[i % 2]
                r0 = pt_row(i * 128)
                k.dma(t_[:], PT[r0:r0 + 128, 768:1536], W=[t_])
                if i < 32:
                    k.dma(rp[:], ROPE_d[i * 128:(i + 1) * 128, :, :], W=[rp])
                k.op("dve", lambda: nc.vector.tensor_tensor(out=sq[:], in0=t_[:, 0:640], in1=t_[:, 0:640], op=ALU.mult), R=[t_], W=[sq])
                k.op("dve", lambda: nc.vector.tensor_reduce(out=ss[:], in_=sq[:].rearrange("p (h d) -> p h d", d=64), axis=AX.X, op=ALU.add), R=[sq], W=[ss])
                k.op("dve", lambda: nc.vector.tensor_scalar(out=ss[:], in0=ss[:], scalar1=1.0 / 64.0, scalar2=QK_EPS, op0=ALU.mult, op1=ALU.add), R=[ss], W=[ss])
                k.op("act", lambda: nc.scalar.activation(out=ss[:], in_=ss[:], func=AF.Sqrt), R=[ss], W=[ss])
                k.op("dve", lambda: nc.vector.reciprocal(out=ss[:], in_=ss[:]), R=[ss], W=[ss])
                for hh in range(10):
                    gsrc = qg if hh < 8 else kg
                    dstt = qn if i < 32 else q_r
                    k.op("dve", lambda: nc.vector.scalar_tensor_tensor(out=dstt[:, hh * 64:(hh + 1) * 64], in0=t_[:, hh * 64:(hh + 1) * 64],
                                                                       scalar=ss[:, hh:hh + 1], in1=gsrc[:], op0=ALU.mult, op1=ALU.mult),
                         R=[t_, ss, gsrc], W=[dstt])
                if i < 32:
                    qv = qn[:].rearrange("p (n two) -> p n two", two=2)
                    ov = q_r[:].rearrange("p (n two) -> p n two", two=2)
                    u1 = qv[:, :, 0]; u2 = qv[:, :, 1]
                    cosv = None
                    for (a0, a1, c0_, c1_) in ((0, 256, 0, 256), (256, 320, 0, 64)):
                        cs = rp[:, 0, c0_:c1_]; sn = rp[:, 1, c0_:c1_]
                        k.op("dve", lambda: nc.vector.tensor_tensor(out=ra[:, a0:a1], in0=u1[:, a0:a1], in1=cs, op=ALU.mult), R=[qn, rp], W=[ra])
                        k.op("dve", lambda: nc.vector.tensor_tensor(out=rb[:, a0:a1], in0=u2[:, a0:a1], in1=sn, op=ALU.mult), R=[qn, rp], W=[rb])
                        k.op("dve", lambda: nc.vector.tensor_tensor(out=ov[:, a0:a1, 0], in0=ra[:, a0:a1], in1=rb[:, a0:a1], op=ALU.subtract), R=[ra, rb], W=[q_r])
                        k.op("dve", lambda: nc.vector.tensor_tensor(out=ra[:, a0:a1], in0=u1[:, a0:a1], in1=sn, op=ALU.mult), R=[qn, rp], W=[ra])
                        k.op("dve", lambda: nc.vector.tensor_tensor(out=rb[:, a0:a1], in0=u2[:, a0:a1], in1=cs, op=ALU.mult), R=[qn, rp], W=[rb])
                        k.op("dve", lambda: nc.vector.tensor_tensor(out=ov[:, a0:a1, 1], in0=ra[:, a0:a1], in1=rb[:, a0:a1], op=ALU.add), R=[ra, rb], W=[q_r])
                for hh in range(8):
                    k.op("pe", lambda: nc.tensor.transpose(pq[:, hh, :], q_r[:, hh * 64:(hh + 1) * 64], identb[:]), R=[q_r, identb], W=[pq])
                for hh in range(2):
                    k.op("pe", lambda: nc.tensor.transpose(ptk[:, hh, :], q_r[:, (8 + hh) * 64:(9 + hh) * 64], identb[:]), R=[q_r, identb], W=[ptk])
                k.op("act", lambda: nc.scalar.copy(out=QT[:, :, i * 128:(i + 1) * 128], in_=pq[:, :, :]), R=[pq], W=[QT])
                k.op("act", lambda: nc.scalar.copy(out=KT[:, :, i * 128:(i + 1) * 128], in_=ptk[:, :, :]), R=[ptk], W=[KT])
                k.op("dve", lambda: nc.vector.tensor_copy(out=VA[:, i, :, 0:64], in_=t_[:, 640:768].rearrange("p (h d) -> p h d", d=64)), R=[t_], W=[VA])
            k.barrier()
            esp.close()
            pS = [ps("pS%d" % i, [128, 512]) for i in range(2)]
            pO = [ps("pO%d" % i, [128, 512]) for i in range(4)]
            Pb = [sb("Pb%d" % i, [128, 512], BF16) for i in range(3)]
            rd = sb("rd", [128, 4]);
            att = [sb("att%d" % i, [128, 4, 512], BF16) for i in range(2)]
            it = 0; io = 0
            jobs = [(qs * 512, 512, list(range(NTILE))) for qs in range(8)] + [(T, 256, [32, 33])]
            for ji, (q0, nq, kchunks) in enumerate(jobs):
                at = att[ji % 2]
                nsub = nq // 128
                for h in range(8):
                    kvh = h // 4
                    io += 1
                    def emit_pv(item):
                        P_, c_, ci_ = item
                        for s in range(nsub):
                            k.op("pe", lambda: nc.tensor.matmul(pO[s][:, 0:65], P_[:, s * 128:(s + 1) * 128], VA[:, c_, kvh, :],
                                                                start=(ci_ == 0), stop=(ci_ == len(kchunks) - 1)), R=[P_, VA], W=[pO[s]])
                    prev = None
                    for ci, c in enumerate(kchunks):
                        S = pS[it % 2]; P = Pb[it % 3]; it += 1
                        k.op("pe", lambda: nc.tensor.matmul(S[:, 0:nq], KT[:, kvh, c * 128:(c + 1) * 128], QT[:, h, q0:q0 + nq], start=True, stop=True),
                             R=[KT, QT], W=[S])
                        k.op("act", lambda: nc.scalar.activation(out=P[:, 0:nq], in_=S[:, 0:nq], func=AF.Exp, scale=0.125), R=[S], W=[P])
                        if prev is not None:
                            emit_pv(prev)
                        prev = (P, c, ci)
                    emit_pv(prev)
                    for s in range(nsub):
                        k.op("dve", lambda: nc.vector.reciprocal(out=rd[:, s:s + 1], in_=pO[s][:, 64:65]), R=[pO[s]], W=[rd])
                        k.op("dve", lambda: nc.vector.tensor_scalar(out=at[:, s, h * 64:(h + 1) * 64], in0=pO[s][:, 0:64], scalar1=rd[:, s:s + 1],
                                                                    scalar2=None, op0=ALU.mult), R=[pO[s], rd], W=[at])
                for s in range(nsub):
                    k.dma(MIXTOK[q0 + s * 128:q0 + (s + 1) * 128, 256:768], at[:, s, :], R=[at])
            k.barrier()

    def hyena_seq(l, es, sb, ps, Lq, tok0, WTd, W0Id, SCd, ZTd, WINd, WIN2d, KF, tag):
        nch = Lq // 128
        slab = [sb("slab%d" % i + tag, [128, nch, 2, 128], BF16) for i in range(3)]
        scs = sb("scs" + tag, [128, nch])
        k.dma(scs[:], SCd[:, :], W=[scs])
        pX = [ps("pX%d" % i + tag, [128, 512]) for i in range(4)]
        pN = ps("pN" + tag, [1, 512])
        kfo = [sb("kfo%d" % i + tag, [128, 2, 512]) for i in range(2)]
        altr = sb("altr" + tag, [1, 128], BF16)
        k.dma(altr[:], ALT_d[:, :], W=[altr])
        si = [0]

        def load_slab(j, inv=False):
            s_ = slab[si[0] % 3]; si[0] += 1
            k.dma(s_[:], W0Id[:, :, :, :] if (inv and j == 0) else WTd[j, :, :, :, :], W=[s_])
            if inv:
                k.dma(s_[0:1, 0, 1, :], ALT_d[:, :], W=[s_])
            return s_
        with ExitStack() as es2:
            def sb2(name, shape, dt=F32):
                return Buf(es2.enter_context(nc.sbuf_tensor(un(name + tag), list(shape), dt)))
            KS = sb2("KS", [128, nch, 512], BF16); KDf = sb2("KD", [128, nch, 512], BF16)
            h2T = sb2("h2T", [64, Lq])
            zT = sb2("zT", [17, Lq]); w1 = sb2("w1", [17, 64]); w2 = sb2("w2", [64, 64]); w3 = sb2("w3", [64, 1024])
            fr = sb2("fr", [64, 1]); b1 = sb2("b1", [64, 1]); b2 = sb2("b2", [64, 1]); fb1 = sb2("fb1", [64, 1]); fb2 = sb2("fb2", [64, 1])
            h1T = sb2("h1T", [64, Lq]); targ = [sb2("targ%d" % i, [64, 512]) for i in range(2)]
            win = [sb2("win%d" % i, [128, 256]) for i in range(2)]; win2 = [sb2("win2%d" % i, [128, 256]) for i in range(2)]
            fa = [sb2("fa%d" % i, [128, 512]) for i in range(2)]; fb_ = [sb2("fb_%d" % i, [128, 512]) for i in range(2)]
            pm = pX
            k.dma(zT[:], ZTd[:, :], W=[zT]); k.dma(w1[:], hy_f_w1[l, :, :], W=[w1]); k.dma(w2[:], hy_f_w2[l, :, :], W=[w2])
            k.dma(w3[:], hy_f_w3[l, :, :], W=[w3])
            k.dma(fr[:], colvec(hy_f_freq[l], 0, 64), W=[fr], allow_slow_non_contiguous=True)
            k.dma(b1[:], colvec(hy_f_b1[l], 0, 64), W=[b1], allow_slow_non_contiguous=True)
            k.dma(b2[:], colvec(hy_f_b2[l], 0, 64), W=[b2], allow_slow_non_contiguous=True)
            OFF = 0.0
            kiq = sb2("kiq", [64, 512], mybir.dt.int32); kfq = sb2("kfq", [64, 512])
            for (bsrc, fdst) in ((b1, fb1), (b2, fb2)):
                k.op("dve", lambda: nc.vector.tensor_tensor(out=fdst[:], in0=bsrc[:], in1=fr[:], op=ALU.mult), R=[bsrc, fr], W=[fdst])
                k.op("dve", lambda: nc.vector.tensor_scalar(out=fdst[:], in0=fdst[:], scalar1=OFF, scalar2=None, op0=ALU.add), R=[fdst], W=[fdst])
            nblk = max(1, Lq // 512); bw = min(512, Lq)

            def sin_layer(wm, src, dstb, fbb, kdim):
                for b in range(nblk):
                    p = pm[b % 2]; ta = targ[b % 2]
                    k.op("pe", lambda: nc.tensor.matmul(p[0:64, 0:bw], wm[:], src[0:kdim, b * bw:(b + 1) * bw], start=True, stop=True), R=[wm, src], W=[p])
                    k.op("dve", lambda: nc.vector.tensor_scalar(out=ta[:, 0:bw], in0=p[0:64, 0:bw], scalar1=fr[:, 0:1], scalar2=fbb[:, 0:1],
                                                                op0=ALU.mult, op1=ALU.add), R=[p, fr, fbb], W=[ta])
                    k.op("dve", lambda: nc.vector.tensor_scalar(out=kiq[:, 0:bw], in0=ta[:, 0:bw], scalar1=1.0 / (2.0 * math.pi), scalar2=None, op0=ALU.mult), R=[ta], W=[kiq])
                    k.op("dve", lambda: nc.vector.tensor_copy(out=kfq[:, 0:bw], in_=kiq[:, 0:bw]), R=[kiq], W=[kfq])
                    k.op("dve", lambda: nc.vector.scalar_tensor_tensor(out=ta[:, 0:bw], in0=kfq[:, 0:bw], scalar=-2.0 * math.pi, in1=ta[:, 0:bw], op0=ALU.mult, op1=ALU.add), R=[kfq, ta], W=[ta])
                    k.op("dve", lambda: nc.vector.tensor_scalar(out=kfq[:, 0:bw], in0=ta[:, 0:bw], scalar1=-math.pi, scalar2=None, op0=ALU.is_lt), R=[ta], W=[kfq])
                    k.op("dve", lambda: nc.vector.scalar_tensor_tensor(out=ta[:, 0:bw], in0=kfq[:, 0:bw], scalar=2.0 * math.pi, in1=ta[:, 0:bw], op0=ALU.mult, op1=ALU.add), R=[kfq, ta], W=[ta])
                    k.op("dve", lambda: nc.vector.tensor_scalar(out=kfq[:, 0:bw], in0=ta[:, 0:bw], scalar1=math.pi, scalar2=None, op0=ALU.is_gt), R=[ta], W=[kfq])
                    k.op("dve", lambda: nc.vector.scalar_tensor_tensor(out=ta[:, 0:bw], in0=kfq[:, 0:bw], scalar=-2.0 * math.pi, in1=ta[:, 0:bw], op0=ALU.mult, op1=ALU.add), R=[kfq, ta], W=[ta])
                    k.op("dve", lambda: nc.vector.tensor_scalar(out=ta[:, 0:bw], in0=ta[:, 0:bw], scalar1=-3.1415925, scalar2=3.1415925,
                                                                op0=ALU.max, op1=ALU.min), R=[ta], W=[ta])
                    k.op("act", lambda: nc.scalar.activation(out=dstb[:, b * bw:(b + 1) * bw], in_=ta[:, 0:bw], func=AF.Sin), R=[ta], W=[dstb])
            sin_layer(w1, zT, h1T, fb1, 17)
            sin_layer(w2, h1T, h2T, fb2, 64)
            for tc in range(nch):
                wn = win[tc % 2]; wn2 = win2[tc % 2]; A_ = fa[tc % 2]; B_ = fb_[tc % 2]
                k.dma(wn[:], WINd[tc * 128:(tc + 1) * 128, :], W=[wn]); k.dma(wn2[:], WIN2d[tc * 128:(tc + 1) * 128, :], W=[wn2])
                p0 = pm[2]; p1 = pm[3]
                k.op("pe", lambda: nc.tensor.matmul(p0[:, :], h2T[:, tc * 128:(tc + 1) * 128], w3[:, 0:512], start=True, stop=True), R=[h2T, w3], W=[p0])
                k.op("pe", lambda: nc.tensor.matmul(p1[:, :], h2T[:, tc * 128:(tc + 1) * 128], w3[:, 512:1024], start=True, stop=True), R=[h2T, w3], W=[p1])
                for o in range(2):
                    k.op("dve", lambda: nc.vector.tensor_tensor(out=A_[:, o * 256:(o + 1) * 256], in0=p0[:, o * 256:(o + 1) * 256], in1=wn[:], op=ALU.mult), R=[p0, wn], W=[A_])
                    k.op("dve", lambda: nc.vector.tensor_tensor(out=B_[:, o * 256:(o + 1) * 256], in0=p1[:, o * 256:(o + 1) * 256], in1=wn2[:], op=ALU.mult), R=[p1, wn2], W=[B_])
                k.op("pool", lambda: nc.gpsimd.tensor_tensor(out=KS[:, tc, :], in0=A_[:], in1=B_[:], op=ALU.add), R=[A_, B_], W=[KS])
                k.op("pool", lambda: nc.gpsimd.tensor_tensor(out=KDf[:, tc, :], in0=A_[:], in1=B_[:], op=ALU.subtract), R=[A_, B_], W=[KDf])
            nxt = load_slab(0)
            for j in range(nch):
                s_ = nxt
                if j + 1 < nch:
                    nxt = load_slab(j + 1)
                pre = pX[(2 * j) % 4]; pim = pX[(2 * j + 1) % 4]; ko = kfo[j % 2]
                for tc in range(nch):
                    k.op("pe", lambda: nc.tensor.matmul(pre[:, :], s_[:, tc, 0, :], KS[:, tc, :], start=(tc == 0), stop=(tc == nch - 1)), R=[s_, KS], W=[pre])
                for tc in range(nch):
                    k.op("pe", lambda: nc.tensor.matmul(pim[:, :], s_[:, tc, 1, :], KDf[:, tc, :], start=(tc == 0), stop=(tc == nch - 1)), R=[s_, KDf], W=[pim])
                if j == 0:
                    for tc in range(nch):
                        k.op("pe", lambda: nc.tensor.matmul(pN[:, :], s_[:, tc, 1, 0:1], KS[:, tc, :], start=(tc == 0), stop=(tc == nch - 1)), R=[s_, KS], W=[pN])
                k.op("act", lambda: nc.scalar.activation(out=ko[:, 0, :], in_=pre[:, :], func=AF.Identity, scale=scs[:, j:j + 1]), R=[pre, scs], W=[ko])
                k.op("dve", lambda: nc.vector.tensor_scalar(out=ko[:, 1, :], in0=pim[:, :], scalar1=scs[:, j:j + 1], scalar2=None, op0=ALU.mult), R=[pim, scs], W=[ko])
                if j == 0:
                    k.op("dve", lambda: nc.vector.tensor_scalar(out=ko[0:1, 1, :], in0=pN[:, :], scalar1=scs[0:1, 0:1], scalar2=None, op0=ALU.mult), R=[pN, scs], W=[ko])
                k.dma(KF[j, :, :, :], ko[:], R=[ko])
            k.barrier()
        V = sb("V" + tag, [128, nch, 256], BF16)
        Y = sb("Y" + tag, [128, nch, 2, 256], BF16)
        wsb = sb("wsb" + tag, [128, 3, 768]); bsb = sb("bsb" + tag, [128, 768]); skb = sb("skb" + tag, [128, 2, 256])
        for jj in range(3):
            k.dma(wsb[:, jj, :], hy_short_w[l, jj, :].partition_broadcast(128), W=[wsb])
        k.dma(bsb[:], hy_short_b[l].partition_broadcast(128), W=[bsb])
        for o in range(2):
            k.dma(skb[:, o, :], hy_skip[l, o, :].partition_broadcast(128), W=[skb])
        pin = [[sb("pin%d_%d" % (a, b) + tag, [128, 768]) for b in range(3)] for a in range(2)]
        cacc = [sb("cacc%d" % i + tag, [128, 768]) for i in range(2)]
        ctmp = [sb("ctmp%d" % i + tag, [128, 768]) for i in range(2)]
        for tc in range(nch):
            tok = tok0 + tc * 128
            r0 = pt_row(tok)
            pp = pin[tc % 2]; ac = cacc[tc % 2]; tm = ctmp[tc % 2]
            for jj in range(3):
                k.dma(pp[jj][:], PT[r0 + jj - 1:r0 + jj - 1 + 128, 0:768], W=[pp[jj]])
            k.op("dve", lambda: nc.vector.tensor_tensor(out=ac[:], in0=pp[0][:], in1=wsb[:, 0, :], op=ALU.mult), R=[pp[0], wsb], W=[ac])
            k.op("pool", lambda: nc.gpsimd.tensor_tensor(out=tm[:], in0=pp[1][:], in1=wsb[:, 1, :], op=ALU.mult), R=[pp[1], wsb], W=[tm])
            k.op("dve", lambda: nc.vector.tensor_tensor(out=ac[:], in0=ac[:], in1=tm[:], op=ALU.add), R=[ac, tm], W=[ac])
            k.op("pool", lambda: nc.gpsimd.tensor_tensor(out=tm[:], in0=pp[2][:], in1=wsb[:, 2, :], op=ALU.mult), R=[pp[2], wsb], W=[tm])
            k.op("dve", lambda: nc.vector.tensor_tensor(out=ac[:], in0=ac[:], in1=tm[:], op=ALU.add), R=[ac, tm], W=[ac])
            k.op("dve", lambda: nc.vector.tensor_tensor(out=ac[:], in0=ac[:], in1=bsb[:], op=ALU.add), R=[ac, bsb], W=[ac])
            k.op("act", lambda: nc.scalar.copy(out=V[:, tc, :], in_=ac[:, 512:768]), R=[ac], W=[V])
            k.dma(HX[tok:tok + 128, :], ac[:], R=[ac])
        k.barrier()
        xs = [sb("xs%d" % i + tag, [128, 2, 256]) for i in range(2)]
        kin = [sb("kin%d" % i + tag, [128, 2, 256]) for i in range(2)]
        ta_ = [sb("ta%d" % i + tag, [128, 256]) for i in range(2)]
        tb_ = [sb("tb%d" % i + tag, [128, 256]) for i in range(2)]
        vin = [sb("vin%d" % i + tag, [128, 256]) for i in range(2)]
        gin = [sb("gin%d" % i + tag, [128, 256]) for i in range(2)]
        yv = [sb("yv%d" % i + tag, [128, 256]) for i in range(2)]
        ybf = [sb("ybf%d" % i + tag, [128, 256], BF16) for i in range(2)]
        nyq = sb("nyq" + tag, [1, 256])
        for n in range(2):
            nxt = load_slab(0)
            for j in range(nch):
                s_ = nxt
                nxt = load_slab(j + 1) if j + 1 < nch else load_slab(0, inv=True)
                pre = pX[(2 * j) % 4]; pim = pX[(2 * j + 1) % 4]
                X = xs[j % 2]; Kc = kin[j % 2]; a_ = ta_[j % 2]; b_ = tb_[j % 2]
                k.dma(Kc[:], KF[j, :, :, n * 256:(n + 1) * 256], W=[Kc])
                for tc in range(nch):
                    k.op("pe", lambda: nc.tensor.matmul(pre[:, 0:256], s_[:, tc, 0, :], V[:, tc, :], start=(tc == 0), stop=(tc == nch - 1)), R=[s_, V], W=[pre])
                for tc in range(nch):
                    k.op("pe", lambda: nc.tensor.matmul(pim[:, 0:256], s_[:, tc, 1, :], V[:, tc, :], start=(tc == 0), stop=(tc == nch - 1)), R=[s_, V], W=[pim])
                k.op("act", lambda: nc.scalar.copy(out=X[:, 0, :], in_=pre[:, 0:256]), R=[pre], W=[X])
                k.op("act", lambda: nc.scalar.copy(out=X[:, 1, :], in_=pim[:, 0:256]), R=[pim], W=[X])
                k.op("dve", lambda: nc.vector.tensor_tensor(out=a_[:], in0=X[:, 0, :], in1=Kc[:, 0, :], op=ALU.mult), R=[X, Kc], W=[a_])
                k.op("pool", lambda: nc.gpsimd.tensor_tensor(out=b_[:], in0=X[:, 1, :], in1=Kc[:, 1, :], op=ALU.mult), R=[X, Kc], W=[b_])
                k.op("dve", lambda: nc.vector.tensor_tensor(out=Y[:, j, 0, :], in0=a_[:], in1=b_[:], op=ALU.subtract), R=[a_, b_], W=[Y])
                if j == 0:
                    k.op("dve", lambda: nc.vector.tensor_copy(out=Y[0:1, j, 0, :], in_=a_[0:1, :]), R=[a_], W=[Y])
                    k.op("dve", lambda: nc.vector.tensor_copy(out=nyq[:], in_=b_[0:1, :]), R=[b_], W=[nyq])
                k.op("dve", lambda: nc.vector.tensor_tensor(out=a_[:], in0=X[:, 0, :], in1=Kc[:, 1, :], op=ALU.mult), R=[X, Kc], W=[a_])
                k.op("pool", lambda: nc.gpsimd.tensor_tensor(out=b_[:], in0=X[:, 1, :], in1=Kc[:, 0, :], op=ALU.mult), R=[X, Kc], W=[b_])
                k.op("dve", lambda: nc.vector.tensor_tensor(out=Y[:, j, 1, :], in0=a_[:], in1=b_[:], op=ALU.add), R=[a_, b_], W=[Y])
                if j == 0:
                    k.op("dve", lambda: nc.vector.tensor_copy(out=Y[0:1, j, 1, :], in_=nyq[:]), R=[nyq], W=[Y])
            for i in range(nch):
                s_ = nxt
                if i + 1 < nch:
                    nxt = load_slab(i + 1, inv=True)
                po_ = pX[i % 4]
                tok = tok0 + i * 128
                vi = vin[i % 2]; gi = gin[i % 2]; y_ = yv[i % 2]; yb = ybf[i % 2]
                if n == 0:
                    k.dma(vi[:], HX[tok:tok + 128, 512:768], W=[vi])
                    k.dma(gi[:], HX[tok:tok + 128, 0:256], W=[gi])
                else:
                    k.dma(vi[:], V1D[tok:tok + 128, :], W=[vi])
                    k.dma(gi[:], HX[tok:tok + 128, 256:512], W=[gi])
                for fc in range(nch):
                    k.op("pe", lambda: nc.tensor.matmul(po_[:, 0:256], s_[:, fc, 0, :], Y[:, fc, 0, :], start=(fc == 0), stop=False), R=[s_, Y], W=[po_])
                    k.op("pe", lambda: nc.tensor.matmul(po_[:, 0:256], s_[:, fc, 1, :], Y[:, fc, 1, :], start=False, stop=(fc == nch - 1)), R=[s_, Y], W=[po_])
                k.op("pool", lambda: nc.gpsimd.tensor_tensor(out=vi[:], in0=vi[:], in1=skb[:, n, :], op=ALU.mult), R=[vi, skb], W=[vi])
                k.op("dve", lambda: nc.vector.tensor_tensor(out=y_[:], in0=po_[:, 0:256], in1=vi[:], op=ALU.add), R=[po_, vi], W=[y_])
                if n == 0:
                    k.op("dve", lambda: nc.vector.tensor_tensor(out=y_[:], in0=y_[:], in1=gi[:], op=ALU.mult), R=[y_, gi], W=[y_])
                    k.op("act", lambda: nc.scalar.copy(out=V[:, i, :], in_=y_[:]), R=[y_], W=[V])
                    k.dma(V1D[tok:tok + 128, :], y_[:], R=[y_])
                else:
                    k.op("dve", lambda: nc.vector.tensor_tensor(out=yb[:], in0=y_[:], in1=gi[:], op=ALU.mult), R=[y_, gi], W=[yb])
                    k.dma(MIXTOK[tok:tok + 128, 0:256], yb[:], R=[yb])
            k.barrier()

    def phase_hyena(l):
        for (Lq, tok0, WTd, W0Id, SCd, ZTd, WINd, WIN2d, KF, tag) in ((T, 0, WT_d, W0I_d, SC_d, ZT_d, WIN_d, WIN2_d, KFD, "L"),
                                                                     (LC, T, WTc_d, W0Ic_d, SCc_d, ZTc_d, WINc_d, WIN2c_d, KFDc, "C")):
            with ExitStack() as es:
                def sb(name, shape, dt=F32):
                    return Buf(es.enter_context(nc.sbuf_tensor(un(name), list(shape), dt)))

                def ps(name, shape, dt=F32):
                    return Buf(es.enter_context(nc.psum_tensor(un(name), list(shape), dt)))
                hyena_seq(l, es, sb, ps, Lq, tok0, WTd, W0Id, SCd, ZTd, WINd, WIN2d, KF, tag)
            k.barrier()

    def resid_ln(sbufs, ysrc, h_t, gb, gam, bet, out_t):
        u_, st, mv = sbufs
        for hf_ in range(2):
            sl = slice(hf_ * 512, (hf_ + 1) * 512)
            yb_, yap = ysrc[hf_]
            k.op("dve", lambda: nc.vector.tensor_tensor(out=u_[:, sl], in0=yap, in1=gb[:, sl], op=ALU.mult), R=[yb_, gb], W=[u_])
        k.op("dve", lambda: nc.vector.scalar_tensor_tensor(out=u_[:], in0=h_t[:], scalar=DN_ALPHA, in1=u_[:], op0=ALU.mult, op1=ALU.add), R=[h_t, u_], W=[u_])
        for hf_ in range(2):
            k.op("dve", lambda: nc.vector.bn_stats(out=st[:, hf_, :], in_=u_[:, hf_ * 512:(hf_ + 1) * 512]), R=[u_], W=[st])
        k.op("dve", lambda: nc.vector.bn_aggr(out=mv[:, 0:2], in_=st[:].rearrange("p a b -> p (a b)")), R=[st], W=[mv])
        k.op("dve", lambda: nc.vector.tensor_scalar(out=mv[:, 1:2], in0=mv[:, 1:2], scalar1=LN_EPS, scalar2=None, op0=ALU.add), R=[mv], W=[mv])
        k.op("act", lambda: nc.scalar.activation(out=mv[:, 1:2], in_=mv[:, 1:2], func=AF.Sqrt), R=[mv], W=[mv])
        k.op("dve", lambda: nc.vector.reciprocal(out=mv[:, 1:2], in_=mv[:, 1:2]), R=[mv], W=[mv])
        k.op("dve", lambda: nc.vector.tensor_scalar(out=u_[:], in0=u_[:], scalar1=mv[:, 0:1], scalar2=mv[:, 1:2], op0=ALU.subtract, op1=ALU.mult), R=[u_, mv], W=[u_])
        k.op("pool", lambda: nc.gpsimd.tensor_tensor(out=u_[:], in0=u_[:], in1=gam[:], op=ALU.mult), R=[u_, gam], W=[u_])
        k.op("pool", lambda: nc.gpsimd.tensor_tensor(out=out_t[:], in0=u_[:], in1=bet[:], op=ALU.add), R=[u_, bet], W=[out_t])

    def phase_outproj(l):
        with ExitStack() as es:
            def sb(name, shape, dt=F32):
                return Buf(es.enter_context(nc.sbuf_tensor(un(name), list(shape), dt)))

            def ps(name, shape, dt=F32):
                return Buf(es.enter_context(nc.psum_tensor(un(name), list(shape), dt)))
            Wst = [sb("wst%d" % i, [128, 8, 512]) for i in range(2)]
            Wb = sb("Wob", [128, 8, D], BF16)
            for cb in range(2):
                st = Wst[cb]
                k.dma(st[:], w_out[l, :, cb * 512:(cb + 1) * 512].rearrange("(k p) n -> p k n", p=128), W=[st])
                k.op("pool", lambda: nc.gpsimd.tensor_copy(out=Wb[:, :, cb * 512:(cb + 1) * 512], in_=st[:]), R=[st], W=[Wb])
            gb = [sb("gb%d" % j, [128, D]) for j in range(2)]
            gam = sb("gam", [128, D]); bet = sb("bet", [128, D])
            for j in range(2):
                k.dma(gb[j][:], GMOD[l, 0, j, :].partition_broadcast(128), W=[gb[j]])
            k.dma(gam[:], ln1_g[l].partition_broadcast(128), W=[gam]); k.dma(bet[:], ln1_b[l].partition_broadcast(128), W=[bet])
            mx = [sb("mx%d" % i, [128, 768], BF16) for i in range(2)]
            mT = [sb("mT%d" % i, [128, 8, 128], BF16) for i in range(2)]
            hf = [sb("hf%d" % i, [128, D]) for i in range(2)]
            ub = [sb("ub%d" % i, [128, D]) for i in range(2)]
            ob = [sb("obo%d" % i, [128, D]) for i in range(2)]
            st6 = [sb("st6%d" % i, [128, 2, 6]) for i in range(2)]; mv = [sb("mv%d" % i, [128, 2]) for i in range(2)]
            tp = [ps("tpo%d" % i, [128, 6, 128], BF16) for i in range(2)]
            py = [[ps("py%d_%d" % (a, b), [128, 512]) for b in range(2)] for a in range(2)]
            for i in range(NTILE):
                j = 0 if i < 32 else 1
                m_ = mx[i % 2]; mt = mT[i % 2]; h_ = hf[i % 2]; tpp = tp[i % 2]; pyy = py[i % 2]
                k.dma(m_[:], MIXTOK[i * 128:(i + 1) * 128, :], W=[m_])
                k.dma(mt[:, 6:8, :], RGT[:, i * 128:(i + 1) * 128].rearrange("(c p) n -> p c n", p=128), W=[mt])
                k.dma(h_[:], hsrc(l, i), W=[h_])
                for kk in range(6):
                    k.op("pe", lambda: nc.tensor.transpose(tpp[:, kk, :], m_[:, kk * 128:(kk + 1) * 128], identb[:]), R=[m_, identb], W=[tpp])
                k.op("act", lambda: nc.scalar.copy(out=mt[:, 0:6, :], in_=tpp[:, :, :]), R=[tpp], W=[mt])
                for nb in range(2):
                    for kk in range(8):
                        k.op("pe", lambda: nc.tensor.matmul(pyy[nb][:, :], mt[:, kk, :], Wb[:, kk, nb * 512:(nb + 1) * 512], start=(kk == 0), stop=(kk == 7)),
                             R=[mt, Wb], W=[pyy[nb]])
                o_ = ob[i % 2]
                resid_ln((ub[i % 2], st6[i % 2], mv[i % 2]), [(pyy[0], pyy[0][:, :]), (pyy[1], pyy[1][:, :])], h_, gb[j], gam, bet, o_)
                k.dma(H1D[i * 128:(i + 1) * 128, :], o_[:], R=[o_])
            k.barrier()

    def phase_moe(l, last):
        passes = [(0, 12), (12, 24), (24, 34)]
        with ExitStack() as es:
            def sb(name, shape, dt=F32):
                return Buf(es.enter_context(nc.sbuf_tensor(un(name), list(shape), dt)))

            def ps(name, shape, dt=F32):
                return Buf(es.enter_context(nc.psum_tensor(un(name), list(shape), dt)))
            yacc = sb("yacc", [128, 12, D])
            mT16 = sb("mT16", [128, 8, 1536], BF16)
            gT = sb("gT", [65, 1536], BF16)
            SEL = sb("SEL", [65, 65 * 128], BF16)
            k.dma(SEL[:], SEL_d[:, :], W=[SEL])
            wst = [sb("west%d" % i, [128, 2048]) for i in range(3)]
            wgb = [sb("wgb%d" % i, [128, 8, 256], BF16) for i in range(2)]
            wub = [sb("wub%d" % i, [128, 8, 256], BF16) for i in range(2)]
            wdb = [sb("wdb%d" % i, [128, 2, D], BF16) for i in range(2)]
            Wr = sb("Wr", [128, 8, 64]); brb = sb("brb", [128, 64])
            k.dma(Wr[:], w_router[l].rearrange("(k p) n -> p k n", p=128), W=[Wr])
            k.dma(brb[:], b_router[l].partition_broadcast(128), W=[brb])
            gb = [sb("g2b%d" % j, [128, D]) for j in range(2)]
            gam = sb("gam2", [128, D]); bet = sb("bet2", [128, D])
            for j in range(2):
                k.dma(gb[j][:], GMOD[l, 1, j, :].partition_broadcast(128), W=[gb[j]])
            k.dma(gam[:], ln2_g[l].partition_broadcast(128), W=[gam]); k.dma(bet[:], ln2_b[l].partition_broadcast(128), W=[bet])
            hf = [sb("hfm%d" % i, [128, D]) for i in range(2)]
            mT32 = sb("mT32", [128, 8, 128])
            sc_ = sb("scr", [128, 64]); sel = sb("selr", [128, 64]); top8 = sb("top8", [128, 8]); den = sb("den", [128, 1])
            G = sb("G", [128, 65])
            sS = [sb("sS%d" % i, [128, 512]) for i in range(2)]
            tS = [sb("tS%d" % i, [128, 512]) for i in range(2)]
            gbs = [sb("gbs%d" % i, [128, 512], BF16) for i in range(2)]
            hT = [[sb("hT%d_%d" % (a, b), [128, 512], BF16) for b in range(2)] for a in range(2)]
            ub = sb("ubm", [128, D]); obm = [sb("obm%d" % i, [128, D]) for i in range(2)]
            st6 = sb("st6m", [128, 2, 6]); mv = sb("mvm", [128, 2])
            pg = [ps("pg%d" % i, [128, 512]) for i in range(2)]
            pu = [ps("pu%d" % i, [128, 512]) for i in range(2)]
            pgb = ps("pgb", [128, 512])
            pd = [ps("pd%d" % i, [128, 512]) for i in range(3)]
            k.op("dve", lambda: nc.vector.memset(G[:], 1.0), W=[G])
            wsti = [0]
            for (t0, t1) in passes:
                ntile = t1 - t0
                ntok = ntile * 128
                for ii, i in enumerate(range(t0, t1)):
                    j = 0 if i < 32 else 1
                    h_ = hf[ii % 2]
                    k.dma(h_[:], H1D[i * 128:(i + 1) * 128, :], W=[h_])
                    for half in range(2):
                        pt_ = pd[half]
                        for k4 in range(4):
                            kk = half * 4 + k4
                            k.op("pe", lambda: nc.tensor.transpose(pt_[:, k4 * 128:(k4 + 1) * 128], h_[:, kk * 128:(kk + 1) * 128], identf[:]),
                                 R=[h_, identf], W=[pt_])
                        for k4 in range(4):
                            kk = half * 4 + k4
                            sc = MOD[:, l, 32 + kk, j:j + 1]; sh = MOD[:, l, 24 + kk, j:j + 1]
                            k.op("act", lambda: nc.scalar.activation(out=mT32[:, kk, :], in_=pt_[:, k4 * 128:(k4 + 1) * 128], func=AF.Identity, bias=sh, scale=sc),
                                 R=[pt_, MOD], W=[mT32])
                            k.op("dve", lambda: nc.vector.tensor_copy(out=mT16[:, kk, ii * 128:(ii + 1) * 128], in_=mT32[:, kk, :]), R=[mT32], W=[mT16])
                    pl = pd[2]
                    for kk in range(8):
                        k.op("pe", lambda: nc.tensor.matmul(pl[:, 0:64], mT32[:, kk, :], Wr[:, kk, :], start=(kk == 0), stop=(kk == 7)), R=[mT32, Wr], W=[pl])
                    k.op("act", lambda: nc.scalar.activation(out=sc_[:], in_=pl[:, 0:64], func=AF.Sigmoid), R=[pl], W=[sc_])
                    k.op("dve", lambda: nc.vector.tensor_tensor(out=sel[:], in0=sc_[:], in1=brb[:], op=ALU.add), R=[sc_, brb], W=[sel])
                    k.op("dve", lambda: nc.vector.max(out=top8[:], in_=sel[:]), R=[sel], W=[top8])
                    k.op("dve", lambda: nc.vector.tensor_scalar(out=sel[:], in0=sel[:], scalar1=top8[:, 7:8], scalar2=None, op0=ALU.is_ge), R=[sel, top8], W=[sel])
                    k.op("dve", lambda: nc.vector.tensor_tensor(out=sel[:], in0=sel[:], in1=sc_[:], op=ALU.mult), R=[sel, sc_], W=[sel])
                    k.op("dve", lambda: nc.vector.tensor_reduce(out=den[:], in_=sel[:], axis=AX.X, op=ALU.add), R=[sel], W=[den])
                    k.op("dve", lambda: nc.vector.reciprocal(out=den[:], in_=den[:]), R=[den], W=[den])
                    k.op("dve", lambda: nc.vector.tensor_scalar(out=G[:, 0:64], in0=sel[:], scalar1=den[:, 0:1], scalar2=2.5, op0=ALU.mult, op1=ALU.mult), R=[sel, den], W=[G])
                    pgt = pg[ii % 2]
                    k.op("pe", lambda: nc.tensor.transpose(pgt[0:65, 0:128], G[:, :], identf[:]), R=[G, identf], W=[pgt])
                    k.op("act", lambda: nc.scalar.copy(out=gT[:, ii * 128:(ii + 1) * 128], in_=pgt[0:65, 0:128]), R=[pgt], W=[gT])
                k.op("dve", lambda: nc.vector.memset(yacc[:], 0.0), W=[yacc])
                blocks = []
                b0 = 0
                while b0 < ntok:
                    nb_ = min(512, ntok - b0)
                    blocks.append((b0, nb_)); b0 += nb_
                pending = None
                wi = 0
                def expert_srcs(e_):
                    return ((w_gate[l, e_], w_up[l, e_], w_down[l, e_]) if e_ < 64 else (ws_gate[l], ws_up[l], ws_down[l]))

                def issue_loads(e_):
                    for mi, src in enumerate(expert_srcs(e_)):
                        st = wst[mi]
                        k.dma(st[:].rearrange("p (k n) -> p k n", n=(256 if mi < 2 else D)), src.rearrange("(k p) n -> p k n", p=128), W=[st])

                def issue_casts(e_):
                    for mi, dstw in enumerate((wgb[e_ % 2], wub[e_ % 2], wdb[e_ % 2])):
                        st = wst[mi]
                        k.op("pool", lambda: nc.gpsimd.tensor_copy(out=dstw[:].rearrange("p k n -> p (k n)"), in_=st[:]), R=[st], W=[dstw])
                issue_loads(0)
                issue_casts(0)
                for e in range(65):
                    Wg_ = wgb[e % 2]; Wu_ = wub[e % 2]; Wd_ = wdb[e % 2]
                    if e + 1 < 65:
                        issue_loads(e + 1)
                    bidx = -1
                    for (bb, nb_) in blocks:
                        hpair = hT[wi % 2]; gb_ = gbs[wi % 2]; wi += 1
                        dn = down_steps(pending, pd, yacc) if pending is not None else iter(())

                        def drain(n_):
                            for _ in range(n_):
                                f_ = next(dn, None)
                                if f_ is None:
                                    return
                                f_()
                        for fc in range(2):
                            g_ = pg[fc]; u_ = pu[fc]; s_ = sS[fc]; t_ = tS[fc]
                            for kk in range(8):
                                k.op("pe", lambda: nc.tensor.matmul(g_[:, 0:nb_], Wg_[:, kk, fc * 128:(fc + 1) * 128], mT16[:, kk, bb:bb + nb_],
                                                                    start=(kk == 0), stop=(kk == 7)), R=[Wg_, mT16], W=[g_])
                            drain(2)
                            for kk in range(8):
                                k.op("pe", lambda: nc.tensor.matmul(u_[:, 0:nb_], Wu_[:, kk, fc * 128:(fc + 1) * 128], mT16[:, kk, bb:bb + nb_],
                                                                    start=(kk == 0), stop=(kk == 7)), R=[Wu_, mT16], W=[u_])
                            if fc == 0:
                                k.op("pe", lambda: nc.tensor.matmul(pgb[:, 0:nb_], SEL[:, e * 128:(e + 1) * 128], gT[:, bb:bb + nb_], start=True, stop=True),
                                     R=[SEL, gT], W=[pgb])
                                k.op("act", lambda: nc.scalar.copy(out=gb_[:, 0:nb_], in_=pgb[:, 0:nb_]), R=[pgb], W=[gb_])
                            k.op("act", lambda: nc.scalar.activation(out=s_[:, 0:nb_], in_=g_[:, 0:nb_], func=AF.Silu), R=[g_], W=[s_])
                            k.op("dve", lambda: nc.vector.tensor_tensor(out=t_[:, 0:nb_], in0=u_[:, 0:nb_], in1=s_[:, 0:nb_], op=ALU.mult), R=[u_, s_], W=[t_])
                            k.op("pool", lambda: nc.gpsimd.tensor_tensor(out=hpair[fc][:, 0:nb_], in0=t_[:, 0:nb_], in1=gb_[:, 0:nb_], op=ALU.mult),
                                 R=[t_, gb_], W=[hpair[fc]])
                            drain(2)
                        drain(100)
                        pending = (hpair, Wd_, bb, nb_)
                        bidx += 1
                        if bidx == 1 and e + 1 < 65:
                            issue_casts(e + 1)
                for f_ in down_steps(pending, pd, yacc):
                    f_()
                for ii, i in enumerate(range(t0, t1)):
                    j = 0 if i < 32 else 1
                    h_ = hf[ii % 2]
                    o_ = obm[ii % 2]
                    k.dma(h_[:], H1D[i * 128:(i + 1) * 128, :], W=[h_])
                    resid_ln((ub, st6, mv), [(yacc, yacc[:, ii, 0:512]), (yacc, yacc[:, ii, 512:1024])], h_, gb[j], gam, bet, o_)
                    if last:
                        if i < 32:
                            k.dma(y_out[i * 128:(i + 1) * 128, :], o_[:], R=[o_])
                    else:
                        k.dma(HD[i * 128:(i + 1) * 128, :], o_[:], R=[o_])
            k.barrier()

    pdi = [0]

    def down_steps(item, pd, yacc):
        hpair, Wd_, bb, nb_ = item
        for sub in range(nb_ // 128):
            ti_ = (bb // 128) + sub
            for dh in range(2):
                def step(sub=sub, ti_=ti_, dh=dh):
                    p = pd[pdi[0] % 3]; pdi[0] += 1
                    for fc in range(2):
                        k.op("pe", lambda: nc.tensor.matmul(p[:, :], hpair[fc][:, sub * 128:(sub + 1) * 128], Wd_[:, fc, dh * 512:(dh + 1) * 512],
                                                            start=(fc == 0), stop=(fc == 1)), R=[hpair[fc], Wd_], W=[p])
                    k.op("dve", lambda: nc.vector.tensor_tensor(out=yacc[:, ti_, dh * 512:(dh + 1) * 512], in0=p[:, :],
                                                                in1=yacc[:, ti_, dh * 512:(dh + 1) * 512], op=ALU.add), R=[p, yacc], W=[yacc])
                yield step

    phase_mod()
    seq = []
    for l in range(L):
        seq += [("inproj", l), ("rg", l), ("attn", l), ("hyena", l), ("outproj", l), ("moe", l)]
    for (ph, l) in seq:
        if ph == "inproj":
            phase_inproj(l)
        elif ph == "rg":
            phase_rg(l)
        elif ph == "attn":
            phase_attn(l)
        elif ph == "hyena":
            phase_hyena(l)
        elif ph == "outproj":
            phase_outproj(l)
        elif ph == "moe":
            phase_moe(l, last=(l == L - 1))
        if stop_after == (ph, l):
            break
    k.barrier()
    gs.close()
    return nc


_CONST = {}


def _consts():
    if _CONST:
        return _CONST
    bf = ml_dtypes.bfloat16

    def dft_tables(Lq):
        N = 2 * Lq
        n = np.arange(Lq, dtype=np.float64)
        ang = 2.0 * np.pi * np.outer(n, n) / N
        C = np.cos(ang)
        S = -np.sin(ang)
        alt = (-1.0) ** n
        S[:, 0] = alt
        nch = Lq // 128
        W = np.stack([C, S], axis=0)
        W = W.reshape(2, nch, 128, nch, 128)
        WTt = np.ascontiguousarray(W.transpose(3, 2, 1, 0, 4)).astype(np.float32).astype(bf)
        sc = np.full((128, nch), 2.0 / N, np.float32)
        sc[0, 0] = 1.0 / N
        W0I = WTt[0].copy()
        W0I[:, :, 1, 0] = 0
        return WTt, sc, W0I

    def filt_tables(Lq):
        t = np.linspace(0.0, 1.0, Lq, dtype=np.float32)[:, None]
        w = (2.0 * np.float32(math.pi) * np.arange(Lq, dtype=np.float32)[:, None] / np.float32(Lq)).astype(np.float32)
        bands = np.linspace(1e-4, 7, 8, dtype=np.float32)
        z = np.concatenate([t, np.cos(bands * w), -np.sin(bands * w)], axis=-1).astype(np.float32)
        mx = math.log(1e-2) / 0.3
        mn = math.log(1e-2) / 1.5
        deltas = np.abs(np.linspace(mn, mx, 256, dtype=np.float32))
        win = np.exp(-t * deltas).astype(np.float32)
        win2 = win.copy()
        win2[0, :] = 0.0
        return np.ascontiguousarray(z.T), win, win2

    WTt, sc, w0i = dft_tables(T)
    WTc, scc, w0ic = dft_tables(LC)
    zt, win, win2 = filt_tables(T)
    ztc, winc, win2c = filt_tables(LC)
    n_rows = T // 64
    row = np.repeat(np.arange(n_rows, dtype=np.float32), 64)
    col = np.tile(np.arange(64, dtype=np.float32), n_rows)
    inv_freq = (np.float32(10000.0) ** (-np.arange(0, 32, 2, dtype=np.float32) / np.float32(32))).astype(np.float32)
    ang = np.concatenate([row[:, None] * inv_freq, col[:, None] * inv_freq], axis=-1).astype(np.float32)
    rope = np.stack([np.tile(np.cos(ang), (1, 8)), np.tile(np.sin(ang), (1, 8))], axis=1).astype(np.float32)
    sel = np.zeros((65, 65, 128), np.float32)
    for e in range(65):
        sel[e, e, :] = 1.0
    _CONST.update(dict(
        identb=np.eye(128, dtype=np.float32).astype(bf), identf=np.eye(128, dtype=np.float32),
        WT=WTt, WTc=WTc, W0I=w0i, W0Ic=w0ic, SC=sc, SCc=scc, ZT=zt, ZTc=ztc, WIN=win, WIN2=win2, WINc=winc, WIN2c=win2c,
        ROPE=rope, SEL=sel.reshape(65, 65 * 128).astype(bf),
        ALT=((-1.0) ** np.arange(128, dtype=np.float32)).reshape(1, 128).astype(bf)))
    return _CONST


WEIGHT_NAMES = ["w_mod", "b_mod", "ln1_g", "ln1_b", "ln2_g", "ln2_b", "w_in", "w_out", "hy_short_w", "hy_short_b",
                "hy_f_w1", "hy_f_b1", "hy_f_freq", "hy_f_w2", "hy_f_b2", "hy_f_w3", "hy_skip", "q_norm", "k_norm",
                "rg_conv_w", "rg_conv_b", "rg_lambda", "rg_w_a", "rg_b_a", "rg_w_x", "rg_b_x", "w_router", "b_router",
                "w_gate", "w_up", "w_down", "ws_gate", "ws_up", "ws_down"]


def make_in_maps(inputs, L, cores):
    cst = _consts()
    shared = {}
    for n in WEIGHT_NAMES:
        arr = np.asarray(inputs[n], dtype=np.float32)
        for l_ in range(L):
            shared["%s_%d" % (n, l_)] = np.ascontiguousarray(arr[l_])
    shared["bmT"] = np.ascontiguousarray(np.asarray(inputs["b_mod"], dtype=np.float32)[:L].reshape(L, 48, 128).transpose(2, 0, 1))
    shared.update(cst)
    c = np.asarray(inputs["c"], np.float32)
    c_ctx = np.asarray(inputs["c_ctx"], np.float32)
    maps = []
    for b in cores:
        m = dict(shared)
        m["x"] = np.ascontiguousarray(np.asarray(inputs["x"], np.float32)[b])
        m["ctx"] = np.ascontiguousarray(np.asarray(inputs["ctx"], np.float32)[b])
        two = np.stack([c[b], c_ctx], axis=-1)
        m["cc"] = np.ascontiguousarray(two.reshape(8, 128, 2).transpose(1, 0, 2))
        maps.append(m)
    return maps


_NC = {}


def kernel(**inputs):
    if "nc" not in _NC:
        _NC["nc"] = build(L=4)
    nc = _NC["nc"]
    maps = make_in_maps(inputs, 4, list(range(8)))
    res = run_bass_kernel_spmd(nc, maps, core_ids=list(range(8)))
    out = np.stack([np.asarray(r["y"], dtype=np.float32) for r in res.results], axis=0)
    return out
```

```python
from contextlib import ExitStack
import math
import numpy as np
import ml_dtypes
import concourse.bass as bass
import concourse.mybir as mybir
from concourse.bass_utils import run_bass_kernel_spmd

F32 = mybir.dt.float32
BF16 = mybir.dt.bfloat16
AF = mybir.ActivationFunctionType
ALU = mybir.AluOpType
AX = mybir.AxisListType

D = 1024
T = 4096
LC = 256
NTOK = T + LC
NTILE = NTOK // 128
PROJ_W = 2048
N_EXP = 64
DN_ALPHA = (2 * 4) ** 0.25
LN_EPS = 1e-6
QK_EPS = 1e-6
PT_ROWS = 2 + T + 2 + LC + 2
PT_W = 1536


def pt_row(tok):
    return 2 + tok if tok < T else 4 + tok


class Buf:
    __slots__ = ("t", "w", "r")

    def __init__(self, t):
        self.t = t
        self.w = None
        self.r = {}

    def __getitem__(self, idx):
        return self.t[idx]


class Res:
    __slots__ = ("w", "r")

    def __init__(self):
        self.w = None
        self.r = {}


class KB:
    NSLOT = 28

    def __init__(self, nc):
        self.nc = nc
        self.eng = {"pe": nc.tensor, "act": nc.scalar, "dve": nc.vector, "pool": nc.gpsimd, "sp": nc.sync}
        self.sem = {q: nc.alloc_semaphore("sem_" + q) for q in ("pe", "act", "dve", "pool")}
        self.cnt = {q: 0 for q in self.sem}
        self.slots = [nc.alloc_semaphore("dsl%d" % i) for i in range(self.NSLOT)]
        self.slotcnt = [0] * self.NSLOT
        self.nextslot = 0
        self.seen = {q: {} for q in self.eng}

    def _semof(self, key):
        return self.sem[key] if isinstance(key, str) else self.slots[key]

    def _wait(self, q, key, val):
        if q == "pe" and key == "pe":
            return
        if self.seen[q].get(key, 0) >= val:
            return
        self.eng[q].wait_ge(self._semof(key), val)
        self.seen[q][key] = val

    def _deps(self, q, R, W):
        for r in R:
            if r.w is not None:
                self._wait(q, *r.w)
        for w in W:
            if w.w is not None:
                self._wait(q, *w.w)
            for kk, v in w.r.items():
                self._wait(q, kk, v)

    @staticmethod
    def _mark(tok, R, W):
        kk, v = tok
        for r in R:
            if r.r.get(kk, 0) < v:
                r.r[kk] = v
        for w in W:
            w.w = tok
            w.r = {}

    def op(self, q, fn, R=(), W=()):
        self._deps(q, R, W)
        inst = fn()
        self.cnt[q] += 1
        inst.then_inc(self.sem[q], 1)
        self._mark((q, self.cnt[q]), R, W)

    def dma(self, out, in_, R=(), W=(), q="sp", **kw):
        sl = self.nextslot
        self.nextslot = (sl + 1) % self.NSLOT
        if self.slotcnt[sl]:
            self._wait(q, sl, self.slotcnt[sl])
        self._deps(q, R, W)
        self.eng[q].dma_start(out=out, in_=in_, **kw).then_inc(self.slots[sl], 16)
        self.slotcnt[sl] += 16
        self._mark((sl, self.slotcnt[sl]), R, W)

    def barrier(self):
        for q in self.eng:
            for key in self.sem:
                if self.cnt[key]:
                    self._wait(q, key, self.cnt[key])
            for sl in range(self.NSLOT):
                if self.slotcnt[sl]:
                    self._wait(q, sl, self.slotcnt[sl])


def build(L=4, dbg=False, stop_after=None):
    nc = bass.Bass("TRN2", target_bir_lowering=False)
    k = KB(nc)

    class LT:
        def __init__(self, aps):
            self.aps = aps

        def __getitem__(self, idx):
            if isinstance(idx, tuple):
                l_, rest = idx[0], idx[1:]
                return self.aps[l_][rest] if rest else self.aps[l_]
            return self.aps[idx]

    def din(name, shape, dt=F32):
        if name in WEIGHT_NAMES:
            return LT([nc.dram_tensor("%s_%d" % (name, l_), list(shape[1:]), dt, kind="ExternalInput").ap() for l_ in range(shape[0])])
        return nc.dram_tensor(name, list(shape), dt, kind="ExternalInput").ap()

    def dscr(name, shape, dt=F32):
        return nc.dram_tensor(name, list(shape), dt, kind="ExternalOutput" if dbg else "Internal").ap()

    x = din("x", [T, D])
    ctx = din("ctx", [LC, D])
    cc = din("cc", [128, 8, 2])
    w_mod = din("w_mod", [L, D, 6 * D])
    b_mod = din("b_mod", [L, 6 * D])
    bmT = din("bmT", [128, L, 48])
    ln1_g = din("ln1_g", [L, D]); ln1_b = din("ln1_b", [L, D])
    ln2_g = din("ln2_g", [L, D]); ln2_b = din("ln2_b", [L, D])
    w_in = din("w_in", [L, D, PROJ_W])
    w_out = din("w_out", [L, D, D])
    hy_short_w = din("hy_short_w", [L, 3, 768]); hy_short_b = din("hy_short_b", [L, 768])
    hy_f_w1 = din("hy_f_w1", [L, 17, 64]); hy_f_b1 = din("hy_f_b1", [L, 64])
    hy_f_freq = din("hy_f_freq", [L, 64])
    hy_f_w2 = din("hy_f_w2", [L, 64, 64]); hy_f_b2 = din("hy_f_b2", [L, 64])
    hy_f_w3 = din("hy_f_w3", [L, 64, 1024]); hy_skip = din("hy_skip", [L, 2, 256])
    q_norm = din("q_norm", [L, 64]); k_norm = din("k_norm", [L, 64])
    rg_conv_w = din("rg_conv_w", [L, 4, 256]); rg_conv_b = din("rg_conv_b", [L, 256])
    rg_lambda = din("rg_lambda", [L, 2, 256])
    rg_w_a = din("rg_w_a", [L, 2, 4, 64, 64]); rg_b_a = din("rg_b_a", [L, 2, 256])
    rg_w_x = din("rg_w_x", [L, 2, 4, 64, 64]); rg_b_x = din("rg_b_x", [L, 2, 256])
    w_router = din("w_router", [L, D, N_EXP]); b_router = din("b_router", [L, N_EXP])
    w_gate = din("w_gate", [L, N_EXP, D, 256]); w_up = din("w_up", [L, N_EXP, D, 256])
    w_down = din("w_down", [L, N_EXP, 256, D])
    ws_gate = din("ws_gate", [L, D, 256]); ws_up = din("ws_up", [L, D, 256]); ws_down = din("ws_down", [L, 256, D])
    identb_d = din("identb", [128, 128], BF16)
    identf_d = din("identf", [128, 128])
    WT_d = din("WT", [32, 128, 32, 2, 128], BF16)
    WTc_d = din("WTc", [2, 128, 2, 2, 128], BF16)
    SC_d = din("SC", [128, 32]); SCc_d = din("SCc", [128, 2])
    ZT_d = din("ZT", [17, T]); ZTc_d = din("ZTc", [17, LC])
    WIN_d = din("WIN", [T, 256]); WIN2_d = din("WIN2", [T, 256])
    WINc_d = din("WINc", [LC, 256]); WIN2c_d = din("WIN2c", [LC, 256])
    ROPE_d = din("ROPE", [T, 2, 256])
    SEL_d = din("SEL", [65, 65 * 128], BF16)
    ALT_d = din("ALT", [1, 128], BF16)
    W0I_d = din("W0I", [128, 32, 2, 128], BF16)
    W0Ic_d = din("W0Ic", [128, 2, 2, 128], BF16)

    y_out = nc.dram_tensor("y", [T, D], F32, kind="ExternalOutput").ap()

    HD = dscr("HD", [NTOK, D])
    H1D = dscr("H1D", [NTOK, D])
    PT = dscr("PT", [PT_ROWS, PT_W])
    PR = dscr("PR", [512, NTOK])
    RGT = dscr("RGT", [256, NTOK], BF16)
    MIXTOK = dscr("MIXTOK", [NTOK, 768], BF16)
    HX = dscr("HX", [NTOK, 768])
    V1D = dscr("V1D", [NTOK, 256])
    KFD = dscr("KFD", [32, 128, 2, 512])
    KFDc = dscr("KFDc", [2, 128, 2, 512])
    GMOD = dscr("GMOD", [L, 2, 2, D])

    gs = ExitStack()
    _uid = [0]

    def un(name):
        _uid[0] += 1
        return "%s_%d" % (name, _uid[0])

    def gsb(name, shape, dt=F32):
        return Buf(gs.enter_context(nc.sbuf_tensor(un(name), list(shape), dt)))

    MOD = gsb("MOD", [128, L, 48, 2])
    identb = gsb("identb_s", [128, 128], BF16)
    identf = gsb("identf_s", [128, 128])
    k.dma(identb[:], identb_d[:, :], W=[identb])
    k.dma(identf[:], identf_d[:, :], W=[identf])

    def hsrc(l, i):
        if l == 0:
            return x[i * 128:(i + 1) * 128, :] if i < 32 else ctx[(i - 32) * 128:(i - 31) * 128, :]
        return HD[i * 128:(i + 1) * 128, :]

    def colvec(v1d, n0, n):
        return v1d[n0:n0 + n].rearrange("(p o) -> p o", o=1)

    def phase_mod():
        with ExitStack() as es:
            def sb(name, shape, dt=F32):
                return Buf(es.enter_context(nc.sbuf_tensor(un(name), list(shape), dt)))

            def ps(name, shape, dt=F32):
                return Buf(es.enter_context(nc.psum_tensor(un(name), list(shape), dt)))
            ccs = sb("ccs", [128, 8, 2]); sT = sb("sT", [128, 8, 2]); sg = sb("sg", [128, 8, 2])
            bmTs = sb("bmTs", [128, L, 48])
            wbuf = [sb("wm%d" % i, [128, 8, 1024]) for i in range(2)]
            brow = sb("brow", [2, 1024]); rows = sb("rows", [2, 1024])
            psf = [ps("psf%d" % i, [128, 8, 2]) for i in range(2)]
            psr = [ps("psr%d" % i, [2, 512]) for i in range(2)]
            k.dma(ccs[:], cc[:, :, :], W=[ccs])
            k.dma(bmTs[:], bmT[:, :, :], W=[bmTs])
            k.op("act", lambda: nc.scalar.activation(out=sg[:], in_=ccs[:], func=AF.Sigmoid), R=[ccs], W=[sg])
            k.op("dve", lambda: nc.vector.tensor_tensor(out=sT[:], in0=ccs[:], in1=sg[:], op=ALU.mult), R=[ccs, sg], W=[sT])
            it = 0
            for l in range(L):
                for blk in range(6):
                    wb = wbuf[it % 2]; pf = psf[it % 2]; it += 1
                    c0 = blk * 1024
                    k.dma(wb[:], w_mod[l, :, c0:c0 + 1024].rearrange("(k p) n -> p k n", p=128), W=[wb])
                    for ch in range(8):
                        for kk in range(8):
                            k.op("pe", lambda: nc.tensor.matmul(pf[:, ch, :], wb[:, kk, ch * 128:(ch + 1) * 128], sT[:, kk, :],
                                                                start=(kk == 0), stop=(kk == 7)), R=[wb, sT], W=[pf])
                    for j in range(2):
                        k.op("dve", lambda: nc.vector.tensor_tensor(out=MOD[:, l, blk * 8:(blk + 1) * 8, j], in0=pf[:, :, j],
                                                                    in1=bmTs[:, l, blk * 8:(blk + 1) * 8], op=ALU.add),
                             R=[pf, bmTs], W=[MOD])
                    if blk in (1, 4):
                        k.op("dve", lambda: nc.vector.tensor_scalar(out=MOD[:, l, blk * 8:(blk + 1) * 8, :], in0=MOD[:, l, blk * 8:(blk + 1) * 8, :],
                                                                    scalar1=1.0, scalar2=None, op0=ALU.add), R=[MOD], W=[MOD])
                    if blk in (2, 5):
                        k.dma(brow[:], b_mod[l, c0:c0 + 1024].partition_broadcast(2), W=[brow])
                        for nb in range(2):
                            for kk in range(8):
                                k.op("pe", lambda: nc.tensor.matmul(psr[nb][:, :], sT[:, kk, :], wb[:, kk, nb * 512:(nb + 1) * 512],
                                                                    start=(kk == 0), stop=(kk == 7)), R=[wb, sT], W=[psr[nb]])
                            k.op("dve", lambda: nc.vector.tensor_tensor(out=rows[:, nb * 512:(nb + 1) * 512], in0=psr[nb][:, :],
                                                                        in1=brow[:, nb * 512:(nb + 1) * 512], op=ALU.add),
                                 R=[psr[nb], brow], W=[rows])
                        k.dma(GMOD[l, 0 if blk == 2 else 1, :, :], rows[:], R=[rows])
            k.barrier()

    def phase_inproj(l):
        with ExitStack() as es:
            def sb(name, shape, dt=F32):
                return Buf(es.enter_context(nc.sbuf_tensor(un(name), list(shape), dt)))

            def ps(name, shape, dt=F32):
                return Buf(es.enter_context(nc.psum_tensor(un(name), list(shape), dt)))
            Wst = [sb("wst%d" % i, [128, 8, 512]) for i in range(2)]
            Wb = sb("Wb", [128, 8, PROJ_W], BF16)
            zt = sb("zt", [2, PT_W])
            hf = [sb("hf%d" % i, [128, D]) for i in range(3)]
            hb = [sb("hb%d" % i, [128, D], BF16) for i in range(2)]
            aT = [sb("aT%d" % i, [128, 8, 512], BF16) for i in range(2)]
            ot = [sb("ot%d" % i, [128, PT_W]) for i in range(2)]
            of = [sb("of%d" % i, [128, 512]) for i in range(2)]
            tp = [ps("tp%d" % i, [128, 8, 128], BF16) for i in range(2)]
            po = [ps("po%d" % i, [128, 512]) for i in range(3)]
            pf = [ps("pf%d" % i, [128, 512]) for i in range(2)]
            for cb in range(4):
                st = Wst[cb % 2]
                k.dma(st[:], w_in[l, :, cb * 512:(cb + 1) * 512].rearrange("(k p) n -> p k n", p=128), W=[st])
                k.op("pool", lambda: nc.gpsimd.tensor_copy(out=Wb[:, :, cb * 512:(cb + 1) * 512], in_=st[:]), R=[st], W=[Wb])
            k.op("dve", lambda: nc.vector.memset(zt[:], 0.0), W=[zt])
            for r0 in (0, 2 + T, PT_ROWS - 2):
                k.dma(PT[r0:r0 + 2, :], zt[:], R=[zt])
            ti = 0
            for g in range(9):
                tiles = list(range(4 * g, min(4 * g + 4, NTILE)))
                nt = len(tiles)
                j = 0 if g < 8 else 1
                A = aT[g % 2]
                for s, i in enumerate(tiles):
                    h_f = hf[ti % 3]; h_b = hb[ti % 2]; tpp = tp[ti % 2]; ti += 1
                    k.dma(h_f[:], hsrc(l, i), W=[h_f])
                    k.op("dve", lambda: nc.vector.tensor_copy(out=h_b[:], in_=h_f[:]), R=[h_f], W=[h_b])
                    for kk in range(8):
                        k.op("pe", lambda: nc.tensor.transpose(tpp[:, kk, :], h_b[:, kk * 128:(kk + 1) * 128], identb[:]),
                             R=[h_b, identb], W=[tpp])
                    for kk in range(8):
                        sc = MOD[:, l, 8 + kk, j:j + 1]; sh = MOD[:, l, kk, j:j + 1]
                        if (ti % 2) == 0:
                            k.op("act", lambda: nc.scalar.activation(out=A[:, kk, s * 128:(s + 1) * 128], in_=tpp[:, kk, :],
                                                                     func=AF.Identity, bias=sh, scale=sc), R=[tpp, MOD], W=[A])
                        else:
                            k.op("dve", lambda: nc.vector.tensor_scalar(out=A[:, kk, s * 128:(s + 1) * 128], in0=tpp[:, kk, :],
                                                                        scalar1=sc, scalar2=sh, op0=ALU.mult, op1=ALU.add),
                                 R=[tpp, MOD], W=[A])
                for s, i in enumerate(tiles):
                    o_t = ot[i % 2]
                    for nb in range(3):
                        p = po[nb]
                        for kk in range(8):
                            k.op("pe", lambda: nc.tensor.matmul(p[:, :], A[:, kk, s * 128:(s + 1) * 128], Wb[:, kk, nb * 512:(nb + 1) * 512],
                                                                start=(kk == 0), stop=(kk == 7)), R=[A, Wb], W=[p])
                        if nb == 1:
                            k.op("dve", lambda: nc.vector.tensor_copy(out=o_t[:, nb * 512:(nb + 1) * 512], in_=p[:, :]), R=[p], W=[o_t])
                        else:
                            k.op("act", lambda: nc.scalar.copy(out=o_t[:, nb * 512:(nb + 1) * 512], in_=p[:, :]), R=[p], W=[o_t])
                    r0 = pt_row(i * 128)
                    k.dma(PT[r0:r0 + 128, :], o_t[:], R=[o_t])
                n = nt * 128
                for c4 in range(4):
                    p = pf[c4 % 2]; o_f = of[c4 % 2]
                    for kk in range(8):
                        k.op("pe", lambda: nc.tensor.matmul(p[:, 0:n], Wb[:, kk, 1536 + c4 * 128:1536 + (c4 + 1) * 128], A[:, kk, 0:n],
                                                            start=(kk == 0), stop=(kk == 7)), R=[A, Wb], W=[p])
                    k.op("act" if c4 % 2 else "dve",
                         (lambda: nc.scalar.copy(out=o_f[:, 0:n], in_=p[:, 0:n])) if c4 % 2 else
                         (lambda: nc.vector.tensor_copy(out=o_f[:, 0:n], in_=p[:, 0:n])), R=[p], W=[o_f])
                    k.dma(PR[c4 * 128:(c4 + 1) * 128, g * 512:g * 512 + n], o_f[:, 0:n], R=[o_f])
            k.barrier()

    def phase_rg(l):
        NB = PT_ROWS
        LAT0, CTX0 = 2, 4 + T
        with ExitStack() as es:
            def sb(name, shape, dt=F32):
                return Buf(es.enter_context(nc.sbuf_tensor(un(name), list(shape), dt)))

            def ps(name, shape, dt=F32):
                return Buf(es.enter_context(nc.psum_tensor(un(name), list(shape), dt)))
            xp = sb("xp", [128, NB]); gp = sb("gp", [128, NTOK]); u = sb("u", [128, NB])
            Aa = sb("Aa", [128, NB]); Bv = sb("Bv", [128, NB]); HS = sb("HS", [128, NTOK]); tmp = sb("tmpS", [128, NTOK])
            ob = sb("ob", [128, NTOK], BF16)
            cw = sb("cw", [128, 4]); cbv = sb("cbv", [128, 1])
            WA = sb("WA", [128, 128]); WX = sb("WX", [128, 128])
            ba = sb("ba", [128, 1]); bx = sb("bx", [128, 1]); lam = sb("lam", [128, 1]); m8 = sb("m8", [128, 1])
            rT = [sb("rT%d" % i, [128, 512]) for i in range(2)]
            iT = [sb("iT%d" % i, [128, 512]) for i in range(2)]
            t1 = [sb("t1%d" % i, [128, 512]) for i in range(2)]
            pr = [ps("pr%d" % i, [128, 512]) for i in range(2)]
            pi = [ps("pi%d" % i, [128, 512]) for i in range(2)]
            segs = [(LAT0 + b * 512, 512) for b in range(8)] + [(CTX0, 256)]
            for c in range(2):
                ch0 = c * 128
                k.op("dve", lambda: nc.vector.memset(xp[:], 0.0), W=[xp])
                k.dma(xp[:, LAT0:LAT0 + T], PR[ch0:ch0 + 128, 0:T], W=[xp])
                k.dma(xp[:, CTX0:CTX0 + LC], PR[ch0:ch0 + 128, T:NTOK], W=[xp])
                k.dma(gp[:], PR[256 + ch0:256 + ch0 + 128, :], W=[gp])
                k.dma(cw[:], rg_conv_w[l, :, ch0:ch0 + 128].rearrange("j p -> p j"), W=[cw], allow_slow_non_contiguous=True)
                k.dma(cbv[:], colvec(rg_conv_b[l], ch0, 128), W=[cbv], allow_slow_non_contiguous=True)
                n = NB - 4
                k.op("dve", lambda: nc.vector.tensor_scalar(out=u[:, 2:2 + n], in0=xp[:, 1:1 + n], scalar1=cw[:, 0:1], scalar2=cbv[:, 0:1],
                                                            op0=ALU.mult, op1=ALU.add), R=[xp, cw, cbv], W=[u])
                for jj in range(1, 4):
                    k.op("dve", lambda: nc.vector.scalar_tensor_tensor(out=u[:, 2:2 + n], in0=xp[:, 1 + jj:1 + jj + n], scalar=cw[:, jj:jj + 1],
                                                                       in1=u[:, 2:2 + n], op0=ALU.mult, op1=ALU.add), R=[xp, cw, u], W=[u])
                for d in range(2):
                    k.op("dve", lambda: nc.vector.memset(WA[:], 0.0), W=[WA])
                    k.op("dve", lambda: nc.vector.memset(WX[:], 0.0), W=[WX])
                    for hh in range(2):
                        k.dma(WA[hh * 64:(hh + 1) * 64, hh * 64:(hh + 1) * 64], rg_w_a[l, d, 2 * c + hh, :, :], W=[WA])
                        k.dma(WX[hh * 64:(hh + 1) * 64, hh * 64:(hh + 1) * 64], rg_w_x[l, d, 2 * c + hh, :, :], W=[WX])
                    k.dma(ba[:], colvec(rg_b_a[l, d], ch0, 128), W=[ba], allow_slow_non_contiguous=True)
                    k.dma(bx[:], colvec(rg_b_x[l, d], ch0, 128), W=[bx], allow_slow_non_contiguous=True)
                    k.dma(lam[:], colvec(rg_lambda[l, d], ch0, 128), W=[lam], allow_slow_non_contiguous=True)
                    k.op("act", lambda: nc.scalar.activation(out=m8[:], in_=lam[:], func=AF.Exp, scale=-1.0), R=[lam], W=[m8])
                    k.op("act", lambda: nc.scalar.activation(out=m8[:], in_=m8[:], func=AF.Ln, bias=1.0), R=[m8], W=[m8])
                    k.op("dve", lambda: nc.vector.tensor_scalar(out=m8[:], in0=m8[:], scalar1=-8.0, scalar2=None, op0=ALU.mult), R=[m8], W=[m8])
                    for bi, (p0, nn) in enumerate(segs):
                        p_r = pr[bi % 2]; p_i = pi[bi % 2]; r_ = rT[bi % 2]; i_ = iT[bi % 2]; t_ = t1[bi % 2]
                        k.op("pe", lambda: nc.tensor.matmul(p_r[:, 0:nn], WA[:], u[:, p0:p0 + nn], start=True, stop=True), R=[WA, u], W=[p_r])
                        k.op("pe", lambda: nc.tensor.matmul(p_i[:, 0:nn], WX[:], u[:, p0:p0 + nn], start=True, stop=True), R=[WX, u], W=[p_i])
                        k.op("act", lambda: nc.scalar.activation(out=r_[:, 0:nn], in_=p_r[:, 0:nn], func=AF.Sigmoid, bias=ba[:, 0:1]), R=[p_r, ba], W=[r_])
                        k.op("act", lambda: nc.scalar.activation(out=Aa[:, p0:p0 + nn], in_=r_[:, 0:nn], func=AF.Exp, scale=m8[:, 0:1]), R=[r_, m8], W=[Aa])
                        k.op("act", lambda: nc.scalar.activation(out=i_[:, 0:nn], in_=p_i[:, 0:nn], func=AF.Sigmoid, bias=bx[:, 0:1]), R=[p_i, bx], W=[i_])
                        k.op("dve", lambda: nc.vector.tensor_tensor(out=t_[:, 0:nn], in0=Aa[:, p0:p0 + nn], in1=Aa[:, p0:p0 + nn], op=ALU.mult), R=[Aa], W=[t_])
                        k.op("dve", lambda: nc.vector.tensor_scalar(out=t_[:, 0:nn], in0=t_[:, 0:nn], scalar1=-1.0, scalar2=1.0, op0=ALU.mult, op1=ALU.add), R=[t_], W=[t_])
                        k.op("act", lambda: nc.scalar.activation(out=t_[:, 0:nn], in_=t_[:, 0:nn], func=AF.Sqrt), R=[t_], W=[t_])
                        k.op("dve", lambda: nc.vector.tensor_tensor(out=i_[:, 0:nn], in0=i_[:, 0:nn], in1=u[:, p0:p0 + nn], op=ALU.mult), R=[i_, u], W=[i_])
                        k.op("dve", lambda: nc.vector.tensor_tensor(out=Bv[:, p0:p0 + nn], in0=t_[:, 0:nn], in1=i_[:, 0:nn], op=ALU.mult), R=[t_, i_], W=[Bv])
                    dst = HS if d == 0 else tmp
                    if d == 0:
                        k.op("dve", lambda: nc.vector.tensor_tensor_scan(out=dst[:, T:NTOK], data0=Aa[:, CTX0:CTX0 + LC], data1=Bv[:, CTX0:CTX0 + LC],
                                                                         initial=0.0, op0=ALU.mult, op1=ALU.add), R=[Aa, Bv], W=[dst])
                        k.op("dve", lambda: nc.vector.tensor_tensor_scan(out=dst[:, 0:T], data0=Aa[:, LAT0:LAT0 + T], data1=Bv[:, LAT0:LAT0 + T],
                                                                         initial=dst[:, NTOK - 1:NTOK], op0=ALU.mult, op1=ALU.add), R=[Aa, Bv, dst], W=[dst])
                    else:
                        k.op("dve", lambda: nc.vector.tensor_tensor_scan(out=dst[:, T:NTOK][:, ::-1], data0=Aa[:, CTX0:CTX0 + LC][:, ::-1],
                                                                         data1=Bv[:, CTX0:CTX0 + LC][:, ::-1],
                                                                         initial=0.0, op0=ALU.mult, op1=ALU.add), R=[Aa, Bv], W=[dst])
                        k.op("dve", lambda: nc.vector.tensor_tensor_scan(out=dst[:, 0:T][:, ::-1], data0=Aa[:, LAT0:LAT0 + T][:, ::-1],
                                                                         data1=Bv[:, LAT0:LAT0 + T][:, ::-1],
                                                                         initial=dst[:, T:T + 1], op0=ALU.mult, op1=ALU.add), R=[Aa, Bv, dst], W=[dst])
                        k.op("dve", lambda: nc.vector.tensor_tensor(out=HS[:], in0=HS[:], in1=tmp[:], op=ALU.add), R=[HS, tmp], W=[HS])
                k.op("dve", lambda: nc.vector.tensor_tensor(out=tmp[:], in0=gp[:], in1=gp[:], op=ALU.mult), R=[gp], W=[tmp])
                k.op("dve", lambda: nc.vector.tensor_scalar(out=tmp[:], in0=tmp[:], scalar1=0.044715, scalar2=1.0, op0=ALU.mult, op1=ALU.add), R=[tmp], W=[tmp])
                k.op("dve", lambda: nc.vector.tensor_tensor(out=tmp[:], in0=tmp[:], in1=gp[:], op=ALU.mult), R=[tmp, gp], W=[tmp])
                k.op("act", lambda: nc.scalar.activation(out=tmp[:], in_=tmp[:], func=AF.Sigmoid, scale=2.0 * math.sqrt(2.0 / math.pi)), R=[tmp], W=[tmp])
                k.op("dve", lambda: nc.vector.tensor_tensor(out=tmp[:], in0=tmp[:], in1=gp[:], op=ALU.mult), R=[tmp, gp], W=[tmp])
                k.op("dve", lambda: nc.vector.tensor_tensor(out=ob[:], in0=tmp[:], in1=HS[:], op=ALU.mult), R=[tmp, HS], W=[ob])
                k.dma(RGT[ch0:ch0 + 128, :], ob[:], R=[ob])
            k.barrier()

    def phase_attn(l):
        with ExitStack() as es:
            def sb(name, shape, dt=F32):
                return Buf(es.enter_context(nc.sbuf_tensor(un(name), list(shape), dt)))

            def ps(name, shape, dt=F32):
                return Buf(es.enter_context(nc.psum_tensor(un(name), list(shape), dt)))
            QT = sb("QT", [64, 8, NTOK], BF16)
            KT = sb("KT", [64, 2, NTOK], BF16)
            VA = sb("VA", [128, NTILE, 2, 65], BF16)
            qg = sb("qg", [128, 64]); kg = sb("kg", [128, 64])
            qkv = [sb("qkv%d" % i, [128, 768]) for i in range(2)]
            rope = [sb("rope%d" % i, [128, 2, 256]) for i in range(2)]
            sq = sb("sq", [128, 640]); ss = sb("ss", [128, 10]); qn = sb("qn", [128, 640])
            ra = sb("ra", [128, 320]); rb = sb("rb", [128, 320])
            qr = [sb("qr%d" % i, [128, 640], BF16) for i in range(2)]
            esp = ExitStack()
            ptq = [Buf(esp.enter_context(nc.psum_tensor(un("ptq%d" % i), [64, 8, 128], BF16))) for i in range(2)]
            ptk = Buf(esp.enter_context(nc.psum_tensor(un("ptk"), [64, 2, 128], BF16)))
            k.dma(qg[:], q_norm[l].partition_broadcast(128), W=[qg])
            k.dma(kg[:], k_norm[l].partition_broadcast(128), W=[kg])
            k.op("dve", lambda: nc.vector.memset(VA[:], 1.0), W=[VA])
            for i in range(NTILE):
                t_ = qkv[i % 2]; rp = rope[i % 2]; q_r = qr[i % 2]; pq = ptq[i % 2]
                r0 = pt_row(i * 128)
                k.dma(t_[:], PT[r0:r0 + 128, 768:1536], W=[t_])
                if i < 32:
                    k.dma(rp[:], ROPE_d[i * 128:(i + 1) * 128, :, :], W=[rp])
                k.op("dve", lambda: nc.vector.tensor_tensor(out=sq[:], in0=t_[:, 0:640], in1=t_[:, 0:640], op=ALU.mult), R=[t_], W=[sq])
                k.op("dve", lambda: nc.vector.tensor_reduce(out=ss[:], in_=sq[:].rearrange("p (h d) -> p h d", d=64), axis=AX.X, op=ALU.add), R=[sq], W=[ss])
                k.op("dve", lambda: nc.vector.tensor_scalar(out=ss[:], in0=ss[:], scalar1=1.0 / 64.0, scalar2=QK_EPS, op0=ALU.mult, op1=ALU.add), R=[ss], W=[ss])
                k.op("act", lambda: nc.scalar.activation(out=ss[:], in_=ss[:], func=AF.Sqrt), R=[ss], W=[ss])
                k.op("dve", lambda: nc.vector.reciprocal(out=ss[:], in_=ss[:]), R=[ss], W=[ss])
                for hh in range(10):
                    gsrc = qg if hh < 8 else kg
                    dstt = qn if i < 32 else q_r
                    k.op("dve", lambda: nc.vector.scalar_tensor_tensor(out=dstt[:, hh * 64:(hh + 1) * 64], in0=t_[:, hh * 64:(hh + 1) * 64],
                                                                       scalar=ss[:, hh:hh + 1], in1=gsrc[:], op0=ALU.mult, op1=ALU.mult),
                         R=[t_, ss, gsrc], W=[dstt])
                if i < 32:
                    qv = qn[:].rearrange("p (n two) -> p n two", two=2)
                    ov = q_r[:].rearrange("p (n two) -> p n two", two=2)
                    u1 = qv[:, :, 0]; u2 = qv[:, :, 1]
                    cosv = None
                    for (a0, a1, c0_, c1_) in ((0, 256, 0, 256), (256, 320, 0, 64)):
                        cs = rp[:, 0, c0_:c1_]; sn = rp[:, 1, c0_:c1_]
                        k.op("dve", lambda: nc.vector.tensor_tensor(out=ra[:, a0:a1], in0=u1[:, a0:a1], in1=cs, op=ALU.mult), R=[qn, rp], W=[ra])
                        k.op("dve", lambda: nc.vector.tensor_tensor(out=rb[:, a0:a1], in0=u2[:, a0:a1], in1=sn, op=ALU.mult), R=[qn, rp], W=[rb])
                        k.op("dve", lambda: nc.vector.tensor_tensor(out=ov[:, a0:a1, 0], in0=ra[:, a0:a1], in1=rb[:, a0:a1], op=ALU.subtract), R=[ra, rb], W=[q_r])
                        k.op("dve", lambda: nc.vector.tensor_tensor(out=ra[:, a0:a1], in0=u1[:, a0:a1], in1=sn, op=ALU.mult), R=[qn, rp], W=[ra])
                        k.op("dve", lambda: nc.vector.tensor_tensor(out=rb[:, a0:a1], in0=u2[:, a0:a1], in1=cs, op=ALU.mult), R=[qn, rp], W=[rb])
                        k.op("dve", lambda: nc.vector.tensor_tensor(out=ov[:, a0:a1, 1], in0=ra[:, a0:a1], in1=rb[:, a0:a1], op=ALU.add), R=[ra, rb], W=[q_r])
                for hh in range(8):
                    k.op("pe", lambda: nc.tensor.transpose(pq[:, hh, :], q_r[:, hh * 64:(hh + 1) * 64], identb[:]), R=[q_r, identb], W=[pq])
                for hh in range(2):
                    k.op("pe", lambda: nc.tensor.transpose(ptk[:, hh, :], q_r[:, (8 + hh) * 64:(9 + hh) * 64], identb[:]), R=[q_r, identb], W=[ptk])
                k.op("act", lambda: nc.scalar.copy(out=QT[:, :, i * 128:(i + 1) * 128], in_=pq[:, :, :]), R=[pq], W=[QT])
                k.op("act", lambda: nc.scalar.copy(out=KT[:, :, i * 128:(i + 1) * 128], in_=ptk[:, :, :]), R=[ptk], W=[KT])
                k.op("dve", lambda: nc.vector.tensor_copy(out=VA[:, i, :, 0:64], in_=t_[:, 640:768].rearrange("p (h d) -> p h d", d=64)), R=[t_], W=[VA])
            k.barrier()
            esp.close()
            pS = [ps("pS%d" % i, [128, 512]) for i in range(2)]
            pO = [ps("pO%d" % i, [128, 512]) for i in range(4)]
            Pb = [sb("Pb%d" % i, [128, 512], BF16) for i in range(3)]
            rd = sb("rd", [128, 4]);
            att = [sb("att%d" % i, [128, 4, 512], BF16) for i in range(2)]
            it = 0; io = 0
            jobs = [(qs * 512, 512, list(range(NTILE))) for qs in range(8)] + [(T, 256, [32, 33])]
            for ji, (q0, nq, kchunks) in enumerate(jobs):
                at = att[ji % 2]
                nsub = nq // 128
                for h in range(8):
                    kvh = h // 4
                    io += 1
                    def emit_pv(item):
                        P_, c_, ci_ = item
                        for s in range(nsub):
                            k.op("pe", lambda: nc.tensor.matmul(pO[s][:, 0:65], P_[:, s * 128:(s + 1) * 128], VA[:, c_, kvh, :],
                                                                start=(ci_ == 0), stop=(ci_ == len(kchunks) - 1)), R=[P_, VA], W=[pO[s]])
                    prev = None
                    for ci, c in enumerate(kchunks):
                        S = pS[it % 2]; P = Pb[it % 3]; it += 1
                        k.op("pe", lambda: nc.tensor.matmul(S[:, 0:nq], KT[:, kvh, c * 128:(c + 1) * 128], QT[:, h, q0:q0 + nq], start=True, stop=True),
                             R=[KT, QT], W=[S])
                        k.op("act", lambda: nc.scalar.activation(out=P[:, 0:nq], in_=S[:, 0:nq], func=AF.Exp, scale=0.125), R=[S], W=[P])
                        if prev is not None:
                            emit_pv(prev)
                        prev = (P, c, ci)
                    emit_pv(prev)
                    for s in range(nsub):
                        k.op("dve", lambda: nc.vector.reciprocal(out=rd[:, s:s + 1], in_=pO[s][:, 64:65]), R=[pO[s]], W=[rd])
                        k.op("dve", lambda: nc.vector.tensor_scalar(out=at[:, s, h * 64:(h + 1) * 64], in0=pO[s][:, 0:64], scalar1=rd[:, s:s + 1],
                                                                    scalar2=None, op0=ALU.mult), R=[pO[s], rd], W=[at])
                for s in range(nsub):
                    k.dma(MIXTOK[q0 + s * 128:q0 + (s + 1) * 128, 256:768], at[:, s, :], R=[at])
            k.barrier()

    def hyena_seq(l, es, sb, ps, Lq, tok0, WTd, W0Id, SCd, ZTd, WINd, WIN2d, KF, tag):
        nch = Lq // 128
        slab = [sb("slab%d" % i + tag, [128, nch, 2, 128], BF16) for i in range(3)]
        scs = sb("scs" + tag, [128, nch])
        k.dma(scs[:], SCd[:, :], W=[scs])
        pX = [ps("pX%d" % i + tag, [128, 512]) for i in range(4)]
        pN = ps("pN" + tag, [1, 512])
        kfo = [sb("kfo%d" % i + tag, [128, 2, 512]) for i in range(2)]
        altr = sb("altr" + tag, [1, 128], BF16)
        k.dma(altr[:], ALT_d[:, :], W=[altr])
        si = [0]

        def load_slab(j, inv=False):
            s_ = slab[si[0] % 3]; si[0] += 1
            k.dma(s_[:], W0Id[:, :, :, :] if (inv and j == 0) else WTd[j, :, :, :, :], W=[s_])
            if inv:
                k.dma(s_[0:1, 0, 1, :], ALT_d[:, :], W=[s_])
            return s_
        with ExitStack() as es2:
            def sb2(name, shape, dt=F32):
                return Buf(es2.enter_context(nc.sbuf_tensor(un(name + tag), list(shape), dt)))
            KS = sb2("KS", [128, nch, 512], BF16); KDf = sb2("KD", [128, nch, 512], BF16)
            h2T = sb2("h2T", [64, Lq])
            zT = sb2("zT", [17, Lq]); w1 = sb2("w1", [17, 64]); w2 = sb2("w2", [64, 64]); w3 = sb2("w3", [64, 1024])
            fr = sb2("fr", [64, 1]); b1 = sb2("b1", [64, 1]); b2 = sb2("b2", [64, 1]); fb1 = sb2("fb1", [64, 1]); fb2 = sb2("fb2", [64, 1])
            h1T = sb2("h1T", [64, Lq]); targ = [sb2("targ%d" % i, [64, 512]) for i in range(2)]
            win = [sb2("win%d" % i, [128, 256]) for i in range(2)]; win2 = [sb2("win2%d" % i, [128, 256]) for i in range(2)]
            fa = [sb2("fa%d" % i, [128, 512]) for i in range(2)]; fb_ = [sb2("fb_%d" % i, [128, 512]) for i in range(2)]
            pm = pX
            k.dma(zT[:], ZTd[:, :], W=[zT]); k.dma(w1[:], hy_f_w1[l, :, :], W=[w1]); k.dma(w2[:], hy_f_w2[l, :, :], W=[w2])
            k.dma(w3[:], hy_f_w3[l, :, :], W=[w3])
            k.dma(fr[:], colvec(hy_f_freq[l], 0, 64), W=[fr], allow_slow_non_contiguous=True)
            k.dma(b1[:], colvec(hy_f_b1[l], 0, 64), W=[b1], allow_slow_non_contiguous=True)
            k.dma(b2[:], colvec(hy_f_b2[l], 0, 64), W=[b2], allow_slow_non_contiguous=True)
            OFF = 0.0
            kiq = sb2("kiq", [64, 512], mybir.dt.int32); kfq = sb2("kfq", [64, 512])
            for (bsrc, fdst) in ((b1, fb1), (b2, fb2)):
                k.op("dve", lambda: nc.vector.tensor_tensor(out=fdst[:], in0=bsrc[:], in1=fr[:], op=ALU.mult), R=[bsrc, fr], W=[fdst])
                k.op("dve", lambda: nc.vector.tensor_scalar(out=fdst[:], in0=fdst[:], scalar1=OFF, scalar2=None, op0=ALU.add), R=[fdst], W=[fdst])
            nblk = max(1, Lq // 512); bw = min(512, Lq)

            def sin_layer(wm, src, dstb, fbb, kdim):
                for b in range(nblk):
                    p = pm[b % 2]; ta = targ[b % 2]
                    k.op("pe", lambda: nc.tensor.matmul(p[0:64, 0:bw], wm[:], src[0:kdim, b * bw:(b + 1) * bw], start=True, stop=True), R=[wm, src], W=[p])
                    k.op("dve", lambda: nc.vector.tensor_scalar(out=ta[:, 0:bw], in0=p[0:64, 0:bw], scalar1=fr[:, 0:1], scalar2=fbb[:, 0:1],
                                                                op0=ALU.mult, op1=ALU.add), R=[p, fr, fbb], W=[ta])
                    k.op("dve", lambda: nc.vector.tensor_scalar(out=kiq[:, 0:bw], in0=ta[:, 0:bw], scalar1=1.0 / (2.0 * math.pi), scalar2=None, op0=ALU.mult), R=[ta], W=[kiq])
                    k.op("dve", lambda: nc.vector.tensor_copy(out=kfq[:, 0:bw], in_=kiq[:, 0:bw]), R=[kiq], W=[kfq])
                    k.op("dve", lambda: nc.vector.scalar_tensor_tensor(out=ta[:, 0:bw], in0=kfq[:, 0:bw], scalar=-2.0 * math.pi, in1=ta[:, 0:bw], op0=ALU.mult, op1=ALU.add), R=[kfq, ta], W=[ta])
                    k.op("dve", lambda: nc.vector.tensor_scalar(out=kfq[:, 0:bw], in0=ta[:, 0:bw], scalar1=-math.pi, scalar2=None, op0=ALU.is_lt), R=[ta], W=[kfq])
                    k.op("dve", lambda: nc.vector.scalar_tensor_tensor(out=ta[:, 0:bw], in0=kfq[:, 0:bw], scalar=2.0 * math.pi, in1=ta[:, 0:bw], op0=ALU.mult, op1=ALU.add), R=[kfq, ta], W=[ta])
                    k.op("dve", lambda: nc.vector.tensor_scalar(out=kfq[:, 0:bw], in0=ta[:, 0:bw], scalar1=math.pi, scalar2=None, op0=ALU.is_gt), R=[ta], W=[kfq])
                    k.op("dve", lambda: nc.vector.scalar_tensor_tensor(out=ta[:, 0:bw], in0=kfq[:, 0:bw], scalar=-2.0 * math.pi, in1=ta[:, 0:bw], op0=ALU.mult, op1=ALU.add), R=[kfq, ta], W=[ta])
                    k.op("dve", lambda: nc.vector.tensor_scalar(out=ta[:, 0:bw], in0=ta[:, 0:bw], scalar1=-3.1415925, scalar2=3.1415925,
                                                                op0=ALU.max, op1=ALU.min), R=[ta], W=[ta])
                    k.op("act", lambda: nc.scalar.activation(out=dstb[:, b * bw:(b + 1) * bw], in_=ta[:, 0:bw], func=AF.Sin), R=[ta], W=[dstb])
            sin_layer(w1, zT, h1T, fb1, 17)
            sin_layer(w2, h1T, h2T, fb2, 64)
            for tc in range(nch):
                wn = win[tc % 2]; wn2 = win2[tc % 2]; A_ = fa[tc % 2]; B_ = fb_[tc % 2]
                k.dma(wn[:], WINd[tc * 128:(tc + 1) * 128, :], W=[wn]); k.dma(wn2[:], WIN2d[tc * 128:(tc + 1) * 128, :], W=[wn2])
                p0 = pm[2]; p1 = pm[3]
                k.op("pe", lambda: nc.tensor.matmul(p0[:, :], h2T[:, tc * 128:(tc + 1) * 128], w3[:, 0:512], start=True, stop=True), R=[h2T, w3], W=[p0])
                k.op("pe", lambda: nc.tensor.matmul(p1[:, :], h2T[:, tc * 128:(tc + 1) * 128], w3[:, 512:1024], start=True, stop=True), R=[h2T, w3], W=[p1])
                for o in range(2):
                    k.op("dve", lambda: nc.vector.tensor_tensor(out=A_[:, o * 256:(o + 1) * 256], in0=p0[:, o * 256:(o + 1) * 256], in1=wn[:], op=ALU.mult), R=[p0, wn], W=[A_])
                    k.op("dve", lambda: nc.vector.tensor_tensor(out=B_[:, o * 256:(o + 1) * 256], in0=p1[:, o * 256:(o + 1) * 256], in1=wn2[:], op=ALU.mult), R=[p1, wn2], W=[B_])
                k.op("pool", lambda: nc.gpsimd.tensor_tensor(out=KS[:, tc, :], in0=A_[:], in1=B_[:], op=ALU.add), R=[A_, B_], W=[KS])
                k.op("pool", lambda: nc.gpsimd.tensor_tensor(out=KDf[:, tc, :], in0=A_[:], in1=B_[:], op=ALU.subtract), R=[A_, B_], W=[KDf])
            nxt = load_slab(0)
            for j in range(nch):
                s_ = nxt
                if j + 1 < nch:
                    nxt = load_slab(j + 1)
                pre = pX[(2 * j) % 4]; pim = pX[(2 * j + 1) % 4]; ko = kfo[j % 2]
                for tc in range(nch):
                    k.op("pe", lambda: nc.tensor.matmul(pre[:, :], s_[:, tc, 0, :], KS[:, tc, :], start=(tc == 0), stop=(tc == nch - 1)), R=[s_, KS], W=[pre])
                for tc in range(nch):
                    k.op("pe", lambda: nc.tensor.matmul(pim[:, :], s_[:, tc, 1, :], KDf[:, tc, :], start=(tc == 0), stop=(tc == nch - 1)), R=[s_, KDf], W=[pim])
                if j == 0:
                    for tc in range(nch):
                        k.op("pe", lambda: nc.tensor.matmul(pN[:, :], s_[:, tc, 1, 0:1], KS[:, tc, :], start=(tc == 0), stop=(tc == nch - 1)), R=[s_, KS], W=[pN])
                k.op("act", lambda: nc.scalar.activation(out=ko[:, 0, :], in_=pre[:, :], func=AF.Identity, scale=scs[:, j:j + 1]), R=[pre, scs], W=[ko])
                k.op("dve", lambda: nc.vector.tensor_scalar(out=ko[:, 1, :], in0=pim[:, :], scalar1=scs[:, j:j + 1], scalar2=None, op0=ALU.mult), R=[pim, scs], W=[ko])
                if j == 0:
                    k.op("dve", lambda: nc.vector.tensor_scalar(out=ko[0:1, 1, :], in0=pN[:, :], scalar1=scs[0:1, 0:1], scalar2=None, op0=ALU.mult), R=[pN, scs], W=[ko])
                k.dma(KF[j, :, :, :], ko[:], R=[ko])
            k.barrier()
        V = sb("V" + tag, [128, nch, 256], BF16)
        Y = sb("Y" + tag, [128, nch, 2, 256], BF16)
        wsb = sb("wsb" + tag, [128, 3, 768]); bsb = sb("bsb" + tag, [128, 768]); skb = sb("skb" + tag, [128, 2, 256])
        for jj in range(3):
            k.dma(wsb[:, jj, :], hy_short_w[l, jj, :].partition_broadcast(128), W=[wsb])
        k.dma(bsb[:], hy_short_b[l].partition_broadcast(128), W=[bsb])
        for o in range(2):
            k.dma(skb[:, o, :], hy_skip[l, o, :].partition_broadcast(128), W=[skb])
        pin = [[sb("pin%d_%d" % (a, b) + tag, [128, 768]) for b in range(3)] for a in range(2)]
        cacc = [sb("cacc%d" % i + tag, [128, 768]) for i in range(2)]
        ctmp = [sb("ctmp%d" % i + tag, [128, 768]) for i in range(2)]
        for tc in range(nch):
            tok = tok0 + tc * 128
            r0 = pt_row(tok)
            pp = pin[tc % 2]; ac = cacc[tc % 2]; tm = ctmp[tc % 2]
            for jj in range(3):
                k.dma(pp[jj][:], PT[r0 + jj - 1:r0 + jj - 1 + 128, 0:768], W=[pp[jj]])
            k.op("dve", lambda: nc.vector.tensor_tensor(out=ac[:], in0=pp[0][:], in1=wsb[:, 0, :], op=ALU.mult), R=[pp[0], wsb], W=[ac])
            k.op("pool", lambda: nc.gpsimd.tensor_tensor(out=tm[:], in0=pp[1][:], in1=wsb[:, 1, :], op=ALU.mult), R=[pp[1], wsb], W=[tm])
            k.op("dve", lambda: nc.vector.tensor_tensor(out=ac[:], in0=ac[:], in1=tm[:], op=ALU.add), R=[ac, tm], W=[ac])
            k.op("pool", lambda: nc.gpsimd.tensor_tensor(out=tm[:], in0=pp[2][:], in1=wsb[:, 2, :], op=ALU.mult), R=[pp[2], wsb], W=[tm])
            k.op("dve", lambda: nc.vector.tensor_tensor(out=ac[:], in0=ac[:], in1=tm[:], op=ALU.add), R=[ac, tm], W=[ac])
            k.op("dve", lambda: nc.vector.tensor_tensor(out=ac[:], in0=ac[:], in1=bsb[:], op=ALU.add), R=[ac, bsb], W=[ac])
            k.op("act", lambda: nc.scalar.copy(out=V[:, tc, :], in_=ac[:, 512:768]), R=[ac], W=[V])
            k.dma(HX[tok:tok + 128, :], ac[:], R=[ac])
        k.barrier()
        xs = [sb("xs%d" % i + tag, [128, 2, 256]) for i in range(2)]
        kin = [sb("kin%d" % i + tag, [128, 2, 256]) for i in range(2)]
        ta_ = [sb("ta%d" % i + tag, [128, 256]) for i in range(2)]
        tb_ = [sb("tb%d" % i + tag, [128, 256]) for i in range(2)]
        vin = [sb("vin%d" % i + tag, [128, 256]) for i in range(2)]
        gin = [sb("gin%d" % i + tag, [128, 256]) for i in range(2)]
        yv = [sb("yv%d" % i + tag, [128, 256]) for i in range(2)]
        ybf = [sb("ybf%d" % i + tag, [128, 256], BF16) for i in range(2)]
        nyq = sb("nyq" + tag, [1, 256])
        for n in range(2):
            nxt = load_slab(0)
            for j in range(nch):
                s_ = nxt
                nxt = load_slab(j + 1) if j + 1 < nch else load_slab(0, inv=True)
                pre = pX[(2 * j) % 4]; pim = pX[(2 * j + 1) % 4]
                X = xs[j % 2]; Kc = kin[j % 2]; a_ = ta_[j % 2]; b_ = tb_[j % 2]
                k.dma(Kc[:], KF[j, :, :, n * 256:(n + 1) * 256], W=[Kc])
                for tc in range(nch):
                    k.op("pe", lambda: nc.tensor.matmul(pre[:, 0:256], s_[:, tc, 0, :], V[:, tc, :], start=(tc == 0), stop=(tc == nch - 1)), R=[s_, V], W=[pre])
                for tc in range(nch):
                    k.op("pe", lambda: nc.tensor.matmul(pim[:, 0:256], s_[:, tc, 1, :], V[:, tc, :], start=(tc == 0), stop=(tc == nch - 1)), R=[s_, V], W=[pim])
                k.op("act", lambda: nc.scalar.copy(out=X[:, 0, :], in_=pre[:, 0:256]), R=[pre], W=[X])
                k.op("act", lambda: nc.scalar.copy(out=X[:, 1, :], in_=pim[:, 0:256]), R=[pim], W=[X])
                k.op("dve", lambda: nc.vector.tensor_tensor(out=a_[:], in0=X[:, 0, :], in1=Kc[:, 0, :], op=ALU.mult), R=[X, Kc], W=[a_])
                k.op("pool", lambda: nc.gpsimd.tensor_tensor(out=b_[:], in0=X[:, 1, :], in1=Kc[:, 1, :], op=ALU.mult), R=[X, Kc], W=[b_])
                k.op("dve", lambda: nc.vector.tensor_tensor(out=Y[:, j, 0, :], in0=a_[:], in1=b_[:], op=ALU.subtract), R=[a_, b_], W=[Y])
                if j == 0:
                    k.op("dve", lambda: nc.vector.tensor_copy(out=Y[0:1, j, 0, :], in_=a_[0:1, :]), R=[a_], W=[Y])
                    k.op("dve", lambda: nc.vector.tensor_copy(out=nyq[:], in_=b_[0:1, :]), R=[b_], W=[nyq])
                k.op("dve", lambda: nc.vector.tensor_tensor(out=a_[:], in0=X[:, 0, :], in1=Kc[:, 1, :], op=ALU.mult), R=[X, Kc], W=[a_])
                k.op("pool", lambda: nc.gpsimd.tensor_tensor(out=b_[:], in0=X[:, 1, :], in1=Kc[:, 0, :], op=ALU.mult), R=[X, Kc], W=[b_])
                k.op("dve", lambda: nc.vector.tensor_tensor(out=Y[:, j, 1, :], in0=a_[:], in1=b_[:], op=ALU.add), R=[a_, b_], W=[Y])
                if j == 0:
                    k.op("dve", lambda: nc.vector.tensor_copy(out=Y[0:1, j, 1, :], in_=nyq[:]), R=[nyq], W=[Y])
            for i in range(nch):
                s_ = nxt
                if i + 1 < nch:
                    nxt = load_slab(i + 1, inv=True)
                po_ = pX[i % 4]
                tok = tok0 + i * 128
                vi = vin[i % 2]; gi = gin[i % 2]; y_ = yv[i % 2]; yb = ybf[i % 2]
                if n == 0:
                    k.dma(vi[:], HX[tok:tok + 128, 512:768], W=[vi])
                    k.dma(gi[:], HX[tok:tok + 128, 0:256], W=[gi])
                else:
                    k.dma(vi[:], V1D[tok:tok + 128, :], W=[vi])
                    k.dma(gi[:], HX[tok:tok + 128, 256:512], W=[gi])
                for fc in range(nch):
                    k.op("pe", lambda: nc.tensor.matmul(po_[:, 0:256], s_[:, fc, 0, :], Y[:, fc, 0, :], start=(fc == 0), stop=False), R=[s_, Y], W=[po_])
                    k.op("pe", lambda: nc.tensor.matmul(po_[:, 0:256], s_[:, fc, 1, :], Y[:, fc, 1, :], start=False, stop=(fc == nch - 1)), R=[s_, Y], W=[po_])
                k.op("pool", lambda: nc.gpsimd.tensor_tensor(out=vi[:], in0=vi[:], in1=skb[:, n, :], op=ALU.mult), R=[vi, skb], W=[vi])
                k.op("dve", lambda: nc.vector.tensor_tensor(out=y_[:], in0=po_[:, 0:256], in1=vi[:], op=ALU.add), R=[po_, vi], W=[y_])
                if n == 0:
                    k.op("dve", lambda: nc.vector.tensor_tensor(out=y_[:], in0=y_[:], in1=gi[:], op=ALU.mult), R=[y_, gi], W=[y_])
                    k.op("act", lambda: nc.scalar.copy(out=V[:, i, :], in_=y_[:]), R=[y_], W=[V])
                    k.dma(V1D[tok:tok + 128, :], y_[:], R=[y_])
                else:
                    k.op("dve", lambda: nc.vector.tensor_tensor(out=yb[:], in0=y_[:], in1=gi[:], op=ALU.mult), R=[y_, gi], W=[yb])
                    k.dma(MIXTOK[tok:tok + 128, 0:256], yb[:], R=[yb])
            k.barrier()

    def phase_hyena(l):
        for (Lq, tok0, WTd, W0Id, SCd, ZTd, WINd, WIN2d, KF, tag) in ((T, 0, WT_d, W0I_d, SC_d, ZT_d, WIN_d, WIN2_d, KFD, "L"),
                                                                     (LC, T, WTc_d, W0Ic_d, SCc_d, ZTc_d, WINc_d, WIN2c_d, KFDc, "C")):
            with ExitStack() as es:
                def sb(name, shape, dt=F32):
                    return Buf(es.enter_context(nc.sbuf_tensor(un(name), list(shape), dt)))

                def ps(name, shape, dt=F32):
                    return Buf(es.enter_context(nc.psum_tensor(un(name), list(shape), dt)))
                hyena_seq(l, es, sb, ps, Lq, tok0, WTd, W0Id, SCd, ZTd, WINd, WIN2d, KF, tag)
            k.barrier()

    def resid_ln(sbufs, ysrc, h_t, gb, gam, bet, out_t):
        u_, st, mv = sbufs
        for hf_ in range(2):
            sl = slice(hf_ * 512, (hf_ + 1) * 512)
            yb_, yap = ysrc[hf_]
            k.op("dve", lambda: nc.vector.tensor_tensor(out=u_[:, sl], in0=yap, in1=gb[:, sl], op=ALU.mult), R=[yb_, gb], W=[u_])
        k.op("dve", lambda: nc.vector.scalar_tensor_tensor(out=u_[:], in0=h_t[:], scalar=DN_ALPHA, in1=u_[:], op0=ALU.mult, op1=ALU.add), R=[h_t, u_], W=[u_])
        for hf_ in range(2):
            k.op("dve", lambda: nc.vector.bn_stats(out=st[:, hf_, :], in_=u_[:, hf_ * 512:(hf_ + 1) * 512]), R=[u_], W=[st])
        k.op("dve", lambda: nc.vector.bn_aggr(out=mv[:, 0:2], in_=st[:].rearrange("p a b -> p (a b)")), R=[st], W=[mv])
        k.op("dve", lambda: nc.vector.tensor_scalar(out=mv[:, 1:2], in0=mv[:, 1:2], scalar1=LN_EPS, scalar2=None, op0=ALU.add), R=[mv], W=[mv])
        k.op("act", lambda: nc.scalar.activation(out=mv[:, 1:2], in_=mv[:, 1:2], func=AF.Sqrt), R=[mv], W=[mv])
        k.op("dve", lambda: nc.vector.reciprocal(out=mv[:, 1:2], in_=mv[:, 1:2]), R=[mv], W=[mv])
        k.op("dve", lambda: nc.vector.tensor_scalar(out=u_[:], in0=u_[:], scalar1=mv[:, 0:1], scalar2=mv[:, 1:2], op0=ALU.subtract, op1=ALU.mult), R=[u_, mv], W=[u_])
        k.op("pool", lambda: nc.gpsimd.tensor_tensor(out=u_[:], in0=u_[:], in1=gam[:], op=ALU.mult), R=[u_, gam], W=[u_])
        k.op("pool", lambda: nc.gpsimd.tensor_tensor(out=out_t[:], in0=u_[:], in1=bet[:], op=ALU.add), R=[u_, bet], W=[out_t])

    def phase_outproj(l):
        with ExitStack() as es:
            def sb(name, shape, dt=F32):
                return Buf(es.enter_context(nc.sbuf_tensor(un(name), list(shape), dt)))

            def ps(name, shape, dt=F32):
                return Buf(es.enter_context(nc.psum_tensor(un(name), list(shape), dt)))
            Wst = [sb("wst%d" % i, [128, 8, 512]) for i in range(2)]
            Wb = sb("Wob", [128, 8, D], BF16)
            for cb in range(2):
                st = Wst[cb]
                k.dma(st[:], w_out[l, :, cb * 512:(cb + 1) * 512].rearrange("(k p) n -> p k n", p=128), W=[st])
                k.op("pool", lambda: nc.gpsimd.tensor_copy(out=Wb[:, :, cb * 512:(cb + 1) * 512], in_=st[:]), R=[st], W=[Wb])
            gb = [sb("gb%d" % j, [128, D]) for j in range(2)]
            gam = sb("gam", [128, D]); bet = sb("bet", [128, D])
            for j in range(2):
                k.dma(gb[j][:], GMOD[l, 0, j, :].partition_broadcast(128), W=[gb[j]])
            k.dma(gam[:], ln1_g[l].partition_broadcast(128), W=[gam]); k.dma(bet[:], ln1_b[l].partition_broadcast(128), W=[bet])
            mx = [sb("mx%d" % i, [128, 768], BF16) for i in range(2)]
            mT = [sb("mT%d" % i, [128, 8, 128], BF16) for i in range(2)]
            hf = [sb("hf%d" % i, [128, D]) for i in range(2)]
            ub = [sb("ub%d" % i, [128, D]) for i in range(2)]
            ob = [sb("obo%d" % i, [128, D]) for i in range(2)]
            st6 = [sb("st6%d" % i, [128, 2, 6]) for i in range(2)]; mv = [sb("mv%d" % i, [128, 2]) for i in range(2)]
            tp = [ps("tpo%d" % i, [128, 6, 128], BF16) for i in range(2)]
            py = [[ps("py%d_%d" % (a, b), [128, 512]) for b in range(2)] for a in range(2)]
            for i in range(NTILE):
                j = 0 if i < 32 else 1
                m_ = mx[i % 2]; mt = mT[i % 2]; h_ = hf[i % 2]; tpp = tp[i % 2]; pyy = py[i % 2]
                k.dma(m_[:], MIXTOK[i * 128:(i + 1) * 128, :], W=[m_])
                k.dma(mt[:, 6:8, :], RGT[:, i * 128:(i + 1) * 128].rearrange("(c p) n -> p c n", p=128), W=[mt])
                k.dma(h_[:], hsrc(l, i), W=[h_])
                for kk in range(6):
                    k.op("pe", lambda: nc.tensor.transpose(tpp[:, kk, :], m_[:, kk * 128:(kk + 1) * 128], identb[:]), R=[m_, identb], W=[tpp])
                k.op("act", lambda: nc.scalar.copy(out=mt[:, 0:6, :], in_=tpp[:, :, :]), R=[tpp], W=[mt])
                for nb in range(2):
                    for kk in range(8):
                        k.op("pe", lambda: nc.tensor.matmul(pyy[nb][:, :], mt[:, kk, :], Wb[:, kk, nb * 512:(nb + 1) * 512], start=(kk == 0), stop=(kk == 7)),
                             R=[mt, Wb], W=[pyy[nb]])
                o_ = ob[i % 2]
                resid_ln((ub[i % 2], st6[i % 2], mv[i % 2]), [(pyy[0], pyy[0][:, :]), (pyy[1], pyy[1][:, :])], h_, gb[j], gam, bet, o_)
                k.dma(H1D[i * 128:(i + 1) * 128, :], o_[:], R=[o_])
            k.barrier()

    def phase_moe(l, last):
        passes = [(0, 12), (12, 24), (24, 34)]
        with ExitStack() as es:
            def sb(name, shape, dt=F32):
                return Buf(es.enter_context(nc.sbuf_tensor(un(name), list(shape), dt)))

            def ps(name, shape, dt=F32):
                return Buf(es.enter_context(nc.psum_tensor(un(name), list(shape), dt)))
            yacc = sb("yacc", [128, 12, D])
            mT16 = sb("mT16", [128, 8, 1536], BF16)
            gT = sb("gT", [65, 1536], BF16)
            SEL = sb("SEL", [65, 65 * 128], BF16)
            k.dma(SEL[:], SEL_d[:, :], W=[SEL])
            wst = [sb("west%d" % i, [128, 2048]) for i in range(3)]
            wgb = [sb("wgb%d" % i, [128, 8, 256], BF16) for i in range(2)]
            wub = [sb("wub%d" % i, [128, 8, 256], BF16) for i in range(2)]
            wdb = [sb("wdb%d" % i, [128, 2, D], BF16) for i in range(2)]
            Wr = sb("Wr", [128, 8, 64]); brb = sb("brb", [128, 64])
            k.dma(Wr[:], w_router[l].rearrange("(k p) n -> p k n", p=128), W=[Wr])
            k.dma(brb[:], b_router[l].partition_broadcast(128), W=[brb])
            gb = [sb("g2b%d" % j, [128, D]) for j in range(2)]
            gam = sb("gam2", [128, D]); bet = sb("bet2", [128, D])
            for j in range(2):
                k.dma(gb[j][:], GMOD[l, 1, j, :].partition_broadcast(128), W=[gb[j]])
            k.dma(gam[:], ln2_g[l].partition_broadcast(128), W=[gam]); k.dma(bet[:], ln2_b[l].partition_broadcast(128), W=[bet])
            hf = [sb("hfm%d" % i, [128, D]) for i in range(2)]
            mT32 = sb("mT32", [128, 8, 128])
            sc_ = sb("scr", [128, 64]); sel = sb("selr", [128, 64]); top8 = sb("top8", [128, 8]); den = sb("den", [128, 1])
            G = sb("G", [128, 65])
            sS = [sb("sS%d" % i, [128, 512]) for i in range(2)]
            tS = [sb("tS%d" % i, [128, 512]) for i in range(2)]
            gbs = [sb("gbs%d" % i, [128, 512], BF16) for i in range(2)]
            hT = [[sb("hT%d_%d" % (a, b), [128, 512], BF16) for b in range(2)] for a in range(2)]
            ub = sb("ubm", [128, D]); obm = [sb("obm%d" % i, [128, D]) for i in range(2)]
            st6 = sb("st6m", [128, 2, 6]); mv = sb("mvm", [128, 2])
            pg = [ps("pg%d" % i, [128, 512]) for i in range(2)]
            pu = [ps("pu%d" % i, [128, 512]) for i in range(2)]
            pgb = ps("pgb", [128, 512])
            pd = [ps("pd%d" % i, [128, 512]) for i in range(3)]
            k.op("dve", lambda: nc.vector.memset(G[:], 1.0), W=[G])
            wsti = [0]
            for (t0, t1) in passes:
                ntile = t1 - t0
                ntok = ntile * 128
                for ii, i in enumerate(range(t0, t1)):
                    j = 0 if i < 32 else 1
                    h_ = hf[ii % 2]
                    k.dma(h_[:], H1D[i * 128:(i + 1) * 128, :], W=[h_])
                    for half in range(2):
                        pt_ = pd[half]
                        for k4 in range(4):
                            kk = half * 4 + k4
                            k.op("pe", lambda: nc.tensor.transpose(pt_[:, k4 * 128:(k4 + 1) * 128], h_[:, kk * 128:(kk + 1) * 128], identf[:]),
                                 R=[h_, identf], W=[pt_])
                        for k4 in range(4):
                            kk = half * 4 + k4
                            sc = MOD[:, l, 32 + kk, j:j + 1]; sh = MOD[:, l, 24 + kk, j:j + 1]
                            k.op("act", lambda: nc.scalar.activation(out=mT32[:, kk, :], in_=pt_[:, k4 * 128:(k4 + 1) * 128], func=AF.Identity, bias=sh, scale=sc),
                                 R=[pt_, MOD], W=[mT32])
                            k.op("dve", lambda: nc.vector.tensor_copy(out=mT16[:, kk, ii * 128:(ii + 1) * 128], in_=mT32[:, kk, :]), R=[mT32], W=[mT16])
                    pl = pd[2]
                    for kk in range(8):
                        k.op("pe", lambda: nc.tensor.matmul(pl[:, 0:64], mT32[:, kk, :], Wr[:, kk, :], start=(kk == 0), stop=(kk == 7)), R=[mT32, Wr], W=[pl])
                    k.op("act", lambda: nc.scalar.activation(out=sc_[:], in_=pl[:, 0:64], func=AF.Sigmoid), R=[pl], W=[sc_])
                    k.op("dve", lambda: nc.vector.tensor_tensor(out=sel[:], in0=sc_[:], in1=brb[:], op=ALU.add), R=[sc_, brb], W=[sel])
                    k.op("dve", lambda: nc.vector.max(out=top8[:], in_=sel[:]), R=[sel], W=[top8])
                    k.op("dve", lambda: nc.vector.tensor_scalar(out=sel[:], in0=sel[:], scalar1=top8[:, 7:8], scalar2=None, op0=ALU.is_ge), R=[sel, top8], W=[sel])
                    k.op("dve", lambda: nc.vector.tensor_tensor(out=sel[:], in0=sel[:], in1=sc_[:], op=ALU.mult), R=[sel, sc_], W=[sel])
                    k.op("dve", lambda: nc.vector.tensor_reduce(out=den[:], in_=sel[:], axis=AX.X, op=ALU.add), R=[sel], W=[den])
                    k.op("dve", lambda: nc.vector.reciprocal(out=den[:], in_=den[:]), R=[den], W=[den])
                    k.op("dve", lambda: nc.vector.tensor_scalar(out=G[:, 0:64], in0=sel[:], scalar1=den[:, 0:1], scalar2=2.5, op0=ALU.mult, op1=ALU.mult), R=[sel, den], W=[G])
                    pgt = pg[ii % 2]
                    k.op("pe", lambda: nc.tensor.transpose(pgt[0:65, 0:128], G[:, :], identf[:]), R=[G, identf], W=[pgt])
                    k.op("act", lambda: nc.scalar.copy(out=gT[:, ii * 128:(ii + 1) * 128], in_=pgt[0:65, 0:128]), R=[pgt], W=[gT])
                k.op("dve", lambda: nc.vector.memset(yacc[:], 0.0), W=[yacc])
                blocks = []
                b0 = 0
                while b0 < ntok:
                    nb_ = min(512, ntok - b0)
                    blocks.append((b0, nb_)); b0 += nb_
                pending = None
                wi = 0
                def expert_srcs(e_):
                    return ((w_gate[l, e_], w_up[l, e_], w_down[l, e_]) if e_ < 64 else (ws_gate[l], ws_up[l], ws_down[l]))

                def issue_loads(e_):
                    for mi, src in enumerate(expert_srcs(e_)):
                        st = wst[mi]
                        k.dma(st[:].rearrange("p (k n) -> p k n", n=(256 if mi < 2 else D)), src.rearrange("(k p) n -> p k n", p=128), W=[st])

                def issue_casts(e_):
                    for mi, dstw in enumerate((wgb[e_ % 2], wub[e_ % 2], wdb[e_ % 2])):
                        st = wst[mi]
                        k.op("pool", lambda: nc.gpsimd.tensor_copy(out=dstw[:].rearrange("p k n -> p (k n)"), in_=st[:]), R=[st], W=[dstw])
                issue_loads(0)
                issue_casts(0)
                for e in range(65):
                    Wg_ = wgb[e % 2]; Wu_ = wub[e % 2]; Wd_ = wdb[e % 2]
                    if e + 1 < 65:
                        issue_loads(e + 1)
                    bidx = -1
                    for (bb, nb_) in blocks:
                        hpair = hT[wi % 2]; gb_ = gbs[wi % 2]; wi += 1
                        dn = down_steps(pending, pd, yacc) if pending is not None else iter(())

                        def drain(n_):
                            for _ in range(n_):
                                f_ = next(dn, None)
                                if f_ is None:
                                    return
                                f_()
                        for fc in range(2):
                            g_ = pg[fc]; u_ = pu[fc]; s_ = sS[fc]; t_ = tS[fc]
                            for kk in range(8):
                                k.op("pe", lambda: nc.tensor.matmul(g_[:, 0:nb_], Wg_[:, kk, fc * 128:(fc + 1) * 128], mT16[:, kk, bb:bb + nb_],
                                                                    start=(kk == 0), stop=(kk == 7)), R=[Wg_, mT16], W=[g_])
                            drain(2)
                            for kk in range(8):
                                k.op("pe", lambda: nc.tensor.matmul(u_[:, 0:nb_], Wu_[:, kk, fc * 128:(fc + 1) * 128], mT16[:, kk, bb:bb + nb_],
                                                                    start=(kk == 0), stop=(kk == 7)), R=[Wu_, mT16], W=[u_])
                            if fc == 0:
                                k.op("pe", lambda: nc.tensor.matmul(pgb[:, 0:nb_], SEL[:, e * 128:(e + 1) * 128], gT[:, bb:bb + nb_], start=True, stop=True),
                                     R=[SEL, gT], W=[pgb])
                                k.op("act", lambda: nc.scalar.copy(out=gb_[:, 0:nb_], in_=pgb[:, 0:nb_]), R=[pgb], W=[gb_])
                            k.op("act", lambda: nc.scalar.activation(out=s_[:, 0:nb_], in_=g_[:, 0:nb_], func=AF.Silu), R=[g_], W=[s_])
                            k.op("dve", lambda: nc.vector.tensor_tensor(out=t_[:, 0:nb_], in0=u_[:, 0:nb_], in1=s_[:, 0:nb_], op=ALU.mult), R=[u_, s_], W=[t_])
                            k.op("pool", lambda: nc.gpsimd.tensor_tensor(out=hpair[fc][:, 0:nb_], in0=t_[:, 0:nb_], in1=gb_[:, 0:nb_], op=ALU.mult),
                                 R=[t_, gb_], W=[hpair[fc]])
                            drain(2)
                        drain(100)
                        pending = (hpair, Wd_, bb, nb_)
                        bidx += 1
                        if bidx == 1 and e + 1 < 65:
                            issue_casts(e + 1)
                for f_ in down_steps(pending, pd, yacc):
                    f_()
                for ii, i in enumerate(range(t0, t1)):
                    j = 0 if i < 32 else 1
                    h_ = hf[ii % 2]
                    o_ = obm[ii % 2]
                    k.dma(h_[:], H1D[i * 128:(i + 1) * 128, :], W=[h_])
                    resid_ln((ub, st6, mv), [(yacc, yacc[:, ii, 0:512]), (yacc, yacc[:, ii, 512:1024])], h_, gb[j], gam, bet, o_)
                    if last:
                        if i < 32:
                            k.dma(y_out[i * 128:(i + 1) * 128, :], o_[:], R=[o_])
                    else:
                        k.dma(HD[i * 128:(i + 1) * 128, :], o_[:], R=[o_])
            k.barrier()

    pdi = [0]

    def down_steps(item, pd, yacc):
        hpair, Wd_, bb, nb_ = item
        for sub in range(nb_ // 128):
            ti_ = (bb // 128) + sub
            for dh in range(2):
                def step(sub=sub, ti_=ti_, dh=dh):
                    p = pd[pdi[0] % 3]; pdi[0] += 1
                    for fc in range(2):
                        k.op("pe", lambda: nc.tensor.matmul(p[:, :], hpair[fc][:, sub * 128:(sub + 1) * 128], Wd_[:, fc, dh * 512:(dh + 1) * 512],
                                                            start=(fc == 0), stop=(fc == 1)), R=[hpair[fc], Wd_], W=[p])
                    k.op("dve", lambda: nc.vector.tensor_tensor(out=yacc[:, ti_, dh * 512:(dh + 1) * 512], in0=p[:, :],
                                                                in1=yacc[:, ti_, dh * 512:(dh + 1) * 512], op=ALU.add), R=[p, yacc], W=[yacc])
                yield step

    phase_mod()
    seq = []
    for l in range(L):
        seq += [("inproj", l), ("rg", l), ("attn", l), ("hyena", l), ("outproj", l), ("moe", l)]
    for (ph, l) in seq:
        if ph == "inproj":
            phase_inproj(l)
        elif ph == "rg":
            phase_rg(l)
        elif ph == "attn":
            phase_attn(l)
        elif ph == "hyena":
            phase_hyena(l)
        elif ph == "outproj":
            phase_outproj(l)
        elif ph == "moe":
            phase_moe(l, last=(l == L - 1))
        if stop_after == (ph, l):
            break
    k.barrier()
    gs.close()
    return nc


_CONST = {}


def _consts():
    if _CONST:
        return _CONST
    bf = ml_dtypes.bfloat16

    def dft_tables(Lq):
        N = 2 * Lq
        n = np.arange(Lq, dtype=np.float64)
        ang = 2.0 * np.pi * np.outer(n, n) / N
        C = np.cos(ang)
        S = -np.sin(ang)
        alt = (-1.0) ** n
        S[:, 0] = alt
        nch = Lq // 128
        W = np.stack([C, S], axis=0)
        W = W.reshape(2, nch, 128, nch, 128)
        WTt = np.ascontiguousarray(W.transpose(3, 2, 1, 0, 4)).astype(np.float32).astype(bf)
        sc = np.full((128, nch), 2.0 / N, np.float32)
        sc[0, 0] = 1.0 / N
        W0I = WTt[0].copy()
        W0I[:, :, 1, 0] = 0
        return WTt, sc, W0I

    def filt_tables(Lq):
        t = np.linspace(0.0, 1.0, Lq, dtype=np.float32)[:, None]
        w = (2.0 * np.float32(math.pi) * np.arange(Lq, dtype=np.float32)[:, None] / np.float32(Lq)).astype(np.float32)
        bands = np.linspace(1e-4, 7, 8, dtype=np.float32)
        z = np.concatenate([t, np.cos(bands * w), -np.sin(bands * w)], axis=-1).astype(np.float32)
        mx = math.log(1e-2) / 0.3
        mn = math.log(1e-2) / 1.5
        deltas = np.abs(np.linspace(mn, mx, 256, dtype=np.float32))
        win = np.exp(-t * deltas).astype(np.float32)
        win2 = win.copy()
        win2[0, :] = 0.0
        return np.ascontiguousarray(z.T), win, win2

    WTt, sc, w0i = dft_tables(T)
    WTc, scc, w0ic = dft_tables(LC)
    zt, win, win2 = filt_tables(T)
    ztc, winc, win2c = filt_tables(LC)
    n_rows = T // 64
    row = np.repeat(np.arange(n_rows, dtype=np.float32), 64)
    col = np.tile(np.arange(64, dtype=np.float32), n_rows)
    inv_freq = (np.float32(10000.0) ** (-np.arange(0, 32, 2, dtype=np.float32) / np.float32(32))).astype(np.float32)
    ang = np.concatenate([row[:, None] * inv_freq, col[:, None] * inv_freq], axis=-1).astype(np.float32)
    rope = np.stack([np.tile(np.cos(ang), (1, 8)), np.tile(np.sin(ang), (1, 8))], axis=1).astype(np.float32)
    sel = np.zeros((65, 65, 128), np.float32)
    for e in range(65):
        sel[e, e, :] = 1.0
    _CONST.update(dict(
        identb=np.eye(128, dtype=np.float32).astype(bf), identf=np.eye(128, dtype=np.float32),
        WT=WTt, WTc=WTc, W0I=w0i, W0Ic=w0ic, SC=sc, SCc=scc, ZT=zt, ZTc=ztc, WIN=win, WIN2=win2, WINc=winc, WIN2c=win2c,
        ROPE=rope, SEL=sel.reshape(65, 65 * 128).astype(bf),
        ALT=((-1.0) ** np.arange(128, dtype=np.float32)).reshape(1, 128).astype(bf)))
    return _CONST


WEIGHT_NAMES = ["w_mod", "b_mod", "ln1_g", "ln1_b", "ln2_g", "ln2_b", "w_in", "w_out", "hy_short_w", "hy_short_b",
                "hy_f_w1", "hy_f_b1", "hy_f_freq", "hy_f_w2", "hy_f_b2", "hy_f_w3", "hy_skip", "q_norm", "k_norm",
                "rg_conv_w", "rg_conv_b", "rg_lambda", "rg_w_a", "rg_b_a", "rg_w_x", "rg_b_x", "w_router", "b_router",
                "w_gate", "w_up", "w_down", "ws_gate", "ws_up", "ws_down"]


def make_in_maps(inputs, L, cores):
    cst = _consts()
    shared = {}
    for n in WEIGHT_NAMES:
        arr = np.asarray(inputs[n], dtype=np.float32)
        for l_ in range(L):
            shared["%s_%d" % (n, l_)] = np.ascontiguousarray(arr[l_])
    shared["bmT"] = np.ascontiguousarray(np.asarray(inputs["b_mod"], dtype=np.float32)[:L].reshape(L, 48, 128).transpose(2, 0, 1))
    shared.update(cst)
    c = np.asarray(inputs["c"], np.float32)
    c_ctx = np.asarray(inputs["c_ctx"], np.float32)
    maps = []
    for b in cores:
        m = dict(shared)
        m["x"] = np.ascontiguousarray(np.asarray(inputs["x"], np.float32)[b])
        m["ctx"] = np.ascontiguousarray(np.asarray(inputs["ctx"], np.float32)[b])
        two = np.stack([c[b], c_ctx], axis=-1)
        m["cc"] = np.ascontiguousarray(two.reshape(8, 128, 2).transpose(1, 0, 2))
        maps.append(m)
    return maps


_NC = {}


def kernel(**inputs):
    if "nc" not in _NC:
        _NC["nc"] = build(L=4)
    nc = _NC["nc"]
    maps = make_in_maps(inputs, 4, list(range(8)))
    res = run_bass_kernel_spmd(nc, maps, core_ids=list(range(8)))
    out = np.stack([np.asarray(r["y"], dtype=np.float32) for r in res.results], axis=0)
    return out
```

```python
from contextlib import ExitStack
import math
import numpy as np
import ml_dtypes
import concourse.bass as bass
import concourse.mybir as mybir
from concourse.bass_utils import run_bass_kernel_spmd

F32 = mybir.dt.float32
BF16 = mybir.dt.bfloat16
AF = mybir.ActivationFunctionType
ALU = mybir.AluOpType
AX = mybir.AxisListType

D = 1024
T = 4096
LC = 256
NTOK = T + LC
NTILE = NTOK // 128
PROJ_W = 2048
N_EXP = 64
DN_ALPHA = (2 * 4) ** 0.25
LN_EPS = 1e-6
QK_EPS = 1e-6
PT_ROWS = 2 + T + 2 + LC + 2
PT_W = 1536


def pt_row(tok):
    return 2 + tok if tok < T else 4 + tok


class Buf:
    __slots__ = ("t", "w", "r")

    def __init__(self, t):
        self.t = t
        self.w = None
        self.r = {}

    def __getitem__(self, idx):
        return self.t[idx]


class Res:
    __slots__ = ("w", "r")

    def __init__(self):
        self.w = None
        self.r = {}


class KB:
    NSLOT = 28

    def __init__(self, nc):
        self.nc = nc
        self.eng = {"pe": nc.tensor, "act": nc.scalar, "dve": nc.vector, "pool": nc.gpsimd, "sp": nc.sync}
        self.sem = {q: nc.alloc_semaphore("sem_" + q) for q in ("pe", "act", "dve", "pool")}
        self.cnt = {q: 0 for q in self.sem}
        self.slots = [nc.alloc_semaphore("dsl%d" % i) for i in range(self.NSLOT)]
        self.slotcnt = [0] * self.NSLOT
        self.nextslot = 0
        self.seen = {q: {} for q in self.eng}

    def _semof(self, key):
        return self.sem[key] if isinstance(key, str) else self.slots[key]

    def _wait(self, q, key, val):
        if q == "pe" and key == "pe":
            return
        if self.seen[q].get(key, 0) >= val:
            return
        self.eng[q].wait_ge(self._semof(key), val)
        self.seen[q][key] = val

    def _deps(self, q, R, W):
        for r in R:
            if r.w is not None:
                self._wait(q, *r.w)
        for w in W:
            if w.w is not None:
                self._wait(q, *w.w)
            for kk, v in w.r.items():
                self._wait(q, kk, v)

    @staticmethod
    def _mark(tok, R, W):
        kk, v = tok
        for r in R:
            if r.r.get(kk, 0) < v:
                r.r[kk] = v
        for w in W:
            w.w = tok
            w.r = {}

    def op(self, q, fn, R=(), W=()):
        self._deps(q, R, W)
        inst = fn()
        self.cnt[q] += 1
        inst.then_inc(self.sem[q], 1)
        self._mark((q, self.cnt[q]), R, W)

    def dma(self, out, in_, R=(), W=(), q="sp", **kw):
        sl = self.nextslot
        self.nextslot = (sl + 1) % self.NSLOT
        if self.slotcnt[sl]:
            self._wait(q, sl, self.slotcnt[sl])
        self._deps(q, R, W)
        self.eng[q].dma_start(out=out, in_=in_, **kw).then_inc(self.slots[sl], 16)
        self.slotcnt[sl] += 16
        self._mark((sl, self.slotcnt[sl]), R, W)

    def barrier(self):
        for q in self.eng:
            for key in self.sem:
                if self.cnt[key]:
                    self._wait(q, key, self.cnt[key])
            for sl in range(self.NSLOT):
                if self.slotcnt[sl]:
                    self._wait(q, sl, self.slotcnt[sl])


def build(L=4, dbg=False, stop_after=None):
    nc = bass.Bass("TRN2", target_bir_lowering=False)
    k = KB(nc)

    class LT:
        def __init__(self, aps):
            self.aps = aps

        def __getitem__(self, idx):
            if isinstance(idx, tuple):
                l_, rest = idx[0], idx[1:]
                return self.aps[l_][rest] if rest else self.aps[l_]
            return self.aps[idx]

    def din(name, shape, dt=F32):
        if name in WEIGHT_NAMES:
            return LT([nc.dram_tensor("%s_%d" % (name, l_), list(shape[1:]), dt, kind="ExternalInput").ap() for l_ in range(shape[0])])
        return nc.dram_tensor(name, list(shape), dt, kind="ExternalInput").ap()

    def dscr(name, shape, dt=F32):
        return nc.dram_tensor(name, list(shape), dt, kind="ExternalOutput" if dbg else "Internal").ap()

    x = din("x", [T, D])
    ctx = din("ctx", [LC, D])
    cc = din("cc", [128, 8, 2])
    w_mod = din("w_mod", [L, D, 6 * D])
    b_mod = din("b_mod", [L, 6 * D])
    bmT = din("bmT", [128, L, 48])
    ln1_g = din("ln1_g", [L, D]); ln1_b = din("ln1_b", [L, D])
    ln2_g = din("ln2_g", [L, D]); ln2_b = din("ln2_b", [L, D])
    w_in = din("w_in", [L, D, PROJ_W])
    w_out = din("w_out", [L, D, D])
    hy_short_w = din("hy_short_w", [L, 3, 768]); hy_short_b = din("hy_short_b", [L, 768])
    hy_f_w1 = din("hy_f_w1", [L, 17, 64]); hy_f_b1 = din("hy_f_b1", [L, 64])
    hy_f_freq = din("hy_f_freq", [L, 64])
    hy_f_w2 = din("hy_f_w2", [L, 64, 64]); hy_f_b2 = din("hy_f_b2", [L, 64])
    hy_f_w3 = din("hy_f_w3", [L, 64, 1024]); hy_skip = din("hy_skip", [L, 2, 256])
    q_norm = din("q_norm", [L, 64]); k_norm = din("k_norm", [L, 64])
    rg_conv_w = din("rg_conv_w", [L, 4, 256]); rg_conv_b = din("rg_conv_b", [L, 256])
    rg_lambda = din("rg_lambda", [L, 2, 256])
    rg_w_a = din("rg_w_a", [L, 2, 4, 64, 64]); rg_b_a = din("rg_b_a", [L, 2, 256])
    rg_w_x = din("rg_w_x", [L, 2, 4, 64, 64]); rg_b_x = din("rg_b_x", [L, 2, 256])
    w_router = din("w_router", [L, D, N_EXP]); b_router = din("b_router", [L, N_EXP])
    w_gate = din("w_gate", [L, N_EXP, D, 256]); w_up = din("w_up", [L, N_EXP, D, 256])
    w_down = din("w_down", [L, N_EXP, 256, D])
    ws_gate = din("ws_gate", [L, D, 256]); ws_up = din("ws_up", [L, D, 256]); ws_down = din("ws_down", [L, 256, D])
    identb_d = din("identb", [128, 128], BF16)
    identf_d = din("identf", [128, 128])
    WT_d = din("WT", [32, 128, 32, 2, 128], BF16)
    WTc_d = din("WTc", [2, 128, 2, 2, 128], BF16)
    SC_d = din("SC", [128, 32]); SCc_d = din("SCc", [128, 2])
    ZT_d = din("ZT", [17, T]); ZTc_d = din("ZTc", [17, LC])
    WIN_d = din("WIN", [T, 256]); WIN2_d = din("WIN2", [T, 256])
    WINc_d = din("WINc", [LC, 256]); WIN2c_d = din("WIN2c", [LC, 256])
    ROPE_d = din("ROPE", [T, 2, 256])
    SEL_d = din("SEL", [65, 65 * 128], BF16)
    ALT_d = din("ALT", [1, 128], BF16)
    W0I_d = din("W0I", [128, 32, 2, 128], BF16)
    W0Ic_d = din("W0Ic", [128, 2, 2, 128], BF16)

    y_out = nc.dram_tensor("y", [T, D], F32, kind="ExternalOutput").ap()

    HD = dscr("HD", [NTOK, D])
    H1D = dscr("H1D", [NTOK, D])
    PT = dscr("PT", [PT_ROWS, PT_W])
    PR = dscr("PR", [512, NTOK])
    RGT = dscr("RGT", [256, NTOK], BF16)
    MIXTOK = dscr("MIXTOK", [NTOK, 768], BF16)
    HX = dscr("HX", [NTOK, 768])
    V1D = dscr("V1D", [NTOK, 256])
    KFD = dscr("KFD", [32, 128, 2, 512])
    KFDc = dscr("KFDc", [2, 128, 2, 512])
    GMOD = dscr("GMOD", [L, 2, 2, D])

    gs = ExitStack()
    _uid = [0]

    def un(name):
        _uid[0] += 1
        return "%s_%d" % (name, _uid[0])

    def gsb(name, shape, dt=F32):
        return Buf(gs.enter_context(nc.sbuf_tensor(un(name), list(shape), dt)))

    MOD = gsb("MOD", [128, L, 48, 2])
    identb = gsb("identb_s", [128, 128], BF16)
    identf = gsb("identf_s", [128, 128])
    k.dma(identb[:], identb_d[:, :], W=[identb])
    k.dma(identf[:], identf_d[:, :], W=[identf])

    def hsrc(l, i):
        if l == 0:
            return x[i * 128:(i + 1) * 128, :] if i < 32 else ctx[(i - 32) * 128:(i - 31) * 128, :]
        return HD[i * 128:(i + 1) * 128, :]

    def colvec(v1d, n0, n):
        return v1d[n0:n0 + n].rearrange("(p o) -> p o", o=1)

    def phase_mod():
        with ExitStack() as es:
            def sb(name, shape, dt=F32):
                return Buf(es.enter_context(nc.sbuf_tensor(un(name), list(shape), dt)))

            def ps(name, shape, dt=F32):
                return Buf(es.enter_context(nc.psum_tensor(un(name), list(shape), dt)))
            ccs = sb("ccs", [128, 8, 2]); sT = sb("sT", [128, 8, 2]); sg = sb("sg", [128, 8, 2])
            bmTs = sb("bmTs", [128, L, 48])
            wbuf = [sb("wm%d" % i, [128, 8, 1024]) for i in range(2)]
            brow = sb("brow", [2, 1024]); rows = sb("rows", [2, 1024])
            psf = [ps("psf%d" % i, [128, 8, 2]) for i in range(2)]
            psr = [ps("psr%d" % i, [2, 512]) for i in range(2)]
            k.dma(ccs[:], cc[:, :, :], W=[ccs])
            k.dma(bmTs[:], bmT[:, :, :], W=[bmTs])
            k.op("act", lambda: nc.scalar.activation(out=sg[:], in_=ccs[:], func=AF.Sigmoid), R=[ccs], W=[sg])
            k.op("dve", lambda: nc.vector.tensor_tensor(out=sT[:], in0=ccs[:], in1=sg[:], op=ALU.mult), R=[ccs, sg], W=[sT])
            it = 0
            for l in range(L):
                for blk in range(6):
                    wb = wbuf[it % 2]; pf = psf[it % 2]; it += 1
                    c0 = blk * 1024
                    k.dma(wb[:], w_mod[l, :, c0:c0 + 1024].rearrange("(k p) n -> p k n", p=128), W=[wb])
                    for ch in range(8):
                        for kk in range(8):
                            k.op("pe", lambda: nc.tensor.matmul(pf[:, ch, :], wb[:, kk, ch * 128:(ch + 1) * 128], sT[:, kk, :],
                                                                start=(kk == 0), stop=(kk == 7)), R=[wb, sT], W=[pf])
                    for j in range(2):
                        k.op("dve", lambda: nc.vector.tensor_tensor(out=MOD[:, l, blk * 8:(blk + 1) * 8, j], in0=pf[:, :, j],
                                                                    in1=bmTs[:, l, blk * 8:(blk + 1) * 8], op=ALU.add),
                             R=[pf, bmTs], W=[MOD])
                    if blk in (1, 4):
                        k.op("dve", lambda: nc.vector.tensor_scalar(out=MOD[:, l, blk * 8:(blk + 1) * 8, :], in0=MOD[:, l, blk * 8:(blk + 1) * 8, :],
                                                                    scalar1=1.0, scalar2=None, op0=ALU.add), R=[MOD], W=[MOD])
                    if blk in (2, 5):
                        k.dma(brow[:], b_mod[l, c0:c0 + 1024].partition_broadcast(2), W=[brow])
                        for nb in range(2):
                            for kk in range(8):
                                k.op("pe", lambda: nc.tensor.matmul(psr[nb][:, :], sT[:, kk, :], wb[:, kk, nb * 512:(nb + 1) * 512],
                                                                    start=(kk == 0), stop=(kk == 7)), R=[wb, sT], W=[psr[nb]])
                            k.op("dve", lambda: nc.vector.tensor_tensor(out=rows[:, nb * 512:(nb + 1) * 512], in0=psr[nb][:, :],
                                                                        in1=brow[:, nb * 512:(nb + 1) * 512], op=ALU.add),
                                 R=[psr[nb], brow], W=[rows])
                        k.dma(GMOD[l, 0 if blk == 2 else 1, :, :], rows[:], R=[rows])
            k.barrier()

    def phase_inproj(l):
        with ExitStack() as es:
            def sb(name, shape, dt=F32):
                return Buf(es.enter_context(nc.sbuf_tensor(un(name), list(shape), dt)))

            def ps(name, shape, dt=F32):
                return Buf(es.enter_context(nc.psum_tensor(un(name), list(shape), dt)))
            Wst = [sb("wst%d" % i, [128, 8, 512]) for i in range(2)]
            Wb = sb("Wb", [128, 8, PROJ_W], BF16)
            zt = sb("zt", [2, PT_W])
            hf = [sb("hf%d" % i, [128, D]) for i in range(3)]
            hb = [sb("hb%d" % i, [128, D], BF16) for i in range(2)]
            aT = [sb("aT%d" % i, [128, 8, 512], BF16) for i in range(2)]
            ot = [sb("ot%d" % i, [128, PT_W]) for i in range(2)]
            of = [sb("of%d" % i, [128, 512]) for i in range(2)]
            tp = [ps("tp%d" % i, [128, 8, 128], BF16) for i in range(2)]
            po = [ps("po%d" % i, [128, 512]) for i in range(3)]
            pf = [ps("pf%d" % i, [128, 512]) for i in range(2)]
            for cb in range(4):
                st = Wst[cb % 2]
                k.dma(st[:], w_in[l, :, cb * 512:(cb + 1) * 512].rearrange("(k p) n -> p k n", p=128), W=[st])
                k.op("pool", lambda: nc.gpsimd.tensor_copy(out=Wb[:, :, cb * 512:(cb + 1) * 512], in_=st[:]), R=[st], W=[Wb])
            k.op("dve", lambda: nc.vector.memset(zt[:], 0.0), W=[zt])
            for r0 in (0, 2 + T, PT_ROWS - 2):
                k.dma(PT[r0:r0 + 2, :], zt[:], R=[zt])
            ti = 0
            for g in range(9):
                tiles = list(range(4 * g, min(4 * g + 4, NTILE)))
                nt = len(tiles)
                j = 0 if g < 8 else 1
                A = aT[g % 2]
                for s, i in enumerate(tiles):
                    h_f = hf[ti % 3]; h_b = hb[ti % 2]; tpp = tp[ti % 2]; ti += 1
                    k.dma(h_f[:], hsrc(l, i), W=[h_f])
                    k.op("dve", lambda: nc.vector.tensor_copy(out=h_b[:], in_=h_f[:]), R=[h_f], W=[h_b])
                    for kk in range(8):
                        k.op("pe", lambda: nc.tensor.transpose(tpp[:, kk, :], h_b[:, kk * 128:(kk + 1) * 128], identb[:]),
                             R=[h_b, identb], W=[tpp])
                    for kk in range(8):
                        sc = MOD[:, l, 8 + kk, j:j + 1]; sh = MOD[:, l, kk, j:j + 1]
                        if (ti % 2) == 0:
                            k.op("act", lambda: nc.scalar.activation(out=A[:, kk, s * 128:(s + 1) * 128], in_=tpp[:, kk, :],
                                                                     func=AF.Identity, bias=sh, scale=sc), R=[tpp, MOD], W=[A])
                        else:
                            k.op("dve", lambda: nc.vector.tensor_scalar(out=A[:, kk, s * 128:(s + 1) * 128], in0=tpp[:, kk, :],
                                                                        scalar1=sc, scalar2=sh, op0=ALU.mult, op1=ALU.add),
                                 R=[tpp, MOD], W=[A])
                for s, i in enumerate(tiles):
                    o_t = ot[i % 2]
                    for nb in range(3):
                        p = po[nb]
                        for kk in range(8):
                            k.op("pe", lambda: nc.tensor.matmul(p[:, :], A[:, kk, s * 128:(s + 1) * 128], Wb[:, kk, nb * 512:(nb + 1) * 512],
                                                                start=(kk == 0), stop=(kk == 7)), R=[A, Wb], W=[p])
                        if nb == 1:
                            k.op("dve", lambda: nc.vector.tensor_copy(out=o_t[:, nb * 512:(nb + 1) * 512], in_=p[:, :]), R=[p], W=[o_t])
                        else:
                            k.op("act", lambda: nc.scalar.copy(out=o_t[:, nb * 512:(nb + 1) * 512], in_=p[:, :]), R=[p], W=[o_t])
                    r0 = pt_row(i * 128)
                    k.dma(PT[r0:r0 + 128, :], o_t[:], R=[o_t])
                n = nt * 128
                for c4 in range(4):
                    p = pf[c4 % 2]; o_f = of[c4 % 2]
                    for kk in range(8):
                        k.op("pe", lambda: nc.tensor.matmul(p[:, 0:n], Wb[:, kk, 1536 + c4 * 128:1536 + (c4 + 1) * 128], A[:, kk, 0:n],
                                                            start=(kk == 0), stop=(kk == 7)), R=[A, Wb], W=[p])
                    k.op("act" if c4 % 2 else "dve",
                         (lambda: nc.scalar.copy(out=o_f[:, 0:n], in_=p[:, 0:n])) if c4 % 2 else
                         (lambda: nc.vector.tensor_copy(out=o_f[:, 0:n], in_=p[:, 0:n])), R=[p], W=[o_f])
                    k.dma(PR[c4 * 128:(c4 + 1) * 128, g * 512:g * 512 + n], o_f[:, 0:n], R=[o_f])
            k.barrier()

    def phase_rg(l):
        NB = PT_ROWS
        LAT0, CTX0 = 2, 4 + T
        with ExitStack() as es:
            def sb(name, shape, dt=F32):
                return Buf(es.enter_context(nc.sbuf_tensor(un(name), list(shape), dt)))

            def ps(name, shape, dt=F32):
                return Buf(es.enter_context(nc.psum_tensor(un(name), list(shape), dt)))
            xp = sb("xp", [128, NB]); gp = sb("gp", [128, NTOK]); u = sb("u", [128, NB])
            Aa = sb("Aa", [128, NB]); Bv = sb("Bv", [128, NB]); HS = sb("HS", [128, NTOK]); tmp = sb("tmpS", [128, NTOK])
            ob = sb("ob", [128, NTOK], BF16)
            cw = sb("cw", [128, 4]); cbv = sb("cbv", [128, 1])
            WA = sb("WA", [128, 128]); WX = sb("WX", [128, 128])
            ba = sb("ba", [128, 1]); bx = sb("bx", [128, 1]); lam = sb("lam", [128, 1]); m8 = sb("m8", [128, 1])
            rT = [sb("rT%d" % i, [128, 512]) for i in range(2)]
            iT = [sb("iT%d" % i, [128, 512]) for i in range(2)]
            t1 = [sb("t1%d" % i, [128, 512]) for i in range(2)]
            pr = [ps("pr%d" % i, [128, 512]) for i in range(2)]
            pi = [ps("pi%d" % i, [128, 512]) for i in range(2)]
            segs = [(LAT0 + b * 512, 512) for b in range(8)] + [(CTX0, 256)]
            for c in range(2):
                ch0 = c * 128
                k.op("dve", lambda: nc.vector.memset(xp[:], 0.0), W=[xp])
                k.dma(xp[:, LAT0:LAT0 + T], PR[ch0:ch0 + 128, 0:T], W=[xp])
                k.dma(xp[:, CTX0:CTX0 + LC], PR[ch0:ch0 + 128, T:NTOK], W=[xp])
                k.dma(gp[:], PR[256 + ch0:256 + ch0 + 128, :], W=[gp])
                k.dma(cw[:], rg_conv_w[l, :, ch0:ch0 + 128].rearrange("j p -> p j"), W=[cw], allow_slow_non_contiguous=True)
                k.dma(cbv[:], colvec(rg_conv_b[l], ch0, 128), W=[cbv], allow_slow_non_contiguous=True)
                n = NB - 4
                k.op("dve", lambda: nc.vector.tensor_scalar(out=u[:, 2:2 + n], in0=xp[:, 1:1 + n], scalar1=cw[:, 0:1], scalar2=cbv[:, 0:1],
                                                            op0=ALU.mult, op1=ALU.add), R=[xp, cw, cbv], W=[u])
                for jj in range(1, 4):
                    k.op("dve", lambda: nc.vector.scalar_tensor_tensor(out=u[:, 2:2 + n], in0=xp[:, 1 + jj:1 + jj + n], scalar=cw[:, jj:jj + 1],
                                                                       in1=u[:, 2:2 + n], op0=ALU.mult, op1=ALU.add), R=[xp, cw, u], W=[u])
                for d in range(2):
                    k.op("dve", lambda: nc.vector.memset(WA[:], 0.0), W=[WA])
                    k.op("dve", lambda: nc.vector.memset(WX[:], 0.0), W=[WX])
                    for hh in range(2):
                        k.dma(WA[hh * 64:(hh + 1) * 64, hh * 64:(hh + 1) * 64], rg_w_a[l, d, 2 * c + hh, :, :], W=[WA])
                        k.dma(WX[hh * 64:(hh + 1) * 64, hh * 64:(hh + 1) * 64], rg_w_x[l, d, 2 * c + hh, :, :], W=[WX])
                    k.dma(ba[:], colvec(rg_b_a[l, d], ch0, 128), W=[ba], allow_slow_non_contiguous=True)
                    k.dma(bx[:], colvec(rg_b_x[l, d], ch0, 128), W=[bx], allow_slow_non_contiguous=True)
                    k.dma(lam[:], colvec(rg_lambda[l, d], ch0, 128), W=[lam], allow_slow_non_contiguous=True)
                    k.op("act", lambda: nc.scalar.activation(out=m8[:], in_=lam[:], func=AF.Exp, scale=-1.0), R=[lam], W=[m8])
                    k.op("act", lambda: nc.scalar.activation(out=m8[:], in_=m8[:], func=AF.Ln, bias=1.0), R=[m8], W=[m8])
                    k.op("dve", lambda: nc.vector.tensor_scalar(out=m8[:], in0=m8[:], scalar1=-8.0, scalar2=None, op0=ALU.mult), R=[m8], W=[m8])
                    for bi, (p0, nn) in enumerate(segs):
                        p_r = pr[bi % 2]; p_i = pi[bi % 2]; r_ = rT[bi % 2]; i_ = iT[bi % 2]; t_ = t1[bi % 2]
                        k.op("pe", lambda: nc.tensor.matmul(p_r[:, 0:nn], WA[:], u[:, p0:p0 + nn], start=True, stop=True), R=[WA, u], W=[p_r])
                        k.op("pe", lambda: nc.tensor.matmul(p_i[:, 0:nn], WX[:], u[:, p0:p0 + nn], start=True, stop=True), R=[WX, u], W=[p_i])
                        k.op("act", lambda: nc.scalar.activation(out=r_[:, 0:nn], in_=p_r[:, 0:nn], func=AF.Sigmoid, bias=ba[:, 0:1]), R=[p_r, ba], W=[r_])
                        k.op("act", lambda: nc.scalar.activation(out=Aa[:, p0:p0 + nn], in_=r_[:, 0:nn], func=AF.Exp, scale=m8[:, 0:1]), R=[r_, m8], W=[Aa])
                        k.op("act", lambda: nc.scalar.activation(out=i_[:, 0:nn], in_=p_i[:, 0:nn], func=AF.Sigmoid, bias=bx[:, 0:1]), R=[p_i, bx], W=[i_])
                        k.op("dve", lambda: nc.vector.tensor_tensor(out=t_[:, 0:nn], in0=Aa[:, p0:p0 + nn], in1=Aa[:, p0:p0 + nn], op=ALU.mult), R=[Aa], W=[t_])
                        k.op("dve", lambda: nc.vector.tensor_scalar(out=t_[:, 0:nn], in0=t_[:, 0:nn], scalar1=-1.0, scalar2=1.0, op0=ALU.mult, op1=ALU.add), R=[t_], W=[t_])
                        k.op("act", lambda: nc.scalar.activation(out=t_[:, 0:nn], in_=t_[:, 0:nn], func=AF.Sqrt), R=[t_], W=[t_])
                        k.op("dve", lambda: nc.vector.tensor_tensor(out=i_[:, 0:nn], in0=i_[:, 0:nn], in1=u[:, p0:p0 + nn], op=ALU.mult), R=[i_, u], W=[i_])
                        k.op("dve", lambda: nc.vector.tensor_tensor(out=Bv[:, p0:p0 + nn], in0=t_[:, 0:nn], in1=i_[:, 0:nn], op=ALU.mult), R=[t_, i_], W=[Bv])
                    dst = HS if d == 0 else tmp
                    if d == 0:
                        k.op("dve", lambda: nc.vector.tensor_tensor_scan(out=dst[:, T:NTOK], data0=Aa[:, CTX0:CTX0 + LC], data1=Bv[:, CTX0:CTX0 + LC],
                                                                         initial=0.0, op0=ALU.mult, op1=ALU.add), R=[Aa, Bv], W=[dst])
                        k.op("dve", lambda: nc.vector.tensor_tensor_scan(out=dst[:, 0:T], data0=Aa[:, LAT0:LAT0 + T], data1=Bv[:, LAT0:LAT0 + T],
                                                                         initial=dst[:, NTOK - 1:NTOK], op0=ALU.mult, op1=ALU.add), R=[Aa, Bv, dst], W=[dst])
                    else:
                        k.op("dve", lambda: nc.vector.tensor_tensor_scan(out=dst[:, T:NTOK][:, ::-1], data0=Aa[:, CTX0:CTX0 + LC][:, ::-1],
                                                                         data1=Bv[:, CTX0:CTX0 + LC][:, ::-1],
                                                                         initial=0.0, op0=ALU.mult, op1=ALU.add), R=[Aa, Bv], W=[dst])
                        k.op("dve", lambda: nc.vector.tensor_tensor_scan(out=dst[:, 0:T][:, ::-1], data0=Aa[:, LAT0:LAT0 + T][:, ::-1],
                                                                         data1=Bv[:, LAT0:LAT0 + T][:, ::-1],
                                                                         initial=dst[:, T:T + 1], op0=ALU.mult, op1=ALU.add), R=[Aa, Bv, dst], W=[dst])
                        k.op("dve", lambda: nc.vector.tensor_tensor(out=HS[:], in0=HS[:], in1=tmp[:], op=ALU.add), R=[HS, tmp], W=[HS])
                k.op("dve", lambda: nc.vector.tensor_tensor(out=tmp[:], in0=gp[:], in1=gp[:], op=ALU.mult), R=[gp], W=[tmp])
                k.op("dve", lambda: nc.vector.tensor_scalar(out=tmp[:], in0=tmp[:], scalar1=0.044715, scalar2=1.0, op0=ALU.mult, op1=ALU.add), R=[tmp], W=[tmp])
                k.op("dve", lambda: nc.vector.tensor_tensor(out=tmp[:], in0=tmp[:], in1=gp[:], op=ALU.mult), R=[tmp, gp], W=[tmp])
                k.op("act", lambda: nc.scalar.activation(out=tmp[:], in_=tmp[:], func=AF.Sigmoid, scale=2.0 * math.sqrt(2.0 / math.pi)), R=[tmp], W=[tmp])
                k.op("dve", lambda: nc.vector.tensor_tensor(out=tmp[:], in0=tmp[:], in1=gp[:], op=ALU.mult), R=[tmp, gp], W=[tmp])
                k.op("dve", lambda: nc.vector.tensor_tensor(out=ob[:], in0=tmp[:], in1=HS[:], op=ALU.mult), R=[tmp, HS], W=[ob])
                k.dma(RGT[ch0:ch0 + 128, :], ob[:], R=[ob])
            k.barrier()

    def phase_attn(l):
        with ExitStack() as es:
            def sb(name, shape, dt=F32):
                return Buf(es.enter_context(nc.sbuf_tensor(un(name), list(shape), dt)))

            def ps(name, shape, dt=F32):
                return Buf(es.enter_context(nc.psum_tensor(un(name), list(shape), dt)))
            QT = sb("QT", [64, 8, NTOK], BF16)
            KT = sb("KT", [64, 2, NTOK], BF16)
            VA = sb("VA", [128, NTILE, 2, 65], BF16)
            qg = sb("qg", [128, 64]); kg = sb("kg", [128, 64])
            qkv = [sb("qkv%d" % i, [128, 768]) for i in range(2)]
            rope = [sb("rope%d" % i, [128, 2, 256]) for i in range(2)]
            sq = sb("sq", [128, 640]); ss = sb("ss", [128, 10]); qn = sb("qn", [128, 640])
            ra = sb("ra", [128, 320]); rb = sb("rb", [128, 320])
            qr = [sb("qr%d" % i, [128, 640], BF16) for i in range(2)]
            esp = ExitStack()
            ptq = [Buf(esp.enter_context(nc.psum_tensor(un("ptq%d" % i), [64, 8, 128], BF16))) for i in range(2)]
            ptk = Buf(esp.enter_context(nc.psum_tensor(un("ptk"), [64, 2, 128], BF16)))
            k.dma(qg[:], q_norm[l].partition_broadcast(128), W=[qg])
            k.dma(kg[:], k_norm[l].partition_broadcast(128), W=[kg])
            k.op("dve", lambda: nc.vector.memset(VA[:], 1.0), W=[VA])
            for i in range(NTILE):
                t_ = qkv[i % 2]; rp = rope[i % 2]; q_r = qr[i % 2]; pq = ptq[i % 2]
                r0 = pt_row(i * 128)
                k.dma(t_[:], PT[r0:r0 + 128, 768:1536], W=[t_])
                if i < 32:
                    k.dma(rp[:], ROPE_d[i * 128:(i + 1) * 128, :, :], W=[rp])
                k.op("dve", lambda: nc.vector.tensor_tensor(out=sq[:], in0=t_[:, 0:640], in1=t_[:, 0:640], op=ALU.mult), R=[t_], W=[sq])
                k.op("dve", lambda: nc.vector.tensor_reduce(out=ss[:], in_=sq[:].rearrange("p (h d) -> p h d", d=64), axis=AX.X, op=ALU.add), R=[sq], W=[ss])
                k.op("dve", lambda: nc.vector.tensor_scalar(out=ss[:], in0=ss[:], scalar1=1.0 / 64.0, scalar2=QK_EPS, op0=ALU.mult, op1=ALU.add), R=[ss], W=[ss])
                k.op("act", lambda: nc.scalar.activation(out=ss[:], in_=ss[:], func=AF.Sqrt), R=[ss], W=[ss])
                k.op("dve", lambda: nc.vector.reciprocal(out=ss[:], in_=ss[:]), R=[ss], W=[ss])
                for hh in range(10):
                    gsrc = qg if hh < 8 else kg
                    dstt = qn if i < 32 else q_r
                    k.op("dve", lambda: nc.vector.scalar_tensor_tensor(out=dstt[:, hh * 64:(hh + 1) * 64], in0=t_[:, hh * 64:(hh + 1) * 64],
                                                                       scalar=ss[:, hh:hh + 1], in1=gsrc[:], op0=ALU.mult, op1=ALU.mult),
                         R=[t_, ss, gsrc], W=[dstt])
                if i < 32:
                    qv = qn[:].rearrange("p (n two) -> p n two", two=2)
                    ov = q_r[:].rearrange("p (n two) -> p n two", two=2)
                    u1 = qv[:, :, 0]; u2 = qv[:, :, 1]
                    cosv = None
                    for (a0, a1, c0_, c1_) in ((0, 256, 0, 256), (256, 320, 0, 64)):
                        cs = rp[:, 0, c0_:c1_]; sn = rp[:, 1, c0_:c1_]
                        k.op("dve", lambda: nc.vector.tensor_tensor(out=ra[:, a0:a1], in0=u1[:, a0:a1], in1=cs, op=ALU.mult), R=[qn, rp], W=[ra])
                        k.op("dve", lambda: nc.vector.tensor_tensor(out=rb[:, a0:a1], in0=u2[:, a0:a1], in1=sn, op=ALU.mult), R=[qn, rp], W=[rb])
                        k.op("dve", lambda: nc.vector.tensor_tensor(out=ov[:, a0:a1, 0], in0=ra[:, a0:a1], in1=rb[:, a0:a1], op=ALU.subtract), R=[ra, rb], W=[q_r])
                        k.op("dve", lambda: nc.vector.tensor_tensor(out=ra[:, a0:a1], in0=u1[:, a0:a1], in1=sn, op=ALU.mult), R=[qn, rp], W=[ra])
                        k.op("dve", lambda: nc.vector.tensor_tensor(out=rb[:, a0:a1], in0=u2[:, a0:a1], in1=cs, op=ALU.mult), R=[qn, rp], W=[rb])
                        k.op("dve", lambda: nc.vector.tensor_tensor(out=ov[:, a0:a1, 1], in0=ra[:, a0:a1], in1=rb[:, a0:a1], op=ALU.add), R=[ra, rb], W=[q_r])
                for hh in range(8):
                    k.op("pe", lambda: nc.tensor.transpose(pq[:, hh, :], q_r[:, hh * 64:(hh + 1) * 64], identb[:]), R=[q_r, identb], W=[pq])
                for hh in range(2):
                    k.op("pe", lambda: nc.tensor.transpose(ptk[:, hh, :], q_r[:, (8 + hh) * 64:(9 + hh) * 64], identb[:]), R=[q_r, identb], W=[ptk])
                k.op("act", lambda: nc.scalar.copy(out=QT[:, :, i * 128:(i + 1) * 128], in_=pq[:, :, :]), R=[pq], W=[QT])
                k.op("act", lambda: nc.scalar.copy(out=KT[:, :, i * 128:(i + 1) * 128], in_=ptk[:, :, :]), R=[ptk], W=[KT])
                k.op("dve", lambda: nc.vector.tensor_copy(out=VA[:, i, :, 0:64], in_=t_[:, 640:768].rearrange("p (h d) -> p h d", d=64)), R=[t_], W=[VA])
            k.barrier()
            esp.close()
            pS = [ps("pS%d" % i, [128, 512]) for i in range(2)]
            pO = [ps("pO%d" % i, [128, 512]) for i in range(4)]
            Pb = [sb("Pb%d" % i, [128, 512], BF16) for i in range(3)]
            rd = sb("rd", [128, 4]);
            att = [sb("att%d" % i, [128, 4, 512], BF16) for i in range(2)]
            it = 0; io = 0
            jobs = [(qs * 512, 512, list(range(NTILE))) for qs in range(8)] + [(T, 256, [32, 33])]
            for ji, (q0, nq, kchunks) in enumerate(jobs):
                at = att[ji % 2]
                nsub = nq // 128
                for h in range(8):
                    kvh = h // 4
                    io += 1
                    def emit_pv(item):
                        P_, c_, ci_ = item
                        for s in range(nsub):
                            k.op("pe", lambda: nc.tensor.matmul(pO[s][:, 0:65], P_[:, s * 128:(s + 1) * 128], VA[:, c_, kvh, :],
                                                                start=(ci_ == 0), stop=(ci_ == len(kchunks) - 1)), R=[P_, VA], W=[pO[s]])
                    prev = None
                    for ci, c in enumerate(kchunks):
                        S = pS[it % 2]; P = Pb[it % 3]; it += 1
                        k.op("pe", lambda: nc.tensor.matmul(S[:, 0:nq], KT[:, kvh, c * 128:(c + 1) * 128], QT[:, h, q0:q0 + nq], start=True, stop=True),
                             R=[KT, QT], W=[S])
                        k.op("act", lambda: nc.scalar.activation(out=P[:, 0:nq], in_=S[:, 0:nq], func=AF.Exp, scale=0.125), R=[S], W=[P])
                        if prev is not None:
                            emit_pv(prev)
                        prev = (P, c, ci)
                    emit_pv(prev)
                    for s in range(nsub):
                        k.op("dve", lambda: nc.vector.reciprocal(out=rd[:, s:s + 1], in_=pO[s][:, 64:65]), R=[pO[s]], W=[rd])
                        k.op("dve", lambda: nc.vector.tensor_scalar(out=at[:, s, h * 64:(h + 1) * 64], in0=pO[s][:, 0:64], scalar1=rd[:, s:s + 1],
                                                                    scalar2=None, op0=ALU.mult), R=[pO[s], rd], W=[at])
                for s in range(nsub):
                    k.dma(MIXTOK[q0 + s * 128:q0 + (s + 1) * 128, 256:768], at[:, s, :], R=[at])
            k.barrier()

    def hyena_seq(l, es, sb, ps, Lq, tok0, WTd, W0Id, SCd, ZTd, WINd, WIN2d, KF, tag):
        nch = Lq // 128
        slab = [sb("slab%d" % i + tag, [128, nch, 2, 128], BF16) for i in range(3)]
        scs = sb("scs" + tag, [128, nch])
        k.dma(scs[:], SCd[:, :], W=[scs])
        pX = [ps("pX%d" % i + tag, [128, 512]) for i in range(4)]
        pN = ps("pN" + tag, [1, 512])
        kfo = [sb("kfo%d" % i + tag, [128, 2, 512]) for i in range(2)]
        altr = sb("altr" + tag, [1, 128], BF16)
        k.dma(altr[:], ALT_d[:, :], W=[altr])
        si = [0]

        def load_slab(j, inv=False):
            s_ = slab[si[0] % 3]; si[0] += 1
            k.dma(s_[:], W0Id[:, :, :, :] if (inv and j == 0) else WTd[j, :, :, :, :], W=[s_])
            if inv:
                k.dma(s_[0:1, 0, 1, :], ALT_d[:, :], W=[s_])
            return s_
        with ExitStack() as es2:
            def sb2(name, shape, dt=F32):
                return Buf(es2.enter_context(nc.sbuf_tensor(un(name + tag), list(shape), dt)))
            KS = sb2("KS", [128, nch, 512], BF16); KDf = sb2("KD", [128, nch, 512], BF16)
            h2T = sb2("h2T", [64, Lq])
            zT = sb2("zT", [17, Lq]); w1 = sb2("w1", [17, 64]); w2 = sb2("w2", [64, 64]); w3 = sb2("w3", [64, 1024])
            fr = sb2("fr", [64, 1]); b1 = sb2("b1", [64, 1]); b2 = sb2("b2", [64, 1]); fb1 = sb2("fb1", [64, 1]); fb2 = sb2("fb2", [64, 1])
            h1T = sb2("h1T", [64, Lq]); targ = [sb2("targ%d" % i, [64, 512]) for i in range(2)]
            win = [sb2("win%d" % i, [128, 256]) for i in range(2)]; win2 = [sb2("win2%d" % i, [128, 256]) for i in range(2)]
            fa = [sb2("fa%d" % i, [128, 512]) for i in range(2)]; fb_ = [sb2("fb_%d" % i, [128, 512]) for i in range(2)]
            pm = pX
            k.dma(zT[:], ZTd[:, :], W=[zT]); k.dma(w1[:], hy_f_w1[l, :, :], W=[w1]); k.dma(w2[:], hy_f_w2[l, :, :], W=[w2])
            k.dma(w3[:], hy_f_w3[l, :, :], W=[w3])
            k.dma(fr[:], colvec(hy_f_freq[l], 0, 64), W=[fr], allow_slow_non_contiguous=True)
            k.dma(b1[:], colvec(hy_f_b1[l], 0, 64), W=[b1], allow_slow_non_contiguous=True)
            k.dma(b2[:], colvec(hy_f_b2[l], 0, 64), W=[b2], allow_slow_non_contiguous=True)
            OFF = 0.0
            kiq = sb2("kiq", [64, 512], mybir.dt.int32); kfq = sb2("kfq", [64, 512])
            for (bsrc, fdst) in ((b1, fb1), (b2, fb2)):
                k.op("dve", lambda: nc.vector.tensor_tensor(out=fdst[:], in0=bsrc[:], in1=fr[:], op=ALU.mult), R=[bsrc, fr], W=[fdst])
                k.op("dve", lambda: nc.vector.tensor_scalar(out=fdst[:], in0=fdst[:], scalar1=OFF, scalar2=None, op0=ALU.add), R=[fdst], W=[fdst])
            nblk = max(1, Lq // 512); bw = min(512, Lq)

            def sin_layer(wm, src, dstb, fbb, kdim):
                for b in range(nblk):
                    p = pm[b % 2]; ta = targ[b % 2]
                    k.op("pe", lambda: nc.tensor.matmul(p[0:64, 0:bw], wm[:], src[0:kdim, b * bw:(b + 1) * bw], start=True, stop=True), R=[wm, src], W=[p])
                    k.op("dve", lambda: nc.vector.tensor_scalar(out=ta[:, 0:bw], in0=p[0:64, 0:bw], scalar1=fr[:, 0:1], scalar2=fbb[:, 0:1],
                                                                op0=ALU.mult, op1=ALU.add), R=[p, fr, fbb], W=[ta])
                    k.op("dve", lambda: nc.vector.tensor_scalar(out=kiq[:, 0:bw], in0=ta[:, 0:bw], scalar1=1.0 / (2.0 * math.pi), scalar2=None, op0=ALU.mult), R=[ta], W=[kiq])
                    k.op("dve", lambda: nc.vector.tensor_copy(out=kfq[:, 0:bw], in_=kiq[:, 0:bw]), R=[kiq], W=[kfq])
                    k.op("dve", lambda: nc.vector.scalar_tensor_tensor(out=ta[:, 0:bw], in0=kfq[:, 0:bw], scalar=-2.0 * math.pi, in1=ta[:, 0:bw], op0=ALU.mult, op1=ALU.add), R=[kfq, ta], W=[ta])
                    k.op("dve", lambda: nc.vector.tensor_scalar(out=kfq[:, 0:bw], in0=ta[:, 0:bw], scalar1=-math.pi, scalar2=None, op0=ALU.is_lt), R=[ta], W=[kfq])
                    k.op("dve", lambda: nc.vector.scalar_tensor_tensor(out=ta[:, 0:bw], in0=kfq[:, 0:bw], scalar=2.0 * math.pi, in1=ta[:, 0:bw], op0=ALU.mult, op1=ALU.add), R=[kfq, ta], W=[ta])
                    k.op("dve", lambda: nc.vector.tensor_scalar(out=kfq[:, 0:bw], in0=ta[:, 0:bw], scalar1=math.pi, scalar2=None, op0=ALU.is_gt), R=[ta], W=[kfq])
                    k.op("dve", lambda: nc.vector.scalar_tensor_tensor(out=ta[:, 0:bw], in0=kfq[:, 0:bw], scalar=-2.0 * math.pi, in1=ta[:, 0:bw], op0=ALU.mult, op1=ALU.add), R=[kfq, ta], W=[ta])
                    k.op("dve", lambda: nc.vector.tensor_scalar(out=ta[:, 0:bw], in0=ta[:, 0:bw], scalar1=-3.1415925, scalar2=3.1415925,
                                                                op0=ALU.max, op1=ALU.min), R=[ta], W=[ta])
                    k.op("act", lambda: nc.scalar.activation(out=dstb[:, b * bw:(b + 1) * bw], in_=ta[:, 0:bw], func=AF.Sin), R=[ta], W=[dstb])
            sin_layer(w1, zT, h1T, fb1, 17)
            sin_layer(w2, h1T, h2T, fb2, 64)
            for tc in range(nch):
                wn = win[tc % 2]; wn2 = win2[tc % 2]; A_ = fa[tc % 2]; B_ = fb_[tc % 2]
                k.dma(wn[:], WINd[tc * 128:(tc + 1) * 128, :], W=[wn]); k.dma(wn2[:], WIN2d[tc * 128:(tc + 1) * 128, :], W=[wn2])
                p0 = pm[2]; p1 = pm[3]
                k.op("pe", lambda: nc.tensor.matmul(p0[:, :], h2T[:, tc * 128:(tc + 1) * 128], w3[:, 0:512], start=True, stop=True), R=[h2T, w3], W=[p0])
                k.op("pe", lambda: nc.tensor.matmul(p1[:, :], h2T[:, tc * 128:(tc + 1) * 128], w3[:, 512:1024], start=True, stop=True), R=[h2T, w3], W=[p1])
                for o in range(2):
                    k.op("dve", lambda: nc.vector.tensor_tensor(out=A_[:, o * 256:(o + 1) * 256], in0=p0[:, o * 256:(o + 1) * 256], in1=wn[:], op=ALU.mult), R=[p0, wn], W=[A_])
                    k.op("dve", lambda: nc.vector.tensor_tensor(out=B_[:, o * 256:(o + 1) * 256], in0=p1[:, o * 256:(o + 1) * 256], in1=wn2[:], op=ALU.mult), R=[p1, wn2], W=[B_])
                k.op("pool", lambda: nc.gpsimd.tensor_tensor(out=KS[:, tc, :], in0=A_[:], in1=B_[:], op=ALU.add), R=[A_, B_], W=[KS])
                k.op("pool", lambda: nc.gpsimd.tensor_tensor(out=KDf[:, tc, :], in0=A_[:], in1=B_[:], op=ALU.subtract), R=[A_, B_], W=[KDf])
            nxt = load_slab(0)
            for j in range(nch):
                s_ = nxt
                if j + 1 < nch:
                    nxt = load_slab(j + 1)
                pre = pX[(2 * j) % 4]; pim = pX[(2 * j + 1) % 4]; ko = kfo[j % 2]
                for tc in range(nch):
                    k.op("pe", lambda: nc.tensor.matmul(pre[:, :], s_[:, tc, 0, :], KS[:, tc, :], start=(tc == 0), stop=(tc == nch - 1)), R=[s_, KS], W=[pre])
                for tc in range(nch):
                    k.op("pe", lambda: nc.tensor.matmul(pim[:, :], s_[:, tc, 1, :], KDf[:, tc, :], start=(tc == 0), stop=(tc == nch - 1)), R=[s_, KDf], W=[pim])
                if j == 0:
                    for tc in range(nch):
                        k.op("pe", lambda: nc.tensor.matmul(pN[:, :], s_[:, tc, 1, 0:1], KS[:, tc, :], start=(tc == 0), stop=(tc == nch - 1)), R=[s_, KS], W=[pN])
                k.op("act", lambda: nc.scalar.activation(out=ko[:, 0, :], in_=pre[:, :], func=AF.Identity, scale=scs[:, j:j + 1]), R=[pre, scs], W=[ko])
                k.op("dve", lambda: nc.vector.tensor_scalar(out=ko[:, 1, :], in0=pim[:, :], scalar1=scs[:, j:j + 1], scalar2=None, op0=ALU.mult), R=[pim, scs], W=[ko])
                if j == 0:
                    k.op("dve", lambda: nc.vector.tensor_scalar(out=ko[0:1, 1, :], in0=pN[:, :], scalar1=scs[0:1, 0:1], scalar2=None, op0=ALU.mult), R=[pN, scs], W=[ko])
                k.dma(KF[j, :, :, :], ko[:], R=[ko])
            k.barrier()
        V = sb("V" + tag, [128, nch, 256], BF16)
        Y = sb("Y" + tag, [128, nch, 2, 256], BF16)
        wsb = sb("wsb" + tag, [128, 3, 768]); bsb = sb("bsb" + tag, [128, 768]); skb = sb("skb" + tag, [128, 2, 256])
        for jj in range(3):
            k.dma(wsb[:, jj, :], hy_short_w[l, jj, :].partition_broadcast(128), W=[wsb])
        k.dma(bsb[:], hy_short_b[l].partition_broadcast(128), W=[bsb])
        for o in range(2):
            k.dma(skb[:, o, :], hy_skip[l, o, :].partition_broadcast(128), W=[skb])
        pin = [[sb("pin%d_%d" % (a, b) + tag, [128, 768]) for b in range(3)] for a in range(2)]
        cacc = [sb("cacc%d" % i + tag, [128, 768]) for i in range(2)]
        ctmp = [sb("ctmp%d" % i + tag, [128, 768]) for i in range(2)]
        for tc in range(nch):
            tok = tok0 + tc * 128
            r0 = pt_row(tok)
            pp = pin[tc % 2]; ac = cacc[tc % 2]; tm = ctmp[tc % 2]
            for jj in range(3):
                k.dma(pp[jj][:], PT[r0 + jj - 1:r0 + jj - 1 + 128, 0:768], W=[pp[jj]])
            k.op("dve", lambda: nc.vector.tensor_tensor(out=ac[:], in0=pp[0][:], in1=wsb[:, 0, :], op=ALU.mult), R=[pp[0], wsb], W=[ac])
            k.op("pool", lambda: nc.gpsimd.tensor_tensor(out=tm[:], in0=pp[1][:], in1=wsb[:, 1, :], op=ALU.mult), R=[pp[1], wsb], W=[tm])
            k.op("dve", lambda: nc.vector.tensor_tensor(out=ac[:], in0=ac[:], in1=tm[:], op=ALU.add), R=[ac, tm], W=[ac])
            k.op("pool", lambda: nc.gpsimd.tensor_tensor(out=tm[:], in0=pp[2][:], in1=wsb[:, 2, :], op=ALU.mult), R=[pp[2], wsb], W=[tm])
            k.op("dve", lambda: nc.vector.tensor_tensor(out=ac[:], in0=ac[:], in1=tm[:], op=ALU.add), R=[ac, tm], W=[ac])
            k.op("dve", lambda: nc.vector.tensor_tensor(out=ac[:], in0=ac[:], in1=bsb[:], op=ALU.add), R=[ac, bsb], W=[ac])
            k.op("act", lambda: nc.scalar.copy(out=V[:, tc, :], in_=ac[:, 512:768]), R=[ac], W=[V])
            k.dma(HX[tok:tok + 128, :], ac[:], R=[ac])
        k.barrier()
        xs = [sb("xs%d" % i + tag, [128, 2, 256]) for i in range(2)]
        kin = [sb("kin%d" % i + tag, [128, 2, 256]) for i in range(2)]
        ta_ = [sb("ta%d" % i + tag, [128, 256]) for i in range(2)]
        tb_ = [sb("tb%d" % i + tag, [128, 256]) for i in range(2)]
        vin = [sb("vin%d" % i + tag, [128, 256]) for i in range(2)]
        gin = [sb("gin%d" % i + tag, [128, 256]) for i in range(2)]
        yv = [sb("yv%d" % i + tag, [128, 256]) for i in range(2)]
        ybf = [sb("ybf%d" % i + tag, [128, 256], BF16) for i in range(2)]
        nyq = sb("nyq" + tag, [1, 256])
        for n in range(2):
            nxt = load_slab(0)
            for j in range(nch):
                s_ = nxt
                nxt = load_slab(j + 1) if j + 1 < nch else load_slab(0, inv=True)
                pre = pX[(2 * j) % 4]; pim = pX[(2 * j + 1) % 4]
                X = xs[j % 2]; Kc = kin[j % 2]; a_ = ta_[j % 2]; b_ = tb_[j % 2]
                k.dma(Kc[:], KF[j, :, :, n * 256:(n + 1) * 256], W=[Kc])
                for tc in range(nch):
                    k.op("pe", lambda: nc.tensor.matmul(pre[:, 0:256], s_[:, tc, 0, :], V[:, tc, :], start=(tc == 0), stop=(tc == nch - 1)), R=[s_, V], W=[pre])
                for tc in range(nch):
                    k.op("pe", lambda: nc.tensor.matmul(pim[:, 0:256], s_[:, tc, 1, :], V[:, tc, :], start=(tc == 0), stop=(tc == nch - 1)), R=[s_, V], W=[pim])
                k.op("act", lambda: nc.scalar.copy(out=X[:, 0, :], in_=pre[:, 0:256]), R=[pre], W=[X])
                k.op("act", lambda: nc.scalar.copy(out=X[:, 1, :], in_=pim[:, 0:256]), R=[pim], W=[X])
                k.op("dve", lambda: nc.vector.tensor_tensor(out=a_[:], in0=X[:, 0, :], in1=Kc[:, 0, :], op=ALU.mult), R=[X, Kc], W=[a_])
                k.op("pool", lambda: nc.gpsimd.tensor_tensor(out=b_[:], in0=X[:, 1, :], in1=Kc[:, 1, :], op=ALU.mult), R=[X, Kc], W=[b_])
                k.op("dve", lambda: nc.vector.tensor_tensor(out=Y[:, j, 0, :], in0=a_[:], in1=b_[:], op=ALU.subtract), R=[a_, b_], W=[Y])
                if j == 0:
                    k.op("dve", lambda: nc.vector.tensor_copy(out=Y[0:1, j, 0, :], in_=a_[0:1, :]), R=[a_], W=[Y])
                    k.op("dve", lambda: nc.vector.tensor_copy(out=nyq[:], in_=b_[0:1, :]), R=[b_], W=[nyq])
                k.op("dve", lambda: nc.vector.tensor_tensor(out=a_[:], in0=X[:, 0, :], in1=Kc[:, 1, :], op=ALU.mult), R=[X, Kc], W=[a_])
                k.op("pool", lambda: nc.gpsimd.tensor_tensor(out=b_[:], in0=X[:, 1, :], in1=Kc[:, 0, :], op=ALU.mult), R=[X, Kc], W=[b_])
                k.op("dve", lambda: nc.vector.tensor_tensor(out=Y[:, j, 1, :], in0=a_[:], in1=b_[:], op=ALU.add), R=[a_, b_], W=[Y])
                if j == 0:
                    k.op("dve", lambda: nc.vector.tensor_copy(out=Y[0:1, j, 1, :], in_=nyq[:]), R=[nyq], W=[Y])
            for i in range(nch):
                s_ = nxt
                if i + 1 < nch:
                    nxt = load_slab(i + 1, inv=True)
                po_ = pX[i % 4]
                tok = tok0 + i * 128
                vi = vin[i % 2]; gi = gin[i % 2]; y_ = yv[i % 2]; yb = ybf[i % 2]
                if n == 0:
                    k.dma(vi[:], HX[tok:tok + 128, 512:768], W=[vi])
                    k.dma(gi[:], HX[tok:tok + 128, 0:256], W=[gi])
                else:
                    k.dma(vi[:], V1D[tok:tok + 128, :], W=[vi])
                    k.dma(gi[:], HX[tok:tok + 128, 256:512], W=[gi])
                for fc in range(nch):
                    k.op("pe", lambda: nc.tensor.matmul(po_[:, 0:256], s_[:, fc, 0, :], Y[:, fc, 0, :], start=(fc == 0), stop=False), R=[s_, Y], W=[po_])
                    k.op("pe", lambda: nc.tensor.matmul(po_[:, 0:256], s_[:, fc, 1, :], Y[:, fc, 1, :], start=False, stop=(fc == nch - 1)), R=[s_, Y], W=[po_])
                k.op("pool", lambda: nc.gpsimd.tensor_tensor(out=vi[:], in0=vi[:], in1=skb[:, n, :], op=ALU.mult), R=[vi, skb], W=[vi])
                k.op("dve", lambda: nc.vector.tensor_tensor(out=y_[:], in0=po_[:, 0:256], in1=vi[:], op=ALU.add), R=[po_, vi], W=[y_])
                if n == 0:
                    k.op("dve", lambda: nc.vector.tensor_tensor(out=y_[:], in0=y_[:], in1=gi[:], op=ALU.mult), R=[y_, gi], W=[y_])
                    k.op("act", lambda: nc.scalar.copy(out=V[:, i, :], in_=y_[:]), R=[y_], W=[V])
                    k.dma(V1D[tok:tok + 128, :], y_[:], R=[y_])
                else:
                    k.op("dve", lambda: nc.vector.tensor_tensor(out=yb[:], in0=y_[:], in1=gi[:], op=ALU.mult), R=[y_, gi], W=[yb])
                    k.dma(MIXTOK[tok:tok + 128, 0:256], yb[:], R=[yb])
            k.barrier()

    def phase_hyena(l):
        for (Lq, tok0, WTd, W0Id, SCd, ZTd, WINd, WIN2d, KF, tag) in ((T, 0, WT_d, W0I_d, SC_d, ZT_d, WIN_d, WIN2_d, KFD, "L"),
                                                                     (LC, T, WTc_d, W0Ic_d, SCc_d, ZTc_d, WINc_d, WIN2c_d, KFDc, "C")):
            with ExitStack() as es:
                def sb(name, shape, dt=F32):
                    return Buf(es.enter_context(nc.sbuf_tensor(un(name), list(shape), dt)))

                def ps(name, shape, dt=F32):
                    return Buf(es.enter_context(nc.psum_tensor(un(name), list(shape), dt)))
                hyena_seq(l, es, sb, ps, Lq, tok0, WTd, W0Id, SCd, ZTd, WINd, WIN2d, KF, tag)
            k.barrier()

    def resid_ln(sbufs, ysrc, h_t, gb, gam, bet, out_t):
        u_, st, mv = sbufs
        for hf_ in range(2):
            sl = slice(hf_ * 512, (hf_ + 1) * 512)
            yb_, yap = ysrc[hf_]
            k.op("dve", lambda: nc.vector.tensor_tensor(out=u_[:, sl], in0=yap, in1=gb[:, sl], op=ALU.mult), R=[yb_, gb], W=[u_])
        k.op("dve", lambda: nc.vector.scalar_tensor_tensor(out=u_[:], in0=h_t[:], scalar=DN_ALPHA, in1=u_[:], op0=ALU.mult, op1=ALU.add), R=[h_t, u_], W=[u_])
        for hf_ in range(2):
            k.op("dve", lambda: nc.vector.bn_stats(out=st[:, hf_, :], in_=u_[:, hf_ * 512:(hf_ + 1) * 512]), R=[u_], W=[st])
        k.op("dve", lambda: nc.vector.bn_aggr(out=mv[:, 0:2], in_=st[:].rearrange("p a b -> p (a b)")), R=[st], W=[mv])
        k.op("dve", lambda: nc.vector.tensor_scalar(out=mv[:, 1:2], in0=mv[:, 1:2], scalar1=LN_EPS, scalar2=None, op0=ALU.add), R=[mv], W=[mv])
        k.op("act", lambda: nc.scalar.activation(out=mv[:, 1:2], in_=mv[:, 1:2], func=AF.Sqrt), R=[mv], W=[mv])
        k.op("dve", lambda: nc.vector.reciprocal(out=mv[:, 1:2], in_=mv[:, 1:2]), R=[mv], W=[mv])
        k.op("dve", lambda: nc.vector.tensor_scalar(out=u_[:], in0=u_[:], scalar1=mv[:, 0:1], scalar2=mv[:, 1:2], op0=ALU.subtract, op1=ALU.mult), R=[u_, mv], W=[u_])
        k.op("pool", lambda: nc.gpsimd.tensor_tensor(out=u_[:], in0=u_[:], in1=gam[:], op=ALU.mult), R=[u_, gam], W=[u_])
        k.op("pool", lambda: nc.gpsimd.tensor_tensor(out=out_t[:], in0=u_[:], in1=bet[:], op=ALU.add), R=[u_, bet], W=[out_t])

    def phase_outproj(l):
        with ExitStack() as es:
            def sb(name, shape, dt=F32):
                return Buf(es.enter_context(nc.sbuf_tensor(un(name), list(shape), dt)))

            def ps(name, shape, dt=F32):
                return Buf(es.enter_context(nc.psum_tensor(un(name), list(shape), dt)))
            Wst = [sb("wst%d" % i, [128, 8, 512]) for i in range(2)]
            Wb = sb("Wob", [128, 8, D], BF16)
            for cb in range(2):
                st = Wst[cb]
                k.dma(st[:], w_out[l, :, cb * 512:(cb + 1) * 512].rearrange("(k p) n -> p k n", p=128), W=[st])
                k.op("pool", lambda: nc.gpsimd.tensor_copy(out=Wb[:, :, cb * 512:(cb + 1) * 512], in_=st[:]), R=[st], W=[Wb])
            gb = [sb("gb%d" % j, [128, D]) for j in range(2)]
            gam = sb("gam", [128, D]); bet = sb("bet", [128, D])
            for j in range(2):
                k.dma(gb[j][:], GMOD[l, 0, j, :].partition_broadcast(128), W=[gb[j]])
            k.dma(gam[:], ln1_g[l].partition_broadcast(128), W=[gam]); k.dma(bet[:], ln1_b[l].partition_broadcast(128), W=[bet])
            mx = [sb("mx%d" % i, [128, 768], BF16) for i in range(2)]
            mT = [sb("mT%d" % i, [128, 8, 128], BF16) for i in range(2)]
            hf = [sb("hf%d" % i, [128, D]) for i in range(2)]
            ub = [sb("ub%d" % i, [128, D]) for i in range(2)]
            ob = [sb("obo%d" % i, [128, D]) for i in range(2)]
            st6 = [sb("st6%d" % i, [128, 2, 6]) for i in range(2)]; mv = [sb("mv%d" % i, [128, 2]) for i in range(2)]
            tp = [ps("tpo%d" % i, [128, 6, 128], BF16) for i in range(2)]
            py = [[ps("py%d_%d" % (a, b), [128, 512]) for b in range(2)] for a in range(2)]
            for i in range(NTILE):
                j = 0 if i < 32 else 1
                m_ = mx[i % 2]; mt = mT[i % 2]; h_ = hf[i % 2]; tpp = tp[i % 2]; pyy = py[i % 2]
                k.dma(m_[:], MIXTOK[i * 128:(i + 1) * 128, :], W=[m_])
                k.dma(mt[:, 6:8, :], RGT[:, i * 128:(i + 1) * 128].rearrange("(c p) n -> p c n", p=128), W=[mt])
                k.dma(h_[:], hsrc(l, i), W=[h_])
                for kk in range(6):
                    k.op("pe", lambda: nc.tensor.transpose(tpp[:, kk, :], m_[:, kk * 128:(kk + 1) * 128], identb[:]), R=[m_, identb], W=[tpp])
                k.op("act", lambda: nc.scalar.copy(out=mt[:, 0:6, :], in_=tpp[:, :, :]), R=[tpp], W=[mt])
                for nb in range(2):
                    for kk in range(8):
                        k.op("pe", lambda: nc.tensor.matmul(pyy[nb][:, :], mt[:, kk, :], Wb[:, kk, nb * 512:(nb + 1) * 512], start=(kk == 0), stop=(kk == 7)),
                             R=[mt, Wb], W=[pyy[nb]])
                o_ = ob[i % 2]
                resid_ln((ub[i % 2], st6[i % 2], mv[i % 2]), [(pyy[0], pyy[0][:, :]), (pyy[1], pyy[1][:, :])], h_, gb[j], gam, bet, o_)
                k.dma(H1D[i * 128:(i + 1) * 128, :], o_[:], R=[o_])
            k.barrier()

    def phase_moe(l, last):
        passes = [(0, 12), (12, 24), (24, 34)]
        with ExitStack() as es:
            def sb(name, shape, dt=F32):
                return Buf(es.enter_context(nc.sbuf_tensor(un(name), list(shape), dt)))

            def ps(name, shape, dt=F32):
                return Buf(es.enter_context(nc.psum_tensor(un(name), list(shape), dt)))
            yacc = sb("yacc", [128, 12, D])
            mT16 = sb("mT16", [128, 8, 1536], BF16)
            gT = sb("gT", [65, 1536], BF16)
            SEL = sb("SEL", [65, 65 * 128], BF16)
            k.dma(SEL[:], SEL_d[:, :], W=[SEL])
            wst = [sb("west%d" % i, [128, 2048]) for i in range(3)]
            wgb = [sb("wgb%d" % i, [128, 8, 256], BF16) for i in range(2)]
            wub = [sb("wub%d" % i, [128, 8, 256], BF16) for i in range(2)]
            wdb = [sb("wdb%d" % i, [128, 2, D], BF16) for i in range(2)]
            Wr = sb("Wr", [128, 8, 64]); brb = sb("brb", [128, 64])
            k.dma(Wr[:], w_router[l].rearrange("(k p) n -> p k n", p=128), W=[Wr])
            k.dma(brb[:], b_router[l].partition_broadcast(128), W=[brb])
            gb = [sb("g2b%d" % j, [128, D]) for j in range(2)]
            gam = sb("gam2", [128, D]); bet = sb("bet2", [128, D])
            for j in range(2):
                k.dma(gb[j][:], GMOD[l, 1, j, :].partition_broadcast(128), W=[gb[j]])
            k.dma(gam[:], ln2_g[l].partition_broadcast(128), W=[gam]); k.dma(bet[:], ln2_b[l].partition_broadcast(128), W=[bet])
            hf = [sb("hfm%d" % i, [128, D]) for i in range(2)]
            mT32 = sb("mT32", [128, 8, 128])
            sc_ = sb("scr", [128, 64]); sel = sb("selr", [128, 64]); top8 = sb("top8", [128, 8]); den = sb("den", [128, 1])
            G = sb("G", [128, 65])
            sS = [sb("sS%d" % i, [128, 512]) for i in range(2)]
            tS = [sb("tS%d" % i, [128, 512]) for i in range(2)]
            gbs = [sb("gbs%d" % i, [128, 512], BF16) for i in range(2)]
            hT = [[sb("hT%d_%d" % (a, b), [128, 512], BF16) for b in range(2)] for a in range(2)]
            ub = sb("ubm", [128, D]); obm = [sb("obm%d" % i, [128, D]) for i in range(2)]
            st6 = sb("st6m", [128, 2, 6]); mv = sb("mvm", [128, 2])
            pg = [ps("pg%d" % i, [128, 512]) for i in range(2)]
            pu = [ps("pu%d" % i, [128, 512]) for i in range(2)]
            pgb = ps("pgb", [128, 512])
            pd = [ps("pd%d" % i, [128, 512]) for i in range(3)]
            k.op("dve", lambda: nc.vector.memset(G[:], 1.0), W=[G])
            wsti = [0]
            for (t0, t1) in passes:
                ntile = t1 - t0
                ntok = ntile * 128
                for ii, i in enumerate(range(t0, t1)):
                    j = 0 if i < 32 else 1
                    h_ = hf[ii % 2]
                    k.dma(h_[:], H1D[i * 128:(i + 1) * 128, :], W=[h_])
                    for half in range(2):
                        pt_ = pd[half]
                        for k4 in range(4):
                            kk = half * 4 + k4
                            k.op("pe", lambda: nc.tensor.transpose(pt_[:, k4 * 128:(k4 + 1) * 128], h_[:, kk * 128:(kk + 1) * 128], identf[:]),
                                 R=[h_, identf], W=[pt_])
                        for k4 in range(4):
                            kk = half * 4 + k4
                            sc = MOD[:, l, 32 + kk, j:j + 1]; sh = MOD[:, l, 24 + kk, j:j + 1]
                            k.op("act", lambda: nc.scalar.activation(out=mT32[:, kk, :], in_=pt_[:, k4 * 128:(k4 + 1) * 128], func=AF.Identity, bias=sh, scale=sc),
                                 R=[pt_, MOD], W=[mT32])
                            k.op("dve", lambda: nc.vector.tensor_copy(out=mT16[:, kk, ii * 128:(ii + 1) * 128], in_=mT32[:, kk, :]), R=[mT32], W=[mT16])
                    pl = pd[2]
                    for kk in range(8):
                        k.op("pe", lambda: nc.tensor.matmul(pl[:, 0:64], mT32[:, kk, :], Wr[:, kk, :], start=(kk == 0), stop=(kk == 7)), R=[mT32, Wr], W=[pl])
                    k.op("act", lambda: nc.scalar.activation(out=sc_[:], in_=pl[:, 0:64], func=AF.Sigmoid), R=[pl], W=[sc_])
                    k.op("dve", lambda: nc.vector.tensor_tensor(out=sel[:], in0=sc_[:], in1=brb[:], op=ALU.add), R=[sc_, brb], W=[sel])
                    k.op("dve", lambda: nc.vector.max(out=top8[:], in_=sel[:]), R=[sel], W=[top8])
                    k.op("dve", lambda: nc.vector.tensor_scalar(out=sel[:], in0=sel[:], scalar1=top8[:, 7:8], scalar2=None, op0=ALU.is_ge), R=[sel, top8], W=[sel])
                    k.op("dve", lambda: nc.vector.tensor_tensor(out=sel[:], in0=sel[:], in1=sc_[:], op=ALU.mult), R=[sel, sc_], W=[sel])
                    k.op("dve", lambda: nc.vector.tensor_reduce(out=den[:], in_=sel[:], axis=AX.X, op=ALU.add), R=[sel], W=[den])
                    k.op("dve", lambda: nc.vector.reciprocal(out=den[:], in_=den[:]), R=[den], W=[den])
                    k.op("dve", lambda: nc.vector.tensor_scalar(out=G[:, 0:64], in0=sel[:], scalar1=den[:, 0:1], scalar2=2.5, op0=ALU.mult, op1=ALU.mult), R=[sel, den], W=[G])
                    pgt = pg[ii % 2]
                    k.op("pe", lambda: nc.tensor.transpose(pgt[0:65, 0:128], G[:, :], identf[:]), R=[G, identf], W=[pgt])
                    k.op("act", lambda: nc.scalar.copy(out=gT[:, ii * 128:(ii + 1) * 128], in_=pgt[0:65, 0:128]), R=[pgt], W=[gT])
                k.op("dve", lambda: nc.vector.memset(yacc[:], 0.0), W=[yacc])
                blocks = []
                b0 = 0
                while b0 < ntok:
                    nb_ = min(512, ntok - b0)
                    blocks.append((b0, nb_)); b0 += nb_
                pending = None
                wi = 0
                def expert_srcs(e_):
                    return ((w_gate[l, e_], w_up[l, e_], w_down[l, e_]) if e_ < 64 else (ws_gate[l], ws_up[l], ws_down[l]))

                def issue_loads(e_):
                    for mi, src in enumerate(expert_srcs(e_)):
                        st = wst[mi]
                        k.dma(st[:].rearrange("p (k n) -> p k n", n=(256 if mi < 2 else D)), src.rearrange("(k p) n -> p k n", p=128), W=[st])

                def issue_casts(e_, which):
                    for mi, dstw in enumerate((wgb[e_ % 2], wub[e_ % 2], wdb[e_ % 2])):
                        if mi not in which:
                            continue
                        st = wst[mi]
                        k.op("act", lambda: nc.scalar.copy(out=dstw[:].rearrange("p k n -> p (k n)"), in_=st[:]), R=[st], W=[dstw])
                issue_loads(0)
                issue_casts(0, (0, 1, 2))
                for e in range(65):
                    Wg_ = wgb[e % 2]; Wu_ = wub[e % 2]; Wd_ = wdb[e % 2]
                    if e + 1 < 65:
                        issue_loads(e + 1)
                    bidx = -1
                    for (bb, nb_) in blocks:
                        hpair = hT[wi % 2]; gb_ = gbs[wi % 2]; wi += 1
                        dn = down_steps(pending, pd, yacc) if pending is not None else iter(())

                        def drain(n_):
                            for _ in range(n_):
                                f_ = next(dn, None)
                                if f_ is None:
                                    return
                                f_()
                        for fc in range(2):
                            g_ = pg[fc]; u_ = pu[fc]; s_ = sS[fc]; t_ = tS[fc]
                            for kk in range(8):
                                k.op("pe", lambda: nc.tensor.matmul(g_[:, 0:nb_], Wg_[:, kk, fc * 128:(fc + 1) * 128], mT16[:, kk, bb:bb + nb_],
                                                                    start=(kk == 0), stop=(kk == 7)), R=[Wg_, mT16], W=[g_])
                            drain(2)
                            for kk in range(8):
                                k.op("pe", lambda: nc.tensor.matmul(u_[:, 0:nb_], Wu_[:, kk, fc * 128:(fc + 1) * 128], mT16[:, kk, bb:bb + nb_],
                                                                    start=(kk == 0), stop=(kk == 7)), R=[Wu_, mT16], W=[u_])
                            if fc == 0:
                                k.op("pe", lambda: nc.tensor.matmul(pgb[:, 0:nb_], SEL[:, e * 128:(e + 1) * 128], gT[:, bb:bb + nb_], start=True, stop=True),
                                     R=[SEL, gT], W=[pgb])
                                k.op("act", lambda: nc.scalar.copy(out=gb_[:, 0:nb_], in_=pgb[:, 0:nb_]), R=[pgb], W=[gb_])
                            k.op("act", lambda: nc.scalar.activation(out=s_[:, 0:nb_], in_=g_[:, 0:nb_], func=AF.Silu), R=[g_], W=[s_])
                            k.op("dve", lambda: nc.vector.tensor_tensor(out=t_[:, 0:nb_], in0=u_[:, 0:nb_], in1=s_[:, 0:nb_], op=ALU.mult), R=[u_, s_], W=[t_])
                            k.op("pool", lambda: nc.gpsimd.tensor_tensor(out=hpair[fc][:, 0:nb_], in0=t_[:, 0:nb_], in1=gb_[:, 0:nb_], op=ALU.mult),
                                 R=[t_, gb_], W=[hpair[fc]])
                            drain(2)
                        drain(100)
                        pending = (hpair, Wd_, bb, nb_)
                        bidx += 1
                        if bidx == 1 and e + 1 < 65:
                            issue_casts(e + 1, (0, 1))
                    if e + 1 < 65:
                        issue_casts(e + 1, (2,))
                for f_ in down_steps(pending, pd, yacc):
                    f_()
                for ii, i in enumerate(range(t0, t1)):
                    j = 0 if i < 32 else 1
                    h_ = hf[ii % 2]
                    o_ = obm[ii % 2]
                    k.dma(h_[:], H1D[i * 128:(i + 1) * 128, :], W=[h_])
                    resid_ln((ub, st6, mv), [(yacc, yacc[:, ii, 0:512]), (yacc, yacc[:, ii, 512:1024])], h_, gb[j], gam, bet, o_)
                    if last:
                        if i < 32:
                            k.dma(y_out[i * 128:(i + 1) * 128, :], o_[:], R=[o_])
                    else:
                        k.dma(HD[i * 128:(i + 1) * 128, :], o_[:], R=[o_])
            k.barrier()

    pdi = [0]

    def down_steps(item, pd, yacc):
        hpair, Wd_, bb, nb_ = item
        for sub in range(nb_ // 128):
            ti_ = (bb // 128) + sub
            for dh in range(2):
                def step(sub=sub, ti_=ti_, dh=dh):
                    p = pd[pdi[0] % 3]; pdi[0] += 1
                    for fc in range(2):
                        k.op("pe", lambda: nc.tensor.matmul(p[:, :], hpair[fc][:, sub * 128:(sub + 1) * 128], Wd_[:, fc, dh * 512:(dh + 1) * 512],
                                                            start=(fc == 0), stop=(fc == 1)), R=[hpair[fc], Wd_], W=[p])
                    k.op("dve", lambda: nc.vector.tensor_tensor(out=yacc[:, ti_, dh * 512:(dh + 1) * 512], in0=p[:, :],
                                                                in1=yacc[:, ti_, dh * 512:(dh + 1) * 512], op=ALU.add), R=[p, yacc], W=[yacc])
                yield step

    phase_mod()
    seq = []
    for l in range(L):
        seq += [("inproj", l), ("rg", l), ("attn", l), ("hyena", l), ("outproj", l), ("moe", l)]
    for (ph, l) in seq:
        if ph == "inproj":
            phase_inproj(l)
        elif ph == "rg":
            phase_rg(l)
        elif ph == "attn":
            phase_attn(l)
        elif ph == "hyena":
            phase_hyena(l)
        elif ph == "outproj":
            phase_outproj(l)
        elif ph == "moe":
            phase_moe(l, last=(l == L - 1))
        if stop_after == (ph, l):
            break
    k.barrier()
    gs.close()
    return nc


_CONST = {}


def _consts():
    if _CONST:
        return _CONST
    bf = ml_dtypes.bfloat16

    def dft_tables(Lq):
        N = 2 * Lq
        n = np.arange(Lq, dtype=np.float64)
        ang = 2.0 * np.pi * np.outer(n, n) / N
        C = np.cos(ang)
        S = -np.sin(ang)
        alt = (-1.0) ** n
        S[:, 0] = alt
        nch = Lq // 128
        W = np.stack([C, S], axis=0)
        W = W.reshape(2, nch, 128, nch, 128)
        WTt = np.ascontiguousarray(W.transpose(3, 2, 1, 0, 4)).astype(np.float32).astype(bf)
        sc = np.full((128, nch), 2.0 / N, np.float32)
        sc[0, 0] = 1.0 / N
        W0I = WTt[0].copy()
        W0I[:, :, 1, 0] = 0
        return WTt, sc, W0I

    def filt_tables(Lq):
        t = np.linspace(0.0, 1.0, Lq, dtype=np.float32)[:, None]
        w = (2.0 * np.float32(math.pi) * np.arange(Lq, dtype=np.float32)[:, None] / np.float32(Lq)).astype(np.float32)
        bands = np.linspace(1e-4, 7, 8, dtype=np.float32)
        z = np.concatenate([t, np.cos(bands * w), -np.sin(bands * w)], axis=-1).astype(np.float32)
        mx = math.log(1e-2) / 0.3
        mn = math.log(1e-2) / 1.5
        deltas = np.abs(np.linspace(mn, mx, 256, dtype=np.float32))
        win = np.exp(-t * deltas).astype(np.float32)
        win2 = win.copy()
        win2[0, :] = 0.0
        return np.ascontiguousarray(z.T), win, win2

    WTt, sc, w0i = dft_tables(T)
    WTc, scc, w0ic = dft_tables(LC)
    zt, win, win2 = filt_tables(T)
    ztc, winc, win2c = filt_tables(LC)
    n_rows = T // 64
    row = np.repeat(np.arange(n_rows, dtype=np.float32), 64)
    col = np.tile(np.arange(64, dtype=np.float32), n_rows)
    inv_freq = (np.float32(10000.0) ** (-np.arange(0, 32, 2, dtype=np.float32) / np.float32(32))).astype(np.float32)
    ang = np.concatenate([row[:, None] * inv_freq, col[:, None] * inv_freq], axis=-1).astype(np.float32)
    rope = np.stack([np.tile(np.cos(ang), (1, 8)), np.tile(np.sin(ang), (1, 8))], axis=1).astype(np.float32)
    sel = np.zeros((65, 65, 128), np.float32)
    for e in range(65):
        sel[e, e, :] = 1.0
    _CONST.update(dict(
        identb=np.eye(128, dtype=np.float32).astype(bf), identf=np.eye(128, dtype=np.float32),
        WT=WTt, WTc=WTc, W0I=w0i, W0Ic=w0ic, SC=sc, SCc=scc, ZT=zt, ZTc=ztc, WIN=win, WIN2=win2, WINc=winc, WIN2c=win2c,
        ROPE=rope, SEL=sel.reshape(65, 65 * 128).astype(bf),
        ALT=((-1.0) ** np.arange(128, dtype=np.float32)).reshape(1, 128).astype(bf)))
    return _CONST


WEIGHT_NAMES = ["w_mod", "b_mod", "ln1_g", "ln1_b", "ln2_g", "ln2_b", "w_in", "w_out", "hy_short_w", "hy_short_b",
                "hy_f_w1", "hy_f_b1", "hy_f_freq", "hy_f_w2", "hy_f_b2", "hy_f_w3", "hy_skip", "q_norm", "k_norm",
                "rg_conv_w", "rg_conv_b", "rg_lambda", "rg_w_a", "rg_b_a", "rg_w_x", "rg_b_x", "w_router", "b_router",
                "w_gate", "w_up", "w_down", "ws_gate", "ws_up", "ws_down"]


def make_in_maps(inputs, L, cores):
    cst = _consts()
    shared = {}
    for n in WEIGHT_NAMES:
        arr = np.asarray(inputs[n], dtype=np.float32)
        for l_ in range(L):
            shared["%s_%d" % (n, l_)] = np.ascontiguousarray(arr[l_])
    shared["bmT"] = np.ascontiguousarray(np.asarray(inputs["b_mod"], dtype=np.float32)[:L].reshape(L, 48, 128).transpose(2, 0, 1))
    shared.update(cst)
    c = np.asarray(inputs["c"], np.float32)
    c_ctx = np.asarray(inputs["c_ctx"], np.float32)
    maps = []
    for b in cores:
        m = dict(shared)
        m["x"] = np.ascontiguousarray(np.asarray(inputs["x"], np.float32)[b])
        m["ctx"] = np.ascontiguousarray(np.asarray(inputs["ctx"], np.float32)[b])
        two = np.stack([c[b], c_ctx], axis=-1)
        m["cc"] = np.ascontiguousarray(two.reshape(8, 128, 2).transpose(1, 0, 2))
        maps.append(m)
    return maps


_NC = {}


def kernel(**inputs):
    if "nc" not in _NC:
        _NC["nc"] = build(L=4)
    nc = _NC["nc"]
    maps = make_in_maps(inputs, 4, list(range(8)))
    res = run_bass_kernel_spmd(nc, maps, core_ids=list(range(8)))
    out = np.stack([np.asarray(r["y"], dtype=np.float32) for r in res.results], axis=0)
    return out
```

```python
from contextlib import ExitStack
import math
import numpy as np
import ml_dtypes
import concourse.bass as bass
import concourse.mybir as mybir
from concourse.bass_utils import run_bass_kernel_spmd

F32 = mybir.dt.float32
BF16 = mybir.dt.bfloat16
AF = mybir.ActivationFunctionType
ALU = mybir.AluOpType
AX = mybir.AxisListType

D = 1024
T = 4096
LC = 256
NTOK = T + LC
NTILE = NTOK // 128
PROJ_W = 2048
N_EXP = 64
DN_ALPHA = (2 * 4) ** 0.25
LN_EPS = 1e-6
QK_EPS = 1e-6
PT_ROWS = 2 + T + 2 + LC + 2
PT_W = 1536


def pt_row(tok):
    return 2 + tok if tok < T else 4 + tok


class Buf:
    __slots__ = ("t", "w", "r")

    def __init__(self, t):
        self.t = t
        self.w = None
        self.r = {}

    def __getitem__(self, idx):
        return self.t[idx]


class Res:
    __slots__ = ("w", "r")

    def __init__(self):
        self.w = None
        self.r = {}


class KB:
    NSLOT = 28

    def __init__(self, nc):
        self.nc = nc
        self.eng = {"pe": nc.tensor, "act": nc.scalar, "dve": nc.vector, "pool": nc.gpsimd, "sp": nc.sync}
        self.sem = {q: nc.alloc_semaphore("sem_" + q) for q in ("pe", "act", "dve", "pool")}
        self.cnt = {q: 0 for q in self.sem}
        self.slots = [nc.alloc_semaphore("dsl%d" % i) for i in range(self.NSLOT)]
        self.slotcnt = [0] * self.NSLOT
        self.nextslot = 0
        self.seen = {q: {} for q in self.eng}

    def _semof(self, key):
        return self.sem[key] if isinstance(key, str) else self.slots[key]

    def _wait(self, q, key, val):
        if q == "pe" and key == "pe":
            return
        if self.seen[q].get(key, 0) >= val:
            return
        self.eng[q].wait_ge(self._semof(key), val)
        self.seen[q][key] = val

    def _deps(self, q, R, W):
        for r in R:
            if r.w is not None:
                self._wait(q, *r.w)
        for w in W:
            if w.w is not None:
                self._wait(q, *w.w)
            for kk, v in w.r.items():
                self._wait(q, kk, v)

    @staticmethod
    def _mark(tok, R, W):
        kk, v = tok
        for r in R:
            if r.r.get(kk, 0) < v:
                r.r[kk] = v
        for w in W:
            w.w = tok
            w.r = {}

    def op(self, q, fn, R=(), W=()):
        self._deps(q, R, W)
        inst = fn()
        self.cnt[q] += 1
        inst.then_inc(self.sem[q], 1)
        self._mark((q, self.cnt[q]), R, W)

    def dma(self, out, in_, R=(), W=(), q="sp", **kw):
        sl = self.nextslot
        self.nextslot = (sl + 1) % self.NSLOT
        if self.slotcnt[sl]:
            self._wait(q, sl, self.slotcnt[sl])
        self._deps(q, R, W)
        self.eng[q].dma_start(out=out, in_=in_, **kw).then_inc(self.slots[sl], 16)
        self.slotcnt[sl] += 16
        self._mark((sl, self.slotcnt[sl]), R, W)

    def barrier(self):
        for q in self.eng:
            for key in self.sem:
                if self.cnt[key]:
                    self._wait(q, key, self.cnt[key])
            for sl in range(self.NSLOT):
                if self.slotcnt[sl]:
                    self._wait(q, sl, self.slotcnt[sl])


def build(L=4, dbg=False, stop_after=None):
    nc = bass.Bass("TRN2", target_bir_lowering=False)
    k = KB(nc)

    class LT:
        def __init__(self, aps):
            self.aps = aps

        def __getitem__(self, idx):
            if isinstance(idx, tuple):
                l_, rest = idx[0], idx[1:]
                return self.aps[l_][rest] if rest else self.aps[l_]
            return self.aps[idx]

    def din(name, shape, dt=F32):
        if name in WEIGHT_NAMES:
            return LT([nc.dram_tensor("%s_%d" % (name, l_), list(shape[1:]), dt, kind="ExternalInput").ap() for l_ in range(shape[0])])
        return nc.dram_tensor(name, list(shape), dt, kind="ExternalInput").ap()

    def dscr(name, shape, dt=F32):
        return nc.dram_tensor(name, list(shape), dt, kind="ExternalOutput" if dbg else "Internal").ap()

    x = din("x", [T, D])
    ctx = din("ctx", [LC, D])
    cc = din("cc", [128, 8, 2])
    w_mod = din("w_mod", [L, D, 6 * D])
    b_mod = din("b_mod", [L, 6 * D])
    bmT = din("bmT", [128, L, 48])
    ln1_g = din("ln1_g", [L, D]); ln1_b = din("ln1_b", [L, D])
    ln2_g = din("ln2_g", [L, D]); ln2_b = din("ln2_b", [L, D])
    w_in = din("w_in", [L, D, PROJ_W])
    w_out = din("w_out", [L, D, D])
    hy_short_w = din("hy_short_w", [L, 3, 768]); hy_short_b = din("hy_short_b", [L, 768])
    hy_f_w1 = din("hy_f_w1", [L, 17, 64]); hy_f_b1 = din("hy_f_b1", [L, 64])
    hy_f_freq = din("hy_f_freq", [L, 64])
    hy_f_w2 = din("hy_f_w2", [L, 64, 64]); hy_f_b2 = din("hy_f_b2", [L, 64])
    hy_f_w3 = din("hy_f_w3", [L, 64, 1024]); hy_skip = din("hy_skip", [L, 2, 256])
    q_norm = din("q_norm", [L, 64]); k_norm = din("k_norm", [L, 64])
    rg_conv_w = din("rg_conv_w", [L, 4, 256]); rg_conv_b = din("rg_conv_b", [L, 256])
    rg_lambda = din("rg_lambda", [L, 2, 256])
    rg_w_a = din("rg_w_a", [L, 2, 4, 64, 64]); rg_b_a = din("rg_b_a", [L, 2, 256])
    rg_w_x = din("rg_w_x", [L, 2, 4, 64, 64]); rg_b_x = din("rg_b_x", [L, 2, 256])
    w_router = din("w_router", [L, D, N_EXP]); b_router = din("b_router", [L, N_EXP])
    w_gate = din("w_gate", [L, N_EXP, D, 256]); w_up = din("w_up", [L, N_EXP, D, 256])
    w_down = din("w_down", [L, N_EXP, 256, D])
    ws_gate = din("ws_gate", [L, D, 256]); ws_up = din("ws_up", [L, D, 256]); ws_down = din("ws_down", [L, 256, D])
    identb_d = din("identb", [128, 128], BF16)
    identf_d = din("identf", [128, 128])
    WT_d = din("WT", [32, 128, 32, 2, 128], BF16)
    WTc_d = din("WTc", [2, 128, 2, 2, 128], BF16)
    SC_d = din("SC", [128, 32]); SCc_d = din("SCc", [128, 2])
    ZT_d = din("ZT", [17, T]); ZTc_d = din("ZTc", [17, LC])
    WIN_d = din("WIN", [T, 256]); WIN2_d = din("WIN2", [T, 256])
    WINc_d = din("WINc", [LC, 256]); WIN2c_d = din("WIN2c", [LC, 256])
    ROPE_d = din("ROPE", [T, 2, 256])
    SEL_d = din("SEL", [65, 65 * 128], BF16)
    ALT_d = din("ALT", [1, 128], BF16)
    W0I_d = din("WTI", [32, 128, 32, 2, 128], BF16)
    W0Ic_d = din("WTIc", [2, 128, 2, 2, 128], BF16)

    y_out = nc.dram_tensor("y", [T, D], F32, kind="ExternalOutput").ap()

    HD = dscr("HD", [NTOK, D])
    H1D = dscr("H1D", [NTOK, D])
    PT = dscr("PT", [PT_ROWS, PT_W])
    PR = dscr("PR", [512, NTOK])
    RGT = dscr("RGT", [256, NTOK], BF16)
    MIXTOK = dscr("MIXTOK", [NTOK, 768], BF16)
    HX = dscr("HX", [NTOK, 768])
    V1D = dscr("V1D", [NTOK, 256])
    KFD = dscr("KFD", [32, 128, 2, 512])
    KFDc = dscr("KFDc", [2, 128, 2, 512])
    GMOD = dscr("GMOD", [L, 2, 2, D])

    gs = ExitStack()
    _uid = [0]

    def un(name):
        _uid[0] += 1
        return "%s_%d" % (name, _uid[0])

    def gsb(name, shape, dt=F32):
        return Buf(gs.enter_context(nc.sbuf_tensor(un(name), list(shape), dt)))

    MOD = gsb("MOD", [128, L, 48, 2])
    identb = gsb("identb_s", [128, 128], BF16)
    identf = gsb("identf_s", [128, 128])
    k.dma(identb[:], identb_d[:, :], W=[identb])
    k.dma(identf[:], identf_d[:, :], W=[identf])

    def hsrc(l, i):
        if l == 0:
            return x[i * 128:(i + 1) * 128, :] if i < 32 else ctx[(i - 32) * 128:(i - 31) * 128, :]
        return HD[i * 128:(i + 1) * 128, :]

    def colvec(v1d, n0, n):
        return v1d[n0:n0 + n].rearrange("(p o) -> p o", o=1)

    def phase_mod():
        with ExitStack() as es:
            def sb(name, shape, dt=F32):
                return Buf(es.enter_context(nc.sbuf_tensor(un(name), list(shape), dt)))

            def ps(name, shape, dt=F32):
                return Buf(es.enter_context(nc.psum_tensor(un(name), list(shape), dt)))
            ccs = sb("ccs", [128, 8, 2]); sT = sb("sT", [128, 8, 2]); sg = sb("sg", [128, 8, 2])
            bmTs = sb("bmTs", [128, L, 48])
            wbuf = [sb("wm%d" % i, [128, 8, 1024]) for i in range(2)]
            brow = sb("brow", [2, 1024]); rows = sb("rows", [2, 1024])
            psf = [ps("psf%d" % i, [128, 8, 2]) for i in range(2)]
            psr = [ps("psr%d" % i, [2, 512]) for i in range(2)]
            k.dma(ccs[:], cc[:, :, :], W=[ccs])
            k.dma(bmTs[:], bmT[:, :, :], W=[bmTs])
            k.op("act", lambda: nc.scalar.activation(out=sg[:], in_=ccs[:], func=AF.Sigmoid), R=[ccs], W=[sg])
            k.op("dve", lambda: nc.vector.tensor_tensor(out=sT[:], in0=ccs[:], in1=sg[:], op=ALU.mult), R=[ccs, sg], W=[sT])
            it = 0
            for l in range(L):
                for blk in range(6):
                    wb = wbuf[it % 2]; pf = psf[it % 2]; it += 1
                    c0 = blk * 1024
                    k.dma(wb[:], w_mod[l, :, c0:c0 + 1024].rearrange("(k p) n -> p k n", p=128), W=[wb])
                    for ch in range(8):
                        for kk in range(8):
                            k.op("pe", lambda: nc.tensor.matmul(pf[:, ch, :], wb[:, kk, ch * 128:(ch + 1) * 128], sT[:, kk, :],
                                                                start=(kk == 0), stop=(kk == 7)), R=[wb, sT], W=[pf])
                    for j in range(2):
                        k.op("dve", lambda: nc.vector.tensor_tensor(out=MOD[:, l, blk * 8:(blk + 1) * 8, j], in0=pf[:, :, j],
                                                                    in1=bmTs[:, l, blk * 8:(blk + 1) * 8], op=ALU.add),
                             R=[pf, bmTs], W=[MOD])
                    if blk in (1, 4):
                        k.op("dve", lambda: nc.vector.tensor_scalar(out=MOD[:, l, blk * 8:(blk + 1) * 8, :], in0=MOD[:, l, blk * 8:(blk + 1) * 8, :],
                                                                    scalar1=1.0, scalar2=None, op0=ALU.add), R=[MOD], W=[MOD])
                    if blk in (2, 5):
                        k.dma(brow[:], b_mod[l, c0:c0 + 1024].partition_broadcast(2), W=[brow])
                        for nb in range(2):
                            for kk in range(8):
                                k.op("pe", lambda: nc.tensor.matmul(psr[nb][:, :], sT[:, kk, :], wb[:, kk, nb * 512:(nb + 1) * 512],
                                                                    start=(kk == 0), stop=(kk == 7)), R=[wb, sT], W=[psr[nb]])
                            k.op("dve", lambda: nc.vector.tensor_tensor(out=rows[:, nb * 512:(nb + 1) * 512], in0=psr[nb][:, :],
                                                                        in1=brow[:, nb * 512:(nb + 1) * 512], op=ALU.add),
                                 R=[psr[nb], brow], W=[rows])
                        k.dma(GMOD[l, 0 if blk == 2 else 1, :, :], rows[:], R=[rows])
            k.barrier()

    def phase_inproj(l):
        with ExitStack() as es:
            def sb(name, shape, dt=F32):
                return Buf(es.enter_context(nc.sbuf_tensor(un(name), list(shape), dt)))

            def ps(name, shape, dt=F32):
                return Buf(es.enter_context(nc.psum_tensor(un(name), list(shape), dt)))
            Wst = [sb("wst%d" % i, [128, 8, 512]) for i in range(2)]
            Wb = sb("Wb", [128, 8, PROJ_W], BF16)
            zt = sb("zt", [2, PT_W])
            hf = [sb("hf%d" % i, [128, D]) for i in range(3)]
            hb = [sb("hb%d" % i, [128, D], BF16) for i in range(2)]
            aT = [sb("aT%d" % i, [128, 8, 512], BF16) for i in range(2)]
            ot = [sb("ot%d" % i, [128, PT_W]) for i in range(2)]
            of = [sb("of%d" % i, [128, 512]) for i in range(2)]
            tp = [ps("tp%d" % i, [128, 8, 128], BF16) for i in range(2)]
            po = [ps("po%d" % i, [128, 512]) for i in range(3)]
            pf = [ps("pf%d" % i, [128, 512]) for i in range(2)]
            for cb in range(4):
                st = Wst[cb % 2]
                k.dma(st[:], w_in[l, :, cb * 512:(cb + 1) * 512].rearrange("(k p) n -> p k n", p=128), W=[st])
                k.op("act", lambda: nc.scalar.copy(out=Wb[:, :, cb * 512:(cb + 1) * 512], in_=st[:]), R=[st], W=[Wb])
            k.op("dve", lambda: nc.vector.memset(zt[:], 0.0), W=[zt])
            for r0 in (0, 2 + T, PT_ROWS - 2):
                k.dma(PT[r0:r0 + 2, :], zt[:], R=[zt])
            ti = 0
            for g in range(9):
                tiles = list(range(4 * g, min(4 * g + 4, NTILE)))
                nt = len(tiles)
                j = 0 if g < 8 else 1
                A = aT[g % 2]
                for s, i in enumerate(tiles):
                    h_f = hf[ti % 3]; h_b = hb[ti % 2]; tpp = tp[ti % 2]; ti += 1
                    k.dma(h_f[:], hsrc(l, i), W=[h_f])
                    k.op("dve", lambda: nc.vector.tensor_copy(out=h_b[:], in_=h_f[:]), R=[h_f], W=[h_b])
                    for kk in range(8):
                        k.op("pe", lambda: nc.tensor.transpose(tpp[:, kk, :], h_b[:, kk * 128:(kk + 1) * 128], identb[:]),
                             R=[h_b, identb], W=[tpp])
                    for kk in range(8):
                        sc = MOD[:, l, 8 + kk, j:j + 1]; sh = MOD[:, l, kk, j:j + 1]
                        if (ti % 2) == 0:
                            k.op("act", lambda: nc.scalar.activation(out=A[:, kk, s * 128:(s + 1) * 128], in_=tpp[:, kk, :],
                                                                     func=AF.Identity, bias=sh, scale=sc), R=[tpp, MOD], W=[A])
                        else:
                            k.op("dve", lambda: nc.vector.tensor_scalar(out=A[:, kk, s * 128:(s + 1) * 128], in0=tpp[:, kk, :],
                                                                        scalar1=sc, scalar2=sh, op0=ALU.mult, op1=ALU.add),
                                 R=[tpp, MOD], W=[A])
                for s, i in enumerate(tiles):
                    o_t = ot[i % 2]
                    for nb in range(3):
                        p = po[nb]
                        for kk in range(8):
                            k.op("pe", lambda: nc.tensor.matmul(p[:, :], A[:, kk, s * 128:(s + 1) * 128], Wb[:, kk, nb * 512:(nb + 1) * 512],
                                                                start=(kk == 0), stop=(kk == 7)), R=[A, Wb], W=[p])
                        if nb == 1:
                            k.op("dve", lambda: nc.vector.tensor_copy(out=o_t[:, nb * 512:(nb + 1) * 512], in_=p[:, :]), R=[p], W=[o_t])
                        else:
                            k.op("act", lambda: nc.scalar.copy(out=o_t[:, nb * 512:(nb + 1) * 512], in_=p[:, :]), R=[p], W=[o_t])
                    r0 = pt_row(i * 128)
                    k.dma(PT[r0:r0 + 128, :], o_t[:], R=[o_t])
                n = nt * 128
                for c4 in range(4):
                    p = pf[c4 % 2]; o_f = of[c4 % 2]
                    for kk in range(8):
                        k.op("pe", lambda: nc.tensor.matmul(p[:, 0:n], Wb[:, kk, 1536 + c4 * 128:1536 + (c4 + 1) * 128], A[:, kk, 0:n],
                                                            start=(kk == 0), stop=(kk == 7)), R=[A, Wb], W=[p])
                    k.op("act" if c4 % 2 else "dve",
                         (lambda: nc.scalar.copy(out=o_f[:, 0:n], in_=p[:, 0:n])) if c4 % 2 else
                         (lambda: nc.vector.tensor_copy(out=o_f[:, 0:n], in_=p[:, 0:n])), R=[p], W=[o_f])
                    k.dma(PR[c4 * 128:(c4 + 1) * 128, g * 512:g * 512 + n], o_f[:, 0:n], R=[o_f])
            k.barrier()

    def phase_rg(l):
        NB = PT_ROWS
        LAT0, CTX0 = 2, 4 + T
        with ExitStack() as es:
            def sb(name, shape, dt=F32):
                return Buf(es.enter_context(nc.sbuf_tensor(un(name), list(shape), dt)))

            def ps(name, shape, dt=F32):
                return Buf(es.enter_context(nc.psum_tensor(un(name), list(shape), dt)))
            xp = sb("xp", [128, NB]); gp = sb("gp", [128, NTOK]); u = sb("u", [128, NB])
            Aa = sb("Aa", [128, NB]); Bv = sb("Bv", [128, NB]); HS = sb("HS", [128, NTOK]); tmp = sb("tmpS", [128, NTOK])
            ob = sb("ob", [128, NTOK], BF16)
            cw = sb("cw", [128, 4]); cbv = sb("cbv", [128, 1])
            WA = sb("WA", [128, 128]); WX = sb("WX", [128, 128])
            ba = sb("ba", [128, 1]); bx = sb("bx", [128, 1]); lam = sb("lam", [128, 1]); m8 = sb("m8", [128, 1])
            rT = [sb("rT%d" % i, [128, 512]) for i in range(2)]
            iT = [sb("iT%d" % i, [128, 512]) for i in range(2)]
            t1 = [sb("t1%d" % i, [128, 512]) for i in range(2)]
            pr = [ps("pr%d" % i, [128, 512]) for i in range(2)]
            pi = [ps("pi%d" % i, [128, 512]) for i in range(2)]
            segs = [(LAT0 + b * 512, 512) for b in range(8)] + [(CTX0, 256)]
            for c in range(2):
                ch0 = c * 128
                k.op("dve", lambda: nc.vector.memset(xp[:], 0.0), W=[xp])
                k.dma(xp[:, LAT0:LAT0 + T], PR[ch0:ch0 + 128, 0:T], W=[xp])
                k.dma(xp[:, CTX0:CTX0 + LC], PR[ch0:ch0 + 128, T:NTOK], W=[xp])
                k.dma(gp[:], PR[256 + ch0:256 + ch0 + 128, :], W=[gp])
                k.dma(cw[:], rg_conv_w[l, :, ch0:ch0 + 128].rearrange("j p -> p j"), W=[cw], allow_slow_non_contiguous=True)
                k.dma(cbv[:], colvec(rg_conv_b[l], ch0, 128), W=[cbv], allow_slow_non_contiguous=True)
                n = NB - 4
                k.op("dve", lambda: nc.vector.tensor_scalar(out=u[:, 2:2 + n], in0=xp[:, 1:1 + n], scalar1=cw[:, 0:1], scalar2=cbv[:, 0:1],
                                                            op0=ALU.mult, op1=ALU.add), R=[xp, cw, cbv], W=[u])
                for jj in range(1, 4):
                    k.op("dve", lambda: nc.vector.scalar_tensor_tensor(out=u[:, 2:2 + n], in0=xp[:, 1 + jj:1 + jj + n], scalar=cw[:, jj:jj + 1],
                                                                       in1=u[:, 2:2 + n], op0=ALU.mult, op1=ALU.add), R=[xp, cw, u], W=[u])
                for d in range(2):
                    k.op("dve", lambda: nc.vector.memset(WA[:], 0.0), W=[WA])
                    k.op("dve", lambda: nc.vector.memset(WX[:], 0.0), W=[WX])
                    for hh in range(2):
                        k.dma(WA[hh * 64:(hh + 1) * 64, hh * 64:(hh + 1) * 64], rg_w_a[l, d, 2 * c + hh, :, :], W=[WA])
                        k.dma(WX[hh * 64:(hh + 1) * 64, hh * 64:(hh + 1) * 64], rg_w_x[l, d, 2 * c + hh, :, :], W=[WX])
                    k.dma(ba[:], colvec(rg_b_a[l, d], ch0, 128), W=[ba], allow_slow_non_contiguous=True)
                    k.dma(bx[:], colvec(rg_b_x[l, d], ch0, 128), W=[bx], allow_slow_non_contiguous=True)
                    k.dma(lam[:], colvec(rg_lambda[l, d], ch0, 128), W=[lam], allow_slow_non_contiguous=True)
                    k.op("act", lambda: nc.scalar.activation(out=m8[:], in_=lam[:], func=AF.Exp, scale=-1.0), R=[lam], W=[m8])
                    k.op("act", lambda: nc.scalar.activation(out=m8[:], in_=m8[:], func=AF.Ln, bias=1.0), R=[m8], W=[m8])
                    k.op("dve", lambda: nc.vector.tensor_scalar(out=m8[:], in0=m8[:], scalar1=-8.0, scalar2=None, op0=ALU.mult), R=[m8], W=[m8])
                    for bi, (p0, nn) in enumerate(segs):
                        p_r = pr[bi % 2]; p_i = pi[bi % 2]; r_ = rT[bi % 2]; i_ = iT[bi % 2]; t_ = t1[bi % 2]
                        k.op("pe", lambda: nc.tensor.matmul(p_r[:, 0:nn], WA[:], u[:, p0:p0 + nn], start=True, stop=True), R=[WA, u], W=[p_r])
                        k.op("pe", lambda: nc.tensor.matmul(p_i[:, 0:nn], WX[:], u[:, p0:p0 + nn], start=True, stop=True), R=[WX, u], W=[p_i])
                        k.op("act", lambda: nc.scalar.activation(out=r_[:, 0:nn], in_=p_r[:, 0:nn], func=AF.Sigmoid, bias=ba[:, 0:1]), R=[p_r, ba], W=[r_])
                        k.op("act", lambda: nc.scalar.activation(out=Aa[:, p0:p0 + nn], in_=r_[:, 0:nn], func=AF.Exp, scale=m8[:, 0:1]), R=[r_, m8], W=[Aa])
                        k.op("act", lambda: nc.scalar.activation(out=i_[:, 0:nn], in_=p_i[:, 0:nn], func=AF.Sigmoid, bias=bx[:, 0:1]), R=[p_i, bx], W=[i_])
                        k.op("dve", lambda: nc.vector.tensor_tensor(out=t_[:, 0:nn], in0=Aa[:, p0:p0 + nn], in1=Aa[:, p0:p0 + nn], op=ALU.mult), R=[Aa], W=[t_])
                        k.op("dve", lambda: nc.vector.tensor_scalar(out=t_[:, 0:nn], in0=t_[:, 0:nn], scalar1=-1.0, scalar2=1.0, op0=ALU.mult, op1=ALU.add), R=[t_], W=[t_])
                        k.op("act", lambda: nc.scalar.activation(out=t_[:, 0:nn], in_=t_[:, 0:nn], func=AF.Sqrt), R=[t_], W=[t_])
                        k.op("dve", lambda: nc.vector.tensor_tensor(out=i_[:, 0:nn], in0=i_[:, 0:nn], in1=u[:, p0:p0 + nn], op=ALU.mult), R=[i_, u], W=[i_])
                        k.op("dve", lambda: nc.vector.tensor_tensor(out=Bv[:, p0:p0 + nn], in0=t_[:, 0:nn], in1=i_[:, 0:nn], op=ALU.mult), R=[t_, i_], W=[Bv])
                    dst = HS if d == 0 else tmp
                    if d == 0:
                        k.op("dve", lambda: nc.vector.tensor_tensor_scan(out=dst[:, T:NTOK], data0=Aa[:, CTX0:CTX0 + LC], data1=Bv[:, CTX0:CTX0 + LC],
                                                                         initial=0.0, op0=ALU.mult, op1=ALU.add), R=[Aa, Bv], W=[dst])
                        k.op("dve", lambda: nc.vector.tensor_tensor_scan(out=dst[:, 0:T], data0=Aa[:, LAT0:LAT0 + T], data1=Bv[:, LAT0:LAT0 + T],
                                                                         initial=dst[:, NTOK - 1:NTOK], op0=ALU.mult, op1=ALU.add), R=[Aa, Bv, dst], W=[dst])
                    else:
                        k.op("dve", lambda: nc.vector.tensor_tensor_scan(out=dst[:, T:NTOK][:, ::-1], data0=Aa[:, CTX0:CTX0 + LC][:, ::-1],
                                                                         data1=Bv[:, CTX0:CTX0 + LC][:, ::-1],
                                                                         initial=0.0, op0=ALU.mult, op1=ALU.add), R=[Aa, Bv], W=[dst])
                        k.op("dve", lambda: nc.vector.tensor_tensor_scan(out=dst[:, 0:T][:, ::-1], data0=Aa[:, LAT0:LAT0 + T][:, ::-1],
                                                                         data1=Bv[:, LAT0:LAT0 + T][:, ::-1],
                                                                         initial=dst[:, T:T + 1], op0=ALU.mult, op1=ALU.add), R=[Aa, Bv, dst], W=[dst])
                        k.op("dve", lambda: nc.vector.tensor_tensor(out=HS[:], in0=HS[:], in1=tmp[:], op=ALU.add), R=[HS, tmp], W=[HS])
                k.op("dve", lambda: nc.vector.tensor_tensor(out=tmp[:], in0=gp[:], in1=gp[:], op=ALU.mult), R=[gp], W=[tmp])
                k.op("dve", lambda: nc.vector.tensor_scalar(out=tmp[:], in0=tmp[:], scalar1=0.044715, scalar2=1.0, op0=ALU.mult, op1=ALU.add), R=[tmp], W=[tmp])
                k.op("dve", lambda: nc.vector.tensor_tensor(out=tmp[:], in0=tmp[:], in1=gp[:], op=ALU.mult), R=[tmp, gp], W=[tmp])
                k.op("act", lambda: nc.scalar.activation(out=tmp[:], in_=tmp[:], func=AF.Sigmoid, scale=2.0 * math.sqrt(2.0 / math.pi)), R=[tmp], W=[tmp])
                k.op("dve", lambda: nc.vector.tensor_tensor(out=tmp[:], in0=tmp[:], in1=gp[:], op=ALU.mult), R=[tmp, gp], W=[tmp])
                k.op("dve", lambda: nc.vector.tensor_tensor(out=ob[:], in0=tmp[:], in1=HS[:], op=ALU.mult), R=[tmp, HS], W=[ob])
                k.dma(RGT[ch0:ch0 + 128, :], ob[:], R=[ob])
            k.barrier()

    def phase_attn(l):
        with ExitStack() as es:
            def sb(name, shape, dt=F32):
                return Buf(es.enter_context(nc.sbuf_tensor(un(name), list(shape), dt)))

            def ps(name, shape, dt=F32):
                return Buf(es.enter_context(nc.psum_tensor(un(name), list(shape), dt)))
            QT = sb("QT", [64, 8, NTOK], BF16)
            KT = sb("KT", [64, 2, NTOK], BF16)
            VA = sb("VA", [128, NTILE, 2, 65], BF16)
            qg = sb("qg", [128, 64]); kg = sb("kg", [128, 64])
            qkv = [sb("qkv%d" % i, [128, 768]) for i in range(2)]
            rope = [sb("rope%d" % i, [128, 2, 256]) for i in range(2)]
            sq = sb("sq", [128, 640]); ss = sb("ss", [128, 10]); qn = sb("qn", [128, 640])
            ra = sb("ra", [128, 320]); rb = sb("rb", [128, 320])
            qr = [sb("qr%d" % i, [128, 640], BF16) for i in range(2)]
            esp = ExitStack()
            ptq = [Buf(esp.enter_context(nc.psum_tensor(un("ptq%d" % i), [64, 8, 128], BF16))) for i in range(2)]
            ptk = Buf(esp.enter_context(nc.psum_tensor(un("ptk"), [64, 2, 128], BF16)))
            k.dma(qg[:], q_norm[l].partition_broadcast(128), W=[qg])
            k.dma(kg[:], k_norm[l].partition_broadcast(128), W=[kg])
            k.op("dve", lambda: nc.vector.memset(VA[:], 1.0), W=[VA])
            for i in range(NTILE):
                t_ = qkv[i % 2]; rp = rope[i % 2]; q_r = qr[i % 2]; pq = ptq[i % 2]
                r0 = pt_row(i * 128)
                k.dma(t_[:], PT[r0:r0 + 128, 768:1536], W=[t_])
                if i < 32:
                    k.dma(rp[:], ROPE_d[i * 128:(i + 1) * 128, :, :], W=[rp])
                k.op("dve", lambda: nc.vector.tensor_tensor(out=sq[:], in0=t_[:, 0:640], in1=t_[:, 0:640], op=ALU.mult), R=[t_], W=[sq])
                k.op("dve", lambda: nc.vector.tensor_reduce(out=ss[:], in_=sq[:].rearrange("p (h d) -> p h d", d=64), axis=AX.X, op=ALU.add), R=[sq], W=[ss])
                k.op("dve", lambda: nc.vector.tensor_scalar(out=ss[:], in0=ss[:], scalar1=1.0 / 64.0, scalar2=QK_EPS, op0=ALU.mult, op1=ALU.add), R=[ss], W=[ss])
                k.op("act", lambda: nc.scalar.activation(out=ss[:], in_=ss[:], func=AF.Sqrt), R=[ss], W=[ss])
                k.op("dve", lambda: nc.vector.reciprocal(out=ss[:], in_=ss[:]), R=[ss], W=[ss])
                for hh in range(10):
                    gsrc = qg if hh < 8 else kg
                    dstt = qn if i < 32 else q_r
                    k.op("dve", lambda: nc.vector.scalar_tensor_tensor(out=dstt[:, hh * 64:(hh + 1) * 64], in0=t_[:, hh * 64:(hh + 1) * 64],
                                                                       scalar=ss[:, hh:hh + 1], in1=gsrc[:], op0=ALU.mult, op1=ALU.mult),
                         R=[t_, ss, gsrc], W=[dstt])
                if i < 32:
                    qv = qn[:].rearrange("p (n two) -> p n two", two=2)
                    ov = q_r[:].rearrange("p (n two) -> p n two", two=2)
                    u1 = qv[:, :, 0]; u2 = qv[:, :, 1]
                    cosv = None
                    for (a0, a1, c0_, c1_) in ((0, 256, 0, 256), (256, 320, 0, 64)):
                        cs = rp[:, 0, c0_:c1_]; sn = rp[:, 1, c0_:c1_]
                        k.op("dve", lambda: nc.vector.tensor_tensor(out=ra[:, a0:a1], in0=u1[:, a0:a1], in1=cs, op=ALU.mult), R=[qn, rp], W=[ra])
                        k.op("dve", lambda: nc.vector.tensor_tensor(out=rb[:, a0:a1], in0=u2[:, a0:a1], in1=sn, op=ALU.mult), R=[qn, rp], W=[rb])
                        k.op("dve", lambda: nc.vector.tensor_tensor(out=ov[:, a0:a1, 0], in0=ra[:, a0:a1], in1=rb[:, a0:a1], op=ALU.subtract), R=[ra, rb], W=[q_r])
                        k.op("dve", lambda: nc.vector.tensor_tensor(out=ra[:, a0:a1], in0=u1[:, a0:a1], in1=sn, op=ALU.mult), R=[qn, rp], W=[ra])
                        k.op("dve", lambda: nc.vector.tensor_tensor(out=rb[:, a0:a1], in0=u2[:, a0:a1], in1=cs, op=ALU.mult), R=[qn, rp], W=[rb])
                        k.op("dve", lambda: nc.vector.tensor_tensor(out=ov[:, a0:a1, 1], in0=ra[:, a0:a1], in1=rb[:, a0:a1], op=ALU.add), R=[ra, rb], W=[q_r])
                for hh in range(8):
                    k.op("pe", lambda: nc.tensor.transpose(pq[:, hh, :], q_r[:, hh * 64:(hh + 1) * 64], identb[:]), R=[q_r, identb], W=[pq])
                for hh in range(2):
                    k.op("pe", lambda: nc.tensor.transpose(ptk[:, hh, :], q_r[:, (8 + hh) * 64:(9 + hh) * 64], identb[:]), R=[q_r, identb], W=[ptk])
                k.op("act", lambda: nc.scalar.copy(out=QT[:, :, i * 128:(i + 1) * 128], in_=pq[:, :, :]), R=[pq], W=[QT])
                k.op("act", lambda: nc.scalar.copy(out=KT[:, :, i * 128:(i + 1) * 128], in_=ptk[:, :, :]), R=[ptk], W=[KT])
                k.op("dve", lambda: nc.vector.tensor_copy(out=VA[:, i, :, 0:64], in_=t_[:, 640:768].rearrange("p (h d) -> p h d", d=64)), R=[t_], W=[VA])
            k.barrier()
            esp.close()
            pS = [ps("pS%d" % i, [128, 512]) for i in range(2)]
            pO = [ps("pO%d" % i, [128, 512]) for i in range(4)]
            Pb = [sb("Pb%d" % i, [128, 512], BF16) for i in range(3)]
            rd = sb("rd", [128, 4]);
            att = [sb("att%d" % i, [128, 4, 512], BF16) for i in range(2)]
            it = 0; io = 0
            jobs = [(qs * 512, 512, list(range(NTILE))) for qs in range(8)] + [(T, 256, [32, 33])]
            for ji, (q0, nq, kchunks) in enumerate(jobs):
                at = att[ji % 2]
                nsub = nq // 128
                for h in range(8):
                    kvh = h // 4
                    io += 1
                    def emit_pv(item):
                        P_, c_, ci_ = item
                        for s in range(nsub):
                            k.op("pe", lambda: nc.tensor.matmul(pO[s][:, 0:65], P_[:, s * 128:(s + 1) * 128], VA[:, c_, kvh, :],
                                                                start=(ci_ == 0), stop=(ci_ == len(kchunks) - 1)), R=[P_, VA], W=[pO[s]])
                    prev = None
                    for ci, c in enumerate(kchunks):
                        S = pS[it % 2]; P = Pb[it % 3]; it += 1
                        k.op("pe", lambda: nc.tensor.matmul(S[:, 0:nq], KT[:, kvh, c * 128:(c + 1) * 128], QT[:, h, q0:q0 + nq], start=True, stop=True),
                             R=[KT, QT], W=[S])
                        k.op("act", lambda: nc.scalar.activation(out=P[:, 0:nq], in_=S[:, 0:nq], func=AF.Exp, scale=0.125), R=[S], W=[P])
                        if prev is not None:
                            emit_pv(prev)
                        prev = (P, c, ci)
                    emit_pv(prev)
                    for s in range(nsub):
                        k.op("dve", lambda: nc.vector.reciprocal(out=rd[:, s:s + 1], in_=pO[s][:, 64:65]), R=[pO[s]], W=[rd])
                        k.op("dve", lambda: nc.vector.tensor_scalar(out=at[:, s, h * 64:(h + 1) * 64], in0=pO[s][:, 0:64], scalar1=rd[:, s:s + 1],
                                                                    scalar2=None, op0=ALU.mult), R=[pO[s], rd], W=[at])
                for s in range(nsub):
                    k.dma(MIXTOK[q0 + s * 128:q0 + (s + 1) * 128, 256:768], at[:, s, :], R=[at])
            k.barrier()

    def hyena_seq(l, es, sb, ps, Lq, tok0, WTd, W0Id, SCd, ZTd, WINd, WIN2d, KF, tag):
        nch = Lq // 128
        slab = [sb("slab%d" % i + tag, [128, nch, 2, 128], BF16) for i in range(3)]
        scs = sb("scs" + tag, [128, nch])
        k.dma(scs[:], SCd[:, :], W=[scs])
        pX = [ps("pX%d" % i + tag, [128, 512]) for i in range(4)]
        pN = ps("pN" + tag, [1, 512])
        kfo = [sb("kfo%d" % i + tag, [128, 2, 512]) for i in range(2)]
        altr = sb("altr" + tag, [1, 128], BF16)
        k.dma(altr[:], ALT_d[:, :], W=[altr])
        si = [0]

        def load_slab(j, inv=False):
            s_ = slab[si[0] % 3]; si[0] += 1
            k.dma(s_[:], (W0Id if inv else WTd)[j, :, :, :, :], W=[s_])
            return s_
        with ExitStack() as es2:
            def sb2(name, shape, dt=F32):
                return Buf(es2.enter_context(nc.sbuf_tensor(un(name + tag), list(shape), dt)))
            KS = sb2("KS", [128, nch, 512], BF16); KDf = sb2("KD", [128, nch, 512], BF16)
            h2T = sb2("h2T", [64, Lq])
            zT = sb2("zT", [17, Lq]); w1 = sb2("w1", [17, 64]); w2 = sb2("w2", [64, 64]); w3 = sb2("w3", [64, 1024])
            fr = sb2("fr", [64, 1]); b1 = sb2("b1", [64, 1]); b2 = sb2("b2", [64, 1]); fb1 = sb2("fb1", [64, 1]); fb2 = sb2("fb2", [64, 1])
            h1T = sb2("h1T", [64, Lq]); targ = [sb2("targ%d" % i, [64, 512]) for i in range(2)]
            win = [sb2("win%d" % i, [128, 256]) for i in range(2)]; win2 = [sb2("win2%d" % i, [128, 256]) for i in range(2)]
            fa = [sb2("fa%d" % i, [128, 512]) for i in range(2)]; fb_ = [sb2("fb_%d" % i, [128, 512]) for i in range(2)]
            pm = pX
            k.dma(zT[:], ZTd[:, :], W=[zT]); k.dma(w1[:], hy_f_w1[l, :, :], W=[w1]); k.dma(w2[:], hy_f_w2[l, :, :], W=[w2])
            k.dma(w3[:], hy_f_w3[l, :, :], W=[w3])
            k.dma(fr[:], colvec(hy_f_freq[l], 0, 64), W=[fr], allow_slow_non_contiguous=True)
            k.dma(b1[:], colvec(hy_f_b1[l], 0, 64), W=[b1], allow_slow_non_contiguous=True)
            k.dma(b2[:], colvec(hy_f_b2[l], 0, 64), W=[b2], allow_slow_non_contiguous=True)
            OFF = 0.0
            kiq = sb2("kiq", [64, 512], mybir.dt.int32); kfq = sb2("kfq", [64, 512])
            for (bsrc, fdst) in ((b1, fb1), (b2, fb2)):
                k.op("dve", lambda: nc.vector.tensor_tensor(out=fdst[:], in0=bsrc[:], in1=fr[:], op=ALU.mult), R=[bsrc, fr], W=[fdst])
                k.op("dve", lambda: nc.vector.tensor_scalar(out=fdst[:], in0=fdst[:], scalar1=OFF, scalar2=None, op0=ALU.add), R=[fdst], W=[fdst])
            nblk = max(1, Lq // 512); bw = min(512, Lq)

            def sin_layer(wm, src, dstb, fbb, kdim):
                for b in range(nblk):
                    p = pm[b % 2]; ta = targ[b % 2]
                    k.op("pe", lambda: nc.tensor.matmul(p[0:64, 0:bw], wm[:], src[0:kdim, b * bw:(b + 1) * bw], start=True, stop=True), R=[wm, src], W=[p])
                    k.op("dve", lambda: nc.vector.tensor_scalar(out=ta[:, 0:bw], in0=p[0:64, 0:bw], scalar1=fr[:, 0:1], scalar2=fbb[:, 0:1],
                                                                op0=ALU.mult, op1=ALU.add), R=[p, fr, fbb], W=[ta])
                    k.op("dve", lambda: nc.vector.tensor_scalar(out=kiq[:, 0:bw], in0=ta[:, 0:bw], scalar1=1.0 / (2.0 * math.pi), scalar2=None, op0=ALU.mult), R=[ta], W=[kiq])
                    k.op("dve", lambda: nc.vector.tensor_copy(out=kfq[:, 0:bw], in_=kiq[:, 0:bw]), R=[kiq], W=[kfq])
                    k.op("dve", lambda: nc.vector.scalar_tensor_tensor(out=ta[:, 0:bw], in0=kfq[:, 0:bw], scalar=-2.0 * math.pi, in1=ta[:, 0:bw], op0=ALU.mult, op1=ALU.add), R=[kfq, ta], W=[ta])
                    k.op("dve", lambda: nc.vector.tensor_scalar(out=kfq[:, 0:bw], in0=ta[:, 0:bw], scalar1=-math.pi, scalar2=None, op0=ALU.is_lt), R=[ta], W=[kfq])
                    k.op("dve", lambda: nc.vector.scalar_tensor_tensor(out=ta[:, 0:bw], in0=kfq[:, 0:bw], scalar=2.0 * math.pi, in1=ta[:, 0:bw], op0=ALU.mult, op1=ALU.add), R=[kfq, ta], W=[ta])
                    k.op("dve", lambda: nc.vector.tensor_scalar(out=kfq[:, 0:bw], in0=ta[:, 0:bw], scalar1=math.pi, scalar2=None, op0=ALU.is_gt), R=[ta], W=[kfq])
                    k.op("dve", lambda: nc.vector.scalar_tensor_tensor(out=ta[:, 0:bw], in0=kfq[:, 0:bw], scalar=-2.0 * math.pi, in1=ta[:, 0:bw], op0=ALU.mult, op1=ALU.add), R=[kfq, ta], W=[ta])
                    k.op("dve", lambda: nc.vector.tensor_scalar(out=ta[:, 0:bw], in0=ta[:, 0:bw], scalar1=-3.1415925, scalar2=3.1415925,
                                                                op0=ALU.max, op1=ALU.min), R=[ta], W=[ta])
                    k.op("act", lambda: nc.scalar.activation(out=dstb[:, b * bw:(b + 1) * bw], in_=ta[:, 0:bw], func=AF.Sin), R=[ta], W=[dstb])
            sin_layer(w1, zT, h1T, fb1, 17)
            sin_layer(w2, h1T, h2T, fb2, 64)
            for tc in range(nch):
                wn = win[tc % 2]; wn2 = win2[tc % 2]; A_ = fa[tc % 2]; B_ = fb_[tc % 2]
                k.dma(wn[:], WINd[tc * 128:(tc + 1) * 128, :], W=[wn]); k.dma(wn2[:], WIN2d[tc * 128:(tc + 1) * 128, :], W=[wn2])
                p0 = pm[2]; p1 = pm[3]
                k.op("pe", lambda: nc.tensor.matmul(p0[:, :], h2T[:, tc * 128:(tc + 1) * 128], w3[:, 0:512], start=True, stop=True), R=[h2T, w3], W=[p0])
                k.op("pe", lambda: nc.tensor.matmul(p1[:, :], h2T[:, tc * 128:(tc + 1) * 128], w3[:, 512:1024], start=True, stop=True), R=[h2T, w3], W=[p1])
                for o in range(2):
                    k.op("dve", lambda: nc.vector.tensor_tensor(out=A_[:, o * 256:(o + 1) * 256], in0=p0[:, o * 256:(o + 1) * 256], in1=wn[:], op=ALU.mult), R=[p0, wn], W=[A_])
                    k.op("dve", lambda: nc.vector.tensor_tensor(out=B_[:, o * 256:(o + 1) * 256], in0=p1[:, o * 256:(o + 1) * 256], in1=wn2[:], op=ALU.mult), R=[p1, wn2], W=[B_])
                k.op("pool", lambda: nc.gpsimd.tensor_tensor(out=KS[:, tc, :], in0=A_[:], in1=B_[:], op=ALU.add), R=[A_, B_], W=[KS])
                k.op("pool", lambda: nc.gpsimd.tensor_tensor(out=KDf[:, tc, :], in0=A_[:], in1=B_[:], op=ALU.subtract), R=[A_, B_], W=[KDf])
            nxt = load_slab(0)
            for j in range(nch):
                s_ = nxt
                if j + 1 < nch:
                    nxt = load_slab(j + 1)
                pre = pX[(2 * j) % 4]; pim = pX[(2 * j + 1) % 4]; ko = kfo[j % 2]
                for tc in range(nch):
                    k.op("pe", lambda: nc.tensor.matmul(pre[:, :], s_[:, tc, 0, :], KS[:, tc, :], start=(tc == 0), stop=(tc == nch - 1)), R=[s_, KS], W=[pre])
                for tc in range(nch):
                    k.op("pe", lambda: nc.tensor.matmul(pim[:, :], s_[:, tc, 1, :], KDf[:, tc, :], start=(tc == 0), stop=(tc == nch - 1)), R=[s_, KDf], W=[pim])
                if j == 0:
                    for tc in range(nch):
                        k.op("pe", lambda: nc.tensor.matmul(pN[:, :], s_[:, tc, 1, 0:1], KS[:, tc, :], start=(tc == 0), stop=(tc == nch - 1)), R=[s_, KS], W=[pN])
                k.op("act", lambda: nc.scalar.activation(out=ko[:, 0, :], in_=pre[:, :], func=AF.Identity, scale=scs[:, j:j + 1]), R=[pre, scs], W=[ko])
                k.op("dve", lambda: nc.vector.tensor_scalar(out=ko[:, 1, :], in0=pim[:, :], scalar1=scs[:, j:j + 1], scalar2=None, op0=ALU.mult), R=[pim, scs], W=[ko])
                if j == 0:
                    k.op("dve", lambda: nc.vector.tensor_scalar(out=ko[0:1, 1, :], in0=pN[:, :], scalar1=scs[0:1, 0:1], scalar2=None, op0=ALU.mult), R=[pN, scs], W=[ko])
                k.dma(KF[j, :, :, :], ko[:], R=[ko])
            k.barrier()
        V = sb("V" + tag, [128, nch, 256], BF16)
        Y = sb("Y" + tag, [128, nch, 2, 256], BF16)
        wsb = sb("wsb" + tag, [128, 3, 768]); bsb = sb("bsb" + tag, [128, 768]); skb = sb("skb" + tag, [128, 2, 256])
        for jj in range(3):
            k.dma(wsb[:, jj, :], hy_short_w[l, jj, :].partition_broadcast(128), W=[wsb])
        k.dma(bsb[:], hy_short_b[l].partition_broadcast(128), W=[bsb])
        for o in range(2):
            k.dma(skb[:, o, :], hy_skip[l, o, :].partition_broadcast(128), W=[skb])
        pin = [[sb("pin%d_%d" % (a, b) + tag, [128, 768]) for b in range(3)] for a in range(2)]
        cacc = [sb("cacc%d" % i + tag, [128, 768]) for i in range(2)]
        ctmp = [sb("ctmp%d" % i + tag, [128, 768]) for i in range(2)]
        for tc in range(nch):
            tok = tok0 + tc * 128
            r0 = pt_row(tok)
            pp = pin[tc % 2]; ac = cacc[tc % 2]; tm = ctmp[tc % 2]
            for jj in range(3):
                k.dma(pp[jj][:], PT[r0 + jj - 1:r0 + jj - 1 + 128, 0:768], W=[pp[jj]])
            k.op("dve", lambda: nc.vector.tensor_tensor(out=ac[:], in0=pp[0][:], in1=wsb[:, 0, :], op=ALU.mult), R=[pp[0], wsb], W=[ac])
            k.op("pool", lambda: nc.gpsimd.tensor_tensor(out=tm[:], in0=pp[1][:], in1=wsb[:, 1, :], op=ALU.mult), R=[pp[1], wsb], W=[tm])
            k.op("dve", lambda: nc.vector.tensor_tensor(out=ac[:], in0=ac[:], in1=tm[:], op=ALU.add), R=[ac, tm], W=[ac])
            k.op("pool", lambda: nc.gpsimd.tensor_tensor(out=tm[:], in0=pp[2][:], in1=wsb[:, 2, :], op=ALU.mult), R=[pp[2], wsb], W=[tm])
            k.op("dve", lambda: nc.vector.tensor_tensor(out=ac[:], in0=ac[:], in1=tm[:], op=ALU.add), R=[ac, tm], W=[ac])
            k.op("dve", lambda: nc.vector.tensor_tensor(out=ac[:], in0=ac[:], in1=bsb[:], op=ALU.add), R=[ac, bsb], W=[ac])
            k.op("act", lambda: nc.scalar.copy(out=V[:, tc, :], in_=ac[:, 512:768]), R=[ac], W=[V])
            k.dma(HX[tok:tok + 128, :], ac[:], R=[ac])
        k.barrier()
        xs = [sb("xs%d" % i + tag, [128, 2, 256]) for i in range(2)]
        kin = [sb("kin%d" % i + tag, [128, 2, 256]) for i in range(2)]
        ta_ = [sb("ta%d" % i + tag, [128, 256]) for i in range(2)]
        tb_ = [sb("tb%d" % i + tag, [128, 256]) for i in range(2)]
        vin = [sb("vin%d" % i + tag, [128, 256]) for i in range(2)]
        gin = [sb("gin%d" % i + tag, [128, 256]) for i in range(2)]
        yv = [sb("yv%d" % i + tag, [128, 256]) for i in range(2)]
        ybf = [sb("ybf%d" % i + tag, [128, 256], BF16) for i in range(2)]
        nyq = sb("nyq" + tag, [1, 256])
        for n in range(2):
            nxt = load_slab(0)
            for j in range(nch):
                s_ = nxt
                nxt = load_slab(j + 1) if j + 1 < nch else load_slab(0, inv=True)
                pre = pX[(2 * j) % 4]; pim = pX[(2 * j + 1) % 4]
                X = xs[j % 2]; Kc = kin[j % 2]; a_ = ta_[j % 2]; b_ = tb_[j % 2]
                k.dma(Kc[:], KF[j, :, :, n * 256:(n + 1) * 256], W=[Kc])
                for tc in range(nch):
                    k.op("pe", lambda: nc.tensor.matmul(pre[:, 0:256], s_[:, tc, 0, :], V[:, tc, :], start=(tc == 0), stop=(tc == nch - 1)), R=[s_, V], W=[pre])
                for tc in range(nch):
                    k.op("pe", lambda: nc.tensor.matmul(pim[:, 0:256], s_[:, tc, 1, :], V[:, tc, :], start=(tc == 0), stop=(tc == nch - 1)), R=[s_, V], W=[pim])
                k.op("act", lambda: nc.scalar.copy(out=X[:, 0, :], in_=pre[:, 0:256]), R=[pre], W=[X])
                k.op("act", lambda: nc.scalar.copy(out=X[:, 1, :], in_=pim[:, 0:256]), R=[pim], W=[X])
                k.op("dve", lambda: nc.vector.tensor_tensor(out=a_[:], in0=X[:, 0, :], in1=Kc[:, 0, :], op=ALU.mult), R=[X, Kc], W=[a_])
                k.op("pool", lambda: nc.gpsimd.tensor_tensor(out=b_[:], in0=X[:, 1, :], in1=Kc[:, 1, :], op=ALU.mult), R=[X, Kc], W=[b_])
                k.op("dve", lambda: nc.vector.tensor_tensor(out=Y[:, j, 0, :], in0=a_[:], in1=b_[:], op=ALU.subtract), R=[a_, b_], W=[Y])
                if j == 0:
                    k.op("dve", lambda: nc.vector.tensor_copy(out=Y[0:1, j, 0, :], in_=a_[0:1, :]), R=[a_], W=[Y])
                    k.op("dve", lambda: nc.vector.tensor_copy(out=nyq[:], in_=b_[0:1, :]), R=[b_], W=[nyq])
                k.op("dve", lambda: nc.vector.tensor_tensor(out=a_[:], in0=X[:, 0, :], in1=Kc[:, 1, :], op=ALU.mult), R=[X, Kc], W=[a_])
                k.op("pool", lambda: nc.gpsimd.tensor_tensor(out=b_[:], in0=X[:, 1, :], in1=Kc[:, 0, :], op=ALU.mult), R=[X, Kc], W=[b_])
                k.op("dve", lambda: nc.vector.tensor_tensor(out=Y[:, j, 1, :], in0=a_[:], in1=b_[:], op=ALU.add), R=[a_, b_], W=[Y])
                if j == 0:
                    k.op("dve", lambda: nc.vector.tensor_copy(out=Y[0:1, j, 1, :], in_=nyq[:]), R=[nyq], W=[Y])
            for i in range(nch):
                s_ = nxt
                if i + 1 < nch:
                    nxt = load_slab(i + 1, inv=True)
                po_ = pX[i % 4]
                tok = tok0 + i * 128
                vi = vin[i % 2]; gi = gin[i % 2]; y_ = yv[i % 2]; yb = ybf[i % 2]
                if n == 0:
                    k.dma(vi[:], HX[tok:tok + 128, 512:768], W=[vi])
                    k.dma(gi[:], HX[tok:tok + 128, 0:256], W=[gi])
                else:
                    k.dma(vi[:], V1D[tok:tok + 128, :], W=[vi])
                    k.dma(gi[:], HX[tok:tok + 128, 256:512], W=[gi])
                for fc in range(nch):
                    k.op("pe", lambda: nc.tensor.matmul(po_[:, 0:256], s_[:, fc, 0, :], Y[:, fc, 0, :], start=(fc == 0), stop=False), R=[s_, Y], W=[po_])
                    k.op("pe", lambda: nc.tensor.matmul(po_[:, 0:256], s_[:, fc, 1, :], Y[:, fc, 1, :], start=False, stop=(fc == nch - 1)), R=[s_, Y], W=[po_])
                k.op("pool", lambda: nc.gpsimd.tensor_tensor(out=vi[:], in0=vi[:], in1=skb[:, n, :], op=ALU.mult), R=[vi, skb], W=[vi])
                k.op("dve", lambda: nc.vector.tensor_tensor(out=y_[:], in0=po_[:, 0:256], in1=vi[:], op=ALU.add), R=[po_, vi], W=[y_])
                if n == 0:
                    k.op("dve", lambda: nc.vector.tensor_tensor(out=y_[:], in0=y_[:], in1=gi[:], op=ALU.mult), R=[y_, gi], W=[y_])
                    k.op("act", lambda: nc.scalar.copy(out=V[:, i, :], in_=y_[:]), R=[y_], W=[V])
                    k.dma(V1D[tok:tok + 128, :], y_[:], R=[y_])
                else:
                    k.op("dve", lambda: nc.vector.tensor_tensor(out=yb[:], in0=y_[:], in1=gi[:], op=ALU.mult), R=[y_, gi], W=[yb])
                    k.dma(MIXTOK[tok:tok + 128, 0:256], yb[:], R=[yb])
            k.barrier()

    def phase_hyena(l):
        for (Lq, tok0, WTd, W0Id, SCd, ZTd, WINd, WIN2d, KF, tag) in ((T, 0, WT_d, W0I_d, SC_d, ZT_d, WIN_d, WIN2_d, KFD, "L"),
                                                                     (LC, T, WTc_d, W0Ic_d, SCc_d, ZTc_d, WINc_d, WIN2c_d, KFDc, "C")):
            with ExitStack() as es:
                def sb(name, shape, dt=F32):
                    return Buf(es.enter_context(nc.sbuf_tensor(un(name), list(shape), dt)))

                def ps(name, shape, dt=F32):
                    return Buf(es.enter_context(nc.psum_tensor(un(name), list(shape), dt)))
                hyena_seq(l, es, sb, ps, Lq, tok0, WTd, W0Id, SCd, ZTd, WINd, WIN2d, KF, tag)
            k.barrier()

    def resid_ln(sbufs, ysrc, h_t, gb, gam, bet, out_t):
        u_, st, mv = sbufs
        for hf_ in range(2):
            sl = slice(hf_ * 512, (hf_ + 1) * 512)
            yb_, yap = ysrc[hf_]
            k.op("dve", lambda: nc.vector.tensor_tensor(out=u_[:, sl], in0=yap, in1=gb[:, sl], op=ALU.mult), R=[yb_, gb], W=[u_])
        k.op("dve", lambda: nc.vector.scalar_tensor_tensor(out=u_[:], in0=h_t[:], scalar=DN_ALPHA, in1=u_[:], op0=ALU.mult, op1=ALU.add), R=[h_t, u_], W=[u_])
        for hf_ in range(2):
            k.op("dve", lambda: nc.vector.bn_stats(out=st[:, hf_, :], in_=u_[:, hf_ * 512:(hf_ + 1) * 512]), R=[u_], W=[st])
        k.op("dve", lambda: nc.vector.bn_aggr(out=mv[:, 0:2], in_=st[:].rearrange("p a b -> p (a b)")), R=[st], W=[mv])
        k.op("dve", lambda: nc.vector.tensor_scalar(out=mv[:, 1:2], in0=mv[:, 1:2], scalar1=LN_EPS, scalar2=None, op0=ALU.add), R=[mv], W=[mv])
        k.op("act", lambda: nc.scalar.activation(out=mv[:, 1:2], in_=mv[:, 1:2], func=AF.Sqrt), R=[mv], W=[mv])
        k.op("dve", lambda: nc.vector.reciprocal(out=mv[:, 1:2], in_=mv[:, 1:2]), R=[mv], W=[mv])
        k.op("dve", lambda: nc.vector.tensor_scalar(out=u_[:], in0=u_[:], scalar1=mv[:, 0:1], scalar2=mv[:, 1:2], op0=ALU.subtract, op1=ALU.mult), R=[u_, mv], W=[u_])
        k.op("pool", lambda: nc.gpsimd.tensor_tensor(out=u_[:], in0=u_[:], in1=gam[:], op=ALU.mult), R=[u_, gam], W=[u_])
        k.op("pool", lambda: nc.gpsimd.tensor_tensor(out=out_t[:], in0=u_[:], in1=bet[:], op=ALU.add), R=[u_, bet], W=[out_t])

    def phase_outproj(l):
        with ExitStack() as es:
            def sb(name, shape, dt=F32):
                return Buf(es.enter_context(nc.sbuf_tensor(un(name), list(shape), dt)))

            def ps(name, shape, dt=F32):
                return Buf(es.enter_context(nc.psum_tensor(un(name), list(shape), dt)))
            Wst = [sb("wst%d" % i, [128, 8, 512]) for i in range(2)]
            Wb = sb("Wob", [128, 8, D], BF16)
            for cb in range(2):
                st = Wst[cb]
                k.dma(st[:], w_out[l, :, cb * 512:(cb + 1) * 512].rearrange("(k p) n -> p k n", p=128), W=[st])
                k.op("act", lambda: nc.scalar.copy(out=Wb[:, :, cb * 512:(cb + 1) * 512], in_=st[:]), R=[st], W=[Wb])
            gb = [sb("gb%d" % j, [128, D]) for j in range(2)]
            gam = sb("gam", [128, D]); bet = sb("bet", [128, D])
            for j in range(2):
                k.dma(gb[j][:], GMOD[l, 0, j, :].partition_broadcast(128), W=[gb[j]])
            k.dma(gam[:], ln1_g[l].partition_broadcast(128), W=[gam]); k.dma(bet[:], ln1_b[l].partition_broadcast(128), W=[bet])
            mx = [sb("mx%d" % i, [128, 768], BF16) for i in range(2)]
            mT = [sb("mT%d" % i, [128, 8, 128], BF16) for i in range(2)]
            hf = [sb("hf%d" % i, [128, D]) for i in range(2)]
            ub = [sb("ub%d" % i, [128, D]) for i in range(2)]
            ob = [sb("obo%d" % i, [128, D]) for i in range(2)]
            st6 = [sb("st6%d" % i, [128, 2, 6]) for i in range(2)]; mv = [sb("mv%d" % i, [128, 2]) for i in range(2)]
            tp = [ps("tpo%d" % i, [128, 6, 128], BF16) for i in range(2)]
            py = [[ps("py%d_%d" % (a, b), [128, 512]) for b in range(2)] for a in range(2)]
            for i in range(NTILE):
                j = 0 if i < 32 else 1
                m_ = mx[i % 2]; mt = mT[i % 2]; h_ = hf[i % 2]; tpp = tp[i % 2]; pyy = py[i % 2]
                k.dma(m_[:], MIXTOK[i * 128:(i + 1) * 128, :], W=[m_])
                k.dma(mt[:, 6:8, :], RGT[:, i * 128:(i + 1) * 128].rearrange("(c p) n -> p c n", p=128), W=[mt])
                k.dma(h_[:], hsrc(l, i), W=[h_])
                for kk in range(6):
                    k.op("pe", lambda: nc.tensor.transpose(tpp[:, kk, :], m_[:, kk * 128:(kk + 1) * 128], identb[:]), R=[m_, identb], W=[tpp])
                k.op("act", lambda: nc.scalar.copy(out=mt[:, 0:6, :], in_=tpp[:, :, :]), R=[tpp], W=[mt])
                for nb in range(2):
                    for kk in range(8):
                        k.op("pe", lambda: nc.tensor.matmul(pyy[nb][:, :], mt[:, kk, :], Wb[:, kk, nb * 512:(nb + 1) * 512], start=(kk == 0), stop=(kk == 7)),
                             R=[mt, Wb], W=[pyy[nb]])
                o_ = ob[i % 2]
                resid_ln((ub[i % 2], st6[i % 2], mv[i % 2]), [(pyy[0], pyy[0][:, :]), (pyy[1], pyy[1][:, :])], h_, gb[j], gam, bet, o_)
                k.dma(H1D[i * 128:(i + 1) * 128, :], o_[:], R=[o_])
            k.barrier()

    def phase_moe(l, last):
        passes = [(0, 12), (12, 24), (24, 34)]
        with ExitStack() as es:
            def sb(name, shape, dt=F32):
                return Buf(es.enter_context(nc.sbuf_tensor(un(name), list(shape), dt)))

            def ps(name, shape, dt=F32):
                return Buf(es.enter_context(nc.psum_tensor(un(name), list(shape), dt)))
            yacc = sb("yacc", [128, 12, D])
            mT16 = sb("mT16", [128, 8, 1536], BF16)
            gT = sb("gT", [65, 1536], BF16)
            SEL = sb("SEL", [65, 65 * 128], BF16)
            k.dma(SEL[:], SEL_d[:, :], W=[SEL])
            wst = [sb("west%d" % i, [128, 2048]) for i in range(3)]
            wgb = [sb("wgb%d" % i, [128, 8, 256], BF16) for i in range(2)]
            wub = [sb("wub%d" % i, [128, 8, 256], BF16) for i in range(2)]
            wdb = [sb("wdb%d" % i, [128, 2, D], BF16) for i in range(2)]
            Wr = sb("Wr", [128, 8, 64]); brb = sb("brb", [128, 64])
            k.dma(Wr[:], w_router[l].rearrange("(k p) n -> p k n", p=128), W=[Wr])
            k.dma(brb[:], b_router[l].partition_broadcast(128), W=[brb])
            gb = [sb("g2b%d" % j, [128, D]) for j in range(2)]
            gam = sb("gam2", [128, D]); bet = sb("bet2", [128, D])
            for j in range(2):
                k.dma(gb[j][:], GMOD[l, 1, j, :].partition_broadcast(128), W=[gb[j]])
            k.dma(gam[:], ln2_g[l].partition_broadcast(128), W=[gam]); k.dma(bet[:], ln2_b[l].partition_broadcast(128), W=[bet])
            hf = [sb("hfm%d" % i, [128, D]) for i in range(2)]
            mT32s = [sb("mT32_%d" % i, [128, 8, 128]) for i in range(2)]
            scs_ = [sb("scr%d" % i, [128, 64]) for i in range(2)]; sels = [sb("selr%d" % i, [128, 64]) for i in range(2)]
            top8s = [sb("top8_%d" % i, [128, 8]) for i in range(2)]; dens = [sb("den%d" % i, [128, 1]) for i in range(2)]
            Gs = [sb("G%d" % i, [128, 65]) for i in range(2)]
            sS = [sb("sS%d" % i, [128, 512]) for i in range(2)]
            tS = [sb("tS%d" % i, [128, 512]) for i in range(2)]
            gbs = [sb("gbs%d" % i, [128, 512], BF16) for i in range(2)]
            hT = [[sb("hT%d_%d" % (a, b), [128, 512], BF16) for b in range(2)] for a in range(2)]
            ubs = [sb("ubm%d" % i, [128, D]) for i in range(2)]; obm = [sb("obm%d" % i, [128, D]) for i in range(2)]
            st6s = [sb("st6m%d" % i, [128, 2, 6]) for i in range(2)]; mvs = [sb("mvm%d" % i, [128, 2]) for i in range(2)]
            pg = [ps("pg%d" % i, [128, 512]) for i in range(2)]
            pu = [ps("pu%d" % i, [128, 512]) for i in range(2)]
            pgb = ps("pgb", [128, 512])
            pd = [ps("pd%d" % i, [128, 512]) for i in range(3)]
            for G in Gs:
                k.op("dve", lambda: nc.vector.memset(G[:], 1.0), W=[G])
            wsti = [0]
            for (t0, t1) in passes:
                ntile = t1 - t0
                ntok = ntile * 128
                for ii, i in enumerate(range(t0, t1)):
                    j = 0 if i < 32 else 1
                    h_ = hf[ii % 2]
                    mT32 = mT32s[0]; sc_ = scs_[0]; sel = sels[0]; top8 = top8s[0]; den = dens[0]; G = Gs[0]
                    k.dma(h_[:], H1D[i * 128:(i + 1) * 128, :], W=[h_])
                    for half in range(2):
                        pt_ = pd[half]
                        for k4 in range(4):
                            kk = half * 4 + k4
                            k.op("pe", lambda: nc.tensor.transpose(pt_[:, k4 * 128:(k4 + 1) * 128], h_[:, kk * 128:(kk + 1) * 128], identf[:]),
                                 R=[h_, identf], W=[pt_])
                        for k4 in range(4):
                            kk = half * 4 + k4
                            sc = MOD[:, l, 32 + kk, j:j + 1]; sh = MOD[:, l, 24 + kk, j:j + 1]
                            k.op("act", lambda: nc.scalar.activation(out=mT32[:, kk, :], in_=pt_[:, k4 * 128:(k4 + 1) * 128], func=AF.Identity, bias=sh, scale=sc),
                                 R=[pt_, MOD], W=[mT32])
                            k.op("dve", lambda: nc.vector.tensor_copy(out=mT16[:, kk, ii * 128:(ii + 1) * 128], in_=mT32[:, kk, :]), R=[mT32], W=[mT16])
                    pl = pd[2]
                    for kk in range(8):
                        k.op("pe", lambda: nc.tensor.matmul(pl[:, 0:64], mT32[:, kk, :], Wr[:, kk, :], start=(kk == 0), stop=(kk == 7)), R=[mT32, Wr], W=[pl])
                    k.op("act", lambda: nc.scalar.activation(out=sc_[:], in_=pl[:, 0:64], func=AF.Sigmoid), R=[pl], W=[sc_])
                    k.op("dve", lambda: nc.vector.tensor_tensor(out=sel[:], in0=sc_[:], in1=brb[:], op=ALU.add), R=[sc_, brb], W=[sel])
                    k.op("dve", lambda: nc.vector.max(out=top8[:], in_=sel[:]), R=[sel], W=[top8])
                    k.op("dve", lambda: nc.vector.tensor_scalar(out=sel[:], in0=sel[:], scalar1=top8[:, 7:8], scalar2=None, op0=ALU.is_ge), R=[sel, top8], W=[sel])
                    k.op("dve", lambda: nc.vector.tensor_tensor(out=sel[:], in0=sel[:], in1=sc_[:], op=ALU.mult), R=[sel, sc_], W=[sel])
                    k.op("dve", lambda: nc.vector.tensor_reduce(out=den[:], in_=sel[:], axis=AX.X, op=ALU.add), R=[sel], W=[den])
                    k.op("dve", lambda: nc.vector.reciprocal(out=den[:], in_=den[:]), R=[den], W=[den])
                    k.op("dve", lambda: nc.vector.tensor_scalar(out=G[:, 0:64], in0=sel[:], scalar1=den[:, 0:1], scalar2=2.5, op0=ALU.mult, op1=ALU.mult), R=[sel, den], W=[G])
                    pgt = pg[ii % 2]
                    k.op("pe", lambda: nc.tensor.transpose(pgt[0:65, 0:128], G[:, :], identf[:]), R=[G, identf], W=[pgt])
                    k.op("act", lambda: nc.scalar.copy(out=gT[:, ii * 128:(ii + 1) * 128], in_=pgt[0:65, 0:128]), R=[pgt], W=[gT])
                k.op("dve", lambda: nc.vector.memset(yacc[:], 0.0), W=[yacc])
                blocks = []
                b0 = 0
                while b0 < ntok:
                    nb_ = min(512, ntok - b0)
                    blocks.append((b0, nb_)); b0 += nb_
                pending = None
                wi = 0
                def expert_srcs(e_):
                    return ((w_gate[l, e_], w_up[l, e_], w_down[l, e_]) if e_ < 64 else (ws_gate[l], ws_up[l], ws_down[l]))

                def issue_loads(e_):
                    for mi, src in enumerate(expert_srcs(e_)):
                        st = wst[mi]
                        k.dma(st[:].rearrange("p (k n) -> p k n", n=(256 if mi < 2 else D)), src.rearrange("(k p) n -> p k n", p=128), W=[st])

                def issue_casts(e_, which):
                    for mi, dstw in enumerate((wgb[e_ % 2], wub[e_ % 2], wdb[e_ % 2])):
                        if mi not in which:
                            continue
                        st = wst[mi]
                        k.op("act", lambda: nc.scalar.copy(out=dstw[:].rearrange("p k n -> p (k n)"), in_=st[:]), R=[st], W=[dstw])
                issue_loads(0)
                issue_casts(0, (0, 1, 2))
                for e in range(65):
                    Wg_ = wgb[e % 2]; Wu_ = wub[e % 2]; Wd_ = wdb[e % 2]
                    if e + 1 < 65:
                        issue_loads(e + 1)
                    bidx = -1
                    for (bb, nb_) in blocks:
                        hpair = hT[wi % 2]; gb_ = gbs[wi % 2]; wi += 1
                        dn = down_steps(pending, pd, yacc) if pending is not None else iter(())

                        def drain(n_):
                            for _ in range(n_):
                                f_ = next(dn, None)
                                if f_ is None:
                                    return
                                f_()
                        for fc in range(2):
                            g_ = pg[fc]; u_ = pu[fc]; s_ = sS[fc]; t_ = tS[fc]
                            for kk in range(8):
                                k.op("pe", lambda: nc.tensor.matmul(g_[:, 0:nb_], Wg_[:, kk, fc * 128:(fc + 1) * 128], mT16[:, kk, bb:bb + nb_],
                                                                    start=(kk == 0), stop=(kk == 7)), R=[Wg_, mT16], W=[g_])
                            drain(2)
                            for kk in range(8):
                                k.op("pe", lambda: nc.tensor.matmul(u_[:, 0:nb_], Wu_[:, kk, fc * 128:(fc + 1) * 128], mT16[:, kk, bb:bb + nb_],
                                                                    start=(kk == 0), stop=(kk == 7)), R=[Wu_, mT16], W=[u_])
                            if fc == 0:
                                k.op("pe", lambda: nc.tensor.matmul(pgb[:, 0:nb_], SEL[:, e * 128:(e + 1) * 128], gT[:, bb:bb + nb_], start=True, stop=True),
                                     R=[SEL, gT], W=[pgb])
                                k.op("act", lambda: nc.scalar.copy(out=gb_[:, 0:nb_], in_=pgb[:, 0:nb_]), R=[pgb], W=[gb_])
                            k.op("act", lambda: nc.scalar.activation(out=s_[:, 0:nb_], in_=g_[:, 0:nb_], func=AF.Silu), R=[g_], W=[s_])
                            k.op("dve", lambda: nc.vector.tensor_tensor(out=t_[:, 0:nb_], in0=u_[:, 0:nb_], in1=s_[:, 0:nb_], op=ALU.mult), R=[u_, s_], W=[t_])
                            k.op("pool", lambda: nc.gpsimd.tensor_tensor(out=hpair[fc][:, 0:nb_], in0=t_[:, 0:nb_], in1=gb_[:, 0:nb_], op=ALU.mult),
                                 R=[t_, gb_], W=[hpair[fc]])
                            drain(2)
                        drain(100)
                        pending = (hpair, Wd_, bb, nb_)
                        bidx += 1
                        if bidx == 1 and e + 1 < 65:
                            issue_casts(e + 1, (0, 1))
                    if e + 1 < 65:
                        issue_casts(e + 1, (2,))
                for f_ in down_steps(pending, pd, yacc):
                    f_()
                for ii, i in enumerate(range(t0, t1)):
                    j = 0 if i < 32 else 1
                    h_ = hf[ii % 2]
                    o_ = obm[ii % 2]
                    k.dma(h_[:], H1D[i * 128:(i + 1) * 128, :], W=[h_])
                    resid_ln((ubs[ii % 2], st6s[ii % 2], mvs[ii % 2]), [(yacc, yacc[:, ii, 0:512]), (yacc, yacc[:, ii, 512:1024])], h_, gb[j], gam, bet, o_)
                    if last:
                        if i < 32:
                            k.dma(y_out[i * 128:(i + 1) * 128, :], o_[:], R=[o_])
                    else:
                        k.dma(HD[i * 128:(i + 1) * 128, :], o_[:], R=[o_])
            k.barrier()

    pdi = [0]

    def down_steps(item, pd, yacc):
        hpair, Wd_, bb, nb_ = item
        for sub in range(nb_ // 128):
            ti_ = (bb // 128) + sub
            for dh in range(2):
                def step(sub=sub, ti_=ti_, dh=dh):
                    p = pd[pdi[0] % 3]; pdi[0] += 1
                    for fc in range(2):
                        k.op("pe", lambda: nc.tensor.matmul(p[:, :], hpair[fc][:, sub * 128:(sub + 1) * 128], Wd_[:, fc, dh * 512:(dh + 1) * 512],
                                                            start=(fc == 0), stop=(fc == 1)), R=[hpair[fc], Wd_], W=[p])
                    k.op("dve", lambda: nc.vector.tensor_tensor(out=yacc[:, ti_, dh * 512:(dh + 1) * 512], in0=p[:, :],
                                                                in1=yacc[:, ti_, dh * 512:(dh + 1) * 512], op=ALU.add), R=[p, yacc], W=[yacc])
                yield step

    phase_mod()
    seq = []
    for l in range(L):
        seq += [("inproj", l), ("rg", l), ("attn", l), ("hyena", l), ("outproj", l), ("moe", l)]
    for (ph, l) in seq:
        if ph == "inproj":
            phase_inproj(l)
        elif ph == "rg":
            phase_rg(l)
        elif ph == "attn":
            phase_attn(l)
        elif ph == "hyena":
            phase_hyena(l)
        elif ph == "outproj":
            phase_outproj(l)
        elif ph == "moe":
            phase_moe(l, last=(l == L - 1))
        if stop_after == (ph, l):
            break
    k.barrier()
    gs.close()
    return nc


_CONST = {}


def _consts():
    if _CONST:
        return _CONST
    bf = ml_dtypes.bfloat16

    def dft_tables(Lq):
        N = 2 * Lq
        n = np.arange(Lq, dtype=np.float64)
        ang = 2.0 * np.pi * np.outer(n, n) / N
        C = np.cos(ang)
        S = -np.sin(ang)
        alt = (-1.0) ** n
        S[:, 0] = alt
        nch = Lq // 128
        W = np.stack([C, S], axis=0)
        W = W.reshape(2, nch, 128, nch, 128)
        WTt = np.ascontiguousarray(W.transpose(3, 2, 1, 0, 4)).astype(np.float32).astype(bf)
        sc = np.full((128, nch), 2.0 / N, np.float32)
        sc[0, 0] = 1.0 / N
        WTI = WTt.copy()
        WTI[0, :, :, 1, 0] = 0
        WTI[:, 0, 0, 1, :] = ((-1.0) ** np.arange(128, dtype=np.float32)).astype(bf)[None, :]
        return WTt, sc, WTI

    def filt_tables(Lq):
        t = np.linspace(0.0, 1.0, Lq, dtype=np.float32)[:, None]
        w = (2.0 * np.float32(math.pi) * np.arange(Lq, dtype=np.float32)[:, None] / np.float32(Lq)).astype(np.float32)
        bands = np.linspace(1e-4, 7, 8, dtype=np.float32)
        z = np.concatenate([t, np.cos(bands * w), -np.sin(bands * w)], axis=-1).astype(np.float32)
        mx = math.log(1e-2) / 0.3
        mn = math.log(1e-2) / 1.5
        deltas = np.abs(np.linspace(mn, mx, 256, dtype=np.float32))
        win = np.exp(-t * deltas).astype(np.float32)
        win2 = win.copy()
        win2[0, :] = 0.0
        return np.ascontiguousarray(z.T), win, win2

    WTt, sc, w0i = dft_tables(T)
    WTc, scc, w0ic = dft_tables(LC)
    zt, win, win2 = filt_tables(T)
    ztc, winc, win2c = filt_tables(LC)
    n_rows = T // 64
    row = np.repeat(np.arange(n_rows, dtype=np.float32), 64)
    col = np.tile(np.arange(64, dtype=np.float32), n_rows)
    inv_freq = (np.float32(10000.0) ** (-np.arange(0, 32, 2, dtype=np.float32) / np.float32(32))).astype(np.float32)
    ang = np.concatenate([row[:, None] * inv_freq, col[:, None] * inv_freq], axis=-1).astype(np.float32)
    rope = np.stack([np.tile(np.cos(ang), (1, 8)), np.tile(np.sin(ang), (1, 8))], axis=1).astype(np.float32)
    sel = np.zeros((65, 65, 128), np.float32)
    for e in range(65):
        sel[e, e, :] = 1.0
    _CONST.update(dict(
        identb=np.eye(128, dtype=np.float32).astype(bf), identf=np.eye(128, dtype=np.float32),
        WT=WTt, WTc=WTc, WTI=w0i, WTIc=w0ic, SC=sc, SCc=scc, ZT=zt, ZTc=ztc, WIN=win, WIN2=win2, WINc=winc, WIN2c=win2c,
        ROPE=rope, SEL=sel.reshape(65, 65 * 128).astype(bf),
        ALT=((-1.0) ** np.arange(128, dtype=np.float32)).reshape(1, 128).astype(bf)))
    return _CONST


WEIGHT_NAMES = ["w_mod", "b_mod", "ln1_g", "ln1_b", "ln2_g", "ln2_b", "w_in", "w_out", "hy_short_w", "hy_short_b",
                "hy_f_w1", "hy_f_b1", "hy_f_freq", "hy_f_w2", "hy_f_b2", "hy_f_w3", "hy_skip", "q_norm", "k_norm",
                "rg_conv_w", "rg_conv_b", "rg_lambda", "rg_w_a", "rg_b_a", "rg_w_x", "rg_b_x", "w_router", "b_router",
                "w_gate", "w_up", "w_down", "ws_gate", "ws_up", "ws_down"]


def make_in_maps(inputs, L, cores):
    cst = _consts()
    shared = {}
    for n in WEIGHT_NAMES:
        arr = np.asarray(inputs[n], dtype=np.float32)
        for l_ in range(L):
            shared["%s_%d" % (n, l_)] = np.ascontiguousarray(arr[l_])
    shared["bmT"] = np.ascontiguousarray(np.asarray(inputs["b_mod"], dtype=np.float32)[:L].reshape(L, 48, 128).transpose(2, 0, 1))
    shared.update(cst)
    c = np.asarray(inputs["c"], np.float32)
    c_ctx = np.asarray(inputs["c_ctx"], np.float32)
    maps = []
    for b in cores:
        m = dict(shared)
        m["x"] = np.ascontiguousarray(np.asarray(inputs["x"], np.float32)[b])
        m["ctx"] = np.ascontiguousarray(np.asarray(inputs["ctx"], np.float32)[b])
        two = np.stack([c[b], c_ctx], axis=-1)
        m["cc"] = np.ascontiguousarray(two.reshape(8, 128, 2).transpose(1, 0, 2))
        maps.append(m)
    return maps


_NC = {}


def kernel(**inputs):
    if "nc" not in _NC:
        _NC["nc"] = build(L=4)
    nc = _NC["nc"]
    maps = make_in_maps(inputs, 4, list(range(8)))
    res = run_bass_kernel_spmd(nc, maps, core_ids=list(range(8)))
    out = np.stack([np.asarray(r["y"], dtype=np.float32) for r in res.results], axis=0)
    return out
```

```python
from contextlib import ExitStack
import math
import numpy as np
import ml_dtypes
import concourse.bass as bass
import concourse.mybir as mybir
from concourse.bass_utils import run_bass_kernel_spmd

F32 = mybir.dt.float32
BF16 = mybir.dt.bfloat16
AF = mybir.ActivationFunctionType
ALU = mybir.AluOpType
AX = mybir.AxisListType

D = 1024
T = 4096
LC = 256
NTOK = T + LC
NTILE = NTOK // 128
PROJ_W = 2048
N_EXP = 64
DN_ALPHA = (2 * 4) ** 0.25
LN_EPS = 1e-6
QK_EPS = 1e-6
PT_ROWS = 2 + T + 2 + LC + 2
PT_W = 1536


def pt_row(tok):
    return 2 + tok if tok < T else 4 + tok


class Buf:
    __slots__ = ("t", "w", "r")

    def __init__(self, t):
        self.t = t
        self.w = None
        self.r = {}

    def __getitem__(self, idx):
        return self.t[idx]


class Res:
    __slots__ = ("w", "r")

    def __init__(self):
        self.w = None
        self.r = {}


class KB:
    NSLOT = 28

    def __init__(self, nc):
        self.nc = nc
        self.eng = {"pe": nc.tensor, "act": nc.scalar, "dve": nc.vector, "pool": nc.gpsimd, "sp": nc.sync}
        self.sem = {q: nc.alloc_semaphore("sem_" + q) for q in ("pe", "act", "dve", "pool")}
        self.cnt = {q: 0 for q in self.sem}
        self.slots = [nc.alloc_semaphore("dsl%d" % i) for i in range(self.NSLOT)]
        self.slotcnt = [0] * self.NSLOT
        self.nextslot = 0
        self.seen = {q: {} for q in self.eng}

    def _semof(self, key):
        return self.sem[key] if isinstance(key, str) else self.slots[key]

    def _wait(self, q, key, val):
        if q == "pe" and key == "pe":
            return
        if self.seen[q].get(key, 0) >= val:
            return
        self.eng[q].wait_ge(self._semof(key), val)
        self.seen[q][key] = val

    def _deps(self, q, R, W):
        for r in R:
            if r.w is not None:
                self._wait(q, *r.w)
        for w in W:
            if w.w is not None:
                self._wait(q, *w.w)
            for kk, v in w.r.items():
                self._wait(q, kk, v)

    @staticmethod
    def _mark(tok, R, W):
        kk, v = tok
        for r in R:
            if r.r.get(kk, 0) < v:
                r.r[kk] = v
        for w in W:
            w.w = tok
            w.r = {}

    def op(self, q, fn, R=(), W=()):
        self._deps(q, R, W)
        inst = fn()
        self.cnt[q] += 1
        inst.then_inc(self.sem[q], 1)
        self._mark((q, self.cnt[q]), R, W)

    def dma(self, out, in_, R=(), W=(), q="sp", **kw):
        sl = self.nextslot
        self.nextslot = (sl + 1) % self.NSLOT
        if self.slotcnt[sl]:
            self._wait(q, sl, self.slotcnt[sl])
        self._deps(q, R, W)
        self.eng[q].dma_start(out=out, in_=in_, **kw).then_inc(self.slots[sl], 16)
        self.slotcnt[sl] += 16
        self._mark((sl, self.slotcnt[sl]), R, W)

    def barrier(self):
        for q in self.eng:
            for key in self.sem:
                if self.cnt[key]:
                    self._wait(q, key, self.cnt[key])
            for sl in range(self.NSLOT):
                if self.slotcnt[sl]:
                    self._wait(q, sl, self.slotcnt[sl])


def build(L=4, dbg=False, stop_after=None):
    nc = bass.Bass("TRN2", target_bir_lowering=False)
    k = KB(nc)

    class LT:
        def __init__(self, aps):
            self.aps = aps

        def __getitem__(self, idx):
            if isinstance(idx, tuple):
                l_, rest = idx[0], idx[1:]
                return self.aps[l_][rest] if rest else self.aps[l_]
            return self.aps[idx]

    def din(name, shape, dt=F32):
        if name in WEIGHT_NAMES:
            return LT([nc.dram_tensor("%s_%d" % (name, l_), list(shape[1:]), dt, kind="ExternalInput").ap() for l_ in range(shape[0])])
        return nc.dram_tensor(name, list(shape), dt, kind="ExternalInput").ap()

    def dscr(name, shape, dt=F32):
        return nc.dram_tensor(name, list(shape), dt, kind="ExternalOutput" if dbg else "Internal").ap()

    x = din("x", [T, D])
    ctx = din("ctx", [LC, D])
    cc = din("cc", [128, 8, 2])
    w_mod = din("w_mod", [L, D, 6 * D])
    b_mod = din("b_mod", [L, 6 * D])
    bmT = din("bmT", [128, L, 48])
    ln1_g = din("ln1_g", [L, D]); ln1_b = din("ln1_b", [L, D])
    ln2_g = din("ln2_g", [L, D]); ln2_b = din("ln2_b", [L, D])
    w_in = din("w_in", [L, D, PROJ_W])
    w_out = din("w_out", [L, D, D])
    hy_short_w = din("hy_short_w", [L, 3, 768]); hy_short_b = din("hy_short_b", [L, 768])
    hy_f_w1 = din("hy_f_w1", [L, 17, 64]); hy_f_b1 = din("hy_f_b1", [L, 64])
    hy_f_freq = din("hy_f_freq", [L, 64])
    hy_f_w2 = din("hy_f_w2", [L, 64, 64]); hy_f_b2 = din("hy_f_b2", [L, 64])
    hy_f_w3 = din("hy_f_w3", [L, 64, 1024]); hy_skip = din("hy_skip", [L, 2, 256])
    q_norm = din("q_norm", [L, 64]); k_norm = din("k_norm", [L, 64])
    rg_conv_w = din("rg_conv_w", [L, 4, 256]); rg_conv_b = din("rg_conv_b", [L, 256])
    rg_lambda = din("rg_lambda", [L, 2, 256])
    rg_w_a = din("rg_w_a", [L, 2, 4, 64, 64]); rg_b_a = din("rg_b_a", [L, 2, 256])
    rg_w_x = din("rg_w_x", [L, 2, 4, 64, 64]); rg_b_x = din("rg_b_x", [L, 2, 256])
    w_router = din("w_router", [L, D, N_EXP]); b_router = din("b_router", [L, N_EXP])
    w_gate = din("w_gate", [L, N_EXP, D, 256]); w_up = din("w_up", [L, N_EXP, D, 256])
    w_down = din("w_down", [L, N_EXP, 256, D])
    ws_gate = din("ws_gate", [L, D, 256]); ws_up = din("ws_up", [L, D, 256]); ws_down = din("ws_down", [L, 256, D])
    identb_d = din("identb", [128, 128], BF16)
    identf_d = din("identf", [128, 128])
    WT_d = din("WT", [32, 128, 32, 2, 128], BF16)
    WTc_d = din("WTc", [2, 128, 2, 2, 128], BF16)
    SC_d = din("SC", [128, 32]); SCc_d = din("SCc", [128, 2])
    ZT_d = din("ZT", [17, T]); ZTc_d = din("ZTc", [17, LC])
    WIN_d = din("WIN", [T, 256]); WIN2_d = din("WIN2", [T, 256])
    WINc_d = din("WINc", [LC, 256]); WIN2c_d = din("WIN2c", [LC, 256])
    ROPE_d = din("ROPE", [T, 2, 256])
    SEL_d = din("SEL", [65, 65 * 128], BF16)
    ALT_d = din("ALT", [1, 128], BF16)
    W0I_d = din("WTI", [32, 128, 32, 2, 128], BF16)
    W0Ic_d = din("WTIc", [2, 128, 2, 2, 128], BF16)

    y_out = nc.dram_tensor("y", [T, D], F32, kind="ExternalOutput").ap()

    HD = dscr("HD", [NTOK, D])
    H1D = dscr("H1D", [NTOK, D])
    PT = dscr("PT", [PT_ROWS, PT_W])
    PR = dscr("PR", [512, NTOK])
    RGT = dscr("RGT", [256, NTOK], BF16)
    MIXTOK = dscr("MIXTOK", [NTOK, 768], BF16)
    HX = dscr("HX", [NTOK, 768])
    V1D = dscr("V1D", [NTOK, 256])
    KFD = dscr("KFD", [32, 128, 2, 512])
    KFDc = dscr("KFDc", [2, 128, 2, 512])
    GMOD = dscr("GMOD", [L, 2, 2, D])

    gs = ExitStack()
    _uid = [0]

    def un(name):
        _uid[0] += 1
        return "%s_%d" % (name, _uid[0])

    def gsb(name, shape, dt=F32):
        return Buf(gs.enter_context(nc.sbuf_tensor(un(name), list(shape), dt)))

    MOD = gsb("MOD", [128, L, 48, 2])
    identb = gsb("identb_s", [128, 128], BF16)
    identf = gsb("identf_s", [128, 128])
    k.dma(identb[:], identb_d[:, :], W=[identb])
    k.dma(identf[:], identf_d[:, :], W=[identf])

    def hsrc(l, i):
        if l == 0:
            return x[i * 128:(i + 1) * 128, :] if i < 32 else ctx[(i - 32) * 128:(i - 31) * 128, :]
        return HD[i * 128:(i + 1) * 128, :]

    def colvec(v1d, n0, n):
        return v1d[n0:n0 + n].rearrange("(p o) -> p o", o=1)

    def phase_mod():
        with ExitStack() as es:
            def sb(name, shape, dt=F32):
                return Buf(es.enter_context(nc.sbuf_tensor(un(name), list(shape), dt)))

            def ps(name, shape, dt=F32):
                return Buf(es.enter_context(nc.psum_tensor(un(name), list(shape), dt)))
            ccs = sb("ccs", [128, 8, 2]); sT = sb("sT", [128, 8, 2]); sg = sb("sg", [128, 8, 2])
            bmTs = sb("bmTs", [128, L, 48])
            wbuf = [sb("wm%d" % i, [128, 8, 1024]) for i in range(2)]
            brow = sb("brow", [2, 1024]); rows = sb("rows", [2, 1024])
            psf = [ps("psf%d" % i, [128, 8, 2]) for i in range(2)]
            psr = [ps("psr%d" % i, [2, 512]) for i in range(2)]
            k.dma(ccs[:], cc[:, :, :], W=[ccs])
            k.dma(bmTs[:], bmT[:, :, :], W=[bmTs])
            k.op("act", lambda: nc.scalar.activation(out=sg[:], in_=ccs[:], func=AF.Sigmoid), R=[ccs], W=[sg])
            k.op("dve", lambda: nc.vector.tensor_tensor(out=sT[:], in0=ccs[:], in1=sg[:], op=ALU.mult), R=[ccs, sg], W=[sT])
            it = 0
            for l in range(L):
                for blk in range(6):
                    wb = wbuf[it % 2]; pf = psf[it % 2]; it += 1
                    c0 = blk * 1024
                    k.dma(wb[:], w_mod[l, :, c0:c0 + 1024].rearrange("(k p) n -> p k n", p=128), W=[wb])
                    for ch in range(8):
                        for kk in range(8):
                            k.op("pe", lambda: nc.tensor.matmul(pf[:, ch, :], wb[:, kk, ch * 128:(ch + 1) * 128], sT[:, kk, :],
                                                                start=(kk == 0), stop=(kk == 7)), R=[wb, sT], W=[pf])
                    for j in range(2):
                        k.op("dve", lambda: nc.vector.tensor_tensor(out=MOD[:, l, blk * 8:(blk + 1) * 8, j], in0=pf[:, :, j],
                                                                    in1=bmTs[:, l, blk * 8:(blk + 1) * 8], op=ALU.add),
                             R=[pf, bmTs], W=[MOD])
                    if blk in (1, 4):
                        k.op("dve", lambda: nc.vector.tensor_scalar(out=MOD[:, l, blk * 8:(blk + 1) * 8, :], in0=MOD[:, l, blk * 8:(blk + 1) * 8, :],
                                                                    scalar1=1.0, scalar2=None, op0=ALU.add), R=[MOD], W=[MOD])
                    if blk in (2, 5):
                        k.dma(brow[:], b_mod[l, c0:c0 + 1024].partition_broadcast(2), W=[brow])
                        for nb in range(2):
                            for kk in range(8):
                                k.op("pe", lambda: nc.tensor.matmul(psr[nb][:, :], sT[:, kk, :], wb[:, kk, nb * 512:(nb + 1) * 512],
                                                                    start=(kk == 0), stop=(kk == 7)), R=[wb, sT], W=[psr[nb]])
                            k.op("dve", lambda: nc.vector.tensor_tensor(out=rows[:, nb * 512:(nb + 1) * 512], in0=psr[nb][:, :],
                                                                        in1=brow[:, nb * 512:(nb + 1) * 512], op=ALU.add),
                                 R=[psr[nb], brow], W=[rows])
                        k.dma(GMOD[l, 0 if blk == 2 else 1, :, :], rows[:], R=[rows])
            k.barrier()

    def phase_inproj(l):
        with ExitStack() as es:
            def sb(name, shape, dt=F32):
                return Buf(es.enter_context(nc.sbuf_tensor(un(name), list(shape), dt)))

            def ps(name, shape, dt=F32):
                return Buf(es.enter_context(nc.psum_tensor(un(name), list(shape), dt)))
            Wst = [sb("wst%d" % i, [128, 8, 512]) for i in range(2)]
            Wb = sb("Wb", [128, 8, PROJ_W], BF16)
            zt = sb("zt", [2, PT_W])
            hf = [sb("hf%d" % i, [128, D]) for i in range(3)]
            hb = [sb("hb%d" % i, [128, D], BF16) for i in range(2)]
            aT = [sb("aT%d" % i, [128, 8, 512], BF16) for i in range(2)]
            ot = [sb("ot%d" % i, [128, PT_W]) for i in range(2)]
            of = [sb("of%d" % i, [128, 512]) for i in range(2)]
            tp = [ps("tp%d" % i, [128, 8, 128], BF16) for i in range(2)]
            po = [ps("po%d" % i, [128, 512]) for i in range(3)]
            pf = [ps("pf%d" % i, [128, 512]) for i in range(2)]
            for cb in range(4):
                st = Wst[cb % 2]
                k.dma(st[:], w_in[l, :, cb * 512:(cb + 1) * 512].rearrange("(k p) n -> p k n", p=128), W=[st])
                k.op("act", lambda: nc.scalar.copy(out=Wb[:, :, cb * 512:(cb + 1) * 512], in_=st[:]), R=[st], W=[Wb])
            k.op("dve", lambda: nc.vector.memset(zt[:], 0.0), W=[zt])
            for r0 in (0, 2 + T, PT_ROWS - 2):
                k.dma(PT[r0:r0 + 2, :], zt[:], R=[zt])
            ti = 0
            for g in range(9):
                tiles = list(range(4 * g, min(4 * g + 4, NTILE)))
                nt = len(tiles)
                j = 0 if g < 8 else 1
                A = aT[g % 2]
                for s, i in enumerate(tiles):
                    h_f = hf[ti % 3]; h_b = hb[ti % 2]; tpp = tp[ti % 2]; ti += 1
                    k.dma(h_f[:], hsrc(l, i), W=[h_f])
                    k.op("dve", lambda: nc.vector.tensor_copy(out=h_b[:], in_=h_f[:]), R=[h_f], W=[h_b])
                    for kk in range(8):
                        k.op("pe", lambda: nc.tensor.transpose(tpp[:, kk, :], h_b[:, kk * 128:(kk + 1) * 128], identb[:]),
                             R=[h_b, identb], W=[tpp])
                    for kk in range(8):
                        sc = MOD[:, l, 8 + kk, j:j + 1]; sh = MOD[:, l, kk, j:j + 1]
                        if (ti % 2) == 0:
                            k.op("act", lambda: nc.scalar.activation(out=A[:, kk, s * 128:(s + 1) * 128], in_=tpp[:, kk, :],
                                                                     func=AF.Identity, bias=sh, scale=sc), R=[tpp, MOD], W=[A])
                        else:
                            k.op("dve", lambda: nc.vector.tensor_scalar(out=A[:, kk, s * 128:(s + 1) * 128], in0=tpp[:, kk, :],
                                                                        scalar1=sc, scalar2=sh, op0=ALU.mult, op1=ALU.add),
                                 R=[tpp, MOD], W=[A])
                for s, i in enumerate(tiles):
                    o_t = ot[i % 2]
                    for nb in range(3):
                        p = po[nb]
                        for kk in range(8):
                            k.op("pe", lambda: nc.tensor.matmul(p[:, :], A[:, kk, s * 128:(s + 1) * 128], Wb[:, kk, nb * 512:(nb + 1) * 512],
                                                                start=(kk == 0), stop=(kk == 7)), R=[A, Wb], W=[p])
                        if nb == 1:
                            k.op("dve", lambda: nc.vector.tensor_copy(out=o_t[:, nb * 512:(nb + 1) * 512], in_=p[:, :]), R=[p], W=[o_t])
                        else:
                            k.op("act", lambda: nc.scalar.copy(out=o_t[:, nb * 512:(nb + 1) * 512], in_=p[:, :]), R=[p], W=[o_t])
                    r0 = pt_row(i * 128)
                    k.dma(PT[r0:r0 + 128, :], o_t[:], R=[o_t])
                n = nt * 128
                for c4 in range(4):
                    p = pf[c4 % 2]; o_f = of[c4 % 2]
                    for kk in range(8):
                        k.op("pe", lambda: nc.tensor.matmul(p[:, 0:n], Wb[:, kk, 1536 + c4 * 128:1536 + (c4 + 1) * 128], A[:, kk, 0:n],
                                                            start=(kk == 0), stop=(kk == 7)), R=[A, Wb], W=[p])
                    k.op("act" if c4 % 2 else "dve",
                         (lambda: nc.scalar.copy(out=o_f[:, 0:n], in_=p[:, 0:n])) if c4 % 2 else
                         (lambda: nc.vector.tensor_copy(out=o_f[:, 0:n], in_=p[:, 0:n])), R=[p], W=[o_f])
                    k.dma(PR[c4 * 128:(c4 + 1) * 128, g * 512:g * 512 + n], o_f[:, 0:n], R=[o_f])
            k.barrier()

    def phase_rg(l):
        NB = PT_ROWS
        LAT0, CTX0 = 2, 4 + T
        with ExitStack() as es:
            def sb(name, shape, dt=F32):
                return Buf(es.enter_context(nc.sbuf_tensor(un(name), list(shape), dt)))

            def ps(name, shape, dt=F32):
                return Buf(es.enter_context(nc.psum_tensor(un(name), list(shape), dt)))
            xp = sb("xp", [128, NB]); gp = sb("gp", [128, NTOK]); u = sb("u", [128, NB])
            Aa = sb("Aa", [128, NB]); Bv = sb("Bv", [128, NB]); HS = sb("HS", [128, NTOK]); tmp = sb("tmpS", [128, NTOK])
            ob = sb("ob", [128, NTOK], BF16)
            cw = sb("cw", [128, 4]); cbv = sb("cbv", [128, 1])
            WA = sb("WA", [128, 128]); WX = sb("WX", [128, 128])
            ba = sb("ba", [128, 1]); bx = sb("bx", [128, 1]); lam = sb("lam", [128, 1]); m8 = sb("m8", [128, 1])
            rbuf = sb("rbuf", [128, NB])
            k.op("dve", lambda: nc.vector.memset(rbuf[:], 0.0), W=[rbuf])
            k.op("dve", lambda: nc.vector.memset(Bv[:], 0.0), W=[Bv])
            k.op("dve", lambda: nc.vector.memset(u[:], 0.0), W=[u])
            pr = [ps("pr%d" % i, [128, 512]) for i in range(2)]
            pi = [ps("pi%d" % i, [128, 512]) for i in range(2)]
            segs = [(LAT0 + b * 512, 512) for b in range(8)] + [(CTX0, 256)]
            for c in range(2):
                ch0 = c * 128
                k.op("dve", lambda: nc.vector.memset(xp[:], 0.0), W=[xp])
                k.dma(xp[:, LAT0:LAT0 + T], PR[ch0:ch0 + 128, 0:T], W=[xp])
                k.dma(xp[:, CTX0:CTX0 + LC], PR[ch0:ch0 + 128, T:NTOK], W=[xp])
                k.dma(gp[:], PR[256 + ch0:256 + ch0 + 128, :], W=[gp])
                k.dma(cw[:], rg_conv_w[l, :, ch0:ch0 + 128].rearrange("j p -> p j"), W=[cw], allow_slow_non_contiguous=True)
                k.dma(cbv[:], colvec(rg_conv_b[l], ch0, 128), W=[cbv], allow_slow_non_contiguous=True)
                n = NB - 4
                k.op("dve", lambda: nc.vector.tensor_scalar(out=u[:, 2:2 + n], in0=xp[:, 1:1 + n], scalar1=cw[:, 0:1], scalar2=cbv[:, 0:1],
                                                            op0=ALU.mult, op1=ALU.add), R=[xp, cw, cbv], W=[u])
                for jj in range(1, 4):
                    k.op("dve", lambda: nc.vector.scalar_tensor_tensor(out=u[:, 2:2 + n], in0=xp[:, 1 + jj:1 + jj + n], scalar=cw[:, jj:jj + 1],
                                                                       in1=u[:, 2:2 + n], op0=ALU.mult, op1=ALU.add), R=[xp, cw, u], W=[u])
                for d in range(2):
                    k.op("dve", lambda: nc.vector.memset(WA[:], 0.0), W=[WA])
                    k.op("dve", lambda: nc.vector.memset(WX[:], 0.0), W=[WX])
                    for hh in range(2):
                        k.dma(WA[hh * 64:(hh + 1) * 64, hh * 64:(hh + 1) * 64], rg_w_a[l, d, 2 * c + hh, :, :], W=[WA])
                        k.dma(WX[hh * 64:(hh + 1) * 64, hh * 64:(hh + 1) * 64], rg_w_x[l, d, 2 * c + hh, :, :], W=[WX])
                    k.dma(ba[:], colvec(rg_b_a[l, d], ch0, 128), W=[ba], allow_slow_non_contiguous=True)
                    k.dma(bx[:], colvec(rg_b_x[l, d], ch0, 128), W=[bx], allow_slow_non_contiguous=True)
                    k.dma(lam[:], colvec(rg_lambda[l, d], ch0, 128), W=[lam], allow_slow_non_contiguous=True)
                    k.op("act", lambda: nc.scalar.activation(out=m8[:], in_=lam[:], func=AF.Exp, scale=-1.0), R=[lam], W=[m8])
                    k.op("act", lambda: nc.scalar.activation(out=m8[:], in_=m8[:], func=AF.Ln, bias=1.0), R=[m8], W=[m8])
                    k.op("dve", lambda: nc.vector.tensor_scalar(out=m8[:], in0=m8[:], scalar1=-8.0, scalar2=None, op0=ALU.mult), R=[m8], W=[m8])
                    for bi, (p0, nn) in enumerate(segs):
                        p_r = pr[bi % 2]; p_i = pi[bi % 2]
                        k.op("pe", lambda: nc.tensor.matmul(p_r[:, 0:nn], WA[:], u[:, p0:p0 + nn], start=True, stop=True), R=[WA, u], W=[p_r])
                        k.op("pe", lambda: nc.tensor.matmul(p_i[:, 0:nn], WX[:], u[:, p0:p0 + nn], start=True, stop=True), R=[WX, u], W=[p_i])
                        k.op("act", lambda: nc.scalar.activation(out=rbuf[:, p0:p0 + nn], in_=p_r[:, 0:nn], func=AF.Sigmoid, bias=ba[:, 0:1]), R=[p_r, ba], W=[rbuf])
                        k.op("act", lambda: nc.scalar.activation(out=Bv[:, p0:p0 + nn], in_=p_i[:, 0:nn], func=AF.Sigmoid, bias=bx[:, 0:1]), R=[p_i, bx], W=[Bv])
                    wl = slice(2, NB - 2)
                    k.op("act", lambda: nc.scalar.activation(out=Aa[:, wl], in_=rbuf[:, wl], func=AF.Exp, scale=m8[:, 0:1]), R=[rbuf, m8], W=[Aa])
                    k.op("dve", lambda: nc.vector.tensor_tensor(out=rbuf[:, wl], in0=Aa[:, wl], in1=Aa[:, wl], op=ALU.mult), R=[Aa], W=[rbuf])
                    k.op("dve", lambda: nc.vector.tensor_scalar(out=rbuf[:, wl], in0=rbuf[:, wl], scalar1=-1.0, scalar2=1.0, op0=ALU.mult, op1=ALU.add), R=[rbuf], W=[rbuf])
                    k.op("act", lambda: nc.scalar.activation(out=rbuf[:, wl], in_=rbuf[:, wl], func=AF.Sqrt), R=[rbuf], W=[rbuf])
                    k.op("dve", lambda: nc.vector.tensor_tensor(out=Bv[:, wl], in0=Bv[:, wl], in1=u[:, wl], op=ALU.mult), R=[Bv, u], W=[Bv])
                    k.op("dve", lambda: nc.vector.tensor_tensor(out=Bv[:, wl], in0=Bv[:, wl], in1=rbuf[:, wl], op=ALU.mult), R=[Bv, rbuf], W=[Bv])
                    dst = HS if d == 0 else tmp
                    if d == 0:
                        k.op("dve", lambda: nc.vector.tensor_tensor_scan(out=dst[:, T:NTOK], data0=Aa[:, CTX0:CTX0 + LC], data1=Bv[:, CTX0:CTX0 + LC],
                                                                         initial=0.0, op0=ALU.mult, op1=ALU.add), R=[Aa, Bv], W=[dst])
                        k.op("dve", lambda: nc.vector.tensor_tensor_scan(out=dst[:, 0:T], data0=Aa[:, LAT0:LAT0 + T], data1=Bv[:, LAT0:LAT0 + T],
                                                                         initial=dst[:, NTOK - 1:NTOK], op0=ALU.mult, op1=ALU.add), R=[Aa, Bv, dst], W=[dst])
                    else:
                        k.op("dve", lambda: nc.vector.tensor_tensor_scan(out=dst[:, T:NTOK][:, ::-1], data0=Aa[:, CTX0:CTX0 + LC][:, ::-1],
                                                                         data1=Bv[:, CTX0:CTX0 + LC][:, ::-1],
                                                                         initial=0.0, op0=ALU.mult, op1=ALU.add), R=[Aa, Bv], W=[dst])
                        k.op("dve", lambda: nc.vector.tensor_tensor_scan(out=dst[:, 0:T][:, ::-1], data0=Aa[:, LAT0:LAT0 + T][:, ::-1],
                                                                         data1=Bv[:, LAT0:LAT0 + T][:, ::-1],
                                                                         initial=dst[:, T:T + 1], op0=ALU.mult, op1=ALU.add), R=[Aa, Bv, dst], W=[dst])
                        k.op("dve", lambda: nc.vector.tensor_tensor(out=HS[:], in0=HS[:], in1=tmp[:], op=ALU.add), R=[HS, tmp], W=[HS])
                k.op("dve", lambda: nc.vector.tensor_tensor(out=tmp[:], in0=gp[:], in1=gp[:], op=ALU.mult), R=[gp], W=[tmp])
                k.op("dve", lambda: nc.vector.tensor_scalar(out=tmp[:], in0=tmp[:], scalar1=0.044715, scalar2=1.0, op0=ALU.mult, op1=ALU.add), R=[tmp], W=[tmp])
                k.op("dve", lambda: nc.vector.tensor_tensor(out=tmp[:], in0=tmp[:], in1=gp[:], op=ALU.mult), R=[tmp, gp], W=[tmp])
                k.op("act", lambda: nc.scalar.activation(out=tmp[:], in_=tmp[:], func=AF.Sigmoid, scale=2.0 * math.sqrt(2.0 / math.pi)), R=[tmp], W=[tmp])
                k.op("dve", lambda: nc.vector.tensor_tensor(out=tmp[:], in0=tmp[:], in1=gp[:], op=ALU.mult), R=[tmp, gp], W=[tmp])
                k.op("dve", lambda: nc.vector.tensor_tensor(out=ob[:], in0=tmp[:], in1=HS[:], op=ALU.mult), R=[tmp, HS], W=[ob])
                k.dma(RGT[ch0:ch0 + 128, :], ob[:], R=[ob])
            k.barrier()

    def phase_attn(l):
        with ExitStack() as es:
            def sb(name, shape, dt=F32):
                return Buf(es.enter_context(nc.sbuf_tensor(un(name), list(shape), dt)))

            def ps(name, shape, dt=F32):
                return Buf(es.enter_context(nc.psum_tensor(un(name), list(shape), dt)))
            QT = sb("QT", [64, 8, NTOK], BF16)
            KT = sb("KT", [64, 2, NTOK], BF16)
            VA = sb("VA", [128, NTILE, 2, 65], BF16)
            qg = sb("qg", [128, 64]); kg = sb("kg", [128, 64])
            qkv = [sb("qkv%d" % i, [128, 768]) for i in range(2)]
            rope = [sb("rope%d" % i, [128, 2, 256]) for i in range(2)]
            sq = sb("sq", [128, 640]); ss = sb("ss", [128, 10]); qn = sb("qn", [128, 640])
            ra = sb("ra", [128, 320]); rb = sb("rb", [128, 320])
            qr = [sb("qr%d" % i, [128, 640], BF16) for i in range(2)]
            esp = ExitStack()
            ptq = [Buf(esp.enter_context(nc.psum_tensor(un("ptq%d" % i), [64, 8, 128], BF16))) for i in range(2)]
            ptk = Buf(esp.enter_context(nc.psum_tensor(un("ptk"), [64, 2, 128], BF16)))
            k.dma(qg[:], q_norm[l].partition_broadcast(128), W=[qg])
            k.dma(kg[:], k_norm[l].partition_broadcast(128), W=[kg])
            k.op("dve", lambda: nc.vector.memset(VA[:], 1.0), W=[VA])
            for i in range(NTILE):
                t_ = qkv[i % 2]; rp = rope[i % 2]; q_r = qr[i % 2]; pq = ptq[i % 2]
                r0 = pt_row(i * 128)
                k.dma(t_[:], PT[r0:r0 + 128, 768:1536], W=[t_])
                if i < 32:
                    k.dma(rp[:], ROPE_d[i * 128:(i + 1) * 128, :, :], W=[rp])
                k.op("dve", lambda: nc.vector.tensor_tensor(out=sq[:], in0=t_[:, 0:640], in1=t_[:, 0:640], op=ALU.mult), R=[t_], W=[sq])
                k.op("dve", lambda: nc.vector.tensor_reduce(out=ss[:], in_=sq[:].rearrange("p (h d) -> p h d", d=64), axis=AX.X, op=ALU.add), R=[sq], W=[ss])
                k.op("dve", lambda: nc.vector.tensor_scalar(out=ss[:], in0=ss[:], scalar1=1.0 / 64.0, scalar2=QK_EPS, op0=ALU.mult, op1=ALU.add), R=[ss], W=[ss])
                k.op("act", lambda: nc.scalar.activation(out=ss[:], in_=ss[:], func=AF.Sqrt), R=[ss], W=[ss])
                k.op("dve", lambda: nc.vector.reciprocal(out=ss[:], in_=ss[:]), R=[ss], W=[ss])
                for hh in range(10):
                    gsrc = qg if hh < 8 else kg
                    dstt = qn if i < 32 else q_r
                    k.op("dve", lambda: nc.vector.scalar_tensor_tensor(out=dstt[:, hh * 64:(hh + 1) * 64], in0=t_[:, hh * 64:(hh + 1) * 64],
                                                                       scalar=ss[:, hh:hh + 1], in1=gsrc[:], op0=ALU.mult, op1=ALU.mult),
                         R=[t_, ss, gsrc], W=[dstt])
                if i < 32:
                    qv = qn[:].rearrange("p (n two) -> p n two", two=2)
                    ov = q_r[:].rearrange("p (n two) -> p n two", two=2)
                    u1 = qv[:, :, 0]; u2 = qv[:, :, 1]
                    cosv = None
                    for (a0, a1, c0_, c1_) in ((0, 256, 0, 256), (256, 320, 0, 64)):
                        cs = rp[:, 0, c0_:c1_]; sn = rp[:, 1, c0_:c1_]
                        k.op("dve", lambda: nc.vector.tensor_tensor(out=ra[:, a0:a1], in0=u1[:, a0:a1], in1=cs, op=ALU.mult), R=[qn, rp], W=[ra])
                        k.op("dve", lambda: nc.vector.tensor_tensor(out=rb[:, a0:a1], in0=u2[:, a0:a1], in1=sn, op=ALU.mult), R=[qn, rp], W=[rb])
                        k.op("dve", lambda: nc.vector.tensor_tensor(out=ov[:, a0:a1, 0], in0=ra[:, a0:a1], in1=rb[:, a0:a1], op=ALU.subtract), R=[ra, rb], W=[q_r])
                        k.op("dve", lambda: nc.vector.tensor_tensor(out=ra[:, a0:a1], in0=u1[:, a0:a1], in1=sn, op=ALU.mult), R=[qn, rp], W=[ra])
                        k.op("dve", lambda: nc.vector.tensor_tensor(out=rb[:, a0:a1], in0=u2[:, a0:a1], in1=cs, op=ALU.mult), R=[qn, rp], W=[rb])
                        k.op("dve", lambda: nc.vector.tensor_tensor(out=ov[:, a0:a1, 1], in0=ra[:, a0:a1], in1=rb[:, a0:a1], op=ALU.add), R=[ra, rb], W=[q_r])
                for hh in range(8):
                    k.op("pe", lambda: nc.tensor.transpose(pq[:, hh, :], q_r[:, hh * 64:(hh + 1) * 64], identb[:]), R=[q_r, identb], W=[pq])
                for hh in range(2):
                    k.op("pe", lambda: nc.tensor.transpose(ptk[:, hh, :], q_r[:, (8 + hh) * 64:(9 + hh) * 64], identb[:]), R=[q_r, identb], W=[ptk])
                k.op("act", lambda: nc.scalar.copy(out=QT[:, :, i * 128:(i + 1) * 128], in_=pq[:, :, :]), R=[pq], W=[QT])
                k.op("act", lambda: nc.scalar.copy(out=KT[:, :, i * 128:(i + 1) * 128], in_=ptk[:, :, :]), R=[ptk], W=[KT])
                k.op("dve", lambda: nc.vector.tensor_copy(out=VA[:, i, :, 0:64], in_=t_[:, 640:768].rearrange("p (h d) -> p h d", d=64)), R=[t_], W=[VA])
            k.barrier()
            esp.close()
            pS = [ps("pS%d" % i, [128, 512]) for i in range(2)]
            pO = [ps("pO%d" % i, [128, 512]) for i in range(4)]
            Pb = [sb("Pb%d" % i, [128, 512], BF16) for i in range(3)]
            rd = sb("rd", [128, 4]);
            att = [sb("att%d" % i, [128, 4, 512], BF16) for i in range(2)]
            it = 0; io = 0
            jobs = [(qs * 512, 512, list(range(NTILE))) for qs in range(8)] + [(T, 256, [32, 33])]
            for ji, (q0, nq, kchunks) in enumerate(jobs):
                at = att[ji % 2]
                nsub = nq // 128
                for h in range(8):
                    kvh = h // 4
                    io += 1
                    def emit_pv(item):
                        P_, c_, ci_ = item
                        for s in range(nsub):
                            k.op("pe", lambda: nc.tensor.matmul(pO[s][:, 0:65], P_[:, s * 128:(s + 1) * 128], VA[:, c_, kvh, :],
                                                                start=(ci_ == 0), stop=(ci_ == len(kchunks) - 1)), R=[P_, VA], W=[pO[s]])
                    prev = None
                    for ci, c in enumerate(kchunks):
                        S = pS[it % 2]; P = Pb[it % 3]; it += 1
                        k.op("pe", lambda: nc.tensor.matmul(S[:, 0:nq], KT[:, kvh, c * 128:(c + 1) * 128], QT[:, h, q0:q0 + nq], start=True, stop=True),
                             R=[KT, QT], W=[S])
                        k.op("act", lambda: nc.scalar.activation(out=P[:, 0:nq], in_=S[:, 0:nq], func=AF.Exp, scale=0.125), R=[S], W=[P])
                        if prev is not None:
                            emit_pv(prev)
                        prev = (P, c, ci)
                    emit_pv(prev)
                    for s in range(nsub):
                        k.op("dve", lambda: nc.vector.reciprocal(out=rd[:, s:s + 1], in_=pO[s][:, 64:65]), R=[pO[s]], W=[rd])
                        k.op("dve", lambda: nc.vector.tensor_scalar(out=at[:, s, h * 64:(h + 1) * 64], in0=pO[s][:, 0:64], scalar1=rd[:, s:s + 1],
                                                                    scalar2=None, op0=ALU.mult), R=[pO[s], rd], W=[at])
                for s in range(nsub):
                    k.dma(MIXTOK[q0 + s * 128:q0 + (s + 1) * 128, 256:768], at[:, s, :], R=[at])
            k.barrier()

    def hyena_seq(l, es, sb, ps, Lq, tok0, WTd, W0Id, SCd, ZTd, WINd, WIN2d, KF, tag):
        nch = Lq // 128
        slab = [sb("slab%d" % i + tag, [128, nch, 2, 128], BF16) for i in range(3)]
        scs = sb("scs" + tag, [128, nch])
        k.dma(scs[:], SCd[:, :], W=[scs])
        pX = [ps("pX%d" % i + tag, [128, 512]) for i in range(4)]
        pN = ps("pN" + tag, [1, 512])
        kfo = [sb("kfo%d" % i + tag, [128, 2, 512]) for i in range(2)]
        altr = sb("altr" + tag, [1, 128], BF16)
        k.dma(altr[:], ALT_d[:, :], W=[altr])
        si = [0]

        def load_slab(j, inv=False):
            s_ = slab[si[0] % 3]; si[0] += 1
            k.dma(s_[:], (W0Id if inv else WTd)[j, :, :, :, :], W=[s_])
            return s_
        with ExitStack() as es2:
            def sb2(name, shape, dt=F32):
                return Buf(es2.enter_context(nc.sbuf_tensor(un(name + tag), list(shape), dt)))
            KS = sb2("KS", [128, nch, 512], BF16); KDf = sb2("KD", [128, nch, 512], BF16)
            h2T = sb2("h2T", [64, Lq])
            zT = sb2("zT", [17, Lq]); w1 = sb2("w1", [17, 64]); w2 = sb2("w2", [64, 64]); w3 = sb2("w3", [64, 1024])
            fr = sb2("fr", [64, 1]); b1 = sb2("b1", [64, 1]); b2 = sb2("b2", [64, 1]); fb1 = sb2("fb1", [64, 1]); fb2 = sb2("fb2", [64, 1])
            h1T = sb2("h1T", [64, Lq]); targ = [sb2("targ%d" % i, [64, 512]) for i in range(2)]
            win = [sb2("win%d" % i, [128, 256]) for i in range(2)]; win2 = [sb2("win2%d" % i, [128, 256]) for i in range(2)]
            fa = [sb2("fa%d" % i, [128, 512]) for i in range(2)]; fb_ = [sb2("fb_%d" % i, [128, 512]) for i in range(2)]
            pm = pX
            k.dma(zT[:], ZTd[:, :], W=[zT]); k.dma(w1[:], hy_f_w1[l, :, :], W=[w1]); k.dma(w2[:], hy_f_w2[l, :, :], W=[w2])
            k.dma(w3[:], hy_f_w3[l, :, :], W=[w3])
            k.dma(fr[:], colvec(hy_f_freq[l], 0, 64), W=[fr], allow_slow_non_contiguous=True)
            k.dma(b1[:], colvec(hy_f_b1[l], 0, 64), W=[b1], allow_slow_non_contiguous=True)
            k.dma(b2[:], colvec(hy_f_b2[l], 0, 64), W=[b2], allow_slow_non_contiguous=True)
            OFF = 0.0
            kiq = sb2("kiq", [64, 512], mybir.dt.int32); kfq = sb2("kfq", [64, 512])
            for (bsrc, fdst) in ((b1, fb1), (b2, fb2)):
                k.op("dve", lambda: nc.vector.tensor_tensor(out=fdst[:], in0=bsrc[:], in1=fr[:], op=ALU.mult), R=[bsrc, fr], W=[fdst])
                k.op("dve", lambda: nc.vector.tensor_scalar(out=fdst[:], in0=fdst[:], scalar1=OFF, scalar2=None, op0=ALU.add), R=[fdst], W=[fdst])
            nblk = max(1, Lq // 512); bw = min(512, Lq)

            def sin_layer(wm, src, dstb, fbb, kdim):
                for b in range(nblk):
                    p = pm[b % 2]; ta = targ[b % 2]
                    k.op("pe", lambda: nc.tensor.matmul(p[0:64, 0:bw], wm[:], src[0:kdim, b * bw:(b + 1) * bw], start=True, stop=True), R=[wm, src], W=[p])
                    k.op("dve", lambda: nc.vector.tensor_scalar(out=ta[:, 0:bw], in0=p[0:64, 0:bw], scalar1=fr[:, 0:1], scalar2=fbb[:, 0:1],
                                                                op0=ALU.mult, op1=ALU.add), R=[p, fr, fbb], W=[ta])
                    k.op("dve", lambda: nc.vector.tensor_scalar(out=kiq[:, 0:bw], in0=ta[:, 0:bw], scalar1=1.0 / (2.0 * math.pi), scalar2=None, op0=ALU.mult), R=[ta], W=[kiq])
                    k.op("dve", lambda: nc.vector.tensor_copy(out=kfq[:, 0:bw], in_=kiq[:, 0:bw]), R=[kiq], W=[kfq])
                    k.op("dve", lambda: nc.vector.scalar_tensor_tensor(out=ta[:, 0:bw], in0=kfq[:, 0:bw], scalar=-2.0 * math.pi, in1=ta[:, 0:bw], op0=ALU.mult, op1=ALU.add), R=[kfq, ta], W=[ta])
                    k.op("dve", lambda: nc.vector.tensor_scalar(out=kfq[:, 0:bw], in0=ta[:, 0:bw], scalar1=-math.pi, scalar2=None, op0=ALU.is_lt), R=[ta], W=[kfq])
                    k.op("dve", lambda: nc.vector.scalar_tensor_tensor(out=ta[:, 0:bw], in0=kfq[:, 0:bw], scalar=2.0 * math.pi, in1=ta[:, 0:bw], op0=ALU.mult, op1=ALU.add), R=[kfq, ta], W=[ta])
                    k.op("dve", lambda: nc.vector.tensor_scalar(out=kfq[:, 0:bw], in0=ta[:, 0:bw], scalar1=math.pi, scalar2=None, op0=ALU.is_gt), R=[ta], W=[kfq])
                    k.op("dve", lambda: nc.vector.scalar_tensor_tensor(out=ta[:, 0:bw], in0=kfq[:, 0:bw], scalar=-2.0 * math.pi, in1=ta[:, 0:bw], op0=ALU.mult, op1=ALU.add), R=[kfq, ta], W=[ta])
                    k.op("dve", lambda: nc.vector.tensor_scalar(out=ta[:, 0:bw], in0=ta[:, 0:bw], scalar1=-3.1415925, scalar2=3.1415925,
                                                                op0=ALU.max, op1=ALU.min), R=[ta], W=[ta])
                    k.op("act", lambda: nc.scalar.activation(out=dstb[:, b * bw:(b + 1) * bw], in_=ta[:, 0:bw], func=AF.Sin), R=[ta], W=[dstb])
            sin_layer(w1, zT, h1T, fb1, 17)
            sin_layer(w2, h1T, h2T, fb2, 64)
            for tc in range(nch):
                wn = win[tc % 2]; wn2 = win2[tc % 2]; A_ = fa[tc % 2]; B_ = fb_[tc % 2]
                k.dma(wn[:], WINd[tc * 128:(tc + 1) * 128, :], W=[wn]); k.dma(wn2[:], WIN2d[tc * 128:(tc + 1) * 128, :], W=[wn2])
                p0 = pm[2]; p1 = pm[3]
                k.op("pe", lambda: nc.tensor.matmul(p0[:, :], h2T[:, tc * 128:(tc + 1) * 128], w3[:, 0:512], start=True, stop=True), R=[h2T, w3], W=[p0])
                k.op("pe", lambda: nc.tensor.matmul(p1[:, :], h2T[:, tc * 128:(tc + 1) * 128], w3[:, 512:1024], start=True, stop=True), R=[h2T, w3], W=[p1])
                for o in range(2):
                    k.op("dve", lambda: nc.vector.tensor_tensor(out=A_[:, o * 256:(o + 1) * 256], in0=p0[:, o * 256:(o + 1) * 256], in1=wn[:], op=ALU.mult), R=[p0, wn], W=[A_])
                    k.op("dve", lambda: nc.vector.tensor_tensor(out=B_[:, o * 256:(o + 1) * 256], in0=p1[:, o * 256:(o + 1) * 256], in1=wn2[:], op=ALU.mult), R=[p1, wn2], W=[B_])
                k.op("pool", lambda: nc.gpsimd.tensor_tensor(out=KS[:, tc, :], in0=A_[:], in1=B_[:], op=ALU.add), R=[A_, B_], W=[KS])
                k.op("pool", lambda: nc.gpsimd.tensor_tensor(out=KDf[:, tc, :], in0=A_[:], in1=B_[:], op=ALU.subtract), R=[A_, B_], W=[KDf])
            nxt = load_slab(0)
            for j in range(nch):
                s_ = nxt
                if j + 1 < nch:
                    nxt = load_slab(j + 1)
                pre = pX[(2 * j) % 4]; pim = pX[(2 * j + 1) % 4]; ko = kfo[j % 2]
                for tc in range(nch):
                    k.op("pe", lambda: nc.tensor.matmul(pre[:, :], s_[:, tc, 0, :], KS[:, tc, :], start=(tc == 0), stop=(tc == nch - 1)), R=[s_, KS], W=[pre])
                for tc in range(nch):
                    k.op("pe", lambda: nc.tensor.matmul(pim[:, :], s_[:, tc, 1, :], KDf[:, tc, :], start=(tc == 0), stop=(tc == nch - 1)), R=[s_, KDf], W=[pim])
                if j == 0:
                    for tc in range(nch):
                        k.op("pe", lambda: nc.tensor.matmul(pN[:, :], s_[:, tc, 1, 0:1], KS[:, tc, :], start=(tc == 0), stop=(tc == nch - 1)), R=[s_, KS], W=[pN])
                k.op("act", lambda: nc.scalar.activation(out=ko[:, 0, :], in_=pre[:, :], func=AF.Identity, scale=scs[:, j:j + 1]), R=[pre, scs], W=[ko])
                k.op("dve", lambda: nc.vector.tensor_scalar(out=ko[:, 1, :], in0=pim[:, :], scalar1=scs[:, j:j + 1], scalar2=None, op0=ALU.mult), R=[pim, scs], W=[ko])
                if j == 0:
                    k.op("dve", lambda: nc.vector.tensor_scalar(out=ko[0:1, 1, :], in0=pN[:, :], scalar1=scs[0:1, 0:1], scalar2=None, op0=ALU.mult), R=[pN, scs], W=[ko])
                k.dma(KF[j, :, :, :], ko[:], R=[ko])
            k.barrier()
        V = sb("V" + tag, [128, nch, 256], BF16)
        Y = sb("Y" + tag, [128, nch, 2, 256], BF16)
        wsb = sb("wsb" + tag, [128, 3, 768]); bsb = sb("bsb" + tag, [128, 768]); skb = sb("skb" + tag, [128, 2, 256])
        for jj in range(3):
            k.dma(wsb[:, jj, :], hy_short_w[l, jj, :].partition_broadcast(128), W=[wsb])
        k.dma(bsb[:], hy_short_b[l].partition_broadcast(128), W=[bsb])
        for o in range(2):
            k.dma(skb[:, o, :], hy_skip[l, o, :].partition_broadcast(128), W=[skb])
        pin = [[sb("pin%d_%d" % (a, b) + tag, [128, 768]) for b in range(3)] for a in range(2)]
        cacc = [sb("cacc%d" % i + tag, [128, 768]) for i in range(2)]
        ctmp = [sb("ctmp%d" % i + tag, [128, 768]) for i in range(2)]
        for tc in range(nch):
            tok = tok0 + tc * 128
            r0 = pt_row(tok)
            pp = pin[tc % 2]; ac = cacc[tc % 2]; tm = ctmp[tc % 2]
            for jj in range(3):
                k.dma(pp[jj][:], PT[r0 + jj - 1:r0 + jj - 1 + 128, 0:768], W=[pp[jj]])
            k.op("dve", lambda: nc.vector.tensor_tensor(out=ac[:], in0=pp[0][:], in1=wsb[:, 0, :], op=ALU.mult), R=[pp[0], wsb], W=[ac])
            k.op("pool", lambda: nc.gpsimd.tensor_tensor(out=tm[:], in0=pp[1][:], in1=wsb[:, 1, :], op=ALU.mult), R=[pp[1], wsb], W=[tm])
            k.op("dve", lambda: nc.vector.tensor_tensor(out=ac[:], in0=ac[:], in1=tm[:], op=ALU.add), R=[ac, tm], W=[ac])
            k.op("pool", lambda: nc.gpsimd.tensor_tensor(out=tm[:], in0=pp[2][:], in1=wsb[:, 2, :], op=ALU.mult), R=[pp[2], wsb], W=[tm])
            k.op("dve", lambda: nc.vector.tensor_tensor(out=ac[:], in0=ac[:], in1=tm[:], op=ALU.add), R=[ac, tm], W=[ac])
            k.op("dve", lambda: nc.vector.tensor_tensor(out=ac[:], in0=ac[:], in1=bsb[:], op=ALU.add), R=[ac, bsb], W=[ac])
            k.op("act", lambda: nc.scalar.copy(out=V[:, tc, :], in_=ac[:, 512:768]), R=[ac], W=[V])
            k.dma(HX[tok:tok + 128, :], ac[:], R=[ac])
        k.barrier()
        xs = [sb("xs%d" % i + tag, [128, 2, 256]) for i in range(2)]
        kin = [sb("kin%d" % i + tag, [128, 2, 256]) for i in range(2)]
        ta_ = [sb("ta%d" % i + tag, [128, 256]) for i in range(2)]
        tb_ = [sb("tb%d" % i + tag, [128, 256]) for i in range(2)]
        vin = [sb("vin%d" % i + tag, [128, 256]) for i in range(2)]
        gin = [sb("gin%d" % i + tag, [128, 256]) for i in range(2)]
        yv = [sb("yv%d" % i + tag, [128, 256]) for i in range(2)]
        ybf = [sb("ybf%d" % i + tag, [128, 256], BF16) for i in range(2)]
        nyq = sb("nyq" + tag, [1, 256])
        for n in range(2):
            nxt = load_slab(0)
            for j in range(nch):
                s_ = nxt
                nxt = load_slab(j + 1) if j + 1 < nch else load_slab(0, inv=True)
                pre = pX[(2 * j) % 4]; pim = pX[(2 * j + 1) % 4]
                X = xs[j % 2]; Kc = kin[j % 2]; a_ = ta_[j % 2]; b_ = tb_[j % 2]
                k.dma(Kc[:], KF[j, :, :, n * 256:(n + 1) * 256], W=[Kc])
                for tc in range(nch):
                    k.op("pe", lambda: nc.tensor.matmul(pre[:, 0:256], s_[:, tc, 0, :], V[:, tc, :], start=(tc == 0), stop=(tc == nch - 1)), R=[s_, V], W=[pre])
                for tc in range(nch):
                    k.op("pe", lambda: nc.tensor.matmul(pim[:, 0:256], s_[:, tc, 1, :], V[:, tc, :], start=(tc == 0), stop=(tc == nch - 1)), R=[s_, V], W=[pim])
                k.op("act", lambda: nc.scalar.copy(out=X[:, 0, :], in_=pre[:, 0:256]), R=[pre], W=[X])
                k.op("act", lambda: nc.scalar.copy(out=X[:, 1, :], in_=pim[:, 0:256]), R=[pim], W=[X])
                k.op("dve", lambda: nc.vector.tensor_tensor(out=a_[:], in0=X[:, 0, :], in1=Kc[:, 0, :], op=ALU.mult), R=[X, Kc], W=[a_])
                k.op("pool", lambda: nc.gpsimd.tensor_tensor(out=b_[:], in0=X[:, 1, :], in1=Kc[:, 1, :], op=ALU.mult), R=[X, Kc], W=[b_])
                k.op("dve", lambda: nc.vector.tensor_tensor(out=Y[:, j, 0, :], in0=a_[:], in1=b_[:], op=ALU.subtract), R=[a_, b_], W=[Y])
                if j == 0:
                    k.op("dve", lambda: nc.vector.tensor_copy(out=Y[0:1, j, 0, :], in_=a_[0:1, :]), R=[a_], W=[Y])
                    k.op("dve", lambda: nc.vector.tensor_copy(out=nyq[:], in_=b_[0:1, :]), R=[b_], W=[nyq])
                k.op("dve", lambda: nc.vector.tensor_tensor(out=a_[:], in0=X[:, 0, :], in1=Kc[:, 1, :], op=ALU.mult), R=[X, Kc], W=[a_])
                k.op("pool", lambda: nc.gpsimd.tensor_tensor(out=b_[:], in0=X[:, 1, :], in1=Kc[:, 0, :], op=ALU.mult), R=[X, Kc], W=[b_])
                k.op("dve", lambda: nc.vector.tensor_tensor(out=Y[:, j, 1, :], in0=a_[:], in1=b_[:], op=ALU.add), R=[a_, b_], W=[Y])
                if j == 0:
                    k.op("dve", lambda: nc.vector.tensor_copy(out=Y[0:1, j, 1, :], in_=nyq[:]), R=[nyq], W=[Y])
            for i in range(nch):
                s_ = nxt
                if i + 1 < nch:
                    nxt = load_slab(i + 1, inv=True)
                po_ = pX[i % 4]
                tok = tok0 + i * 128
                vi = vin[i % 2]; gi = gin[i % 2]; y_ = yv[i % 2]; yb = ybf[i % 2]
                if n == 0:
                    k.dma(vi[:], HX[tok:tok + 128, 512:768], W=[vi])
                    k.dma(gi[:], HX[tok:tok + 128, 0:256], W=[gi])
                else:
                    k.dma(vi[:], V1D[tok:tok + 128, :], W=[vi])
                    k.dma(gi[:], HX[tok:tok + 128, 256:512], W=[gi])
                for fc in range(nch):
                    k.op("pe", lambda: nc.tensor.matmul(po_[:, 0:256], s_[:, fc, 0, :], Y[:, fc, 0, :], start=(fc == 0), stop=False), R=[s_, Y], W=[po_])
                    k.op("pe", lambda: nc.tensor.matmul(po_[:, 0:256], s_[:, fc, 1, :], Y[:, fc, 1, :], start=False, stop=(fc == nch - 1)), R=[s_, Y], W=[po_])
                k.op("pool", lambda: nc.gpsimd.tensor_tensor(out=vi[:], in0=vi[:], in1=skb[:, n, :], op=ALU.mult), R=[vi, skb], W=[vi])
                k.op("dve", lambda: nc.vector.tensor_tensor(out=y_[:], in0=po_[:, 0:256], in1=vi[:], op=ALU.add), R=[po_, vi], W=[y_])
                if n == 0:
                    k.op("dve", lambda: nc.vector.tensor_tensor(out=y_[:], in0=y_[:], in1=gi[:], op=ALU.mult), R=[y_, gi], W=[y_])
                    k.op("act", lambda: nc.scalar.copy(out=V[:, i, :], in_=y_[:]), R=[y_], W=[V])
                    k.dma(V1D[tok:tok + 128, :], y_[:], R=[y_])
                else:
                    k.op("dve", lambda: nc.vector.tensor_tensor(out=yb[:], in0=y_[:], in1=gi[:], op=ALU.mult), R=[y_, gi], W=[yb])
                    k.dma(MIXTOK[tok:tok + 128, 0:256], yb[:], R=[yb])
            k.barrier()

    def phase_hyena(l):
        for (Lq, tok0, WTd, W0Id, SCd, ZTd, WINd, WIN2d, KF, tag) in ((T, 0, WT_d, W0I_d, SC_d, ZT_d, WIN_d, WIN2_d, KFD, "L"),
                                                                     (LC, T, WTc_d, W0Ic_d, SCc_d, ZTc_d, WINc_d, WIN2c_d, KFDc, "C")):
            with ExitStack() as es:
                def sb(name, shape, dt=F32):
                    return Buf(es.enter_context(nc.sbuf_tensor(un(name), list(shape), dt)))

                def ps(name, shape, dt=F32):
                    return Buf(es.enter_context(nc.psum_tensor(un(name), list(shape), dt)))
                hyena_seq(l, es, sb, ps, Lq, tok0, WTd, W0Id, SCd, ZTd, WINd, WIN2d, KF, tag)
            k.barrier()

    def resid_ln(sbufs, ysrc, h_t, gb, gam, bet, out_t):
        u_, st, mv = sbufs
        for hf_ in range(2):
            sl = slice(hf_ * 512, (hf_ + 1) * 512)
            yb_, yap = ysrc[hf_]
            k.op("dve", lambda: nc.vector.tensor_tensor(out=u_[:, sl], in0=yap, in1=gb[:, sl], op=ALU.mult), R=[yb_, gb], W=[u_])
        k.op("dve", lambda: nc.vector.scalar_tensor_tensor(out=u_[:], in0=h_t[:], scalar=DN_ALPHA, in1=u_[:], op0=ALU.mult, op1=ALU.add), R=[h_t, u_], W=[u_])
        for hf_ in range(2):
            k.op("dve", lambda: nc.vector.bn_stats(out=st[:, hf_, :], in_=u_[:, hf_ * 512:(hf_ + 1) * 512]), R=[u_], W=[st])
        k.op("dve", lambda: nc.vector.bn_aggr(out=mv[:, 0:2], in_=st[:].rearrange("p a b -> p (a b)")), R=[st], W=[mv])
        k.op("dve", lambda: nc.vector.tensor_scalar(out=mv[:, 1:2], in0=mv[:, 1:2], scalar1=LN_EPS, scalar2=None, op0=ALU.add), R=[mv], W=[mv])
        k.op("act", lambda: nc.scalar.activation(out=mv[:, 1:2], in_=mv[:, 1:2], func=AF.Sqrt), R=[mv], W=[mv])
        k.op("dve", lambda: nc.vector.reciprocal(out=mv[:, 1:2], in_=mv[:, 1:2]), R=[mv], W=[mv])
        k.op("dve", lambda: nc.vector.tensor_scalar(out=u_[:], in0=u_[:], scalar1=mv[:, 0:1], scalar2=mv[:, 1:2], op0=ALU.subtract, op1=ALU.mult), R=[u_, mv], W=[u_])
        k.op("pool", lambda: nc.gpsimd.tensor_tensor(out=u_[:], in0=u_[:], in1=gam[:], op=ALU.mult), R=[u_, gam], W=[u_])
        k.op("pool", lambda: nc.gpsimd.tensor_tensor(out=out_t[:], in0=u_[:], in1=bet[:], op=ALU.add), R=[u_, bet], W=[out_t])

    def phase_outproj(l):
        with ExitStack() as es:
            def sb(name, shape, dt=F32):
                return Buf(es.enter_context(nc.sbuf_tensor(un(name), list(shape), dt)))

            def ps(name, shape, dt=F32):
                return Buf(es.enter_context(nc.psum_tensor(un(name), list(shape), dt)))
            Wst = [sb("wst%d" % i, [128, 8, 512]) for i in range(2)]
            Wb = sb("Wob", [128, 8, D], BF16)
            for cb in range(2):
                st = Wst[cb]
                k.dma(st[:], w_out[l, :, cb * 512:(cb + 1) * 512].rearrange("(k p) n -> p k n", p=128), W=[st])
                k.op("act", lambda: nc.scalar.copy(out=Wb[:, :, cb * 512:(cb + 1) * 512], in_=st[:]), R=[st], W=[Wb])
            gb = [sb("gb%d" % j, [128, D]) for j in range(2)]
            gam = sb("gam", [128, D]); bet = sb("bet", [128, D])
            for j in range(2):
                k.dma(gb[j][:], GMOD[l, 0, j, :].partition_broadcast(128), W=[gb[j]])
            k.dma(gam[:], ln1_g[l].partition_broadcast(128), W=[gam]); k.dma(bet[:], ln1_b[l].partition_broadcast(128), W=[bet])
            mx = [sb("mx%d" % i, [128, 768], BF16) for i in range(2)]
            mT = [sb("mT%d" % i, [128, 8, 128], BF16) for i in range(2)]
            hf = [sb("hf%d" % i, [128, D]) for i in range(2)]
            ub = [sb("ub%d" % i, [128, D]) for i in range(2)]
            ob = [sb("obo%d" % i, [128, D]) for i in range(2)]
            st6 = [sb("st6%d" % i, [128, 2, 6]) for i in range(2)]; mv = [sb("mv%d" % i, [128, 2]) for i in range(2)]
            tp = [ps("tpo%d" % i, [128, 6, 128], BF16) for i in range(2)]
            py = [[ps("py%d_%d" % (a, b), [128, 512]) for b in range(2)] for a in range(2)]
            for i in range(NTILE):
                j = 0 if i < 32 else 1
                m_ = mx[i % 2]; mt = mT[i % 2]; h_ = hf[i % 2]; tpp = tp[i % 2]; pyy = py[i % 2]
                k.dma(m_[:], MIXTOK[i * 128:(i + 1) * 128, :], W=[m_])
                k.dma(mt[:, 6:8, :], RGT[:, i * 128:(i + 1) * 128].rearrange("(c p) n -> p c n", p=128), W=[mt])
                k.dma(h_[:], hsrc(l, i), W=[h_])
                for kk in range(6):
                    k.op("pe", lambda: nc.tensor.transpose(tpp[:, kk, :], m_[:, kk * 128:(kk + 1) * 128], identb[:]), R=[m_, identb], W=[tpp])
                k.op("act", lambda: nc.scalar.copy(out=mt[:, 0:6, :], in_=tpp[:, :, :]), R=[tpp], W=[mt])
                for nb in range(2):
                    for kk in range(8):
                        k.op("pe", lambda: nc.tensor.matmul(pyy[nb][:, :], mt[:, kk, :], Wb[:, kk, nb * 512:(nb + 1) * 512], start=(kk == 0), stop=(kk == 7)),
                             R=[mt, Wb], W=[pyy[nb]])
                o_ = ob[i % 2]
                resid_ln((ub[i % 2], st6[i % 2], mv[i % 2]), [(pyy[0], pyy[0][:, :]), (pyy[1], pyy[1][:, :])], h_, gb[j], gam, bet, o_)
                k.dma(H1D[i * 128:(i + 1) * 128, :], o_[:], R=[o_])
            k.barrier()

    def phase_moe(l, last):
        passes = [(0, 12), (12, 24), (24, 34)]
        with ExitStack() as es:
            def sb(name, shape, dt=F32):
                return Buf(es.enter_context(nc.sbuf_tensor(un(name), list(shape), dt)))

            def ps(name, shape, dt=F32):
                return Buf(es.enter_context(nc.psum_tensor(un(name), list(shape), dt)))
            yacc = sb("yacc", [128, 12, D])
            mT16 = sb("mT16", [128, 8, 1536], BF16)
            gT = sb("gT", [65, 1536], BF16)
            SEL = sb("SEL", [65, 65 * 128], BF16)
            k.dma(SEL[:], SEL_d[:, :], W=[SEL])
            wst = [sb("west%d" % i, [128, 2048]) for i in range(3)]
            wgb = [sb("wgb%d" % i, [128, 8, 256], BF16) for i in range(2)]
            wub = [sb("wub%d" % i, [128, 8, 256], BF16) for i in range(2)]
            wdb = [sb("wdb%d" % i, [128, 2, D], BF16) for i in range(2)]
            Wr = sb("Wr", [128, 8, 64]); brb = sb("brb", [128, 64])
            k.dma(Wr[:], w_router[l].rearrange("(k p) n -> p k n", p=128), W=[Wr])
            k.dma(brb[:], b_router[l].partition_broadcast(128), W=[brb])
            gb = [sb("g2b%d" % j, [128, D]) for j in range(2)]
            gam = sb("gam2", [128, D]); bet = sb("bet2", [128, D])
            for j in range(2):
                k.dma(gb[j][:], GMOD[l, 1, j, :].partition_broadcast(128), W=[gb[j]])
            k.dma(gam[:], ln2_g[l].partition_broadcast(128), W=[gam]); k.dma(bet[:], ln2_b[l].partition_broadcast(128), W=[bet])
            hf = [sb("hfm%d" % i, [128, D]) for i in range(2)]
            mT32s = [sb("mT32_%d" % i, [128, 8, 128]) for i in range(2)]
            scs_ = [sb("scr%d" % i, [128, 64]) for i in range(2)]; sels = [sb("selr%d" % i, [128, 64]) for i in range(2)]
            top8s = [sb("top8_%d" % i, [128, 8]) for i in range(2)]; dens = [sb("den%d" % i, [128, 1]) for i in range(2)]
            Gs = [sb("G%d" % i, [128, 65]) for i in range(2)]
            sS = [sb("sS%d" % i, [128, 512]) for i in range(2)]
            tS = [sb("tS%d" % i, [128, 512]) for i in range(2)]
            gbs = [sb("gbs%d" % i, [128, 512], BF16) for i in range(2)]
            hT = [[sb("hT%d_%d" % (a, b), [128, 512], BF16) for b in range(2)] for a in range(2)]
            ubs = [sb("ubm%d" % i, [128, D]) for i in range(2)]; obm = [sb("obm%d" % i, [128, D]) for i in range(2)]
            st6s = [sb("st6m%d" % i, [128, 2, 6]) for i in range(2)]; mvs = [sb("mvm%d" % i, [128, 2]) for i in range(2)]
            pg = [ps("pg%d" % i, [128, 512]) for i in range(2)]
            pu = [ps("pu%d" % i, [128, 512]) for i in range(2)]
            pgb = ps("pgb", [128, 512])
            pd = [ps("pd%d" % i, [128, 512]) for i in range(3)]
            for G in Gs:
                k.op("dve", lambda: nc.vector.memset(G[:], 1.0), W=[G])
            wsti = [0]
            for (t0, t1) in passes:
                ntile = t1 - t0
                ntok = ntile * 128
                for ii, i in enumerate(range(t0, t1)):
                    j = 0 if i < 32 else 1
                    h_ = hf[ii % 2]
                    mT32 = mT32s[0]; sc_ = scs_[0]; sel = sels[0]; top8 = top8s[0]; den = dens[0]; G = Gs[0]
                    k.dma(h_[:], H1D[i * 128:(i + 1) * 128, :], W=[h_])
                    for half in range(2):
                        pt_ = pd[half]
                        for k4 in range(4):
                            kk = half * 4 + k4
                            k.op("pe", lambda: nc.tensor.transpose(pt_[:, k4 * 128:(k4 + 1) * 128], h_[:, kk * 128:(kk + 1) * 128], identf[:]),
                                 R=[h_, identf], W=[pt_])
                        for k4 in range(4):
                            kk = half * 4 + k4
                            sc = MOD[:, l, 32 + kk, j:j + 1]; sh = MOD[:, l, 24 + kk, j:j + 1]
                            k.op("act", lambda: nc.scalar.activation(out=mT32[:, kk, :], in_=pt_[:, k4 * 128:(k4 + 1) * 128], func=AF.Identity, bias=sh, scale=sc),
                                 R=[pt_, MOD], W=[mT32])
                            k.op("dve", lambda: nc.vector.tensor_copy(out=mT16[:, kk, ii * 128:(ii + 1) * 128], in_=mT32[:, kk, :]), R=[mT32], W=[mT16])
                    pl = pd[2]
                    for kk in range(8):
                        k.op("pe", lambda: nc.tensor.matmul(pl[:, 0:64], mT32[:, kk, :], Wr[:, kk, :], start=(kk == 0), stop=(kk == 7)), R=[mT32, Wr], W=[pl])
                    k.op("act", lambda: nc.scalar.activation(out=sc_[:], in_=pl[:, 0:64], func=AF.Sigmoid), R=[pl], W=[sc_])
                    k.op("dve", lambda: nc.vector.tensor_tensor(out=sel[:], in0=sc_[:], in1=brb[:], op=ALU.add), R=[sc_, brb], W=[sel])
                    k.op("dve", lambda: nc.vector.max(out=top8[:], in_=sel[:]), R=[sel], W=[top8])
                    k.op("dve", lambda: nc.vector.tensor_scalar(out=sel[:], in0=sel[:], scalar1=top8[:, 7:8], scalar2=None, op0=ALU.is_ge), R=[sel, top8], W=[sel])
                    k.op("dve", lambda: nc.vector.tensor_tensor(out=sel[:], in0=sel[:], in1=sc_[:], op=ALU.mult), R=[sel, sc_], W=[sel])
                    k.op("dve", lambda: nc.vector.tensor_reduce(out=den[:], in_=sel[:], axis=AX.X, op=ALU.add), R=[sel], W=[den])
                    k.op("dve", lambda: nc.vector.reciprocal(out=den[:], in_=den[:]), R=[den], W=[den])
                    k.op("dve", lambda: nc.vector.tensor_scalar(out=G[:, 0:64], in0=sel[:], scalar1=den[:, 0:1], scalar2=2.5, op0=ALU.mult, op1=ALU.mult), R=[sel, den], W=[G])
                    pgt = pg[ii % 2]
                    k.op("pe", lambda: nc.tensor.transpose(pgt[0:65, 0:128], G[:, :], identf[:]), R=[G, identf], W=[pgt])
                    k.op("act", lambda: nc.scalar.copy(out=gT[:, ii * 128:(ii + 1) * 128], in_=pgt[0:65, 0:128]), R=[pgt], W=[gT])
                k.op("dve", lambda: nc.vector.memset(yacc[:], 0.0), W=[yacc])
                blocks = []
                b0 = 0
                while b0 < ntok:
                    nb_ = min(512, ntok - b0)
                    blocks.append((b0, nb_)); b0 += nb_
                pending = None
                wi = 0
                def expert_srcs(e_):
                    return ((w_gate[l, e_], w_up[l, e_], w_down[l, e_]) if e_ < 64 else (ws_gate[l], ws_up[l], ws_down[l]))

                def issue_loads(e_):
                    for mi, src in enumerate(expert_srcs(e_)):
                        st = wst[mi]
                        k.dma(st[:].rearrange("p (k n) -> p k n", n=(256 if mi < 2 else D)), src.rearrange("(k p) n -> p k n", p=128), W=[st])

                def issue_casts(e_, which):
                    for mi, dstw in enumerate((wgb[e_ % 2], wub[e_ % 2], wdb[e_ % 2])):
                        if mi not in which:
                            continue
                        st = wst[mi]
                        k.op("act", lambda: nc.scalar.copy(out=dstw[:].rearrange("p k n -> p (k n)"), in_=st[:]), R=[st], W=[dstw])
                issue_loads(0)
                issue_casts(0, (0, 1, 2))
                for e in range(65):
                    Wg_ = wgb[e % 2]; Wu_ = wub[e % 2]; Wd_ = wdb[e % 2]
                    if e + 1 < 65:
                        issue_loads(e + 1)
                    bidx = -1
                    for (bb, nb_) in blocks:
                        hpair = hT[wi % 2]; gb_ = gbs[wi % 2]; wi += 1
                        dn = down_steps(pending, pd, yacc) if pending is not None else iter(())

                        def drain(n_):
                            for _ in range(n_):
                                f_ = next(dn, None)
                                if f_ is None:
                                    return
                                f_()
                        for fc in range(2):
                            g_ = pg[fc]; u_ = pu[fc]; s_ = sS[fc]; t_ = tS[fc]
                            for kk in range(8):
                                k.op("pe", lambda: nc.tensor.matmul(g_[:, 0:nb_], Wg_[:, kk, fc * 128:(fc + 1) * 128], mT16[:, kk, bb:bb + nb_],
                                                                    start=(kk == 0), stop=(kk == 7)), R=[Wg_, mT16], W=[g_])
                            drain(2)
                            for kk in range(8):
                                k.op("pe", lambda: nc.tensor.matmul(u_[:, 0:nb_], Wu_[:, kk, fc * 128:(fc + 1) * 128], mT16[:, kk, bb:bb + nb_],
                                                                    start=(kk == 0), stop=(kk == 7)), R=[Wu_, mT16], W=[u_])
                            if fc == 0:
                                k.op("pe", lambda: nc.tensor.matmul(pgb[:, 0:nb_], SEL[:, e * 128:(e + 1) * 128], gT[:, bb:bb + nb_], start=True, stop=True),
                                     R=[SEL, gT], W=[pgb])
                                k.op("act", lambda: nc.scalar.copy(out=gb_[:, 0:nb_], in_=pgb[:, 0:nb_]), R=[pgb], W=[gb_])
                            k.op("act", lambda: nc.scalar.activation(out=s_[:, 0:nb_], in_=g_[:, 0:nb_], func=AF.Silu), R=[g_], W=[s_])
                            k.op("dve", lambda: nc.vector.tensor_tensor(out=t_[:, 0:nb_], in0=u_[:, 0:nb_], in1=s_[:, 0:nb_], op=ALU.mult), R=[u_, s_], W=[t_])
                            k.op("pool", lambda: nc.gpsimd.tensor_tensor(out=hpair[fc][:, 0:nb_], in0=t_[:, 0:nb_], in1=gb_[:, 0:nb_], op=ALU.mult),
                                 R=[t_, gb_], W=[hpair[fc]])
                            drain(2)
                        drain(100)
                        pending = (hpair, Wd_, bb, nb_)
                        bidx += 1
                        if bidx == 1 and e + 1 < 65:
                            issue_casts(e + 1, (0, 1))
                    if e + 1 < 65:
                        issue_casts(e + 1, (2,))
                for f_ in down_steps(pending, pd, yacc):
                    f_()
                for ii, i in enumerate(range(t0, t1)):
                    j = 0 if i < 32 else 1
                    h_ = hf[ii % 2]
                    o_ = obm[ii % 2]
                    k.dma(h_[:], H1D[i * 128:(i + 1) * 128, :], W=[h_])
                    resid_ln((ubs[ii % 2], st6s[ii % 2], mvs[ii % 2]), [(yacc, yacc[:, ii, 0:512]), (yacc, yacc[:, ii, 512:1024])], h_, gb[j], gam, bet, o_)
                    if last:
                        if i < 32:
                            k.dma(y_out[i * 128:(i + 1) * 128, :], o_[:], R=[o_])
                    else:
                        k.dma(HD[i * 128:(i + 1) * 128, :], o_[:], R=[o_])
            k.barrier()

    pdi = [0]

    def down_steps(item, pd, yacc):
        hpair, Wd_, bb, nb_ = item
        for sub in range(nb_ // 128):
            ti_ = (bb // 128) + sub
            for dh in range(2):
                def step(sub=sub, ti_=ti_, dh=dh):
                    p = pd[pdi[0] % 3]; pdi[0] += 1
                    for fc in range(2):
                        k.op("pe", lambda: nc.tensor.matmul(p[:, :], hpair[fc][:, sub * 128:(sub + 1) * 128], Wd_[:, fc, dh * 512:(dh + 1) * 512],
                                                            start=(fc == 0), stop=(fc == 1)), R=[hpair[fc], Wd_], W=[p])
                    k.op("dve", lambda: nc.vector.tensor_tensor(out=yacc[:, ti_, dh * 512:(dh + 1) * 512], in0=p[:, :],
                                                                in1=yacc[:, ti_, dh * 512:(dh + 1) * 512], op=ALU.add), R=[p, yacc], W=[yacc])
                yield step

    phase_mod()
    seq = []
    for l in range(L):
        seq += [("inproj", l), ("rg", l), ("attn", l), ("hyena", l), ("outproj", l), ("moe", l)]
    for (ph, l) in seq:
        if ph == "inproj":
            phase_inproj(l)
        elif ph == "rg":
            phase_rg(l)
        elif ph == "attn":
            phase_attn(l)
        elif ph == "hyena":
            phase_hyena(l)
        elif ph == "outproj":
            phase_outproj(l)
        elif ph == "moe":
            phase_moe(l, last=(l == L - 1))
        if stop_after == (ph, l):
            break
    k.barrier()
    gs.close()
    return nc


_CONST = {}


def _consts():
    if _CONST:
        return _CONST
    bf = ml_dtypes.bfloat16

    def dft_tables(Lq):
        N = 2 * Lq
        n = np.arange(Lq, dtype=np.float64)
        ang = 2.0 * np.pi * np.outer(n, n) / N
        C = np.cos(ang)
        S = -np.sin(ang)
        alt = (-1.0) ** n
        S[:, 0] = alt
        nch = Lq // 128
        W = np.stack([C, S], axis=0)
        W = W.reshape(2, nch, 128, nch, 128)
        WTt = np.ascontiguousarray(W.transpose(3, 2, 1, 0, 4)).astype(np.float32).astype(bf)
        sc = np.full((128, nch), 2.0 / N, np.float32)
        sc[0, 0] = 1.0 / N
        WTI = WTt.copy()
        WTI[0, :, :, 1, 0] = 0
        WTI[:, 0, 0, 1, :] = ((-1.0) ** np.arange(128, dtype=np.float32)).astype(bf)[None, :]
        return WTt, sc, WTI

    def filt_tables(Lq):
        t = np.linspace(0.0, 1.0, Lq, dtype=np.float32)[:, None]
        w = (2.0 * np.float32(math.pi) * np.arange(Lq, dtype=np.float32)[:, None] / np.float32(Lq)).astype(np.float32)
        bands = np.linspace(1e-4, 7, 8, dtype=np.float32)
        z = np.concatenate([t, np.cos(bands * w), -np.sin(bands * w)], axis=-1).astype(np.float32)
        mx = math.log(1e-2) / 0.3
        mn = math.log(1e-2) / 1.5
        deltas = np.abs(np.linspace(mn, mx, 256, dtype=np.float32))
        win = np.exp(-t * deltas).astype(np.float32)
        win2 = win.copy()
        win2[0, :] = 0.0
        return np.ascontiguousarray(z.T), win, win2

    WTt, sc, w0i = dft_tables(T)
    WTc, scc, w0ic = dft_tables(LC)
    zt, win, win2 = filt_tables(T)
    ztc, winc, win2c = filt_tables(LC)
    n_rows = T // 64
    row = np.repeat(np.arange(n_rows, dtype=np.float32), 64)
    col = np.tile(np.arange(64, dtype=np.float32), n_rows)
    inv_freq = (np.float32(10000.0) ** (-np.arange(0, 32, 2, dtype=np.float32) / np.float32(32))).astype(np.float32)
    ang = np.concatenate([row[:, None] * inv_freq, col[:, None] * inv_freq], axis=-1).astype(np.float32)
    rope = np.stack([np.tile(np.cos(ang), (1, 8)), np.tile(np.sin(ang), (1, 8))], axis=1).astype(np.float32)
    sel = np.zeros((65, 65, 128), np.float32)
    for e in range(65):
        sel[e, e, :] = 1.0
    _CONST.update(dict(
        identb=np.eye(128, dtype=np.float32).astype(bf), identf=np.eye(128, dtype=np.float32),
        WT=WTt, WTc=WTc, WTI=w0i, WTIc=w0ic, SC=sc, SCc=scc, ZT=zt, ZTc=ztc, WIN=win, WIN2=win2, WINc=winc, WIN2c=win2c,
        ROPE=rope, SEL=sel.reshape(65, 65 * 128).astype(bf),
        ALT=((-1.0) ** np.arange(128, dtype=np.float32)).reshape(1, 128).astype(bf)))
    return _CONST


WEIGHT_NAMES = ["w_mod", "b_mod", "ln1_g", "ln1_b", "ln2_g", "ln2_b", "w_in", "w_out", "hy_short_w", "hy_short_b",
                "hy_f_w1", "hy_f_b1", "hy_f_freq", "hy_f_w2", "hy_f_b2", "hy_f_w3", "hy_skip", "q_norm", "k_norm",
                "rg_conv_w", "rg_conv_b", "rg_lambda", "rg_w_a", "rg_b_a", "rg_w_x", "rg_b_x", "w_router", "b_router",
                "w_gate", "w_up", "w_down", "ws_gate", "ws_up", "ws_down"]


def make_in_maps(inputs, L, cores):
    cst = _consts()
    shared = {}
    for n in WEIGHT_NAMES:
        arr = np.asarray(inputs[n], dtype=np.float32)
        for l_ in range(L):
            shared["%s_%d" % (n, l_)] = np.ascontiguousarray(arr[l_])
    shared["bmT"] = np.ascontiguousarray(np.asarray(inputs["b_mod"], dtype=np.float32)[:L].reshape(L, 48, 128).transpose(2, 0, 1))
    shared.update(cst)
    c = np.asarray(inputs["c"], np.float32)
    c_ctx = np.asarray(inputs["c_ctx"], np.float32)
    maps = []
    for b in cores:
        m = dict(shared)
        m["x"] = np.ascontiguousarray(np.asarray(inputs["x"], np.float32)[b])
        m["ctx"] = np.ascontiguousarray(np.asarray(inputs["ctx"], np.float32)[b])
        two = np.stack([c[b], c_ctx], axis=-1)
        m["cc"] = np.ascontiguousarray(two.reshape(8, 128, 2).transpose(1, 0, 2))
        maps.append(m)
    return maps


_NC = {}


def kernel(**inputs):
    if "nc" not in _NC:
        _NC["nc"] = build(L=4)
    nc = _NC["nc"]
    maps = make_in_maps(inputs, 4, list(range(8)))
    res = run_bass_kernel_spmd(nc, maps, core_ids=list(range(8)))
    out = np.stack([np.asarray(r["y"], dtype=np.float32) for r in res.results], axis=0)
    return out
```

```python
from contextlib import ExitStack
import math
import numpy as np
import ml_dtypes
import concourse.bass as bass
import concourse.mybir as mybir
from concourse.bass_utils import run_bass_kernel_spmd

F32 = mybir.dt.float32
BF16 = mybir.dt.bfloat16
AF = mybir.ActivationFunctionType
ALU = mybir.AluOpType
AX = mybir.AxisListType

D = 1024
T = 4096
LC = 256
NTOK = T + LC
NTILE = NTOK // 128
PROJ_W = 2048
N_EXP = 64
DN_ALPHA = (2 * 4) ** 0.25
LN_EPS = 1e-6
QK_EPS = 1e-6
PT_ROWS = 2 + T + 2 + LC + 2
PT_W = 1536


def pt_row(tok):
    return 2 + tok if tok < T else 4 + tok


class Buf:
    __slots__ = ("t", "w", "r")

    def __init__(self, t):
        self.t = t
        self.w = None
        self.r = {}

    def __getitem__(self, idx):
        return self.t[idx]


class Res:
    __slots__ = ("w", "r")

    def __init__(self):
        self.w = None
        self.r = {}


class KB:
    NSLOT = 28

    def __init__(self, nc):
        self.nc = nc
        self.eng = {"pe": nc.tensor, "act": nc.scalar, "dve": nc.vector, "pool": nc.gpsimd, "sp": nc.sync}
        self.sem = {q: nc.alloc_semaphore("sem_" + q) for q in ("pe", "act", "dve", "pool")}
        self.cnt = {q: 0 for q in self.sem}
        self.slots = [nc.alloc_semaphore("dsl%d" % i) for i in range(self.NSLOT)]
        self.slotcnt = [0] * self.NSLOT
        self.nextslot = 0
        self.seen = {q: {} for q in self.eng}

    def _semof(self, key):
        return self.sem[key] if isinstance(key, str) else self.slots[key]

    def _wait(self, q, key, val):
        if q == "pe" and key == "pe":
            return
        if self.seen[q].get(key, 0) >= val:
            return
        self.eng[q].wait_ge(self._semof(key), val)
        self.seen[q][key] = val

    def _deps(self, q, R, W):
        for r in R:
            if r.w is not None:
                self._wait(q, *r.w)
        for w in W:
            if w.w is not None:
                self._wait(q, *w.w)
            for kk, v in w.r.items():
                self._wait(q, kk, v)

    @staticmethod
    def _mark(tok, R, W):
        kk, v = tok
        for r in R:
            if r.r.get(kk, 0) < v:
                r.r[kk] = v
        for w in W:
            w.w = tok
            w.r = {}

    def op(self, q, fn, R=(), W=()):
        self._deps(q, R, W)
        inst = fn()
        self.cnt[q] += 1
        inst.then_inc(self.sem[q], 1)
        self._mark((q, self.cnt[q]), R, W)

    def dma(self, out, in_, R=(), W=(), q="sp", **kw):
        sl = self.nextslot
        self.nextslot = (sl + 1) % self.NSLOT
        if self.slotcnt[sl]:
            self._wait(q, sl, self.slotcnt[sl])
        self._deps(q, R, W)
        self.eng[q].dma_start(out=out, in_=in_, **kw).then_inc(self.slots[sl], 16)
        self.slotcnt[sl] += 16
        self._mark((sl, self.slotcnt[sl]), R, W)

    def barrier(self):
        for q in self.eng:
            for key in self.sem:
                if self.cnt[key]:
                    self._wait(q, key, self.cnt[key])
            for sl in range(self.NSLOT):
                if self.slotcnt[sl]:
                    self._wait(q, sl, self.slotcnt[sl])


def build(L=4, dbg=False, stop_after=None):
    nc = bass.Bass("TRN2", target_bir_lowering=False)
    k = KB(nc)

    class LT:
        def __init__(self, aps):
            self.aps = aps

        def __getitem__(self, idx):
            if isinstance(idx, tuple):
                l_, rest = idx[0], idx[1:]
                return self.aps[l_][rest] if rest else self.aps[l_]
            return self.aps[idx]

    def din(name, shape, dt=F32):
        if name in WEIGHT_NAMES:
            return LT([nc.dram_tensor("%s_%d" % (name, l_), list(shape[1:]), dt, kind="ExternalInput").ap() for l_ in range(shape[0])])
        return nc.dram_tensor(name, list(shape), dt, kind="ExternalInput").ap()

    def dscr(name, shape, dt=F32):
        return nc.dram_tensor(name, list(shape), dt, kind="ExternalOutput" if dbg else "Internal").ap()

    x = din("x", [T, D])
    ctx = din("ctx", [LC, D])
    cc = din("cc", [128, 8, 2])
    w_mod = din("w_mod", [L, D, 6 * D])
    b_mod = din("b_mod", [L, 6 * D])
    bmT = din("bmT", [128, L, 48])
    ln1_g = din("ln1_g", [L, D]); ln1_b = din("ln1_b", [L, D])
    ln2_g = din("ln2_g", [L, D]); ln2_b = din("ln2_b", [L, D])
    w_in = din("w_in", [L, D, PROJ_W])
    w_out = din("w_out", [L, D, D])
    hy_short_w = din("hy_short_w", [L, 3, 768]); hy_short_b = din("hy_short_b", [L, 768])
    hy_f_w1 = din("hy_f_w1", [L, 17, 64]); hy_f_b1 = din("hy_f_b1", [L, 64])
    hy_f_freq = din("hy_f_freq", [L, 64])
    hy_f_w2 = din("hy_f_w2", [L, 64, 64]); hy_f_b2 = din("hy_f_b2", [L, 64])
    hy_f_w3 = din("hy_f_w3", [L, 64, 1024]); hy_skip = din("hy_skip", [L, 2, 256])
    q_norm = din("q_norm", [L, 64]); k_norm = din("k_norm", [L, 64])
    rg_conv_w = din("rg_conv_w", [L, 4, 256]); rg_conv_b = din("rg_conv_b", [L, 256])
    rg_lambda = din("rg_lambda", [L, 2, 256])
    rg_w_a = din("rg_w_a", [L, 2, 4, 64, 64]); rg_b_a = din("rg_b_a", [L, 2, 256])
    rg_w_x = din("rg_w_x", [L, 2, 4, 64, 64]); rg_b_x = din("rg_b_x", [L, 2, 256])
    w_router = din("w_router", [L, D, N_EXP]); b_router = din("b_router", [L, N_EXP])
    w_gate = din("w_gate", [L, N_EXP, D, 256]); w_up = din("w_up", [L, N_EXP, D, 256])
    w_down = din("w_down", [L, N_EXP, 256, D])
    ws_gate = din("ws_gate", [L, D, 256]); ws_up = din("ws_up", [L, D, 256]); ws_down = din("ws_down", [L, 256, D])
    identb_d = din("identb", [128, 128], BF16)
    identf_d = din("identf", [128, 128])
    WT_d = din("WT", [32, 128, 32, 2, 128], BF16)
    WTc_d = din("WTc", [2, 128, 2, 2, 128], BF16)
    SC_d = din("SC", [128, 32]); SCc_d = din("SCc", [128, 2])
    ZT_d = din("ZT", [17, T]); ZTc_d = din("ZTc", [17, LC])
    WIN_d = din("WIN", [T, 256]); WIN2_d = din("WIN2", [T, 256])
    WINc_d = din("WINc", [LC, 256]); WIN2c_d = din("WIN2c", [LC, 256])
    ROPE_d = din("ROPE", [T, 2, 256])
    SEL_d = din("SEL", [65, 65 * 128], BF16)
    ALT_d = din("ALT", [1, 128], BF16)
    W0I_d = din("WTI", [32, 128, 32, 2, 128], BF16)
    W0Ic_d = din("WTIc", [2, 128, 2, 2, 128], BF16)

    y_out = nc.dram_tensor("y", [T, D], F32, kind="ExternalOutput").ap()

    HD = dscr("HD", [NTOK, D])
    H1D = dscr("H1D", [NTOK, D])
    PT = dscr("PT", [PT_ROWS, PT_W])
    PR = dscr("PR", [512, NTOK])
    RGT = dscr("RGT", [256, NTOK], BF16)
    MIXTOK = dscr("MIXTOK", [NTOK, 768], BF16)
    HX = dscr("HX", [NTOK, 768])
    V1D = dscr("V1D", [NTOK, 256])
    KFD = dscr("KFD", [32, 128, 2, 512])
    KFDc = dscr("KFDc", [2, 128, 2, 512])
    GMOD = dscr("GMOD", [L, 2, 2, D])

    gs = ExitStack()
    _uid = [0]

    def un(name):
        _uid[0] += 1
        return "%s_%d" % (name, _uid[0])

    def gsb(name, shape, dt=F32):
        return Buf(gs.enter_context(nc.sbuf_tensor(un(name), list(shape), dt)))

    MOD = gsb("MOD", [128, L, 48, 2])
    identb = gsb("identb_s", [128, 128], BF16)
    identf = gsb("identf_s", [128, 128])
    k.dma(identb[:], identb_d[:, :], W=[identb])
    k.dma(identf[:], identf_d[:, :], W=[identf])

    def hsrc(l, i):
        if l == 0:
            return x[i * 128:(i + 1) * 128, :] if i < 32 else ctx[(i - 32) * 128:(i - 31) * 128, :]
        return HD[i * 128:(i + 1) * 128, :]

    def colvec(v1d, n0, n):
        return v1d[n0:n0 + n].rearrange("(p o) -> p o", o=1)

    def phase_mod():
        with ExitStack() as es:
            def sb(name, shape, dt=F32):
                return Buf(es.enter_context(nc.sbuf_tensor(un(name), list(shape), dt)))

            def ps(name, shape, dt=F32):
                return Buf(es.enter_context(nc.psum_tensor(un(name), list(shape), dt)))
            ccs = sb("ccs", [128, 8, 2]); sT = sb("sT", [128, 8, 2]); sg = sb("sg", [128, 8, 2])
            bmTs = sb("bmTs", [128, L, 48])
            wbuf = [sb("wm%d" % i, [128, 8, 1024]) for i in range(2)]
            brow = sb("brow", [2, 1024]); rows = sb("rows", [2, 1024])
            psf = [ps("psf%d" % i, [128, 8, 2]) for i in range(2)]
            psr = [ps("psr%d" % i, [2, 512]) for i in range(2)]
            k.dma(ccs[:], cc[:, :, :], W=[ccs])
            k.dma(bmTs[:], bmT[:, :, :], W=[bmTs])
            k.op("act", lambda: nc.scalar.activation(out=sg[:], in_=ccs[:], func=AF.Sigmoid), R=[ccs], W=[sg])
            k.op("dve", lambda: nc.vector.tensor_tensor(out=sT[:], in0=ccs[:], in1=sg[:], op=ALU.mult), R=[ccs, sg], W=[sT])
            it = 0
            rows2 = [rows, sb("rows_b", [2, 1024])]
            brow2 = [brow, sb("brow_b", [2, 1024])]
            for l in range(L):
                for blk in range(6):
                    wb = wbuf[it % 2]; pf = psf[it % 2]; rw = rows2[it % 2]; br = brow2[it % 2]; it += 1
                    c0 = blk * 1024
                    k.dma(wb[:], w_mod[l, :, c0:c0 + 1024].rearrange("(k p) n -> p k n", p=128), W=[wb])
                    k.dma(br[:], b_mod[l, c0:c0 + 1024].partition_broadcast(2), W=[br])
                    for nb in range(2):
                        for kk in range(8):
                            k.op("pe", lambda: nc.tensor.matmul(psr[nb][:, :], sT[:, kk, :], wb[:, kk, nb * 512:(nb + 1) * 512],
                                                                start=(kk == 0), stop=(kk == 7)), R=[wb, sT], W=[psr[nb]])
                        k.op("dve", lambda: nc.vector.tensor_tensor(out=rw[:, nb * 512:(nb + 1) * 512], in0=psr[nb][:, :],
                                                                    in1=br[:, nb * 512:(nb + 1) * 512], op=ALU.add),
                             R=[psr[nb], br], W=[rw])
                    if blk in (1, 4):
                        k.op("dve", lambda: nc.vector.tensor_scalar(out=rw[:], in0=rw[:], scalar1=1.0, scalar2=None, op0=ALU.add), R=[rw], W=[rw])
                    if blk in (2, 5):
                        k.dma(GMOD[l, 0 if blk == 2 else 1, :, :], rw[:], R=[rw])
                    for ch in range(8):
                        k.op("pe", lambda: nc.tensor.transpose(pf[:, ch, :], rw[0:2, ch * 128:(ch + 1) * 128], identf[0:2, 0:2]), R=[rw, identf], W=[pf])
                    k.op("dve", lambda: nc.vector.tensor_copy(out=MOD[:, l, blk * 8:(blk + 1) * 8, :], in_=pf[:, :, :]), R=[pf], W=[MOD])
            k.barrier()

    def phase_inproj(l):
        with ExitStack() as es:
            def sb(name, shape, dt=F32):
                return Buf(es.enter_context(nc.sbuf_tensor(un(name), list(shape), dt)))

            def ps(name, shape, dt=F32):
                return Buf(es.enter_context(nc.psum_tensor(un(name), list(shape), dt)))
            Wst = [sb("wst%d" % i, [128, 8, 512]) for i in range(2)]
            Wb = sb("Wb", [128, 8, PROJ_W], BF16)
            zt = sb("zt", [2, PT_W])
            hf = [sb("hf%d" % i, [128, D]) for i in range(3)]
            hb = [sb("hb%d" % i, [128, D], BF16) for i in range(2)]
            aT = [sb("aT%d" % i, [128, 8, 512], BF16) for i in range(2)]
            ot = [sb("ot%d" % i, [128, PT_W]) for i in range(2)]
            of = [sb("of%d" % i, [128, 512]) for i in range(2)]
            tp = [ps("tp%d" % i, [128, 8, 128], BF16) for i in range(2)]
            po = [ps("po%d" % i, [128, 512]) for i in range(3)]
            pf = [ps("pf%d" % i, [128, 512]) for i in range(2)]
            for cb in range(4):
                st = Wst[cb % 2]
                k.dma(st[:], w_in[l, :, cb * 512:(cb + 1) * 512].rearrange("(k p) n -> p k n", p=128), W=[st])
                k.op("act", lambda: nc.scalar.copy(out=Wb[:, :, cb * 512:(cb + 1) * 512], in_=st[:]), R=[st], W=[Wb])
            k.op("dve", lambda: nc.vector.memset(zt[:], 0.0), W=[zt])
            for r0 in (0, 2 + T, PT_ROWS - 2):
                k.dma(PT[r0:r0 + 2, :], zt[:], R=[zt])
            ti = 0
            for g in range(9):
                tiles = list(range(4 * g, min(4 * g + 4, NTILE)))
                nt = len(tiles)
                j = 0 if g < 8 else 1
                A = aT[g % 2]
                for s, i in enumerate(tiles):
                    h_f = hf[ti % 3]; h_b = hb[ti % 2]; tpp = tp[ti % 2]; ti += 1
                    k.dma(h_f[:], hsrc(l, i), W=[h_f])
                    k.op("dve", lambda: nc.vector.tensor_copy(out=h_b[:], in_=h_f[:]), R=[h_f], W=[h_b])
                    for kk in range(8):
                        k.op("pe", lambda: nc.tensor.transpose(tpp[:, kk, :], h_b[:, kk * 128:(kk + 1) * 128], identb[:]),
                             R=[h_b, identb], W=[tpp])
                    for kk in range(8):
                        sc = MOD[:, l, 8 + kk, j:j + 1]; sh = MOD[:, l, kk, j:j + 1]
                        if (ti % 2) == 0:
                            k.op("act", lambda: nc.scalar.activation(out=A[:, kk, s * 128:(s + 1) * 128], in_=tpp[:, kk, :],
                                                                     func=AF.Identity, bias=sh, scale=sc), R=[tpp, MOD], W=[A])
                        else:
                            k.op("dve", lambda: nc.vector.tensor_scalar(out=A[:, kk, s * 128:(s + 1) * 128], in0=tpp[:, kk, :],
                                                                        scalar1=sc, scalar2=sh, op0=ALU.mult, op1=ALU.add),
                                 R=[tpp, MOD], W=[A])
                for s, i in enumerate(tiles):
                    o_t = ot[i % 2]
                    for nb in range(3):
                        p = po[nb]
                        for kk in range(8):
                            k.op("pe", lambda: nc.tensor.matmul(p[:, :], A[:, kk, s * 128:(s + 1) * 128], Wb[:, kk, nb * 512:(nb + 1) * 512],
                                                                start=(kk == 0), stop=(kk == 7)), R=[A, Wb], W=[p])
                        if nb == 1:
                            k.op("dve", lambda: nc.vector.tensor_copy(out=o_t[:, nb * 512:(nb + 1) * 512], in_=p[:, :]), R=[p], W=[o_t])
                        else:
                            k.op("act", lambda: nc.scalar.copy(out=o_t[:, nb * 512:(nb + 1) * 512], in_=p[:, :]), R=[p], W=[o_t])
                    r0 = pt_row(i * 128)
                    k.dma(PT[r0:r0 + 128, :], o_t[:], R=[o_t])
                n = nt * 128
                for c4 in range(4):
                    p = pf[c4 % 2]; o_f = of[c4 % 2]
                    for kk in range(8):
                        k.op("pe", lambda: nc.tensor.matmul(p[:, 0:n], Wb[:, kk, 1536 + c4 * 128:1536 + (c4 + 1) * 128], A[:, kk, 0:n],
                                                            start=(kk == 0), stop=(kk == 7)), R=[A, Wb], W=[p])
                    k.op("act" if c4 % 2 else "dve",
                         (lambda: nc.scalar.copy(out=o_f[:, 0:n], in_=p[:, 0:n])) if c4 % 2 else
                         (lambda: nc.vector.tensor_copy(out=o_f[:, 0:n], in_=p[:, 0:n])), R=[p], W=[o_f])
                    k.dma(PR[c4 * 128:(c4 + 1) * 128, g * 512:g * 512 + n], o_f[:, 0:n], R=[o_f])
            k.barrier()

    def phase_rg(l):
        NB = PT_ROWS
        LAT0, CTX0 = 2, 4 + T
        with ExitStack() as es:
            def sb(name, shape, dt=F32):
                return Buf(es.enter_context(nc.sbuf_tensor(un(name), list(shape), dt)))

            def ps(name, shape, dt=F32):
                return Buf(es.enter_context(nc.psum_tensor(un(name), list(shape), dt)))
            xp = sb("xp", [128, NB]); gp = sb("gp", [128, NTOK]); u = sb("u", [128, NB])
            Aa = sb("Aa", [128, NB]); Bv = sb("Bv", [128, NB]); HS = sb("HS", [128, NTOK]); tmp = sb("tmpS", [128, NTOK])
            ob = sb("ob", [128, NTOK], BF16)
            cw = sb("cw", [128, 4]); cbv = sb("cbv", [128, 1])
            WA = sb("WA", [128, 128]); WX = sb("WX", [128, 128])
            ba = sb("ba", [128, 1]); bx = sb("bx", [128, 1]); lam = sb("lam", [128, 1]); m8 = sb("m8", [128, 1])
            rbuf = sb("rbuf", [128, NB])
            k.op("dve", lambda: nc.vector.memset(rbuf[:], 0.0), W=[rbuf])
            k.op("dve", lambda: nc.vector.memset(Bv[:], 0.0), W=[Bv])
            k.op("dve", lambda: nc.vector.memset(u[:], 0.0), W=[u])
            pr = [ps("pr%d" % i, [128, 512]) for i in range(2)]
            pi = [ps("pi%d" % i, [128, 512]) for i in range(2)]
            segs = [(LAT0 + b * 512, 512) for b in range(8)] + [(CTX0, 256)]
            for c in range(2):
                ch0 = c * 128
                k.op("dve", lambda: nc.vector.memset(xp[:], 0.0), W=[xp])
                k.dma(xp[:, LAT0:LAT0 + T], PR[ch0:ch0 + 128, 0:T], W=[xp])
                k.dma(xp[:, CTX0:CTX0 + LC], PR[ch0:ch0 + 128, T:NTOK], W=[xp])
                k.dma(gp[:], PR[256 + ch0:256 + ch0 + 128, :], W=[gp])
                k.dma(cw[:], rg_conv_w[l, :, ch0:ch0 + 128].rearrange("j p -> p j"), W=[cw], allow_slow_non_contiguous=True)
                k.dma(cbv[:], colvec(rg_conv_b[l], ch0, 128), W=[cbv], allow_slow_non_contiguous=True)
                n = NB - 4
                k.op("dve", lambda: nc.vector.tensor_scalar(out=u[:, 2:2 + n], in0=xp[:, 1:1 + n], scalar1=cw[:, 0:1], scalar2=cbv[:, 0:1],
                                                            op0=ALU.mult, op1=ALU.add), R=[xp, cw, cbv], W=[u])
                for jj in range(1, 4):
                    k.op("dve", lambda: nc.vector.scalar_tensor_tensor(out=u[:, 2:2 + n], in0=xp[:, 1 + jj:1 + jj + n], scalar=cw[:, jj:jj + 1],
                                                                       in1=u[:, 2:2 + n], op0=ALU.mult, op1=ALU.add), R=[xp, cw, u], W=[u])
                for d in range(2):
                    k.op("dve", lambda: nc.vector.memset(WA[:], 0.0), W=[WA])
                    k.op("dve", lambda: nc.vector.memset(WX[:], 0.0), W=[WX])
                    for hh in range(2):
                        k.dma(WA[hh * 64:(hh + 1) * 64, hh * 64:(hh + 1) * 64], rg_w_a[l, d, 2 * c + hh, :, :], W=[WA])
                        k.dma(WX[hh * 64:(hh + 1) * 64, hh * 64:(hh + 1) * 64], rg_w_x[l, d, 2 * c + hh, :, :], W=[WX])
                    k.dma(ba[:], colvec(rg_b_a[l, d], ch0, 128), W=[ba], allow_slow_non_contiguous=True)
                    k.dma(bx[:], colvec(rg_b_x[l, d], ch0, 128), W=[bx], allow_slow_non_contiguous=True)
                    k.dma(lam[:], colvec(rg_lambda[l, d], ch0, 128), W=[lam], allow_slow_non_contiguous=True)
                    k.op("act", lambda: nc.scalar.activation(out=m8[:], in_=lam[:], func=AF.Exp, scale=-1.0), R=[lam], W=[m8])
                    k.op("act", lambda: nc.scalar.activation(out=m8[:], in_=m8[:], func=AF.Ln, bias=1.0), R=[m8], W=[m8])
                    k.op("dve", lambda: nc.vector.tensor_scalar(out=m8[:], in0=m8[:], scalar1=-8.0, scalar2=None, op0=ALU.mult), R=[m8], W=[m8])
                    for bi, (p0, nn) in enumerate(segs):
                        p_r = pr[bi % 2]; p_i = pi[bi % 2]
                        k.op("pe", lambda: nc.tensor.matmul(p_r[:, 0:nn], WA[:], u[:, p0:p0 + nn], start=True, stop=True), R=[WA, u], W=[p_r])
                        k.op("pe", lambda: nc.tensor.matmul(p_i[:, 0:nn], WX[:], u[:, p0:p0 + nn], start=True, stop=True), R=[WX, u], W=[p_i])
                        k.op("act", lambda: nc.scalar.activation(out=rbuf[:, p0:p0 + nn], in_=p_r[:, 0:nn], func=AF.Sigmoid, bias=ba[:, 0:1]), R=[p_r, ba], W=[rbuf])
                        k.op("act", lambda: nc.scalar.activation(out=Bv[:, p0:p0 + nn], in_=p_i[:, 0:nn], func=AF.Sigmoid, bias=bx[:, 0:1]), R=[p_i, bx], W=[Bv])
                    wl = slice(2, NB - 2)
                    k.op("act", lambda: nc.scalar.activation(out=Aa[:, wl], in_=rbuf[:, wl], func=AF.Exp, scale=m8[:, 0:1]), R=[rbuf, m8], W=[Aa])
                    k.op("dve", lambda: nc.vector.tensor_tensor(out=rbuf[:, wl], in0=Aa[:, wl], in1=Aa[:, wl], op=ALU.mult), R=[Aa], W=[rbuf])
                    k.op("dve", lambda: nc.vector.tensor_scalar(out=rbuf[:, wl], in0=rbuf[:, wl], scalar1=-1.0, scalar2=1.0, op0=ALU.mult, op1=ALU.add), R=[rbuf], W=[rbuf])
                    k.op("act", lambda: nc.scalar.activation(out=rbuf[:, wl], in_=rbuf[:, wl], func=AF.Sqrt), R=[rbuf], W=[rbuf])
                    k.op("dve", lambda: nc.vector.tensor_tensor(out=Bv[:, wl], in0=Bv[:, wl], in1=u[:, wl], op=ALU.mult), R=[Bv, u], W=[Bv])
                    k.op("dve", lambda: nc.vector.tensor_tensor(out=Bv[:, wl], in0=Bv[:, wl], in1=rbuf[:, wl], op=ALU.mult), R=[Bv, rbuf], W=[Bv])
                    dst = HS if d == 0 else tmp
                    if d == 0:
                        k.op("dve", lambda: nc.vector.tensor_tensor_scan(out=dst[:, T:NTOK], data0=Aa[:, CTX0:CTX0 + LC], data1=Bv[:, CTX0:CTX0 + LC],
                                                                         initial=0.0, op0=ALU.mult, op1=ALU.add), R=[Aa, Bv], W=[dst])
                        k.op("dve", lambda: nc.vector.tensor_tensor_scan(out=dst[:, 0:T], data0=Aa[:, LAT0:LAT0 + T], data1=Bv[:, LAT0:LAT0 + T],
                                                                         initial=dst[:, NTOK - 1:NTOK], op0=ALU.mult, op1=ALU.add), R=[Aa, Bv, dst], W=[dst])
                    else:
                        k.op("dve", lambda: nc.vector.tensor_tensor_scan(out=dst[:, T:NTOK][:, ::-1], data0=Aa[:, CTX0:CTX0 + LC][:, ::-1],
                                                                         data1=Bv[:, CTX0:CTX0 + LC][:, ::-1],
                                                                         initial=0.0, op0=ALU.mult, op1=ALU.add), R=[Aa, Bv], W=[dst])
                        k.op("dve", lambda: nc.vector.tensor_tensor_scan(out=dst[:, 0:T][:, ::-1], data0=Aa[:, LAT0:LAT0 + T][:, ::-1],
                                                                         data1=Bv[:, LAT0:LAT0 + T][:, ::-1],
                                                                         initial=dst[:, T:T + 1], op0=ALU.mult, op1=ALU.add), R=[Aa, Bv, dst], W=[dst])
                        k.op("dve", lambda: nc.vector.tensor_tensor(out=HS[:], in0=HS[:], in1=tmp[:], op=ALU.add), R=[HS, tmp], W=[HS])
                k.op("dve", lambda: nc.vector.tensor_tensor(out=tmp[:], in0=gp[:], in1=gp[:], op=ALU.mult), R=[gp], W=[tmp])
                k.op("dve", lambda: nc.vector.tensor_scalar(out=tmp[:], in0=tmp[:], scalar1=0.044715, scalar2=1.0, op0=ALU.mult, op1=ALU.add), R=[tmp], W=[tmp])
                k.op("dve", lambda: nc.vector.tensor_tensor(out=tmp[:], in0=tmp[:], in1=gp[:], op=ALU.mult), R=[tmp, gp], W=[tmp])
                k.op("act", lambda: nc.scalar.activation(out=tmp[:], in_=tmp[:], func=AF.Sigmoid, scale=2.0 * math.sqrt(2.0 / math.pi)), R=[tmp], W=[tmp])
                k.op("dve", lambda: nc.vector.tensor_tensor(out=tmp[:], in0=tmp[:], in1=gp[:], op=ALU.mult), R=[tmp, gp], W=[tmp])
                k.op("dve", lambda: nc.vector.tensor_tensor(out=ob[:], in0=tmp[:], in1=HS[:], op=ALU.mult), R=[tmp, HS], W=[ob])
                k.dma(RGT[ch0:ch0 + 128, :], ob[:], R=[ob])
            k.barrier()

    def phase_attn(l):
        with ExitStack() as es:
            def sb(name, shape, dt=F32):
                return Buf(es.enter_context(nc.sbuf_tensor(un(name), list(shape), dt)))

            def ps(name, shape, dt=F32):
                return Buf(es.enter_context(nc.psum_tensor(un(name), list(shape), dt)))
            QT = sb("QT", [64, 8, NTOK], BF16)
            KT = sb("KT", [64, 2, NTOK], BF16)
            VA = sb("VA", [128, NTILE, 2, 65], BF16)
            qg = sb("qg", [128, 64]); kg = sb("kg", [128, 64])
            qkv = [sb("qkv%d" % i, [128, 768]) for i in range(2)]
            rope = [sb("rope%d" % i, [128, 2, 256]) for i in range(2)]
            sq = sb("sq", [128, 640]); ss = sb("ss", [128, 10]); qn = sb("qn", [128, 640])
            ra = sb("ra", [128, 320]); rb = sb("rb", [128, 320])
            qr = [sb("qr%d" % i, [128, 640], BF16) for i in range(2)]
            esp = ExitStack()
            ptq = [Buf(esp.enter_context(nc.psum_tensor(un("ptq%d" % i), [64, 8, 128], BF16))) for i in range(2)]
            ptk = Buf(esp.enter_context(nc.psum_tensor(un("ptk"), [64, 2, 128], BF16)))
            k.dma(qg[:], q_norm[l].partition_broadcast(128), W=[qg])
            k.dma(kg[:], k_norm[l].partition_broadcast(128), W=[kg])
            k.op("dve", lambda: nc.vector.memset(VA[:], 1.0), W=[VA])
            for i in range(NTILE):
                t_ = qkv[i % 2]; rp = rope[i % 2]; q_r = qr[i % 2]; pq = ptq[i % 2]
                r0 = pt_row(i * 128)
                k.dma(t_[:], PT[r0:r0 + 128, 768:1536], W=[t_])
                if i < 32:
                    k.dma(rp[:], ROPE_d[i * 128:(i + 1) * 128, :, :], W=[rp])
                k.op("dve", lambda: nc.vector.tensor_tensor(out=sq[:], in0=t_[:, 0:640], in1=t_[:, 0:640], op=ALU.mult), R=[t_], W=[sq])
                k.op("dve", lambda: nc.vector.tensor_reduce(out=ss[:], in_=sq[:].rearrange("p (h d) -> p h d", d=64), axis=AX.X, op=ALU.add), R=[sq], W=[ss])
                k.op("dve", lambda: nc.vector.tensor_scalar(out=ss[:], in0=ss[:], scalar1=1.0 / 64.0, scalar2=QK_EPS, op0=ALU.mult, op1=ALU.add), R=[ss], W=[ss])
                k.op("act", lambda: nc.scalar.activation(out=ss[:], in_=ss[:], func=AF.Sqrt), R=[ss], W=[ss])
                k.op("dve", lambda: nc.vector.reciprocal(out=ss[:], in_=ss[:]), R=[ss], W=[ss])
                for hh in range(10):
                    gsrc = qg if hh < 8 else kg
                    dstt = qn if i < 32 else q_r
                    k.op("dve", lambda: nc.vector.scalar_tensor_tensor(out=dstt[:, hh * 64:(hh + 1) * 64], in0=t_[:, hh * 64:(hh + 1) * 64],
                                                                       scalar=ss[:, hh:hh + 1], in1=gsrc[:], op0=ALU.mult, op1=ALU.mult),
                         R=[t_, ss, gsrc], W=[dstt])
                if i < 32:
                    qv = qn[:].rearrange("p (n two) -> p n two", two=2)
                    ov = q_r[:].rearrange("p (n two) -> p n two", two=2)
                    u1 = qv[:, :, 0]; u2 = qv[:, :, 1]
                    cosv = None
                    for (a0, a1, c0_, c1_) in ((0, 256, 0, 256), (256, 320, 0, 64)):
                        cs = rp[:, 0, c0_:c1_]; sn = rp[:, 1, c0_:c1_]
                        k.op("dve", lambda: nc.vector.tensor_tensor(out=ra[:, a0:a1], in0=u1[:, a0:a1], in1=cs, op=ALU.mult), R=[qn, rp], W=[ra])
                        k.op("dve", lambda: nc.vector.tensor_tensor(out=rb[:, a0:a1], in0=u2[:, a0:a1], in1=sn, op=ALU.mult), R=[qn, rp], W=[rb])
                        k.op("dve", lambda: nc.vector.tensor_tensor(out=ov[:, a0:a1, 0], in0=ra[:, a0:a1], in1=rb[:, a0:a1], op=ALU.subtract), R=[ra, rb], W=[q_r])
                        k.op("dve", lambda: nc.vector.tensor_tensor(out=ra[:, a0:a1], in0=u1[:, a0:a1], in1=sn, op=ALU.mult), R=[qn, rp], W=[ra])
                        k.op("dve", lambda: nc.vector.tensor_tensor(out=rb[:, a0:a1], in0=u2[:, a0:a1], in1=cs, op=ALU.mult), R=[qn, rp], W=[rb])
                        k.op("dve", lambda: nc.vector.tensor_tensor(out=ov[:, a0:a1, 1], in0=ra[:, a0:a1], in1=rb[:, a0:a1], op=ALU.add), R=[ra, rb], W=[q_r])
                for hh in range(8):
                    k.op("pe", lambda: nc.tensor.transpose(pq[:, hh, :], q_r[:, hh * 64:(hh + 1) * 64], identb[:]), R=[q_r, identb], W=[pq])
                for hh in range(2):
                    k.op("pe", lambda: nc.tensor.transpose(ptk[:, hh, :], q_r[:, (8 + hh) * 64:(9 + hh) * 64], identb[:]), R=[q_r, identb], W=[ptk])
                k.op("act", lambda: nc.scalar.copy(out=QT[:, :, i * 128:(i + 1) * 128], in_=pq[:, :, :]), R=[pq], W=[QT])
                k.op("act", lambda: nc.scalar.copy(out=KT[:, :, i * 128:(i + 1) * 128], in_=ptk[:, :, :]), R=[ptk], W=[KT])
                k.op("dve", lambda: nc.vector.tensor_copy(out=VA[:, i, :, 0:64], in_=t_[:, 640:768].rearrange("p (h d) -> p h d", d=64)), R=[t_], W=[VA])
            k.barrier()
            esp.close()
            pS = [ps("pS%d" % i, [128, 512]) for i in range(2)]
            pO = [ps("pO%d" % i, [128, 512]) for i in range(4)]
            Pb = [sb("Pb%d" % i, [128, 512], BF16) for i in range(3)]
            rd = sb("rd", [128, 4]);
            att = [sb("att%d" % i, [128, 4, 512], BF16) for i in range(2)]
            it = 0; io = 0
            jobs = [(qs * 512, 512, list(range(NTILE))) for qs in range(8)] + [(T, 256, [32, 33])]
            for ji, (q0, nq, kchunks) in enumerate(jobs):
                at = att[ji % 2]
                nsub = nq // 128
                for h in range(8):
                    kvh = h // 4
                    io += 1
                    def emit_pv(item):
                        P_, c_, ci_ = item
                        for s in range(nsub):
                            k.op("pe", lambda: nc.tensor.matmul(pO[s][:, 0:65], P_[:, s * 128:(s + 1) * 128], VA[:, c_, kvh, :],
                                                                start=(ci_ == 0), stop=(ci_ == len(kchunks) - 1)), R=[P_, VA], W=[pO[s]])
                    prev = None
                    for ci, c in enumerate(kchunks):
                        S = pS[it % 2]; P = Pb[it % 3]; it += 1
                        k.op("pe", lambda: nc.tensor.matmul(S[:, 0:nq], KT[:, kvh, c * 128:(c + 1) * 128], QT[:, h, q0:q0 + nq], start=True, stop=True),
                             R=[KT, QT], W=[S])
                        k.op("act", lambda: nc.scalar.activation(out=P[:, 0:nq], in_=S[:, 0:nq], func=AF.Exp, scale=0.125), R=[S], W=[P])
                        if prev is not None:
                            emit_pv(prev)
                        prev = (P, c, ci)
                    emit_pv(prev)
                    for s in range(nsub):
                        k.op("dve", lambda: nc.vector.reciprocal(out=rd[:, s:s + 1], in_=pO[s][:, 64:65]), R=[pO[s]], W=[rd])
                        k.op("dve", lambda: nc.vector.tensor_scalar(out=at[:, s, h * 64:(h + 1) * 64], in0=pO[s][:, 0:64], scalar1=rd[:, s:s + 1],
                                                                    scalar2=None, op0=ALU.mult), R=[pO[s], rd], W=[at])
                for s in range(nsub):
                    k.dma(MIXTOK[q0 + s * 128:q0 + (s + 1) * 128, 256:768], at[:, s, :], R=[at])
            k.barrier()

    def hyena_seq(l, es, sb, ps, Lq, tok0, WTd, W0Id, SCd, ZTd, WINd, WIN2d, KF, tag):
        nch = Lq // 128
        slab = [sb("slab%d" % i + tag, [128, nch, 2, 128], BF16) for i in range(3)]
        scs = sb("scs" + tag, [128, nch])
        k.dma(scs[:], SCd[:, :], W=[scs])
        pX = [ps("pX%d" % i + tag, [128, 512]) for i in range(4)]
        pN = ps("pN" + tag, [1, 512])
        kfo = [sb("kfo%d" % i + tag, [128, 2, 512]) for i in range(2)]
        altr = sb("altr" + tag, [1, 128], BF16)
        k.dma(altr[:], ALT_d[:, :], W=[altr])
        si = [0]

        def load_slab(j, inv=False):
            s_ = slab[si[0] % 3]; si[0] += 1
            k.dma(s_[:], (W0Id if inv else WTd)[j, :, :, :, :], W=[s_])
            return s_
        with ExitStack() as es2:
            def sb2(name, shape, dt=F32):
                return Buf(es2.enter_context(nc.sbuf_tensor(un(name + tag), list(shape), dt)))
            KS = sb2("KS", [128, nch, 512], BF16); KDf = sb2("KD", [128, nch, 512], BF16)
            h2T = sb2("h2T", [64, Lq])
            zT = sb2("zT", [17, Lq]); w1 = sb2("w1", [17, 64]); w2 = sb2("w2", [64, 64]); w3 = sb2("w3", [64, 1024])
            fr = sb2("fr", [64, 1]); b1 = sb2("b1", [64, 1]); b2 = sb2("b2", [64, 1]); fb1 = sb2("fb1", [64, 1]); fb2 = sb2("fb2", [64, 1])
            h1T = sb2("h1T", [64, Lq]); targ = [sb2("targ%d" % i, [64, 512]) for i in range(2)]
            win = [sb2("win%d" % i, [128, 256]) for i in range(2)]; win2 = [sb2("win2%d" % i, [128, 256]) for i in range(2)]
            fa = [sb2("fa%d" % i, [128, 512]) for i in range(2)]; fb_ = [sb2("fb_%d" % i, [128, 512]) for i in range(2)]
            pm = pX
            k.dma(zT[:], ZTd[:, :], W=[zT]); k.dma(w1[:], hy_f_w1[l, :, :], W=[w1]); k.dma(w2[:], hy_f_w2[l, :, :], W=[w2])
            k.dma(w3[:], hy_f_w3[l, :, :], W=[w3])
            k.dma(fr[:], colvec(hy_f_freq[l], 0, 64), W=[fr], allow_slow_non_contiguous=True)
            k.dma(b1[:], colvec(hy_f_b1[l], 0, 64), W=[b1], allow_slow_non_contiguous=True)
            k.dma(b2[:], colvec(hy_f_b2[l], 0, 64), W=[b2], allow_slow_non_contiguous=True)
            OFF = 0.0
            kiq = sb2("kiq", [64, 512], mybir.dt.int32); kfq = sb2("kfq", [64, 512])
            for (bsrc, fdst) in ((b1, fb1), (b2, fb2)):
                k.op("dve", lambda: nc.vector.tensor_tensor(out=fdst[:], in0=bsrc[:], in1=fr[:], op=ALU.mult), R=[bsrc, fr], W=[fdst])
                k.op("dve", lambda: nc.vector.tensor_scalar(out=fdst[:], in0=fdst[:], scalar1=OFF, scalar2=None, op0=ALU.add), R=[fdst], W=[fdst])
            nblk = max(1, Lq // 512); bw = min(512, Lq)

            def sin_layer(wm, src, dstb, fbb, kdim):
                for b in range(nblk):
                    p = pm[b % 2]; ta = targ[b % 2]
                    k.op("pe", lambda: nc.tensor.matmul(p[0:64, 0:bw], wm[:], src[0:kdim, b * bw:(b + 1) * bw], start=True, stop=True), R=[wm, src], W=[p])
                    k.op("dve", lambda: nc.vector.tensor_scalar(out=ta[:, 0:bw], in0=p[0:64, 0:bw], scalar1=fr[:, 0:1], scalar2=fbb[:, 0:1],
                                                                op0=ALU.mult, op1=ALU.add), R=[p, fr, fbb], W=[ta])
                    k.op("dve", lambda: nc.vector.tensor_scalar(out=kiq[:, 0:bw], in0=ta[:, 0:bw], scalar1=1.0 / (2.0 * math.pi), scalar2=None, op0=ALU.mult), R=[ta], W=[kiq])
                    k.op("dve", lambda: nc.vector.tensor_copy(out=kfq[:, 0:bw], in_=kiq[:, 0:bw]), R=[kiq], W=[kfq])
                    k.op("dve", lambda: nc.vector.scalar_tensor_tensor(out=ta[:, 0:bw], in0=kfq[:, 0:bw], scalar=-2.0 * math.pi, in1=ta[:, 0:bw], op0=ALU.mult, op1=ALU.add), R=[kfq, ta], W=[ta])
                    k.op("dve", lambda: nc.vector.tensor_scalar(out=kfq[:, 0:bw], in0=ta[:, 0:bw], scalar1=-math.pi, scalar2=None, op0=ALU.is_lt), R=[ta], W=[kfq])
                    k.op("dve", lambda: nc.vector.scalar_tensor_tensor(out=ta[:, 0:bw], in0=kfq[:, 0:bw], scalar=2.0 * math.pi, in1=ta[:, 0:bw], op0=ALU.mult, op1=ALU.add), R=[kfq, ta], W=[ta])
                    k.op("dve", lambda: nc.vector.tensor_scalar(out=kfq[:, 0:bw], in0=ta[:, 0:bw], scalar1=math.pi, scalar2=None, op0=ALU.is_gt), R=[ta], W=[kfq])
                    k.op("dve", lambda: nc.vector.scalar_tensor_tensor(out=ta[:, 0:bw], in0=kfq[:, 0:bw], scalar=-2.0 * math.pi, in1=ta[:, 0:bw], op0=ALU.mult, op1=ALU.add), R=[kfq, ta], W=[ta])
                    k.op("dve", lambda: nc.vector.tensor_scalar(out=ta[:, 0:bw], in0=ta[:, 0:bw], scalar1=-3.1415925, scalar2=3.1415925,
                                                                op0=ALU.max, op1=ALU.min), R=[ta], W=[ta])
                    k.op("act", lambda: nc.scalar.activation(out=dstb[:, b * bw:(b + 1) * bw], in_=ta[:, 0:bw], func=AF.Sin), R=[ta], W=[dstb])
            sin_layer(w1, zT, h1T, fb1, 17)
            sin_layer(w2, h1T, h2T, fb2, 64)
            for tc in range(nch):
                wn = win[tc % 2]; wn2 = win2[tc % 2]; A_ = fa[tc % 2]; B_ = fb_[tc % 2]
                k.dma(wn[:], WINd[tc * 128:(tc + 1) * 128, :], W=[wn]); k.dma(wn2[:], WIN2d[tc * 128:(tc + 1) * 128, :], W=[wn2])
                p0 = pm[2]; p1 = pm[3]
                k.op("pe", lambda: nc.tensor.matmul(p0[:, :], h2T[:, tc * 128:(tc + 1) * 128], w3[:, 0:512], start=True, stop=True), R=[h2T, w3], W=[p0])
                k.op("pe", lambda: nc.tensor.matmul(p1[:, :], h2T[:, tc * 128:(tc + 1) * 128], w3[:, 512:1024], start=True, stop=True), R=[h2T, w3], W=[p1])
                for o in range(2):
                    k.op("dve", lambda: nc.vector.tensor_tensor(out=A_[:, o * 256:(o + 1) * 256], in0=p0[:, o * 256:(o + 1) * 256], in1=wn[:], op=ALU.mult), R=[p0, wn], W=[A_])
                    k.op("dve", lambda: nc.vector.tensor_tensor(out=B_[:, o * 256:(o + 1) * 256], in0=p1[:, o * 256:(o + 1) * 256], in1=wn2[:], op=ALU.mult), R=[p1, wn2], W=[B_])
                k.op("pool", lambda: nc.gpsimd.tensor_tensor(out=KS[:, tc, :], in0=A_[:], in1=B_[:], op=ALU.add), R=[A_, B_], W=[KS])
                k.op("pool", lambda: nc.gpsimd.tensor_tensor(out=KDf[:, tc, :], in0=A_[:], in1=B_[:], op=ALU.subtract), R=[A_, B_], W=[KDf])
            nxt = load_slab(0)
            for j in range(nch):
                s_ = nxt
                if j + 1 < nch:
                    nxt = load_slab(j + 1)
                pre = pX[(2 * j) % 4]; pim = pX[(2 * j + 1) % 4]; ko = kfo[j % 2]
                for tc in range(nch):
                    k.op("pe", lambda: nc.tensor.matmul(pre[:, :], s_[:, tc, 0, :], KS[:, tc, :], start=(tc == 0), stop=(tc == nch - 1)), R=[s_, KS], W=[pre])
                for tc in range(nch):
                    k.op("pe", lambda: nc.tensor.matmul(pim[:, :], s_[:, tc, 1, :], KDf[:, tc, :], start=(tc == 0), stop=(tc == nch - 1)), R=[s_, KDf], W=[pim])
                if j == 0:
                    for tc in range(nch):
                        k.op("pe", lambda: nc.tensor.matmul(pN[:, :], s_[:, tc, 1, 0:1], KS[:, tc, :], start=(tc == 0), stop=(tc == nch - 1)), R=[s_, KS], W=[pN])
                k.op("act", lambda: nc.scalar.activation(out=ko[:, 0, :], in_=pre[:, :], func=AF.Identity, scale=scs[:, j:j + 1]), R=[pre, scs], W=[ko])
                k.op("dve", lambda: nc.vector.tensor_scalar(out=ko[:, 1, :], in0=pim[:, :], scalar1=scs[:, j:j + 1], scalar2=None, op0=ALU.mult), R=[pim, scs], W=[ko])
                if j == 0:
                    k.op("dve", lambda: nc.vector.tensor_scalar(out=ko[0:1, 1, :], in0=pN[:, :], scalar1=scs[0:1, 0:1], scalar2=None, op0=ALU.mult), R=[pN, scs], W=[ko])
                k.dma(KF[j, :, :, :], ko[:], R=[ko])
            k.barrier()
        V = sb("V" + tag, [128, nch, 256], BF16)
        Y = sb("Y" + tag, [128, nch, 2, 256], BF16)
        wsb = sb("wsb" + tag, [128, 3, 768]); bsb = sb("bsb" + tag, [128, 768]); skb = sb("skb" + tag, [128, 2, 256])
        for jj in range(3):
            k.dma(wsb[:, jj, :], hy_short_w[l, jj, :].partition_broadcast(128), W=[wsb])
        k.dma(bsb[:], hy_short_b[l].partition_broadcast(128), W=[bsb])
        for o in range(2):
            k.dma(skb[:, o, :], hy_skip[l, o, :].partition_broadcast(128), W=[skb])
        pin = [[sb("pin%d_%d" % (a, b) + tag, [128, 768]) for b in range(3)] for a in range(2)]
        cacc = [sb("cacc%d" % i + tag, [128, 768]) for i in range(2)]
        ctmp = [sb("ctmp%d" % i + tag, [128, 768]) for i in range(2)]
        ctmp2 = [sb("ctmpb%d" % i + tag, [128, 768]) for i in range(2)]
        for tc in range(nch):
            tok = tok0 + tc * 128
            r0 = pt_row(tok)
            pp = pin[tc % 2]; ac = cacc[tc % 2]; tm = ctmp[tc % 2]
            for jj in range(3):
                k.dma(pp[jj][:], PT[r0 + jj - 1:r0 + jj - 1 + 128, 0:768], W=[pp[jj]])
            tm2 = ctmp2[tc % 2]
            k.op("dve", lambda: nc.vector.tensor_tensor(out=ac[:], in0=pp[0][:], in1=wsb[:, 0, :], op=ALU.mult), R=[pp[0], wsb], W=[ac])
            k.op("pool", lambda: nc.gpsimd.tensor_tensor(out=tm[:], in0=pp[1][:], in1=wsb[:, 1, :], op=ALU.mult), R=[pp[1], wsb], W=[tm])
            k.op("pool", lambda: nc.gpsimd.tensor_tensor(out=tm2[:], in0=pp[2][:], in1=wsb[:, 2, :], op=ALU.mult), R=[pp[2], wsb], W=[tm2])
            k.op("dve", lambda: nc.vector.tensor_tensor(out=ac[:], in0=ac[:], in1=bsb[:], op=ALU.add), R=[ac, bsb], W=[ac])
            k.op("dve", lambda: nc.vector.tensor_tensor(out=ac[:], in0=ac[:], in1=tm[:], op=ALU.add), R=[ac, tm], W=[ac])
            k.op("dve", lambda: nc.vector.tensor_tensor(out=ac[:], in0=ac[:], in1=tm2[:], op=ALU.add), R=[ac, tm2], W=[ac])
            k.op("act", lambda: nc.scalar.copy(out=V[:, tc, :], in_=ac[:, 512:768]), R=[ac], W=[V])
            k.dma(HX[tok:tok + 128, :], ac[:], R=[ac])
        k.barrier()
        xs = [sb("xs%d" % i + tag, [128, 2, 256]) for i in range(2)]
        kin = [sb("kin%d" % i + tag, [128, 2, 256]) for i in range(2)]
        ta_ = [sb("ta%d" % i + tag, [128, 256]) for i in range(2)]
        tb_ = [sb("tb%d" % i + tag, [128, 256]) for i in range(2)]
        vin = [sb("vin%d" % i + tag, [128, 256]) for i in range(2)]
        gin = [sb("gin%d" % i + tag, [128, 256]) for i in range(2)]
        yv = [sb("yv%d" % i + tag, [128, 256]) for i in range(2)]
        ybf = [sb("ybf%d" % i + tag, [128, 256], BF16) for i in range(2)]
        nyq = sb("nyq" + tag, [1, 256])
        for n in range(2):
            nxt = load_slab(0)
            for j in range(nch):
                s_ = nxt
                nxt = load_slab(j + 1) if j + 1 < nch else load_slab(0, inv=True)
                pre = pX[(2 * j) % 4]; pim = pX[(2 * j + 1) % 4]
                X = xs[j % 2]; Kc = kin[j % 2]; a_ = ta_[j % 2]; b_ = tb_[j % 2]
                k.dma(Kc[:], KF[j, :, :, n * 256:(n + 1) * 256], W=[Kc])
                for tc in range(nch):
                    k.op("pe", lambda: nc.tensor.matmul(pre[:, 0:256], s_[:, tc, 0, :], V[:, tc, :], start=(tc == 0), stop=(tc == nch - 1)), R=[s_, V], W=[pre])
                for tc in range(nch):
                    k.op("pe", lambda: nc.tensor.matmul(pim[:, 0:256], s_[:, tc, 1, :], V[:, tc, :], start=(tc == 0), stop=(tc == nch - 1)), R=[s_, V], W=[pim])
                k.op("act", lambda: nc.scalar.copy(out=X[:, 0, :], in_=pre[:, 0:256]), R=[pre], W=[X])
                k.op("act", lambda: nc.scalar.copy(out=X[:, 1, :], in_=pim[:, 0:256]), R=[pim], W=[X])
                k.op("dve", lambda: nc.vector.tensor_tensor(out=a_[:], in0=X[:, 0, :], in1=Kc[:, 0, :], op=ALU.mult), R=[X, Kc], W=[a_])
                k.op("pool", lambda: nc.gpsimd.tensor_tensor(out=b_[:], in0=X[:, 1, :], in1=Kc[:, 1, :], op=ALU.mult), R=[X, Kc], W=[b_])
                k.op("dve", lambda: nc.vector.tensor_tensor(out=Y[:, j, 0, :], in0=a_[:], in1=b_[:], op=ALU.subtract), R=[a_, b_], W=[Y])
                if j == 0:
                    k.op("dve", lambda: nc.vector.tensor_copy(out=Y[0:1, j, 0, :], in_=a_[0:1, :]), R=[a_], W=[Y])
                    k.op("dve", lambda: nc.vector.tensor_copy(out=nyq[:], in_=b_[0:1, :]), R=[b_], W=[nyq])
                k.op("dve", lambda: nc.vector.tensor_tensor(out=a_[:], in0=X[:, 0, :], in1=Kc[:, 1, :], op=ALU.mult), R=[X, Kc], W=[a_])
                k.op("pool", lambda: nc.gpsimd.tensor_tensor(out=b_[:], in0=X[:, 1, :], in1=Kc[:, 0, :], op=ALU.mult), R=[X, Kc], W=[b_])
                k.op("dve", lambda: nc.vector.tensor_tensor(out=Y[:, j, 1, :], in0=a_[:], in1=b_[:], op=ALU.add), R=[a_, b_], W=[Y])
                if j == 0:
                    k.op("dve", lambda: nc.vector.tensor_copy(out=Y[0:1, j, 1, :], in_=nyq[:]), R=[nyq], W=[Y])
            for i in range(nch):
                s_ = nxt
                if i + 1 < nch:
                    nxt = load_slab(i + 1, inv=True)
                po_ = pX[i % 4]
                tok = tok0 + i * 128
                vi = vin[i % 2]; gi = gin[i % 2]; y_ = yv[i % 2]; yb = ybf[i % 2]
                if n == 0:
                    k.dma(vi[:], HX[tok:tok + 128, 512:768], W=[vi])
                    k.dma(gi[:], HX[tok:tok + 128, 0:256], W=[gi])
                else:
                    k.dma(vi[:], V1D[tok:tok + 128, :], W=[vi])
                    k.dma(gi[:], HX[tok:tok + 128, 256:512], W=[gi])
                for fc in range(nch):
                    k.op("pe", lambda: nc.tensor.matmul(po_[:, 0:256], s_[:, fc, 0, :], Y[:, fc, 0, :], start=(fc == 0), stop=False), R=[s_, Y], W=[po_])
                    k.op("pe", lambda: nc.tensor.matmul(po_[:, 0:256], s_[:, fc, 1, :], Y[:, fc, 1, :], start=False, stop=(fc == nch - 1)), R=[s_, Y], W=[po_])
                k.op("pool", lambda: nc.gpsimd.tensor_tensor(out=vi[:], in0=vi[:], in1=skb[:, n, :], op=ALU.mult), R=[vi, skb], W=[vi])
                k.op("dve", lambda: nc.vector.tensor_tensor(out=y_[:], in0=po_[:, 0:256], in1=vi[:], op=ALU.add), R=[po_, vi], W=[y_])
                if n == 0:
                    k.op("dve", lambda: nc.vector.tensor_tensor(out=y_[:], in0=y_[:], in1=gi[:], op=ALU.mult), R=[y_, gi], W=[y_])
                    k.op("act", lambda: nc.scalar.copy(out=V[:, i, :], in_=y_[:]), R=[y_], W=[V])
                    k.dma(V1D[tok:tok + 128, :], y_[:], R=[y_])
                else:
                    k.op("dve", lambda: nc.vector.tensor_tensor(out=yb[:], in0=y_[:], in1=gi[:], op=ALU.mult), R=[y_, gi], W=[yb])
                    k.dma(MIXTOK[tok:tok + 128, 0:256], yb[:], R=[yb])
            k.barrier()

    def phase_hyena(l):
        for (Lq, tok0, WTd, W0Id, SCd, ZTd, WINd, WIN2d, KF, tag) in ((T, 0, WT_d, W0I_d, SC_d, ZT_d, WIN_d, WIN2_d, KFD, "L"),
                                                                     (LC, T, WTc_d, W0Ic_d, SCc_d, ZTc_d, WINc_d, WIN2c_d, KFDc, "C")):
            with ExitStack() as es:
                def sb(name, shape, dt=F32):
                    return Buf(es.enter_context(nc.sbuf_tensor(un(name), list(shape), dt)))

                def ps(name, shape, dt=F32):
                    return Buf(es.enter_context(nc.psum_tensor(un(name), list(shape), dt)))
                hyena_seq(l, es, sb, ps, Lq, tok0, WTd, W0Id, SCd, ZTd, WINd, WIN2d, KF, tag)
            k.barrier()

    def resid_ln(sbufs, ysrc, h_t, gb, gam, bet, out_t):
        u_, st, mv = sbufs
        for hf_ in range(2):
            sl = slice(hf_ * 512, (hf_ + 1) * 512)
            yb_, yap = ysrc[hf_]
            k.op("dve", lambda: nc.vector.tensor_tensor(out=u_[:, sl], in0=yap, in1=gb[:, sl], op=ALU.mult), R=[yb_, gb], W=[u_])
        k.op("dve", lambda: nc.vector.scalar_tensor_tensor(out=u_[:], in0=h_t[:], scalar=DN_ALPHA, in1=u_[:], op0=ALU.mult, op1=ALU.add), R=[h_t, u_], W=[u_])
        for hf_ in range(2):
            k.op("dve", lambda: nc.vector.bn_stats(out=st[:, hf_, :], in_=u_[:, hf_ * 512:(hf_ + 1) * 512]), R=[u_], W=[st])
        k.op("dve", lambda: nc.vector.bn_aggr(out=mv[:, 0:2], in_=st[:].rearrange("p a b -> p (a b)")), R=[st], W=[mv])
        k.op("dve", lambda: nc.vector.tensor_scalar(out=mv[:, 1:2], in0=mv[:, 1:2], scalar1=LN_EPS, scalar2=None, op0=ALU.add), R=[mv], W=[mv])
        k.op("act", lambda: nc.scalar.activation(out=mv[:, 1:2], in_=mv[:, 1:2], func=AF.Sqrt), R=[mv], W=[mv])
        k.op("dve", lambda: nc.vector.reciprocal(out=mv[:, 1:2], in_=mv[:, 1:2]), R=[mv], W=[mv])
        k.op("dve", lambda: nc.vector.tensor_scalar(out=u_[:], in0=u_[:], scalar1=mv[:, 0:1], scalar2=mv[:, 1:2], op0=ALU.subtract, op1=ALU.mult), R=[u_, mv], W=[u_])
        k.op("pool", lambda: nc.gpsimd.tensor_tensor(out=u_[:], in0=u_[:], in1=gam[:], op=ALU.mult), R=[u_, gam], W=[u_])
        k.op("pool", lambda: nc.gpsimd.tensor_tensor(out=out_t[:], in0=u_[:], in1=bet[:], op=ALU.add), R=[u_, bet], W=[out_t])

    def phase_outproj(l):
        with ExitStack() as es:
            def sb(name, shape, dt=F32):
                return Buf(es.enter_context(nc.sbuf_tensor(un(name), list(shape), dt)))

            def ps(name, shape, dt=F32):
                return Buf(es.enter_context(nc.psum_tensor(un(name), list(shape), dt)))
            Wst = [sb("wst%d" % i, [128, 8, 512]) for i in range(2)]
            Wb = sb("Wob", [128, 8, D], BF16)
            for cb in range(2):
                st = Wst[cb]
                k.dma(st[:], w_out[l, :, cb * 512:(cb + 1) * 512].rearrange("(k p) n -> p k n", p=128), W=[st])
                k.op("act", lambda: nc.scalar.copy(out=Wb[:, :, cb * 512:(cb + 1) * 512], in_=st[:]), R=[st], W=[Wb])
            gb = [sb("gb%d" % j, [128, D]) for j in range(2)]
            gam = sb("gam", [128, D]); bet = sb("bet", [128, D])
            for j in range(2):
                k.dma(gb[j][:], GMOD[l, 0, j, :].partition_broadcast(128), W=[gb[j]])
            k.dma(gam[:], ln1_g[l].partition_broadcast(128), W=[gam]); k.dma(bet[:], ln1_b[l].partition_broadcast(128), W=[bet])
            mx = [sb("mx%d" % i, [128, 768], BF16) for i in range(2)]
            mT = [sb("mT%d" % i, [128, 8, 128], BF16) for i in range(2)]
            hf = [sb("hf%d" % i, [128, D]) for i in range(2)]
            ub = [sb("ub%d" % i, [128, D]) for i in range(2)]
            ob = [sb("obo%d" % i, [128, D]) for i in range(2)]
            st6 = [sb("st6%d" % i, [128, 2, 6]) for i in range(2)]; mv = [sb("mv%d" % i, [128, 2]) for i in range(2)]
            tp = [ps("tpo%d" % i, [128, 6, 128], BF16) for i in range(2)]
            py = [[ps("py%d_%d" % (a, b), [128, 512]) for b in range(2)] for a in range(2)]
            for i in range(NTILE):
                j = 0 if i < 32 else 1
                m_ = mx[i % 2]; mt = mT[i % 2]; h_ = hf[i % 2]; tpp = tp[i % 2]; pyy = py[i % 2]
                k.dma(m_[:], MIXTOK[i * 128:(i + 1) * 128, :], W=[m_])
                k.dma(mt[:, 6:8, :], RGT[:, i * 128:(i + 1) * 128].rearrange("(c p) n -> p c n", p=128), W=[mt])
                k.dma(h_[:], hsrc(l, i), W=[h_])
                for kk in range(6):
                    k.op("pe", lambda: nc.tensor.transpose(tpp[:, kk, :], m_[:, kk * 128:(kk + 1) * 128], identb[:]), R=[m_, identb], W=[tpp])
                k.op("act", lambda: nc.scalar.copy(out=mt[:, 0:6, :], in_=tpp[:, :, :]), R=[tpp], W=[mt])
                for nb in range(2):
                    for kk in range(8):
                        k.op("pe", lambda: nc.tensor.matmul(pyy[nb][:, :], mt[:, kk, :], Wb[:, kk, nb * 512:(nb + 1) * 512], start=(kk == 0), stop=(kk == 7)),
                             R=[mt, Wb], W=[pyy[nb]])
                o_ = ob[i % 2]
                resid_ln((ub[i % 2], st6[i % 2], mv[i % 2]), [(pyy[0], pyy[0][:, :]), (pyy[1], pyy[1][:, :])], h_, gb[j], gam, bet, o_)
                k.dma(H1D[i * 128:(i + 1) * 128, :], o_[:], R=[o_])
            k.barrier()

    def phase_moe(l, last):
        passes = [(0, 12), (12, 24), (24, 34)]
        with ExitStack() as es:
            def sb(name, shape, dt=F32):
                return Buf(es.enter_context(nc.sbuf_tensor(un(name), list(shape), dt)))

            def ps(name, shape, dt=F32):
                return Buf(es.enter_context(nc.psum_tensor(un(name), list(shape), dt)))
            yacc = sb("yacc", [128, 12, D])
            mT16 = sb("mT16", [128, 8, 1536], BF16)
            gT = sb("gT", [65, 1536], BF16)
            SEL = sb("SEL", [65, 65 * 128], BF16)
            k.dma(SEL[:], SEL_d[:, :], W=[SEL])
            wst = [sb("west%d" % i, [128, 2048]) for i in range(3)]
            wgb = [sb("wgb%d" % i, [128, 8, 256], BF16) for i in range(2)]
            wub = [sb("wub%d" % i, [128, 8, 256], BF16) for i in range(2)]
            wdb = [sb("wdb%d" % i, [128, 2, D], BF16) for i in range(2)]
            Wr = sb("Wr", [128, 8, 64]); brb = sb("brb", [128, 64])
            k.dma(Wr[:], w_router[l].rearrange("(k p) n -> p k n", p=128), W=[Wr])
            k.dma(brb[:], b_router[l].partition_broadcast(128), W=[brb])
            gb = [sb("g2b%d" % j, [128, D]) for j in range(2)]
            gam = sb("gam2", [128, D]); bet = sb("bet2", [128, D])
            for j in range(2):
                k.dma(gb[j][:], GMOD[l, 1, j, :].partition_broadcast(128), W=[gb[j]])
            k.dma(gam[:], ln2_g[l].partition_broadcast(128), W=[gam]); k.dma(bet[:], ln2_b[l].partition_broadcast(128), W=[bet])
            hf = [sb("hfm%d" % i, [128, D]) for i in range(2)]
            mT32s = [sb("mT32_%d" % i, [128, 8, 128]) for i in range(2)]
            scs_ = [sb("scr%d" % i, [128, 64]) for i in range(2)]; sels = [sb("selr%d" % i, [128, 64]) for i in range(2)]
            top8s = [sb("top8_%d" % i, [128, 8]) for i in range(2)]; dens = [sb("den%d" % i, [128, 1]) for i in range(2)]
            Gs = [sb("G%d" % i, [128, 65]) for i in range(2)]
            sS = [sb("sS%d" % i, [128, 512]) for i in range(2)]
            tS = [sb("tS%d" % i, [128, 512]) for i in range(2)]
            gbs = [sb("gbs%d" % i, [128, 512], BF16) for i in range(2)]
            hT = [[sb("hT%d_%d" % (a, b), [128, 512], BF16) for b in range(2)] for a in range(2)]
            ubs = [sb("ubm%d" % i, [128, D]) for i in range(2)]; obm = [sb("obm%d" % i, [128, D]) for i in range(2)]
            st6s = [sb("st6m%d" % i, [128, 2, 6]) for i in range(2)]; mvs = [sb("mvm%d" % i, [128, 2]) for i in range(2)]
            pg = [ps("pg%d" % i, [128, 512]) for i in range(2)]
            pu = [ps("pu%d" % i, [128, 512]) for i in range(2)]
            pgb = ps("pgb", [128, 512])
            pd = [ps("pd%d" % i, [128, 512]) for i in range(3)]
            for G in Gs:
                k.op("dve", lambda: nc.vector.memset(G[:], 1.0), W=[G])
            wsti = [0]
            for (t0, t1) in passes:
                ntile = t1 - t0
                ntok = ntile * 128
                for ii, i in enumerate(range(t0, t1)):
                    j = 0 if i < 32 else 1
                    h_ = hf[ii % 2]
                    mT32 = mT32s[0]; sc_ = scs_[0]; sel = sels[0]; top8 = top8s[0]; den = dens[0]; G = Gs[0]
                    k.dma(h_[:], H1D[i * 128:(i + 1) * 128, :], W=[h_])
                    for half in range(2):
                        pt_ = pd[half]
                        for k4 in range(4):
                            kk = half * 4 + k4
                            k.op("pe", lambda: nc.tensor.transpose(pt_[:, k4 * 128:(k4 + 1) * 128], h_[:, kk * 128:(kk + 1) * 128], identf[:]),
                                 R=[h_, identf], W=[pt_])
                        for k4 in range(4):
                            kk = half * 4 + k4
                            sc = MOD[:, l, 32 + kk, j:j + 1]; sh = MOD[:, l, 24 + kk, j:j + 1]
                            k.op("act", lambda: nc.scalar.activation(out=mT32[:, kk, :], in_=pt_[:, k4 * 128:(k4 + 1) * 128], func=AF.Identity, bias=sh, scale=sc),
                                 R=[pt_, MOD], W=[mT32])
                            k.op("dve", lambda: nc.vector.tensor_copy(out=mT16[:, kk, ii * 128:(ii + 1) * 128], in_=mT32[:, kk, :]), R=[mT32], W=[mT16])
                    pl = pd[2]
                    for kk in range(8):
                        k.op("pe", lambda: nc.tensor.matmul(pl[:, 0:64], mT32[:, kk, :], Wr[:, kk, :], start=(kk == 0), stop=(kk == 7)), R=[mT32, Wr], W=[pl])
                    k.op("act", lambda: nc.scalar.activation(out=sc_[:], in_=pl[:, 0:64], func=AF.Sigmoid), R=[pl], W=[sc_])
                    k.op("dve", lambda: nc.vector.tensor_tensor(out=sel[:], in0=sc_[:], in1=brb[:], op=ALU.add), R=[sc_, brb], W=[sel])
                    k.op("dve", lambda: nc.vector.max(out=top8[:], in_=sel[:]), R=[sel], W=[top8])
                    k.op("dve", lambda: nc.vector.tensor_scalar(out=sel[:], in0=sel[:], scalar1=top8[:, 7:8], scalar2=None, op0=ALU.is_ge), R=[sel, top8], W=[sel])
                    k.op("dve", lambda: nc.vector.tensor_tensor(out=sel[:], in0=sel[:], in1=sc_[:], op=ALU.mult), R=[sel, sc_], W=[sel])
                    k.op("dve", lambda: nc.vector.tensor_reduce(out=den[:], in_=sel[:], axis=AX.X, op=ALU.add), R=[sel], W=[den])
                    k.op("dve", lambda: nc.vector.reciprocal(out=den[:], in_=den[:]), R=[den], W=[den])
                    k.op("dve", lambda: nc.vector.tensor_scalar(out=G[:, 0:64], in0=sel[:], scalar1=den[:, 0:1], scalar2=2.5, op0=ALU.mult, op1=ALU.mult), R=[sel, den], W=[G])
                    pgt = pg[ii % 2]
                    k.op("pe", lambda: nc.tensor.transpose(pgt[0:65, 0:128], G[:, :], identf[:]), R=[G, identf], W=[pgt])
                    k.op("act", lambda: nc.scalar.copy(out=gT[:, ii * 128:(ii + 1) * 128], in_=pgt[0:65, 0:128]), R=[pgt], W=[gT])
                k.op("dve", lambda: nc.vector.memset(yacc[:], 0.0), W=[yacc])
                blocks = []
                b0 = 0
                while b0 < ntok:
                    nb_ = min(512, ntok - b0)
                    blocks.append((b0, nb_)); b0 += nb_
                pending = None
                wi = 0
                def expert_srcs(e_):
                    return ((w_gate[l, e_], w_up[l, e_], w_down[l, e_]) if e_ < 64 else (ws_gate[l], ws_up[l], ws_down[l]))

                def issue_loads(e_):
                    for mi, src in enumerate(expert_srcs(e_)):
                        st = wst[mi]
                        k.dma(st[:].rearrange("p (k n) -> p k n", n=(256 if mi < 2 else D)), src.rearrange("(k p) n -> p k n", p=128), W=[st])

                def issue_casts(e_, which):
                    for mi, dstw in enumerate((wgb[e_ % 2], wub[e_ % 2], wdb[e_ % 2])):
                        if mi not in which:
                            continue
                        st = wst[mi]
                        k.op("act", lambda: nc.scalar.copy(out=dstw[:].rearrange("p k n -> p (k n)"), in_=st[:]), R=[st], W=[dstw])
                issue_loads(0)
                issue_casts(0, (0, 1, 2))
                for e in range(65):
                    Wg_ = wgb[e % 2]; Wu_ = wub[e % 2]; Wd_ = wdb[e % 2]
                    if e + 1 < 65:
                        issue_loads(e + 1)
                    bidx = -1
                    for (bb, nb_) in blocks:
                        hpair = hT[wi % 2]; gb_ = gbs[wi % 2]; wi += 1
                        dn = down_steps(pending, pd, yacc) if pending is not None else iter(())

                        def drain(n_):
                            for _ in range(n_):
                                f_ = next(dn, None)
                                if f_ is None:
                                    return
                                f_()
                        for fc in range(2):
                            g_ = pg[fc]; u_ = pu[fc]; s_ = sS[fc]; t_ = tS[fc]
                            for kk in range(8):
                                k.op("pe", lambda: nc.tensor.matmul(g_[:, 0:nb_], Wg_[:, kk, fc * 128:(fc + 1) * 128], mT16[:, kk, bb:bb + nb_],
                                                                    start=(kk == 0), stop=(kk == 7)), R=[Wg_, mT16], W=[g_])
                            drain(2)
                            for kk in range(8):
                                k.op("pe", lambda: nc.tensor.matmul(u_[:, 0:nb_], Wu_[:, kk, fc * 128:(fc + 1) * 128], mT16[:, kk, bb:bb + nb_],
                                                                    start=(kk == 0), stop=(kk == 7)), R=[Wu_, mT16], W=[u_])
                            if fc == 0:
                                k.op("pe", lambda: nc.tensor.matmul(pgb[:, 0:nb_], SEL[:, e * 128:(e + 1) * 128], gT[:, bb:bb + nb_], start=True, stop=True),
                                     R=[SEL, gT], W=[pgb])
                                k.op("act", lambda: nc.scalar.copy(out=gb_[:, 0:nb_], in_=pgb[:, 0:nb_]), R=[pgb], W=[gb_])
                            k.op("act", lambda: nc.scalar.activation(out=s_[:, 0:nb_], in_=g_[:, 0:nb_], func=AF.Silu), R=[g_], W=[s_])
                            k.op("dve", lambda: nc.vector.tensor_tensor(out=t_[:, 0:nb_], in0=u_[:, 0:nb_], in1=s_[:, 0:nb_], op=ALU.mult), R=[u_, s_], W=[t_])
                            k.op("pool", lambda: nc.gpsimd.tensor_tensor(out=hpair[fc][:, 0:nb_], in0=t_[:, 0:nb_], in1=gb_[:, 0:nb_], op=ALU.mult),
                                 R=[t_, gb_], W=[hpair[fc]])
                            drain(2)
                        drain(100)
                        pending = (hpair, Wd_, bb, nb_)
                        bidx += 1
                        if bidx == 1 and e + 1 < 65:
                            issue_casts(e + 1, (0, 1))
                    if e + 1 < 65:
                        issue_casts(e + 1, (2,))
                for f_ in down_steps(pending, pd, yacc):
                    f_()
                for ii, i in enumerate(range(t0, t1)):
                    j = 0 if i < 32 else 1
                    h_ = hf[ii % 2]
                    o_ = obm[ii % 2]
                    k.dma(h_[:], H1D[i * 128:(i + 1) * 128, :], W=[h_])
                    resid_ln((ubs[ii % 2], st6s[ii % 2], mvs[ii % 2]), [(yacc, yacc[:, ii, 0:512]), (yacc, yacc[:, ii, 512:1024])], h_, gb[j], gam, bet, o_)
                    if last:
                        if i < 32:
                            k.dma(y_out[i * 128:(i + 1) * 128, :], o_[:], R=[o_])
                    else:
                        k.dma(HD[i * 128:(i + 1) * 128, :], o_[:], R=[o_])
            k.barrier()

    pdi = [0]

    def down_steps(item, pd, yacc):
        hpair, Wd_, bb, nb_ = item
        for sub in range(nb_ // 128):
            ti_ = (bb // 128) + sub
            for dh in range(2):
                def step(sub=sub, ti_=ti_, dh=dh):
                    p = pd[pdi[0] % 3]; pdi[0] += 1
                    for fc in range(2):
                        k.op("pe", lambda: nc.tensor.matmul(p[:, :], hpair[fc][:, sub * 128:(sub + 1) * 128], Wd_[:, fc, dh * 512:(dh + 1) * 512],
                                                            start=(fc == 0), stop=(fc == 1)), R=[hpair[fc], Wd_], W=[p])
                    k.op("dve", lambda: nc.vector.tensor_tensor(out=yacc[:, ti_, dh * 512:(dh + 1) * 512], in0=p[:, :],
                                                                in1=yacc[:, ti_, dh * 512:(dh + 1) * 512], op=ALU.add), R=[p, yacc], W=[yacc])
                yield step

    phase_mod()
    seq = []
    for l in range(L):
        seq += [("inproj", l), ("rg", l), ("attn", l), ("hyena", l), ("outproj", l), ("moe", l)]
    for (ph, l) in seq:
        if ph == "inproj":
            phase_inproj(l)
        elif ph == "rg":
            phase_rg(l)
        elif ph == "attn":
            phase_attn(l)
        elif ph == "hyena":
            phase_hyena(l)
        elif ph == "outproj":
            phase_outproj(l)
        elif ph == "moe":
            phase_moe(l, last=(l == L - 1))
        if stop_after == (ph, l):
            break
    k.barrier()
    gs.close()
    return nc


_CONST = {}


def _consts():
    if _CONST:
        return _CONST
    bf = ml_dtypes.bfloat16

    def dft_tables(Lq):
        N = 2 * Lq
        n = np.arange(Lq, dtype=np.float64)
        ang = 2.0 * np.pi * np.outer(n, n) / N
        C = np.cos(ang)
        S = -np.sin(ang)
        alt = (-1.0) ** n
        S[:, 0] = alt
        nch = Lq // 128
        W = np.stack([C, S], axis=0)
        W = W.reshape(2, nch, 128, nch, 128)
        WTt = np.ascontiguousarray(W.transpose(3, 2, 1, 0, 4)).astype(np.float32).astype(bf)
        sc = np.full((128, nch), 2.0 / N, np.float32)
        sc[0, 0] = 1.0 / N
        WTI = WTt.copy()
        WTI[0, :, :, 1, 0] = 0
        WTI[:, 0, 0, 1, :] = ((-1.0) ** np.arange(128, dtype=np.float32)).astype(bf)[None, :]
        return WTt, sc, WTI

    def filt_tables(Lq):
        t = np.linspace(0.0, 1.0, Lq, dtype=np.float32)[:, None]
        w = (2.0 * np.float32(math.pi) * np.arange(Lq, dtype=np.float32)[:, None] / np.float32(Lq)).astype(np.float32)
        bands = np.linspace(1e-4, 7, 8, dtype=np.float32)
        z = np.concatenate([t, np.cos(bands * w), -np.sin(bands * w)], axis=-1).astype(np.float32)
        mx = math.log(1e-2) / 0.3
        mn = math.log(1e-2) / 1.5
        deltas = np.abs(np.linspace(mn, mx, 256, dtype=np.float32))
        win = np.exp(-t * deltas).astype(np.float32)
        win2 = win.copy()
        win2[0, :] = 0.0
        return np.ascontiguousarray(z.T), win, win2

    WTt, sc, w0i = dft_tables(T)
    WTc, scc, w0ic = dft_tables(LC)
    zt, win, win2 = filt_tables(T)
    ztc, winc, win2c = filt_tables(LC)
    n_rows = T // 64
    row = np.repeat(np.arange(n_rows, dtype=np.float32), 64)
    col = np.tile(np.arange(64, dtype=np.float32), n_rows)
    inv_freq = (np.float32(10000.0) ** (-np.arange(0, 32, 2, dtype=np.float32) / np.float32(32))).astype(np.float32)
    ang = np.concatenate([row[:, None] * inv_freq, col[:, None] * inv_freq], axis=-1).astype(np.float32)
    rope = np.stack([np.tile(np.cos(ang), (1, 8)), np.tile(np.sin(ang), (1, 8))], axis=1).astype(np.float32)
    sel = np.zeros((65, 65, 128), np.float32)
    for e in range(65):
        sel[e, e, :] = 1.0
    _CONST.update(dict(
        identb=np.eye(128, dtype=np.float32).astype(bf), identf=np.eye(128, dtype=np.float32),
        WT=WTt, WTc=WTc, WTI=w0i, WTIc=w0ic, SC=sc, SCc=scc, ZT=zt, ZTc=ztc, WIN=win, WIN2=win2, WINc=winc, WIN2c=win2c,
        ROPE=rope, SEL=sel.reshape(65, 65 * 128).astype(bf),
        ALT=((-1.0) ** np.arange(128, dtype=np.float32)).reshape(1, 128).astype(bf)))
    return _CONST


WEIGHT_NAMES = ["w_mod", "b_mod", "ln1_g", "ln1_b", "ln2_g", "ln2_b", "w_in", "w_out", "hy_short_w", "hy_short_b",
                "hy_f_w1", "hy_f_b1", "hy_f_freq", "hy_f_w2", "hy_f_b2", "hy_f_w3", "hy_skip", "q_norm", "k_norm",
                "rg_conv_w", "rg_conv_b", "rg_lambda", "rg_w_a", "rg_b_a", "rg_w_x", "rg_b_x", "w_router", "b_router",
                "w_gate", "w_up", "w_down", "ws_gate", "ws_up", "ws_down"]


def make_in_maps(inputs, L, cores):
    cst = _consts()
    shared = {}
    for n in WEIGHT_NAMES:
        arr = np.asarray(inputs[n], dtype=np.float32)
        for l_ in range(L):
            shared["%s_%d" % (n, l_)] = np.ascontiguousarray(arr[l_])
    shared["bmT"] = np.ascontiguousarray(np.asarray(inputs["b_mod"], dtype=np.float32)[:L].reshape(L, 48, 128).transpose(2, 0, 1))
    shared.update(cst)
    c = np.asarray(inputs["c"], np.float32)
    c_ctx = np.asarray(inputs["c_ctx"], np.float32)
    maps = []
    for b in cores:
        m = dict(shared)
        m["x"] = np.ascontiguousarray(np.asarray(inputs["x"], np.float32)[b])
        m["ctx"] = np.ascontiguousarray(np.asarray(inputs["ctx"], np.float32)[b])
        two = np.stack([c[b], c_ctx], axis=-1)
        m["cc"] = np.ascontiguousarray(two.reshape(8, 128, 2).transpose(1, 0, 2))
        maps.append(m)
    return maps


_NC = {}


def kernel(**inputs):
    if "nc" not in _NC:
        _NC["nc"] = build(L=4)
    nc = _NC["nc"]
    maps = make_in_maps(inputs, 4, list(range(8)))
    res = run_bass_kernel_spmd(nc, maps, core_ids=list(range(8)))
    out = np.stack([np.asarray(r["y"], dtype=np.float32) for r in res.results], axis=0)
    return out
```
